# Optimizing a Trainium2 kernel written in Bass

```python
import math
import jax
import jax.numpy as jnp
from jax import lax
import numpy as np


D_MODEL = 1024
BATCH = 4
SEQ = 4096
DEPTH = 2

GRID_W = 64
CTX_LEN = 256
N_MIXERS = 2

DEEPNORM_ALPHA = (2 * DEPTH) ** 0.25
DEEPNORM_BETA = (8 * DEPTH) ** -0.25
LN_EPS = 1e-5
RMS_EPS = 1e-6
GATED_NORM_EPS = 1e-5

SSD_D_INNER = 2 * D_MODEL
SSD_HEAD_DIM = 64
SSD_HEADS = SSD_D_INNER // SSD_HEAD_DIM
SSD_GROUPS = 8
SSD_STATE = 128
SSD_CONV = 5
SSD_CHUNK = 128
SSD_CONV_DIM = SSD_D_INNER + 2 * SSD_GROUPS * SSD_STATE
SSD_IN_DIM = SSD_D_INNER + SSD_CONV_DIM + 2 * SSD_HEADS
N_SSD_LAYERS = (DEPTH + 1) // 2

MLA_HEADS = 8
MLA_NOPE = 128
MLA_ROPE = 64
MLA_V = 128
MLA_Q_LORA = 384
MLA_KV_LORA = 256
MLA_IN_DIM = MLA_Q_LORA + MLA_KV_LORA + MLA_ROPE
MLA_SCALE = (MLA_NOPE + MLA_ROPE) ** -0.5
ROPE_FREQS = MLA_ROPE // 4
ROPE_BASE = 10000.0
Q_BLOCK = 128
N_MLA_LAYERS = DEPTH // 2

N_EXPERTS = 32
TOP_K = 4
D_FF = D_MODEL
SWIGLU_LIMIT = 7.0
SWIGLU_ALPHA = 1.702
MOE_BLOCK = 128

kernel_name = 'hybrid_ssd_mla_moe_flow_block'


def layer_norm(x, g, b):
    xf = x.astype(jnp.float32)
    mu = jnp.mean(xf, axis=-1, keepdims=True)
    var = jnp.mean(jnp.square(xf - mu), axis=-1, keepdims=True)
    return ((xf - mu) * lax.rsqrt(var + LN_EPS) * g + b).astype(x.dtype)


def rms_norm(x, g):
    xf = x.astype(jnp.float32)
    return (xf * lax.rsqrt(jnp.mean(xf * xf, axis=-1, keepdims=True) + RMS_EPS) * g).astype(x.dtype)


def centred_depthwise_conv(u, w, b):
    pad = SSD_CONV // 2
    y = lax.conv_general_dilated(u, w[:, None, :], (1,), [(pad, pad)],
                                 dimension_numbers=('NWC', 'WIO', 'NWC'),
                                 feature_group_count=u.shape[-1])
    return y + b


def ssd_chunked(xh, dt, a, bm, cm, s0):
    f32 = jnp.float32
    bsz, L, H, P = xh.shape
    G, N = bm.shape[2], bm.shape[3]
    R = H // G
    nc = L // SSD_CHUNK
    x = (xh.astype(f32) * dt[..., None]).reshape(bsz, nc, SSD_CHUNK, G, R, P)
    la = (dt * a).reshape(bsz, nc, SSD_CHUNK, G, R)
    bc = bm.astype(f32).reshape(bsz, nc, SSD_CHUNK, G, N)
    cc = cm.astype(f32).reshape(bsz, nc, SSD_CHUNK, G, N)
    cum = jnp.cumsum(la, axis=2)
    lower = jnp.tril(jnp.ones((SSD_CHUNK, SSD_CHUNK), bool))[:, :, None, None]
    seg = cum[:, :, :, None] - cum[:, :, None, :]
    decay = jnp.where(lower, jnp.exp(jnp.where(lower, seg, 0.0)), 0.0)
    cb = jnp.einsum('bclgn,bcsgn->bclsg', cc, bc)
    y_diag = jnp.einsum('bclsg,bclsgr,bcsgrp->bclgrp', cb, decay, x)
    to_end = jnp.exp(cum[:, :, -1:] - cum)
    chunk_states = jnp.einsum('bclgn,bclgr,bclgrp->bcgrpn', bc, to_end, x)
    chunk_decay = jnp.exp(cum[:, :, -1])

    def carry_state(s, inp):
        cs, cd = inp
        return s * cd[..., None, None] + cs, s

    s_final, s_in = lax.scan(carry_state, s0.astype(f32).reshape(bsz, G, R, P, N),
                             (jnp.moveaxis(chunk_states, 1, 0), jnp.moveaxis(chunk_decay, 1, 0)))
    s_in = jnp.moveaxis(s_in, 0, 1)
    y_off = jnp.einsum('bclgn,bcgrpn,bclgr->bclgrp', cc, s_in, jnp.exp(cum))
    y = (y_diag + y_off).reshape(bsz, L, H, P)
    return y, s_final.reshape(bsz, H, P, N)


def ssd_mixer(h_lat, h_ctx, w_in, conv_w, conv_b, dt_bias, a_log, d_skip, norm_w, w_out, need_ctx):
    f32 = jnp.float32
    a = -jnp.exp(a_log.astype(f32))

    def branch(h):
        bsz, L = h.shape[:2]
        u = h @ w_in
        z, xbc, dt_raw = jnp.split(u, [SSD_D_INNER, SSD_D_INNER + SSD_CONV_DIM], axis=-1)
        xbc = jax.nn.silu(centred_depthwise_conv(xbc, conv_w, conv_b))
        xs, bm, cm = jnp.split(xbc, [SSD_D_INNER, SSD_D_INNER + SSD_GROUPS * SSD_STATE], axis=-1)
        xh = xs.reshape(bsz, L, SSD_HEADS, SSD_HEAD_DIM)
        bm = bm.reshape(bsz, L, SSD_GROUPS, SSD_STATE)
        cm = cm.reshape(bsz, L, SSD_GROUPS, SSD_STATE)
        dt = jax.nn.softplus(dt_raw.astype(f32).reshape(bsz, L, 2, SSD_HEADS) + dt_bias)
        return z, xh, bm, cm, dt

    def flip(t):
        return jnp.flip(t, axis=1)

    zc, xc, bc, cc, dtc = branch(h_ctx)
    zl, xl, bl, cl, dtl = branch(h_lat)
    bsz = h_lat.shape[0]
    s0 = jnp.zeros((bsz, SSD_HEADS, SSD_HEAD_DIM, SSD_STATE), f32)
    yc_f, sc_f = ssd_chunked(xc, dtc[:, :, 0], a[0], bc, cc, s0)
    yl_f, _ = ssd_chunked(xl, dtl[:, :, 0], a[0], bl, cl, sc_f)
    yc_b, sc_b = ssd_chunked(flip(xc), flip(dtc[:, :, 1]), a[1], flip(bc), flip(cc), s0)
    yl_b, _ = ssd_chunked(flip(xl), flip(dtl[:, :, 1]), a[1], flip(bl), flip(cl), sc_b)

    def finish(xh, y_fwd, y_bwd_rev, z):
        bsz_, L = xh.shape[:2]
        y = y_fwd + flip(y_bwd_rev) + xh.astype(f32) * d_skip[:, None]
        y = y.reshape(bsz_, L, SSD_D_INNER) * jax.nn.silu(z.astype(f32))
        yg = y.reshape(bsz_, L, SSD_GROUPS, SSD_D_INNER // SSD_GROUPS)
        yg = yg * lax.rsqrt(jnp.mean(yg * yg, axis=-1, keepdims=True) + GATED_NORM_EPS)
        y = yg.reshape(bsz_, L, SSD_D_INNER) * norm_w
        return y.astype(w_out.dtype) @ w_out

    y_lat = finish(xl, yl_f, yl_b, zl)
    y_ctx = finish(xc, yc_f, yc_b, zc) if need_ctx else None
    return y_lat, y_ctx


def axial_rope_tables(seq_len):
    t = jnp.arange(seq_len)
    row = (t // GRID_W).astype(jnp.float32)
    col = (t % GRID_W).astype(jnp.float32)
    inv = ROPE_BASE ** (-jnp.arange(ROPE_FREQS, dtype=jnp.float32) / ROPE_FREQS)
    ang = jnp.stack([row[:, None] * inv, col[:, None] * inv], axis=1)
    return jnp.cos(ang), jnp.sin(ang)


def apply_rope_2d(t, cos, sin):
    tr = t.reshape(t.shape[:-1] + (2, 2, ROPE_FREQS)).astype(jnp.float32)
    t1, t2 = tr[..., 0, :], tr[..., 1, :]
    out = jnp.stack([t1 * cos - t2 * sin, t2 * cos + t1 * sin], axis=-2)
    return out.reshape(t.shape).astype(t.dtype)


def mla_attend(qn, qr, kn, kr, v):
    s = (jnp.einsum('bqhd,bkhd->bhqk', qn, kn, preferred_element_type=jnp.float32)
         + jnp.einsum('bqhr,bkr->bhqk', qr, kr, preferred_element_type=jnp.float32))
    p = jax.nn.softmax(s * MLA_SCALE, axis=-1)
    return jnp.einsum('bhqk,bkhv->bqhv', p.astype(v.dtype), v)


def mla_blocked(qn, qr, kn, kr, v):
    bsz, L = qn.shape[:2]
    nb = L // Q_BLOCK

    def to_blocks(t):
        return jnp.moveaxis(t.reshape((bsz, nb, Q_BLOCK) + t.shape[2:]), 1, 0)

    out = lax.map(lambda qs: mla_attend(qs[0], qs[1], kn, kr, v), (to_blocks(qn), to_blocks(qr)))
    return jnp.moveaxis(out, 0, 1).reshape(bsz, L, MLA_HEADS, MLA_V)


def mla_mixer(h_lat, h_ctx, w_a, q_norm, kv_norm, w_qb, w_kvb, w_o, rope_cos, rope_sin, need_ctx):
    def project(h):
        bsz, L = h.shape[:2]
        q_lat, kv_lat, k_rope = jnp.split(h @ w_a, [MLA_Q_LORA, MLA_Q_LORA + MLA_KV_LORA], axis=-1)
        kv = (rms_norm(kv_lat, kv_norm) @ w_kvb).reshape(bsz, L, MLA_HEADS, MLA_NOPE + MLA_V)
        return q_lat, kv[..., :MLA_NOPE], kv[..., MLA_NOPE:], k_rope

    def queries(q_lat):
        bsz, L = q_lat.shape[:2]
        q = (rms_norm(q_lat, q_norm) @ w_qb).reshape(bsz, L, MLA_HEADS, MLA_NOPE + MLA_ROPE)
        return q[..., :MLA_NOPE], q[..., MLA_NOPE:]

    bsz, L = h_lat.shape[:2]
    ql_lat, kn_l, v_l, kr_l = project(h_lat)
    qn_l, qr_l = queries(ql_lat)
    qr_l = apply_rope_2d(qr_l, rope_cos[:, None], rope_sin[:, None])
    kr_l = apply_rope_2d(kr_l, rope_cos, rope_sin)
    ql_ctx, kn_c, v_c, kr_c = project(h_ctx)
    kn = jnp.concatenate([kn_c, kn_l], axis=1)
    kr = jnp.concatenate([kr_c, kr_l], axis=1)
    v = jnp.concatenate([v_c, v_l], axis=1)
    o_lat = mla_blocked(qn_l, qr_l, kn, kr, v)
    y_lat = o_lat.reshape(bsz, L, MLA_HEADS * MLA_V) @ w_o
    y_ctx = None
    if need_ctx:
        qn_c, qr_c = queries(ql_ctx)
        o_ctx = mla_attend(qn_c, qr_c, kn_c, kr_c, v_c)
        y_ctx = o_ctx.reshape(bsz, h_ctx.shape[1], MLA_HEADS * MLA_V) @ w_o
    return y_lat, y_ctx


def clamped_swiglu(u):
    glu, lin = jnp.split(u, 2, axis=-1)
    glu = jnp.minimum(glu, SWIGLU_LIMIT)
    lin = jnp.clip(lin, -SWIGLU_LIMIT, SWIGLU_LIMIT)
    return glu * jax.nn.sigmoid(SWIGLU_ALPHA * glu) * (lin + 1.0)


def moe_ffn(h, router_w, router_b, w_in, b_in, w_out, b_out):
    T, D = h.shape
    logits = jnp.dot(h, router_w, preferred_element_type=jnp.float32) + router_b
    top_logit, top_e = lax.top_k(logits, TOP_K)
    gate = jax.nn.softmax(top_logit, axis=-1)
    n_assign = T * TOP_K
    flat_e = top_e.reshape(-1)
    order = jnp.argsort(flat_e, stable=True)
    sorted_e = flat_e[order]
    sorted_tok = order // TOP_K
    sorted_gate = gate.reshape(-1)[order]
    counts = jnp.bincount(flat_e, length=N_EXPERTS)
    padded = (counts + MOE_BLOCK - 1) // MOE_BLOCK * MOE_BLOCK
    padded_end = jnp.cumsum(padded)
    group_start = jnp.cumsum(counts) - counts
    slot = (padded_end - padded)[sorted_e] + jnp.arange(n_assign) - group_start[sorted_e]
    n_blocks = (n_assign + N_EXPERTS * (MOE_BLOCK - 1) + MOE_BLOCK - 1) // MOE_BLOCK
    n_slots = n_blocks * MOE_BLOCK
    slot_tok = jnp.zeros((n_slots,), jnp.int32).at[slot].set(sorted_tok)
    block_e = jnp.minimum(jnp.searchsorted(padded_end, jnp.arange(n_blocks) * MOE_BLOCK, side='right'),
                          N_EXPERTS - 1)
    xb = h[slot_tok].reshape(n_blocks, MOE_BLOCK, D)

    def expert_block(args):
        xblk, e = args
        u = xblk @ w_in[e] + b_in[e]
        return clamped_swiglu(u) @ w_out[e] + b_out[e]

    yb = lax.map(expert_block, (xb, block_e)).reshape(n_slots, D)
    contrib = yb[slot].astype(jnp.float32) * sorted_gate[:, None]
    return jax.ops.segment_sum(contrib, sorted_tok, num_segments=T).astype(h.dtype)


def setup_inputs(seed: int = 0) -> dict:
    key = jax.random.key(seed)
    keys = iter(jax.random.split(key, 48))
    f32 = jnp.float32

    def normal(shape, std):
        return jax.random.normal(next(keys), shape, f32) * std

    def dense(shape, fan_in, gain=1.0):
        return normal(shape, gain * fan_in ** -0.5)

    def near_one(shape):
        return 1.0 + normal(shape, 0.02)

    def small(shape):
        return normal(shape, 0.02)

    dt0 = jnp.exp(jax.random.uniform(next(keys), (N_SSD_LAYERS, 2, SSD_HEADS), f32)
                  * (math.log(0.1) - math.log(0.001)) + math.log(0.001))
    dt_bias = dt0 + jnp.log(-jnp.expm1(-dt0))
    a_log = jnp.log(jax.random.uniform(next(keys), (N_SSD_LAYERS, 2, SSD_HEADS), f32, 1.0, 16.0))
    return {
        'x': normal((BATCH, SEQ, D_MODEL), 1.0),
        'c': normal((BATCH, D_MODEL), 1.0),
        'ctx': normal((BATCH, CTX_LEN, D_MODEL), 1.0),
        'c_ctx': normal((D_MODEL,), 1.0),
        'mod_w': dense((DEPTH, D_MODEL, 6 * D_MODEL), D_MODEL),
        'mod_b': small((DEPTH, 6 * D_MODEL)),
        'ln1_g': near_one((DEPTH, D_MODEL)),
        'ln1_b': small((DEPTH, D_MODEL)),
        'ln2_g': near_one((DEPTH, D_MODEL)),
        'ln2_b': small((DEPTH, D_MODEL)),
        'ssd_w_in': dense((N_SSD_LAYERS, D_MODEL, SSD_IN_DIM), D_MODEL),
        'ssd_conv_w': dense((N_SSD_LAYERS, SSD_CONV, SSD_CONV_DIM), SSD_CONV),
        'ssd_conv_b': small((N_SSD_LAYERS, SSD_CONV_DIM)),
        'ssd_dt_bias': dt_bias,
        'ssd_a_log': a_log,
        'ssd_d': near_one((N_SSD_LAYERS, SSD_HEADS)),
        'ssd_norm_w': near_one((N_SSD_LAYERS, SSD_D_INNER)),
        'ssd_w_out': dense((N_SSD_LAYERS, SSD_D_INNER, D_MODEL), SSD_D_INNER, DEEPNORM_BETA),
        'mla_w_a': dense((N_MLA_LAYERS, D_MODEL, MLA_IN_DIM), D_MODEL),
        'mla_q_norm': near_one((N_MLA_LAYERS, MLA_Q_LORA)),
        'mla_kv_norm': near_one((N_MLA_LAYERS, MLA_KV_LORA)),
        'mla_w_qb': dense((N_MLA_LAYERS, MLA_Q_LORA, MLA_HEADS * (MLA_NOPE + MLA_ROPE)), MLA_Q_LORA),
        'mla_w_kvb': dense((N_MLA_LAYERS, MLA_KV_LORA, MLA_HEADS * (MLA_NOPE + MLA_V)), MLA_KV_LORA),
        'mla_w_o': dense((N_MLA_LAYERS, MLA_HEADS * MLA_V, D_MODEL), MLA_HEADS * MLA_V, DEEPNORM_BETA),
        'router_w': dense((DEPTH, D_MODEL, N_EXPERTS), D_MODEL),
        'router_b': normal((DEPTH, N_EXPERTS), 0.01),
        'moe_w_in': dense((DEPTH, N_EXPERTS, D_MODEL, 2 * D_FF), D_MODEL),
        'moe_b_in': small((DEPTH, N_EXPERTS, 2 * D_FF)),
        'moe_w_out': dense((DEPTH, N_EXPERTS, D_FF, D_MODEL), D_FF, DEEPNORM_BETA),
        'moe_b_out': small((DEPTH, N_EXPERTS, D_MODEL)),
    }


def reference(x, c, ctx, c_ctx, mod_w, mod_b, ln1_g, ln1_b, ln2_g, ln2_b,
              ssd_w_in, ssd_conv_w, ssd_conv_b, ssd_dt_bias, ssd_a_log, ssd_d, ssd_norm_w, ssd_w_out,
              mla_w_a, mla_q_norm, mla_kv_norm, mla_w_qb, mla_w_kvb, mla_w_o,
              router_w, router_b, moe_w_in, moe_b_in, moe_w_out, moe_b_out):
    bsz, seq_len, d = x.shape
    n_lat = bsz * seq_len
    rope_cos, rope_sin = axial_rope_tables(seq_len)
    for i in range(DEPTH):
        last = i == DEPTH - 1
        j = i // N_MIXERS
        mod_l = jax.nn.silu(c) @ mod_w[i] + mod_b[i]
        mod_c = jax.nn.silu(c_ctx) @ mod_w[i] + mod_b[i]
        sh1, sc1, g1, sh2, sc2, g2 = jnp.split(mod_l[:, None, :], 6, axis=-1)
        csh1, csc1, cg1, csh2, csc2, cg2 = jnp.split(mod_c, 6, axis=-1)
        h_lat = x * (1.0 + sc1) + sh1
        h_ctx = ctx * (1.0 + csc1) + csh1
        if i % N_MIXERS == 0:
            y_lat, y_ctx = ssd_mixer(h_lat, h_ctx, ssd_w_in[j], ssd_conv_w[j], ssd_conv_b[j],
                                     ssd_dt_bias[j], ssd_a_log[j], ssd_d[j], ssd_norm_w[j],
                                     ssd_w_out[j], not last)
        else:
            y_lat, y_ctx = mla_mixer(h_lat, h_ctx, mla_w_a[j], mla_q_norm[j], mla_kv_norm[j],
                                     mla_w_qb[j], mla_w_kvb[j], mla_w_o[j], rope_cos, rope_sin,
                                     not last)
        x = layer_norm(DEEPNORM_ALPHA * x + g1 * y_lat, ln1_g[i], ln1_b[i])
        tokens = (x * (1.0 + sc2) + sh2).reshape(n_lat, d)
        if not last:
            ctx = layer_norm(DEEPNORM_ALPHA * ctx + cg1 * y_ctx, ln1_g[i], ln1_b[i])
            tokens = jnp.concatenate([tokens, (ctx * (1.0 + csc2) + csh2).reshape(-1, d)], axis=0)
        f = moe_ffn(tokens, router_w[i], router_b[i], moe_w_in[i], moe_b_in[i], moe_w_out[i], moe_b_out[i])
        x = layer_norm(DEEPNORM_ALPHA * x + g2 * f[:n_lat].reshape(x.shape), ln2_g[i], ln2_b[i])
        if not last:
            ctx = layer_norm(DEEPNORM_ALPHA * ctx + cg2 * f[n_lat:].reshape(ctx.shape), ln2_g[i], ln2_b[i])
    return x
```

```python
import numpy as np
import concourse.bass as bass
import concourse.mybir as mybir
from concourse.bass_utils import run_bass_kernel_spmd
from contextlib import ExitStack

F32 = mybir.dt.float32
BF16 = mybir.dt.bfloat16
U32 = mybir.dt.uint32
I32 = mybir.dt.int32
AF = mybir.ActivationFunctionType
ALU = mybir.AluOpType
AX = mybir.AxisListType


class Tile:
    def __init__(self, name, h, nreg=1):
        self.name = name
        self.h = h
        self.nreg = nreg

    def k(self, i=0):
        return (self.name, i)

    def all(self):
        return [(self.name, i) for i in range(self.nreg)]

    def __getitem__(self, idx):
        return self.h[idx]


class Sched:
    ENGS = ("pe", "act", "dve", "pool", "sp")

    def __init__(self, nc, es, n_dma_sems=8, sync_same=True):
        self.nc = nc
        self.es = es
        self.sync_same = sync_same
        self.q = {e: [] for e in self.ENGS}
        self.csem = {e: es.enter_context(nc.semaphore("c_" + e)) for e in self.ENGS}
        self.ccount = {e: 0 for e in self.ENGS}
        self.dq = ("sp", "pool", "act")
        self.dsems = {e: [es.enter_context(nc.semaphore("d_%s_%d" % (e, i))) for i in range(n_dma_sems)]
                      for e in self.dq}
        self.duse = {e: [0] * n_dma_sems for e in self.dq}
        self.dnext = {e: 0 for e in self.dq}
        self.lastw = {}
        self.readers = {}
        self.known = {e: {} for e in self.ENGS}
        self.ntile = 0

    def sb(self, name, shape, dtype, nreg=1):
        h = self.es.enter_context(self.nc.sbuf_tensor(name, list(shape), dtype))
        return Tile(name, h, nreg)

    def ps(self, name, shape, dtype, nreg=1):
        h = self.es.enter_context(self.nc.psum_tensor(name, list(shape), dtype))
        return Tile(name, h, nreg)

    def dram(self, name, shape, dtype, kind, nreg=1):
        name = getattr(self, "prefix", "") + name
        if not hasattr(self.nc, "_in_names"):
            self.nc._in_names = []
        if kind == "ExternalInput":
            self.nc._in_names.append(name)
        h = self.nc.dram_tensor(name, list(shape), dtype, kind=kind)
        return Tile(name, h.ap(), nreg)

    def arena(self, nfloats):
        self.ar = self.sb("arena", [128, nfloats], F32)
        self.ar_n = nfloats
        self.ar_off = 0
        self.ar_base = 0

    def carve(self, name, shape, dtype, nreg=1, parts=128):
        n = 1
        for s in shape:
            n *= s
        nf = n if dtype == F32 else (n + 1) // 2
        nf = (nf + 7) // 8 * 8
        assert self.ar_off + nf <= self.ar_n, ("arena overflow", name, self.ar_off, nf, self.ar_n)
        ap = self.ar.h[0:parts, self.ar_off:self.ar_off + nf]
        if dtype != F32:
            ap = ap.bitcast(dtype)
        ap = ap[:, 0:n]
        if len(shape) == 2:
            ap = ap.rearrange("p (a b) -> p a b", b=shape[1])
        elif len(shape) == 3:
            ap = ap.rearrange("p (a b c) -> p a b c", b=shape[1], c=shape[2])
        self.ar_off += nf
        self.ntile += 1
        return Tile("%s#%d" % (name, self.ntile), ap, nreg)

    def mark(self):
        return self.ar_off

    def reset(self, mark):
        self.barrier()
        self.ar_off = mark

    def barrier(self):
        allt = {}
        for e in self.ENGS:
            if self.ccount[e] > 0:
                allt[("c", e)] = self.ccount[e]
        for e in self.dq:
            for i, u in enumerate(self.duse[e]):
                if u > 0:
                    allt[("d", e, i)] = u
        self.pending = {e: dict(allt) for e in self.ENGS}

    def op(self, eng, fn, reads=(), writes=(), dma=False, same=None, inc=16):
        deps = {}
        pend = getattr(self, "pending", None)
        if pend and pend.get(eng):
            for sk, v in pend[eng].items():
                if deps.get(sk, 0) < v:
                    deps[sk] = v
            pend[eng] = None

        def add(tok):
            sk, v = tok
            if deps.get(sk, 0) < v:
                deps[sk] = v

        for k in reads:
            if k in self.lastw:
                add(self.lastw[k])
        for k in writes:
            if k in self.lastw:
                add(self.lastw[k])
            for sk, v in self.readers.get(k, {}).items():
                add((sk, v))
        if dma:
            i = self.dnext[eng]
            self.dnext[eng] = (i + 1) % len(self.dsems[eng])
            prev = self.duse[eng][i]
            self.duse[eng][i] = prev + inc
            tok = (("d", eng, i), prev + inc)
            if prev > 0:
                add((("d", eng, i), prev))
        else:
            self.ccount[eng] += 1
            tok = (("c", eng), self.ccount[eng])
        same = self.sync_same if same is None else same
        waits = []
        for sk, v in deps.items():
            if sk == ("c", eng) and not same:
                continue
            if self.known[eng].get(sk, 0) >= v:
                continue
            self.known[eng][sk] = v
            waits.append((sk, v))
        self.q[eng].append((waits, fn, tok, inc if dma else 1))
        for k in reads:
            r = self.readers.setdefault(k, {})
            if r.get(tok[0], 0) < tok[1]:
                r[tok[0]] = tok[1]
        for k in writes:
            self.lastw[k] = tok
            self.readers[k] = {}
        return tok

    def dma(self, out, in_, reads, writes, eng="sp", **kw):
        return self.op(eng, lambda e: e.dma_start(out=out, in_=in_, **kw), reads, writes, dma=True)

    def mm(self, out, lhsT, rhs, start, stop, reads, writes):
        return self.op("pe", lambda e: e.matmul(out, lhsT, rhs, start=start, stop=stop), reads, writes, same=False)

    def tr(self, out, in_, ident, reads, writes):
        return self.op("pe", lambda e: e.transpose(out, in_, ident), reads, writes, same=False)

    def act(self, out, in_, func, reads, writes, eng="act", **kw):
        return self.op(eng, lambda e: e.activation(out=out, in_=in_, func=func, **kw), reads, writes)

    def tt(self, out, in0, in1, op, reads, writes, eng="dve"):
        return self.op(eng, lambda e: e.tensor_tensor(out=out, in0=in0, in1=in1, op=op), reads, writes)

    def ts(self, out, in0, s1, s2, op0, op1, reads, writes, eng="dve", **kw):
        if op1 is None:
            return self.op(eng, lambda e: e.tensor_scalar(out=out, in0=in0, scalar1=s1, scalar2=None, op0=op0, **kw),
                           reads, writes)
        return self.op(eng, lambda e: e.tensor_scalar(out=out, in0=in0, scalar1=s1, scalar2=s2, op0=op0, op1=op1, **kw),
                       reads, writes)

    def cp(self, out, in_, reads, writes, eng="dve"):
        if eng == "act":
            return self.op(eng, lambda e: e.copy(out=out, in_=in_), reads, writes)
        return self.op(eng, lambda e: e.tensor_copy(out=out, in_=in_), reads, writes)

    def stt(self, out, in0, scalar, in1, op0, op1, reads, writes):
        return self.op("dve", lambda e: e.scalar_tensor_tensor(out=out, in0=in0, scalar=scalar, in1=in1, op0=op0, op1=op1),
                       reads, writes)

    def recip(self, out, in_, reads, writes):
        return self.op("dve", lambda e: e.reciprocal(out=out, in_=in_), reads, writes)

    def max8(self, out, in_, reads, writes):
        return self.op("dve", lambda e: e.max(out=out, in_=in_), reads, writes)

    def reduce(self, out, in_, axis, op, reads, writes):
        return self.op("dve", lambda e: e.tensor_reduce(out=out, in_=in_, axis=axis, op=op), reads, writes)

    def memset(self, out, val, writes, eng="dve"):
        return self.op(eng, lambda e: e.memset(out, val), (), writes)

    def _semh(self, sk):
        if sk[0] == "c":
            return self.csem[sk[1]]
        return self.dsems[sk[1]][sk[2]]

    def finish(self, out_keys):
        waits = []
        for k in out_keys:
            if k in self.lastw:
                waits.append(self.lastw[k])
        self.final_waits = waits

    def emit(self):
        nc = self.nc
        counts = {e: len(self.q[e]) for e in self.ENGS}
        nw = {e: sum(len(w[0]) for w in self.q[e]) for e in self.ENGS}
        print("SCHED instr counts", counts, "waits", nw, flush=True)
        with nc.Block() as block:
            def mk(engname):
                def body(e):
                    for waits, fn, tok, inc_ in self.q[engname]:
                        for sk, v in waits:
                            e.wait_ge(self._semh(sk), v)
                        ins = fn(e)
                        ins.then_inc(self._semh(tok[0]), inc_)
                    if engname == "sp":
                        done = {}
                        for sk, v in getattr(self, "final_waits", []):
                            if done.get(sk, 0) < v:
                                done[sk] = v
                        for sk, v in done.items():
                            e.wait_ge(self._semh(sk), v)
                return body

            block.tensor(mk("pe"))
            block.scalar(mk("act"))
            block.vector(mk("dve"))
            block.gpsimd(mk("pool"))
            block.sync(mk("sp"))


ALPHA = 4.0 ** 0.25
LN_EPS = 1e-5
SH1, SC1, G1, SH2, SC2, G2 = 0, 8, 16, 24, 32, 40


def R(*tiles):
    out = []
    for t in tiles:
        out += t.all()
    return out


class Ctx:
    pass


def setup_common(S, C, layer_has_ctx):
    if hasattr(S, "_common"):
        C.__dict__.update(S._common.__dict__)
        return
    S._common = C
    C.PS = [S.ps("ps%d" % i, [128, 512], F32) for i in range(8)]
    pre, S.prefix = getattr(S, "prefix", ""), ""
    C.cst = S.dram("cst", [128, 1024], F32, "ExternalInput")
    S.prefix = pre
    C.cs = S.sb("cs", [128, 1024], F32)
    S.dma(C.cs[:], C.cst[:], [], R(C.cs))
    C.ident = C.cs[:, 0:128]
    C.ones = C.cs[:, 128:256]
    C.U = [C.cs[:, 256:384], C.cs[:, 384:512]]
    C.NEG1 = [C.cs[:, 512:640], C.cs[:, 640:768]]
    C.eye32 = C.cs[0:32, 768:800]
    C.identbf = S.sb("identbf", [128, 128], BF16)
    S.cp(C.identbf[:], C.ident, R(C.cs), R(C.identbf))


def modulation(S, C, mod_w, mod_bT, cT):
    PS = C.PS
    m0 = S.mark()
    cs = S.carve("cTs", [8, 2], F32)
    S.dma(cs[:], cT[:], [], R(cs))
    csl = S.carve("csl", [8, 2], F32)
    S.act(csl[:], cs[:], AF.Silu, R(cs), R(csl))
    wb = [S.carve("modw%d" % i, [8, 512], F32) for i in range(2)]
    mb = S.carve("modb", [48], F32)
    S.dma(mb[:], mod_bT[:], [], R(mb))
    mwv = mod_w.h.rearrange("(kc p) f -> p kc f", p=128)
    for fg in range(12):
        w = wb[fg % 2]
        S.dma(w[:], mwv[:, :, fg * 512:(fg + 1) * 512], [], R(w))
        for fl in range(4):
            fc = fg * 4 + fl
            for kc in range(8):
                S.mm(PS[7][:, fc * 2:fc * 2 + 2], w[:, kc, fl * 128:(fl + 1) * 128], csl[:, kc, :], kc == 0, kc == 7,
                     R(w, csl), R(PS[7]))
    S.tt(C.mod[:], PS[7][:, 0:96].rearrange("p (f j) -> p f j", j=2), mb[:].unsqueeze(2).to_broadcast([128, 48, 2]),
         ALU.add, R(PS[7], mb), R(C.mod))
    for base in (SC1, SC2):
        S.ts(C.mod[:, base:base + 8, :], C.mod[:, base:base + 8, :], 1.0, None, ALU.add, None, R(C.mod), R(C.mod))
    S.reset(m0)


def layer_norm_fm(S, C, r, n, gi, out, tmp):
    PS = C.PS
    sq, mean, msq, var, rs = tmp["sq"], tmp["mean"], tmp["msq"], tmp["var"], tmp["rs"]
    for c in range(8):
        S.mm(PS[6][:, 0:n], C.ones, r[:, c, 0:n], c == 0, c == 7, R(r, C.cs), R(PS[6]))
    S.tt(sq[:, :, 0:n], r[:, :, 0:n], r[:, :, 0:n], ALU.mult, R(r), R(sq), eng="pool")
    for c in range(8):
        S.mm(PS[7][:, 0:n], C.ones, sq[:, c, 0:n], c == 0, c == 7, R(sq, C.cs), R(PS[7]))
    S.act(mean[:, 0:n], PS[6][:, 0:n], AF.Identity, R(PS[6]), R(mean), scale=1.0 / 1024)
    S.tt(msq[:, 0:n], mean[:, 0:n], mean[:, 0:n], ALU.mult, R(mean), R(msq), eng="pool")
    S.stt(var[:, 0:n], PS[7][:, 0:n], 1.0 / 1024, msq[:, 0:n], ALU.mult, ALU.subtract, R(PS[7], msq), R(var))
    S.act(var[:, 0:n], var[:, 0:n], AF.Sqrt, R(var), R(var), bias=LN_EPS)
    S.recip(rs[:, 0:n], var[:, 0:n], R(var), R(rs))
    for c in range(8):
        S.tt(r[:, c, 0:n], r[:, c, 0:n], mean[:, 0:n], ALU.subtract, R(r, mean), R(r))
        S.tt(r[:, c, 0:n], r[:, c, 0:n], rs[:, 0:n], ALU.mult, R(r, rs), R(r), eng="pool")
        S.act(out(c), r[:, c, 0:n], AF.Identity, R(r, C.lnp), tmp["outkeys"],
              scale=C.lnp[:, gi, c:c + 1], bias=C.lnp[:, gi + 1, c:c + 1])


def ln_tmp(S, nmax, outkeys):
    return {"sq": S.carve("lnsq", [8, nmax], F32), "mean": S.carve("lnmean", [nmax], F32),
            "msq": S.carve("lnmsq", [nmax], F32), "var": S.carve("lnvar", [nmax], F32),
            "rs": S.carve("lnrs", [nmax], F32), "outkeys": outkeys}


def router(S, C, Xf, n, o0, rw, rb, gateT, rt):
    PS = C.PS
    for i in range(n // 128):
        for kc in range(8):
            S.mm(PS[5][:, 0:32], Xf[:, kc, i * 128:(i + 1) * 128], rw[:, kc, :], kc == 0, kc == 7, R(Xf, rw), R(PS[5]))
        lg, m8, msk, ex, sm = rt["lg"], rt["m8"], rt["msk"], rt["ex"], rt["sm"]
        S.tt(lg[:], PS[5][:, 0:32], rb[:], ALU.add, R(PS[5], rb), R(lg))
        S.max8(m8[:], lg[:], R(lg), R(m8))
        S.ts(msk[:], lg[:], m8[:, 3:4], None, ALU.is_ge, None, R(lg, m8), R(msk))
        S.ts(sm[:, 0:1], m8[:, 0:1], -1.0, None, ALU.mult, None, R(m8), R(sm))
        S.act(ex[:], lg[:], AF.Exp, R(lg, sm), R(ex), bias=sm[:, 0:1])
        S.tt(ex[:], ex[:], msk[:], ALU.mult, R(ex, msk), R(ex))
        S.reduce(sm[:, 1:2], ex[:], AX.X, ALU.add, R(ex), R(sm))
        S.recip(sm[:, 2:3], sm[:, 1:2], R(sm), R(sm))
        S.ts(ex[:], ex[:], sm[:, 2:3], None, ALU.mult, None, R(ex, sm), R(ex))
        S.tr(PS[5][0:32, 128:256], ex[:], C.ident, R(ex, C.cs), R(PS[5]))
        S.cp(gateT[0:32, o0 + i * 128:o0 + (i + 1) * 128], PS[5][0:32, 128:256], R(PS[5]), R(gateT), eng="act")


def router_tmp(S):
    return {"lg": S.carve("rlg", [32], F32), "m8": S.carve("rm8", [8], F32), "msk": S.carve("rmsk", [32], F32),
            "ex": S.carve("rex", [32], F32), "sm": S.carve("rsm", [4], F32)}


def moe_block(S, C, XT, gateT, chunks, w_in, w_out, b_inT, b_out, acc):
    PS = C.PS
    tot = sum(n for _, n in chunks)
    offs = []
    o = 0
    for lo, n in chunks:
        offs.append(o)
        o += n
    wi = S.carve("wi", [8, 2048], BF16, nreg=2)
    wo = S.carve("wo", [8, 1024], BF16)
    actT = S.carve("actT", [8, tot], BF16, nreg=8)
    Gb = S.carve("Gb", [tot], F32)
    ge = S.carve("ge", [tot], F32, parts=32)
    bo = S.carve("bo", [1024], F32, parts=32)
    S.dma(bo[:], b_out[:], [], R(bo))
    tg = [S.carve("tg%d" % i, [512], F32) for i in range(2)]
    tsg = [S.carve("tsg%d" % i, [512], F32) for i in range(2)]
    tl = [S.carve("tl%d" % i, [512], F32) for i in range(2)]
    it = 0
    for dc in range(8):
        for ci, (lo, n) in enumerate(chunks):
            p = PS[4 + it % 2]
            it += 1
            S.mm(p[:, 0:n], bo[:, dc * 128:(dc + 1) * 128], gateT[0:32, lo:lo + n], True, True, R(bo, gateT), R(p))
            S.cp(acc[:, dc, offs[ci]:offs[ci] + n], p[:, 0:n], R(p), [acc.k(dc)], eng="act")
    wiv = w_in.h.rearrange("e (kc p) f -> e p kc f", p=128)
    wov = w_out.h.rearrange("e (kc p) f -> e p kc f", p=128)

    def load_wi(e, h):
        S.dma(wi[:, :, h * 1024:(h + 1) * 1024], wiv[e, :, :, h * 1024:(h + 1) * 1024], [], [wi.k(h)], eng="pool")

    load_wi(0, 0)
    load_wi(0, 1)
    it = 0
    for e in range(32):
        S.dma(wo[:], wov[e], [], R(wo), eng="pool")
        for ci, (lo, n) in enumerate(chunks):
            S.ts(ge[0:32, offs[ci]:offs[ci] + n], gateT[0:32, lo:lo + n], C.eye32[:, e:e + 1], None, ALU.mult, None,
                 R(gateT, C.cs), R(ge))
            S.mm(PS[6][:, 0:n], C.ones[0:32, :], ge[0:32, offs[ci]:offs[ci] + n], True, True, R(ge, C.cs), R(PS[6]))
            S.cp(Gb[:, offs[ci]:offs[ci] + n], PS[6][:, 0:n], R(PS[6]), R(Gb), eng="act")
        for j in range(8):
            hh, jj = j // 4, j % 4
            cg = hh * 1024 + jj * 128
            cl = hh * 1024 + 512 + jj * 128
            for ci, (lo, n) in enumerate(chunks):
                pa, pb = (PS[0], PS[1]) if it % 2 == 0 else (PS[2], PS[3])
                q = it % 2
                it += 1
                for kc in range(8):
                    S.mm(pa[:, 0:n], wi[:, kc, cg:cg + 128], XT[:, kc, lo:lo + n], kc == 0, kc == 7,
                         [wi.k(hh)] + R(XT), R(pa))
                for kc in range(8):
                    S.mm(pb[:, 0:n], wi[:, kc, cl:cl + 128], XT[:, kc, lo:lo + n], kc == 0, kc == 7,
                         [wi.k(hh)] + R(XT), R(pb))
                g, sg, l = tg[q], tsg[q], tl[q]
                S.ts(g[:, 0:n], pa[:, 0:n], b_inT[:, e, j:j + 1], 7.0, ALU.add, ALU.min, R(pa, b_inT), R(g))
                S.act(sg[:, 0:n], g[:, 0:n], AF.Sigmoid, R(g), R(sg), scale=1.702)
                S.ts(l[:, 0:n], pb[:, 0:n], b_inT[:, e, 8 + j:9 + j], 8.0, ALU.add, ALU.min, R(pb, b_inT), R(l))
                S.stt(l[:, 0:n], l[:, 0:n], -6.0, Gb[:, offs[ci]:offs[ci] + n], ALU.max, ALU.mult, R(l, Gb), R(l))
                S.tt(g[:, 0:n], g[:, 0:n], sg[:, 0:n], ALU.mult, R(g, sg), R(g), eng="pool")
                S.tt(actT[:, j, offs[ci]:offs[ci] + n], g[:, 0:n], l[:, 0:n], ALU.mult, R(g, l), [actT.k(j)])
            if e + 1 < 32 and j in (3, 7):
                load_wi(e + 1, j // 4)
        for dc in range(8):
            for ci, (lo, n) in enumerate(chunks):
                p = PS[4 + it % 2]
                it += 1
                for j in range(8):
                    S.mm(p[:, 0:n], wo[:, j, dc * 128:(dc + 1) * 128], actT[:, j, offs[ci]:offs[ci] + n], j == 0, j == 7,
                         R(wo) + [actT.k(j)], R(p))
                S.tt(acc[:, dc, offs[ci]:offs[ci] + n], acc[:, dc, offs[ci]:offs[ci] + n], p[:, 0:n], ALU.add,
                     R(p) + [acc.k(dc)], [acc.k(dc)])
    return offs


def ln2_out(S, C, acc, offs, chunks, cols, x1d, xout, tmp, mk):
    PS = C.PS
    x1b = S.carve("x1b", [8, 512], F32)
    r = S.carve("r2", [8, 512], F32)
    x2 = S.carve("x2", [8, 512], F32)
    tA = S.carve("tA2", [512], F32)
    orow = [S.carve("orow%d" % i, [1024], F32) for i in range(2)]
    lt = ln_tmp(S, 512, R(x2))
    it = 0
    for ci, (lo, n) in enumerate(chunks):
        col = cols[ci]
        S.dma(x1b[:, :, 0:n], x1d[:, :, lo:lo + n], R(x1d), R(x1b))
        for dc in range(8):
            S.act(tA[:, 0:n], acc[:, dc, offs[ci]:offs[ci] + n], AF.Identity, [acc.k(dc)] + R(C.mod), R(tA),
                  scale=C.mod[:, G2 + dc, col:col + 1])
            S.stt(r[:, dc, 0:n], x1b[:, dc, 0:n], ALPHA, tA[:, 0:n], ALU.mult, ALU.add, R(x1b, tA), R(r))
        layer_norm_fm(S, C, r, n, 2, lambda c: x2[:, c, 0:n], lt)
        for i in range(n // 128):
            for dc in range(8):
                p = PS[dc // 4]
                S.tr(p[:, (dc % 4) * 128:(dc % 4 + 1) * 128], x2[:, dc, i * 128:(i + 1) * 128], C.ident, R(x2, C.cs), R(p))
            ot = orow[it % 2]
            it += 1
            S.cp(ot[:, 0:512], PS[0][:, :], R(PS[0]), R(ot), eng="act")
            S.cp(ot[:, 512:1024], PS[1][:, :], R(PS[1]), R(ot))
            S.dma(xout[lo + i * 128:lo + (i + 1) * 128, :], ot[:], R(ot), R(xout))


NT = 34
TOK = 4352
OWN = [0] + list(range(2, 18))
NOWN = 2176


def own_segments(t0, t1):
    segs = []
    for lo, hi, base in ((0, 128, 0), (256, 2304, 128)):
        a, b = max(t0, lo), min(t1, hi)
        if a < b:
            segs.append((a, b - a, base + a - lo))
    return segs


class _Stop(Exception):
    pass


def build_l0(debug=False):
    import os
    stop = int(os.environ.get("STOP", "99"))

    def chk(n):
        if stop == n:
            raise _Stop()

    nc = bass.Bass("TRN2", target_bir_lowering=False)
    es = ExitStack()
    with es:
      S = Sched(nc, es)
      try:
        _build(S, nc, chk)
      except _Stop:
        print("STOPPED at", stop)
        S.finish([])
      S.emit()
    return nc


def _build(S, nc, chk, fused=False):
    if True:
        C = Ctx()
        D = lambda name, shape, dt=F32, kind="ExternalInput": S.dram(name, shape, dt, kind)
        xs = D("xs", [TOK, 1024])
        cT = D("cT", [128, 8, 2])
        mod_w = D("mod_w", [1024, 6144])
        mod_bT = D("mod_bT", [128, 48])
        lnp_d = D("lnp", [128, 4, 8])
        win = D("win", [8, 1024, 776])
        convp_d = D("convp", [128, 8, 4, 6])
        ssdp_d = D("ssdp", [1, 160])
        normw_d = D("normw", [128, 16])
        wout_d = D("wout", [2048, 1024])
        rw_d = D("rw", [128, 8, 32])
        rb_d = D("rb", [1, 32])
        xout = D("xout", [NOWN, 1024], F32, "Internal" if fused else "ExternalOutput")
        hTd = D("hTd", [128, 8, TOK], BF16, "Internal")
        ygd = D("ygd", [128, 16, NOWN], BF16, "Internal")
        x1d = D("x1d", [128, 8, NOWN], F32, "Internal")
        setup_common(S, C, True)
        PS = C.PS
        if not hasattr(S, "ar"):
            S.arena(49800)
        C.mod = S.carve("mod", [48, 2], F32)
        C.lnp = S.carve("lnp", [4, 8], F32)
        S.dma(C.lnp[:], lnp_d[:], [], R(C.lnp))
        modulation(S, C, mod_w, mod_bT, cT)
        base = S.mark()
        chk(0)

        xr = [S.carve("xr%d" % i, [1024], F32) for i in range(2)]
        hb = [S.carve("hb%d" % i, [8, 512], BF16) for i in range(2)]
        for blk in range(9):
            t0 = blk * 4
            nt = min(4, NT - t0)
            h = hb[blk % 2]
            for i in range(nt):
                t = t0 + i
                col = 1 if t < 2 else 0
                x = xr[t % 2]
                S.dma(x[:], xs[t * 128:(t + 1) * 128, :], [], R(x))
                for kc in range(8):
                    p = PS[kc // 4 + 2 * (t % 2)]
                    S.tr(p[:, (kc % 4) * 128:(kc % 4 + 1) * 128], x[:, kc * 128:(kc + 1) * 128], C.ident, R(x, C.cs), R(p))
                for kc in range(8):
                    p = PS[kc // 4 + 2 * (t % 2)]
                    S.act(h[:, kc, i * 128:(i + 1) * 128], p[:, (kc % 4) * 128:(kc % 4 + 1) * 128], AF.Identity,
                          R(p, C.mod), R(h), scale=C.mod[:, SC1 + kc, col:col + 1], bias=C.mod[:, SH1 + kc, col:col + 1])
            S.dma(hTd[:, :, t0 * 128:(t0 + nt) * 128], h[:, :, 0:nt * 128], R(h), R(hTd))
        S.reset(base)
        chk(1)

        convp = S.carve("convp", [8, 4, 6], F32)
        S.dma(convp[:], convp_d[:], [], R(convp))
        ssdp = S.carve("ssdp", [8, 20], F32)
        S.dma(ssdp[:], ssdp_d.h.partition_broadcast(128).rearrange("p o (g k) -> p (o g) k", k=20), [], R(ssdp))
        normw = S.carve("normw", [16], F32)
        S.dma(normw[:], normw_d[:], [], R(normw))
        wg = [S.carve("wg%d" % i, [8, 776], BF16) for i in range(2)]
        hblk = [S.carve("hblk%d" % i, [8, 512], BF16) for i in range(2)]
        raw = [S.carve("raw%d" % i, [4358], BF16) for i in range(2)]
        for r_ in raw:
            S.memset(r_[:], 0.0, R(r_))
        cv = [S.carve("cv%d" % i, [1024], F32) for i in range(2)]
        fm = S.carve("fm", [TOK], BF16)
        bT = S.carve("bT", [TOK], BF16)
        cTt = S.carve("cTt", [2304], BF16)
        zT = S.carve("zT", [2, NOWN], BF16)
        xtok = S.carve("xtok", [NT, 256], BF16)
        btok = S.carve("btok", [NT, 128], BF16)
        yacc = S.carve("yacc", [17, 256], F32, nreg=17)
        ygs = S.carve("ygs", [2, NOWN], BF16)
        dtv = S.carve("dtv", [2, NT, 4], F32)
        dte = S.carve("dte", [2, NT, 4], F32)
        lndt = S.carve("lndt", [2, NT, 4], F32)
        la = S.carve("la", [2, NT, 4], F32)
        biasL = S.carve("biasL", [2, NT, 4], F32)
        ecum = S.carve("ecum", [2, NT, 4], F32)
        wdec = S.carve("wdec", [2, NT, 4], F32)
        cdec = S.carve("cdec", [2, NT, 4], F32)
        aneg = S.carve("aneg", [8], F32)
        laU = [S.carve("laU%d" % i, [4, 128], F32) for i in range(2)]
        NEG4 = [S.carve("NEG4%d" % i, [4, 128], F32) for i in range(2)]
        for d in range(2):
            S.cp(NEG4[d][:], C.NEG1[d].unsqueeze(1).to_broadcast([128, 4, 128]), R(C.cs), R(NEG4[d]))
        Lp = [S.carve("Lp%d" % i, [4, 128], F32) for i in range(2)]
        Mp = [S.carve("Mp%d" % i, [4, 128], BF16) for i in range(2)]
        t1 = [S.carve("t1%d" % i, [4, 64], F32) for i in range(2)]
        t2 = [S.carve("t2%d" % i, [4, 64], F32) for i in range(2)]
        xdec = [S.carve("xdec%d" % i, [4, 64], BF16) for i in range(2)]
        st = [S.carve("st%d" % i, [4, 64], F32) for i in range(2)]
        stbf = [S.carve("stbf%d" % i, [4, 64], BF16) for i in range(2)]
        yz = S.carve("yz", [2, 512], F32)
        sqg = S.carve("sqg", [2, 512], F32)
        rsg = S.carve("rsg", [512], F32)
        hv = hTd.h
        blocks = [(b * 512, min(512, TOK - b * 512)) for b in range(9)]
        orderA = list(range(NT))
        orderB = [1, 0] + list(range(NT - 1, 1, -1))
        flat = lambda t: t[:].rearrange("p a b -> p (a b)")

        for g in range(8):
            w = wg[g % 2]
            S.dma(w[:], win.h[g].rearrange("(kc p) f -> p kc f", p=128), [], R(w), eng="pool")

            def inproj_pass(chunks, evac, with_dt):
                for bi, (b0, n) in enumerate(blocks):
                    hbk = hblk[bi % 2]
                    S.dma(hbk[:, :, 0:n], hv[:, :, b0:b0 + n], R(hTd), R(hbk))
                    for ci, off in enumerate(chunks):
                        p = PS[ci % 2]
                        for kc in range(8):
                            S.mm(p[:, 0:n], w[:, kc, off:off + 128], hbk[:, kc, 0:n], kc == 0, kc == 7, R(w, hbk), R(p))
                        evac(ci, p, b0, n)
                    if with_dt:
                        for i in range(n // 128):
                            t = b0 // 128 + i
                            for kc in range(8):
                                S.mm(PS[2][:, t * 8:(t + 1) * 8], hbk[:, kc, i * 128:(i + 1) * 128], w[:, kc, 768:776],
                                     kc == 0, kc == 7, R(w, hbk), R(PS[2]))

            def evac_raw(ci, p, b0, n):
                r_ = raw[ci]
                for lo, hi, sh in ((0, 256, 2), (256, TOK, 4)):
                    a, b = max(b0, lo), min(b0 + n, hi)
                    if a < b:
                        S.cp(r_[:, a + sh:b + sh], p[:, a - b0:b - b0], R(p), R(r_), eng="act")

            def conv_silu(slot, ch, dst, limit=TOK):
                r_ = raw[slot]
                segs = [(0, 256, 0)] + [(256 + 1024 * k, 1024, 258 + 1024 * k) for k in range(4)]
                segs = [sg for sg in segs if sg[0] < limit]
                for si, (olo, n, rlo) in enumerate(segs):
                    c_ = cv[si % 2]
                    S.ts(c_[:, 0:n], r_[:, rlo:rlo + n], convp[:, g, ch, 0:1], convp[:, g, ch, 5:6], ALU.mult, ALU.add,
                         R(r_, convp), R(c_))
                    for j in range(1, 5):
                        S.stt(c_[:, 0:n], r_[:, rlo + j:rlo + j + n], convp[:, g, ch, j:j + 1], c_[:, 0:n], ALU.mult, ALU.add,
                              R(r_, convp, c_), R(c_))
                    S.act(dst[:, olo:olo + n], c_[:, 0:n], AF.Silu, R(c_), R(dst))

            if g == 0: chk(20)
            inproj_pass([256, 384], evac_raw, False)
            if g == 0: chk(21)
            for xi in range(2):
                conv_silu(xi, xi, fm)
                for t0 in range(0, NT, 4):
                    nt = min(4, NT - t0)
                    pb = PS[3 + (t0 // 4) % 2][:, 0:256].bitcast(BF16)
                    pk = PS[3 + (t0 // 4) % 2]
                    for i in range(nt):
                        S.tr(pb[:, i * 128:(i + 1) * 128], fm[:, (t0 + i) * 128:(t0 + i + 1) * 128], C.identbf[:],
                             R(fm, C.identbf), R(pk))
                    S.cp(xtok[:, t0:t0 + nt, xi * 128:(xi + 1) * 128],
                         pb[:, 0:nt * 128].rearrange("p (a b) -> p a b", b=128), R(pk), R(xtok), eng="act")
            if g == 0: chk(22)
            inproj_pass([512, 640], evac_raw, False)
            conv_silu(0, 2, bT)
            conv_silu(1, 3, cTt, 2304)
            for t0 in range(0, NT, 4):
                nt = min(4, NT - t0)
                pb = PS[3 + (t0 // 4) % 2][:, 0:256].bitcast(BF16)
                pk = PS[3 + (t0 // 4) % 2]
                for i in range(nt):
                    S.tr(pb[:, i * 128:(i + 1) * 128], bT[:, (t0 + i) * 128:(t0 + i + 1) * 128], C.identbf[:],
                         R(bT, C.identbf), R(pk))
                S.cp(btok[:, t0:t0 + nt, :], pb[:, 0:nt * 128].rearrange("p (a b) -> p a b", b=128), R(pk), R(btok), eng="act")

            if g == 0: chk(23)
            def evac_z(ci, p, b0, n):
                for a, m, o in own_segments(b0, b0 + n):
                    S.act(zT[:, ci, o:o + m], p[:, a - b0:a - b0 + m], AF.Silu, R(p), R(zT))

            inproj_pass([0, 128], evac_z, True)
            if g == 0: chk(24)
            dtb = ssdp[:, g, 0:8].rearrange("p (d h) -> p d h", h=4).unsqueeze(2).to_broadcast([128, 2, NT, 4])
            if g == 0: chk(240)
            S.tt(dtv[:], PS[2][:, 0:NT * 8].rearrange("p (c d h) -> p d c h", d=2, h=4), dtb, ALU.add, R(PS[2], ssdp), R(dtv))
            if g == 0: chk(241)
            S.act(dte[:], dtv[:], AF.Exp, R(dtv), R(dte))
            if g == 0: chk(242)
            S.act(dtv[:], dte[:], AF.Ln, R(dte), R(dtv), bias=1.0)
            if g == 0: chk(243)
            S.act(lndt[:], dtv[:], AF.Ln, R(dtv), R(lndt))
            if g == 0: chk(244)
            S.act(aneg[:], ssdp[:, g, 8:16], AF.Exp, R(ssdp), R(aneg))
            if g == 0: chk(245)
            S.ts(aneg[:], aneg[:], -1.0, None, ALU.mult, None, R(aneg), R(aneg))
            if g == 0: chk(246)
            S.tt(la[:], dtv[:], aneg[:].rearrange("p (d h) -> p d h", h=4).unsqueeze(2).to_broadcast([128, 2, NT, 4]),
                 ALU.mult, R(dtv, aneg), R(la))
            laf = la[:].rearrange("p d c h -> p (d c h)")
            if g == 0: chk(247)
            for d in range(2):
                S.mm(PS[5][:, d * 136:(d + 1) * 136], C.U[d], laf[:, d * 136:(d + 1) * 136], True, True, R(la, C.cs), R(PS[5]))
            if g == 0: chk(248)
            S.mm(PS[6][:, 0:272], C.ones, laf, True, True, R(la, C.cs), R(PS[6]))
            fl = lambda t: t[:].rearrange("p d c h -> p (d c h)")
            if g == 0: chk(249)
            S.tt(fl(biasL), fl(lndt), PS[5][:, 0:272], ALU.subtract, R(lndt, PS[5]), R(biasL))
            if g == 0: chk(250)
            S.cp(fl(ecum), PS[5][:, 0:272], R(PS[5]), R(ecum))
            S.act(fl(ecum), fl(ecum), AF.Exp, R(ecum), R(ecum))
            if g == 0: chk(251)
            S.tt(fl(wdec), fl(biasL), PS[6][:, 0:272], ALU.add, R(biasL, PS[6]), R(wdec))
            if g == 0: chk(252)
            S.act(fl(wdec), fl(wdec), AF.Exp, R(wdec), R(wdec))
            if g == 0: chk(253)
            S.cp(fl(cdec), PS[6][:, 0:272], R(PS[6]), R(cdec))
            S.act(fl(cdec), fl(cdec), AF.Exp, R(cdec), R(cdec))

            if g == 0: chk(25)
            Dh = ssdp[:, g, 16:20].unsqueeze(2).to_broadcast([128, 4, 64])
            for oc, c in enumerate(OWN):
                S.tt(yacc[:, oc, :].rearrange("p (h q) -> p h q", q=64), xtok[:, c, :].rearrange("p (h q) -> p h q", q=64),
                     Dh, ALU.mult, R(xtok, ssdp), [yacc.k(oc)], eng="pool")
            for d in range(2):
                S.memset(st[d][:], 0.0, R(st[d]))
                S.memset(stbf[d][:], 0.0, R(stbf[d]))
            step = 0
            for i in range(NT):
                for d, order in ((0, orderA), (1, orderB)):
                    c = order[i]
                    par = step % 2
                    step += 1
                    tc0 = c * 128
                    if c in OWN:
                        oc = OWN.index(c)
                        pCB, pseg, pY = PS[0], PS[1 + par], PS[3 + par]
                        S.mm(pCB[:, 0:128], bT[:, tc0:tc0 + 128], cTt[:, tc0:tc0 + 128], True, True, R(bT, cTt), R(pCB))
                        S.tt(laU[par][:], C.U[d].unsqueeze(1).to_broadcast([128, 4, 128]),
                             la[:, d, c, :].unsqueeze(2).to_broadcast([128, 4, 128]), ALU.mult, R(la, C.cs), R(laU[par]))
                        S.mm(pseg[:, :], C.ones, flat(laU[par]), True, False, R(laU[par], C.cs), R(pseg))
                        S.mm(pseg[:, :], C.ident, flat(NEG4[d]), False, True, R(NEG4[d], C.cs), R(pseg))
                        for h in range(4):
                            S.act(Lp[par][:, h, :], pseg[:, h * 128:(h + 1) * 128], AF.Exp, R(pseg, biasL), R(Lp[par]),
                                  bias=biasL[:, d, c, h:h + 1])
                        S.tt(Mp[par][:], Lp[par][:], pCB[:, 0:128].unsqueeze(1).to_broadcast([128, 4, 128]), ALU.mult,
                             R(Lp[par], pCB), R(Mp[par]))
                        for h in range(4):
                            S.mm(pY[:, h * 64:(h + 1) * 64], Mp[par][:, h, :], xtok[:, c, h * 64:(h + 1) * 64], True, True,
                                 R(Mp[par], xtok), R(pY))
                        S.mm(pY[:, 256:512], cTt[:, tc0:tc0 + 128], flat(stbf[d]), True, True, R(cTt, stbf[d]), R(pY))
                        S.tt(t1[par][:], pY[:, 256:512].rearrange("p (h q) -> p h q", q=64),
                             ecum[:, d, c, :].unsqueeze(2).to_broadcast([128, 4, 64]), ALU.mult, R(pY, ecum), R(t1[par]))
                        S.tt(flat(t2[par]), flat(t1[par]), pY[:, 0:256], ALU.add, R(t1[par], pY), R(t2[par]))
                        S.tt(yacc[:, oc, :], yacc[:, oc, :], flat(t2[par]), ALU.add, R(t2[par]) + [yacc.k(oc)], [yacc.k(oc)],
                             eng="pool")
                    if any(cc in OWN for cc in order[i + 1:]):
                        pds = PS[5 + par]
                        S.tt(xdec[par][:], xtok[:, c, :].rearrange("p (h q) -> p h q", q=64),
                             wdec[:, d, c, :].unsqueeze(2).to_broadcast([128, 4, 64]), ALU.mult, R(xtok, wdec), R(xdec[par]),
                             eng="pool")
                        S.mm(pds[:, 0:256], btok[:, c, :], flat(xdec[par]), True, True, R(btok, xdec[par]), R(pds))
                        S.tt(st[d][:], st[d][:], cdec[:, d, c, :].unsqueeze(2).to_broadcast([128, 4, 64]), ALU.mult,
                             R(st[d], cdec), R(st[d]), eng="pool")
                        S.tt(flat(st[d]), flat(st[d]), pds[:, 0:256], ALU.add, R(st[d], pds), R(st[d]))
                        S.cp(stbf[d][:], st[d][:], R(st[d]), R(stbf[d]), eng="act")

            if g == 0: chk(26)
            for o0 in range(0, 17, 4):
                nt = min(4, 17 - o0)
                n = nt * 128
                for i in range(nt):
                    for j in range(2):
                        S.tr(PS[5 + j][:, i * 128:(i + 1) * 128], yacc[:, o0 + i, j * 128:(j + 1) * 128], C.ident,
                             [yacc.k(o0 + i)] + R(C.cs), R(PS[5 + j]))
                for j in range(2):
                    S.tt(yz[:, j, 0:n], PS[5 + j][:, 0:n], zT[:, j, o0 * 128:o0 * 128 + n], ALU.mult, R(PS[5 + j], zT), R(yz))
                S.tt(sqg[:, :, 0:n], yz[:, :, 0:n], yz[:, :, 0:n], ALU.mult, R(yz), R(sqg), eng="pool")
                for j in range(2):
                    S.mm(PS[7][:, 0:n], C.ones, sqg[:, j, 0:n], j == 0, j == 1, R(sqg, C.cs), R(PS[7]))
                S.act(rsg[:, 0:n], PS[7][:, 0:n], AF.Sqrt, R(PS[7]), R(rsg), scale=1.0 / 256, bias=1e-5)
                S.recip(rsg[:, 0:n], rsg[:, 0:n], R(rsg), R(rsg))
                S.tt(yz[:, :, 0:n], yz[:, :, 0:n], rsg[:, 0:n].unsqueeze(1).to_broadcast([128, 2, n]), ALU.mult,
                     R(yz, rsg), R(yz))
                for j in range(2):
                    S.act(ygs[:, j, o0 * 128:o0 * 128 + n], yz[:, j, 0:n], AF.Identity, R(yz, normw), R(ygs),
                          scale=normw[:, 2 * g + j:2 * g + j + 1])
            S.dma(ygd[:, 2 * g:2 * g + 2, :], ygs[:], R(ygs), R(ygd))
            if g == 0: chk(27)
        S.reset(base)
        chk(2)

        XT = S.carve("XT", [8, NOWN], BF16)
        gateT = S.carve("gateT", [NOWN], F32, parts=32)
        p3 = S.mark()
        wout = S.carve("wout", [16, 1024], BF16)
        for h in range(2):
            S.dma(wout[:, h * 8:(h + 1) * 8, :], wout_d.h.rearrange("(kc p) f -> p kc f", p=128)[:, h * 8:(h + 1) * 8, :],
                  [], R(wout), eng="pool")
        rw = S.carve("rw", [8, 32], F32)
        S.dma(rw[:], rw_d[:], [], R(rw))
        rb = S.carve("rb", [32], F32)
        S.dma(rb[:], rb_d.h.partition_broadcast(128).rearrange("p o k -> p (o k)"), [], R(rb))
        ygb = S.carve("ygb", [16, 512], BF16)
        xrw = S.carve("xrw", [4, 1024], F32)
        r = S.carve("r1", [8, 512], F32)
        x1t = S.carve("x1t", [8, 512], F32)
        Xf = S.carve("Xf", [8, 512], F32)
        tA = S.carve("tA", [512], F32)
        lt = ln_tmp(S, 512, R(x1t))
        rt = router_tmp(S)
        chunks3 = [(0, 128, 1)] + [(128 + 512 * k, 512, 0) for k in range(4)]
        for (o0, n, col) in chunks3:
            row0 = o0 if o0 < 128 else o0 + 128
            S.dma(ygb[:, :, 0:n], ygd[:, :, o0:o0 + n], R(ygd), R(ygb))
            S.dma(xrw[:, 0:n // 128, :], xs[row0:row0 + n, :].rearrange("(i p) f -> p i f", p=128), [], R(xrw))
            for dc in range(8):
                py, px = PS[dc % 2], PS[2 + dc % 2]
                for kc in range(16):
                    S.mm(py[:, 0:n], wout[:, kc, dc * 128:(dc + 1) * 128], ygb[:, kc, 0:n], kc == 0, kc == 15, R(wout, ygb), R(py))
                for i in range(n // 128):
                    S.tr(px[:, i * 128:(i + 1) * 128], xrw[:, i, dc * 128:(dc + 1) * 128], C.ident, R(xrw, C.cs), R(px))
                S.act(tA[:, 0:n], py[:, 0:n], AF.Identity, R(py, C.mod), R(tA), scale=C.mod[:, G1 + dc, col:col + 1])
                S.stt(r[:, dc, 0:n], px[:, 0:n], ALPHA, tA[:, 0:n], ALU.mult, ALU.add, R(px, tA), R(r))
            layer_norm_fm(S, C, r, n, 0, lambda c: x1t[:, c, 0:n], lt)
            S.dma(x1d[:, :, o0:o0 + n], x1t[:, :, 0:n], R(x1t), R(x1d))
            for dc in range(8):
                S.act(Xf[:, dc, 0:n], x1t[:, dc, 0:n], AF.Identity, R(x1t, C.mod), R(Xf),
                      scale=C.mod[:, SC2 + dc, col:col + 1], bias=C.mod[:, SH2 + dc, col:col + 1])
            S.cp(XT[:, :, o0:o0 + n], Xf[:, :, 0:n], R(Xf), R(XT), eng="pool")
            router(S, C, Xf, n, o0, rw, rb, gateT, rt)
        S.reset(p3)
        chk(3)

        mw_in = D("mw_in", [32, 1024, 2048])
        mb_inT = D("mb_inT", [128, 32, 16])
        mw_out = D("mw_out", [32, 1024, 1024])
        mb_out = D("mb_out", [32, 1024])
        b_inT = S.carve("b_inT", [32, 16], F32)
        S.dma(b_inT[:], mb_inT[:], [], R(b_inT))
        S.ts(b_inT[:, :, 8:16], b_inT[:, :, 8:16], 1.0, None, ALU.add, None, R(b_inT), R(b_inT))
        p4 = S.mark()
        tblocks = [([(0, 128), (128, 512), (640, 512)], [1, 0, 0]), ([(1152, 512), (1664, 512)], [0, 0])]
        for chunks, cols in tblocks:
            acc = S.carve("acc", [8, sum(n for _, n in chunks)], F32, nreg=8)
            m = S.mark()
            offs = moe_block(S, C, XT, gateT, chunks, mw_in, mw_out, b_inT, mb_out, acc)
            S.reset(m)
            ln2_out(S, C, acc, offs, chunks, cols, x1d, xout, None, None)
            S.reset(p4)
        if not fused:
            S.finish(R(xout))
        return xout


TOK1 = 4352
NQ = 2048
NT1 = 34
MLA_SCALE = 192.0 ** -0.5
RMS_EPS = 1e-6


class _Stop1(Exception):
    pass


def build_l1():
    import os
    stop = int(os.environ.get("STOP", "99"))

    def chk(n):
        if stop == n:
            raise _Stop1()

    nc = bass.Bass("TRN2", target_bir_lowering=False)
    es = ExitStack()
    with es:
        S = Sched(nc, es)
        try:
            _build1(S, nc, chk)
        except _Stop1:
            print("STOPPED at", stop)
            S.finish([])
        S.emit()
    return nc


def _build1(S, nc, chk, fused=False, xown=None, xall=None):
    C = Ctx()
    D = lambda name, shape, dt=F32, kind="ExternalInput": S.dram(name, shape, dt, kind)
    xs = None if fused else D("xs", [TOK1, 1024])
    NTQ = 16 if fused else 0
    cT = D("cT", [128, 8, 2])
    mod_w = D("mod_w", [1024, 6144])
    mod_bT = D("mod_bT", [128, 48])
    lnp_d = D("lnp", [128, 4, 8])
    wa_d = D("wa", [1024, 704])
    qnorm_d = D("qnorm", [128, 3])
    kvnorm_d = D("kvnorm", [128, 2])
    wqb_d = D("wqb", [384, 1536])
    wkvb_d = D("wkvb", [256, 2048])
    wo_d = D("wo", [1024, 1024])
    ropeq_d = D("ropeq", [64, 2, NQ])
    ropek_d = D("ropek", [64, 2, TOK1])
    rm_d = D("rm", [64, 64])
    rw_d = D("rw", [128, 8, 32])
    rb_d = D("rb", [1, 32])
    xout = D("xout", [NQ, 1024], F32, "ExternalOutput")
    hTd = D("hTd", [128, 8, TOK1 + NTQ * 128], BF16, "Internal")
    x1d = D("x1d", [128, 8, NQ], F32, "Internal")
    setup_common(S, C, True)
    PS = C.PS
    if not hasattr(S, "ar"):
        S.arena(49800)
    C.mod = S.carve("mod", [48, 2], F32)
    C.lnp = S.carve("lnp", [4, 8], F32)
    S.dma(C.lnp[:], lnp_d[:], [], R(C.lnp))
    onesbf = S.carve("onesbf", [128], BF16)
    S.cp(onesbf[:], C.ones, R(C.cs), R(onesbf))
    c128 = S.carve("c128", [128], BF16)
    S.ts(c128[:], C.ones, 1.0 / 128, None, ALU.mult, None, R(C.cs), R(c128))
    rm = S.carve("rm", [64], F32, parts=64)
    S.dma(rm[:], rm_d[:], [], R(rm))
    modulation(S, C, mod_w, mod_bT, cT)
    XT = S.carve("XT", [8, NQ], BF16)
    gateT = S.carve("gateT", [NQ], F32, parts=32)
    markA = S.mark()
    oT = S.carve("oT", [8, NQ], BF16, nreg=8)
    markB = S.mark()
    chk(0)

    xr = [S.carve("xr%d" % i, [1024], F32) for i in range(2)]
    hb = [S.carve("hb%d" % i, [8, 512], BF16) for i in range(2)]
    ntl = NT1 + NTQ
    for blk in range((ntl + 3) // 4):
        t0 = blk * 4
        nt = min(4, ntl - t0)
        h = hb[blk % 2]
        for i in range(nt):
            t = t0 + i
            x = xr[t % 2]
            if not fused:
                col = 1 if t >= 32 else 0
                S.dma(x[:], xs[t * 128:(t + 1) * 128, :], [], R(x))
            elif t < NT1:
                col = 1 if t in (0, 1) else 0
                S.dma(x[:], xall[t * 128:(t + 1) * 128, :], R(xall), R(x))
            else:
                col = 0
                S.dma(x[:], xown[128 + (t - NT1) * 128:128 + (t - NT1 + 1) * 128, :], R(xown), R(x))
            for kc in range(8):
                p = PS[kc // 4 + 2 * (t % 2)]
                S.tr(p[:, (kc % 4) * 128:(kc % 4 + 1) * 128], x[:, kc * 128:(kc + 1) * 128], C.ident, R(x, C.cs), R(p))
            for kc in range(8):
                p = PS[kc // 4 + 2 * (t % 2)]
                S.act(h[:, kc, i * 128:(i + 1) * 128], p[:, (kc % 4) * 128:(kc % 4 + 1) * 128], AF.Identity,
                      R(p, C.mod), R(h), scale=C.mod[:, SC1 + kc, col:col + 1], bias=C.mod[:, SH1 + kc, col:col + 1])
        S.dma(hTd[:, :, t0 * 128:(t0 + nt) * 128], h[:, :, 0:nt * 128], R(h), R(hTd))
    S.reset(markB)
    chk(1)

    kvn = S.carve("kvn", [2, TOK1], BF16)
    qln = S.carve("qln", [3, NQ], BF16)
    KrT = S.carve("KrT", [TOK1], BF16)
    S.memset(KrT[64:128, :], 0.0, R(KrT))
    markC = S.mark()
    wa = S.carve("wa", [8, 704], BF16)
    S.dma(wa[:], wa_d.h.rearrange("(kc p) f -> p kc f", p=128), [], R(wa), eng="pool")
    qnorm = S.carve("qnorm", [3], F32)
    S.dma(qnorm[:], qnorm_d[:], [], R(qnorm))
    kvnorm = S.carve("kvnorm", [2], F32)
    S.dma(kvnorm[:], kvnorm_d[:], [], R(kvnorm))
    hblk = [S.carve("hblk%d" % i, [8, 512], BF16) for i in range(2)]
    kvl = S.carve("kvl", [2, 512], F32)
    ql = S.carve("ql", [3, 512], F32)
    krl = S.carve("krl", [512], F32)
    sq = S.carve("sq", [3, 512], F32)
    rs = S.carve("rs", [512], F32)
    rk = S.carve("rk", [2, 512], F32)
    t1 = S.carve("t1", [512], F32)
    t2 = S.carve("t2", [512], F32)
    blocks = [(b * 512, min(512, TOK1 - b * 512)) for b in range(9)]

    def rms_fm(src, nch, width, g, dst, d0, n):
        S.tt(sq[:, 0:nch, 0:n], src[:, 0:nch, 0:n], src[:, 0:nch, 0:n], ALU.mult, R(src), R(sq), eng="pool")
        for c in range(nch):
            S.mm(PS[6][:, 0:n], C.ones, sq[:, c, 0:n], c == 0, c == nch - 1, R(sq, C.cs), R(PS[6]))
        S.cp(rs[:, 0:n], PS[6][:, 0:n], R(PS[6]), R(rs))
        S.act(rs[:, 0:n], rs[:, 0:n], AF.Sqrt, R(rs), R(rs), scale=1.0 / width, bias=RMS_EPS)
        S.recip(rs[:, 0:n], rs[:, 0:n], R(rs), R(rs))
        S.tt(src[:, 0:nch, 0:n], src[:, 0:nch, 0:n], rs[:, 0:n].unsqueeze(1).to_broadcast([128, nch, n]), ALU.mult,
             R(src, rs), R(src))
        for c in range(nch):
            S.act(dst[:, c, d0:d0 + n], src[:, c, 0:n], AF.Identity, R(src, g), R(dst), scale=g[:, c:c + 1])

    def rope(src, tab, dst_ap, n, dstkeys):
        S.mm(PS[7][0:64, 0:n], rm[0:64, 0:64], src[0:64, 0:n], True, True, R(src, rm), R(PS[7]))
        S.tt(t1[0:64, 0:n], src[0:64, 0:n], tab[0:64, 0, 0:n], ALU.mult, R(src, tab), R(t1))
        S.tt(t2[0:64, 0:n], PS[7][0:64, 0:n], tab[0:64, 1, 0:n], ALU.mult, R(PS[7], tab), R(t2))
        S.tt(dst_ap, t1[0:64, 0:n], t2[0:64, 0:n], ALU.add, R(t1, t2), dstkeys, eng="pool")

    for bi, (b0, n) in enumerate(blocks):
        hbk = hblk[bi % 2]
        S.dma(hbk[:, :, 0:n], hTd.h[:, :, b0:b0 + n], R(hTd), R(hbk))
        S.dma(rk[0:64, :, 0:n], ropek_d[:, :, b0:b0 + n], [], R(rk))
        it = 0
        for c in range(2):
            p = PS[it % 2]
            it += 1
            for kc in range(8):
                S.mm(p[:, 0:n], wa[:, kc, 384 + c * 128:384 + (c + 1) * 128], hbk[:, kc, 0:n], kc == 0, kc == 7, R(wa, hbk), R(p))
            S.cp(kvl[:, c, 0:n], p[:, 0:n], R(p), R(kvl))
        p = PS[2]
        for kc in range(8):
            S.mm(p[0:64, 0:n], wa[:, kc, 640:704], hbk[:, kc, 0:n], kc == 0, kc == 7, R(wa, hbk), R(p))
        S.cp(krl[0:64, 0:n], p[0:64, 0:n], R(p), R(krl))
        if b0 < NQ and not fused:
            for c in range(3):
                p = PS[it % 2]
                it += 1
                for kc in range(8):
                    S.mm(p[:, 0:n], wa[:, kc, c * 128:(c + 1) * 128], hbk[:, kc, 0:n], kc == 0, kc == 7, R(wa, hbk), R(p))
                S.cp(ql[:, c, 0:n], p[:, 0:n], R(p), R(ql))
            rms_fm(ql, 3, 384, qnorm, qln, b0, n)
        rms_fm(kvl, 2, 256, kvnorm, kvn, b0, n)
        rope(krl, rk, KrT[0:64, b0:b0 + n], n, R(KrT))
    if fused:
        for qi in range(4):
            hbk = hblk[qi % 2]
            n = 512
            b0 = qi * 512
            S.dma(hbk[:, :, 0:n], hTd.h[:, :, TOK1 + b0:TOK1 + b0 + n], R(hTd), R(hbk))
            for c in range(3):
                p = PS[c % 2]
                for kc in range(8):
                    S.mm(p[:, 0:n], wa[:, kc, c * 128:(c + 1) * 128], hbk[:, kc, 0:n], kc == 0, kc == 7, R(wa, hbk), R(p))
                S.cp(ql[:, c, 0:n], p[:, 0:n], R(p), R(ql))
            rms_fm(ql, 3, 384, qnorm, qln, b0, n)
    S.reset(markC)
    chk(2)

    wqb = S.carve("wqb", [3, 1536], BF16)
    S.dma(wqb[:], wqb_d.h.rearrange("(kc p) f -> p kc f", p=128), [], R(wqb), eng="pool")
    wkvb = S.carve("wkvb", [2, 2048], BF16)
    S.dma(wkvb[:], wkvb_d.h.rearrange("(kc p) f -> p kc f", p=128), [], R(wkvb), eng="pool")
    rq = S.carve("rq", [2, 512], F32)
    KnT = S.carve("KnT", [TOK1], BF16)
    V = S.carve("V", [NT1, 128], BF16)
    QnT = S.carve("QnT", [NQ], BF16)
    QrT = S.carve("QrT", [NQ], BF16)
    S.memset(QrT[64:128, :], 0.0, R(QrT))
    negm = S.carve("negm", [NQ], BF16)
    pT = [S.carve("pT%d" % i, [512], BF16) for i in range(3)]
    sqb = S.carve("sqb", [512], BF16)
    sqr = S.carve("sqr", [512], BF16)
    qrl = S.carve("qrl", [512], F32)
    t1 = S.carve("t1b", [512], F32)
    t2 = S.carve("t2b", [512], F32)
    tq = S.carve("tq", [512], F32)
    rden = S.carve("rden", [512], F32)
    mx = S.carve("mx", [16], F32)
    kmax = S.carve("kmax", [4], F32)
    for bi, (b0, n) in enumerate(blocks):
        S.tt(sqr[0:64, 0:n], KrT[0:64, b0:b0 + n], KrT[0:64, b0:b0 + n], ALU.mult, R(KrT), R(sqr), eng="pool")
        S.mm(PS[6][:, 0:n], onesbf[0:64, :], sqr[0:64, 0:n], True, True, R(sqr, onesbf), R(PS[6]))
        S.reduce(mx[:, bi:bi + 1], PS[6][:, 0:n], AX.X, ALU.max, R(PS[6]), R(mx))
    S.reduce(kmax[:, 0:1], mx[:, 0:9], AX.X, ALU.max, R(mx), R(kmax))
    qblocks = [(q * 512, 512) for q in range(4)]
    step = 0
    for h in range(8):
        for bi, (b0, n) in enumerate(blocks):
            p = PS[bi % 2]
            for kc in range(2):
                S.mm(p[:, 0:n], wkvb[:, kc, h * 128:(h + 1) * 128], kvn[:, kc, b0:b0 + n], kc == 0, kc == 1, R(wkvb, kvn), R(p))
            S.cp(KnT[:, b0:b0 + n], p[:, 0:n], R(p), R(KnT), eng="act")
            S.tt(sqb[:, 0:n], KnT[:, b0:b0 + n], KnT[:, b0:b0 + n], ALU.mult, R(KnT), R(sqb), eng="pool")
            S.mm(PS[6][:, 0:n], onesbf[:], sqb[:, 0:n], True, True, R(sqb, onesbf), R(PS[6]))
            S.reduce(mx[:, bi:bi + 1], PS[6][:, 0:n], AX.X, ALU.max, R(PS[6]), R(mx))
        S.reduce(kmax[:, 1:2], mx[:, 0:9], AX.X, ALU.max, R(mx), R(kmax))
        S.tt(kmax[:, 2:3], kmax[:, 0:1], kmax[:, 1:2], ALU.add, R(kmax), R(kmax))
        for t0 in range(0, NT1, 4):
            nt = min(4, NT1 - t0)
            p = PS[2 + (t0 // 4) % 2]
            for i in range(nt):
                for kc in range(2):
                    S.mm(p[:, i * 128:(i + 1) * 128], kvn[:, kc, (t0 + i) * 128:(t0 + i + 1) * 128],
                         wkvb[:, kc, 1024 + h * 128:1024 + (h + 1) * 128], kc == 0, kc == 1, R(wkvb, kvn), R(p))
            S.cp(V[:, t0:t0 + nt, :], p[:, 0:nt * 128].rearrange("p (a b) -> p a b", b=128), R(p), R(V), eng="act")
        for (q0, n) in qblocks:
            p = PS[0]
            for kc in range(3):
                S.mm(p[:, 0:n], wqb[:, kc, h * 128:(h + 1) * 128], qln[:, kc, q0:q0 + n], kc == 0, kc == 2, R(wqb, qln), R(p))
            S.cp(QnT[:, q0:q0 + n], p[:, 0:n], R(p), R(QnT), eng="act")
            p = PS[1]
            for kc in range(3):
                S.mm(p[0:64, 0:n], wqb[:, kc, 1024 + h * 64:1024 + (h + 1) * 64], qln[:, kc, q0:q0 + n], kc == 0, kc == 2,
                     R(wqb, qln), R(p))
            S.cp(qrl[0:64, 0:n], p[0:64, 0:n], R(p), R(qrl))
            S.dma(rq[0:64, :, 0:n], ropeq_d[:, :, q0:q0 + n], [], R(rq))
            S.mm(PS[7][0:64, 0:n], rm[0:64, 0:64], qrl[0:64, 0:n], True, True, R(qrl, rm), R(PS[7]))
            S.tt(t1[0:64, 0:n], qrl[0:64, 0:n], rq[0:64, 0, 0:n], ALU.mult, R(qrl, rq), R(t1))
            S.tt(t2[0:64, 0:n], PS[7][0:64, 0:n], rq[0:64, 1, 0:n], ALU.mult, R(PS[7], rq), R(t2))
            S.tt(QrT[0:64, q0:q0 + n], t1[0:64, 0:n], t2[0:64, 0:n], ALU.add, R(t1, t2), R(QrT), eng="pool")
            S.tt(sqb[:, 0:n], QnT[:, q0:q0 + n], QnT[:, q0:q0 + n], ALU.mult, R(QnT), R(sqb), eng="pool")
            S.tt(sqr[0:64, 0:n], QrT[0:64, q0:q0 + n], QrT[0:64, q0:q0 + n], ALU.mult, R(QrT), R(sqr), eng="pool")
            S.mm(PS[6][:, 0:n], onesbf[:], sqb[:, 0:n], True, False, R(sqb, onesbf), R(PS[6]))
            S.mm(PS[6][:, 0:n], onesbf[0:64, :], sqr[0:64, 0:n], False, True, R(sqr, onesbf), R(PS[6]))
            S.cp(tq[:, 0:n], PS[6][:, 0:n], R(PS[6]), R(tq))
            S.act(tq[:, 0:n], tq[:, 0:n], AF.Sqrt, R(tq, kmax), R(tq), scale=kmax[:, 2:3])
            S.ts(negm[:, q0:q0 + n], tq[:, 0:n], -1.0, None, ALU.mult, None, R(tq), R(negm))
        for (q0, n) in qblocks:
            po, pd = PS[4], PS[5]
            def scores(kt):
                par = kt % 3
                p = PS[par]
                k0 = kt * 128
                S.mm(p[:, 0:n], KnT[:, k0:k0 + 128], QnT[:, q0:q0 + n], True, False, R(KnT, QnT), R(p))
                S.mm(p[:, 0:n], KrT[:, k0:k0 + 128], QrT[:, q0:q0 + n], False, False, R(KrT, QrT), R(p))
                S.mm(p[:, 0:n], c128[:], negm[:, q0:q0 + n], False, True, R(c128, negm), R(p))
                S.act(pT[par][:, 0:n], p[:, 0:n], AF.Exp, R(p), R(pT[par]), scale=MLA_SCALE)

            scores(0)
            scores(1)
            for kt in range(NT1):
                if kt + 2 < NT1:
                    scores(kt + 2)
                par = kt % 3
                S.mm(po[:, 0:n], V[:, kt, :], pT[par][:, 0:n], kt == 0, kt == NT1 - 1, R(V, pT[par]), R(po))
                S.mm(pd[:, 0:n], onesbf[:], pT[par][:, 0:n], kt == 0, kt == NT1 - 1, R(onesbf, pT[par]), R(pd))
            S.recip(rden[:, 0:n], pd[:, 0:n], R(pd), R(rden))
            S.tt(oT[:, h, q0:q0 + n], po[:, 0:n], rden[:, 0:n], ALU.mult, R(po, rden), [oT.k(h)])
        if h == 0:
            chk(20)
    S.reset(markB)
    chk(3)

    wo = S.carve("wo", [8, 1024], BF16)
    S.dma(wo[:], wo_d.h.rearrange("(kc p) f -> p kc f", p=128), [], R(wo), eng="pool")
    rw = S.carve("rw", [8, 32], F32)
    S.dma(rw[:], rw_d[:], [], R(rw))
    rb = S.carve("rb", [32], F32)
    S.dma(rb[:], rb_d.h.partition_broadcast(128).rearrange("p o k -> p (o k)"), [], R(rb))
    xrw = S.carve("xrw", [4, 1024], F32)
    r = S.carve("r1", [8, 512], F32)
    x1t = S.carve("x1t", [8, 512], F32)
    Xf = S.carve("Xf", [8, 512], F32)
    tA = S.carve("tA", [512], F32)
    lt = ln_tmp(S, 512, R(x1t))
    rt = router_tmp(S)
    for (o0, n) in qblocks:
        if fused:
            S.dma(xrw[:, 0:n // 128, :], xown[128 + o0:128 + o0 + n, :].rearrange("(i p) f -> p i f", p=128), R(xown), R(xrw))
        else:
            S.dma(xrw[:, 0:n // 128, :], xs[o0:o0 + n, :].rearrange("(i p) f -> p i f", p=128), [], R(xrw))
        for dc in range(8):
            py, px = PS[dc % 2], PS[2 + dc % 2]
            for kc in range(8):
                S.mm(py[:, 0:n], wo[:, kc, dc * 128:(dc + 1) * 128], oT[:, kc, o0:o0 + n], kc == 0, kc == 7, R(wo, oT), R(py))
            for i in range(n // 128):
                S.tr(px[:, i * 128:(i + 1) * 128], xrw[:, i, dc * 128:(dc + 1) * 128], C.ident, R(xrw, C.cs), R(px))
            S.act(tA[:, 0:n], py[:, 0:n], AF.Identity, R(py, C.mod), R(tA), scale=C.mod[:, G1 + dc, 0:1])
            S.stt(r[:, dc, 0:n], px[:, 0:n], ALPHA, tA[:, 0:n], ALU.mult, ALU.add, R(px, tA), R(r))
        layer_norm_fm(S, C, r, n, 0, lambda c: x1t[:, c, 0:n], lt)
        S.dma(x1d[:, :, o0:o0 + n], x1t[:, :, 0:n], R(x1t), R(x1d))
        for dc in range(8):
            S.act(Xf[:, dc, 0:n], x1t[:, dc, 0:n], AF.Identity, R(x1t, C.mod), R(Xf),
                  scale=C.mod[:, SC2 + dc, 0:1], bias=C.mod[:, SH2 + dc, 0:1])
        S.cp(XT[:, :, o0:o0 + n], Xf[:, :, 0:n], R(Xf), R(XT), eng="pool")
        router(S, C, Xf, n, o0, rw, rb, gateT, rt)
    S.reset(markA)
    chk(4)

    mw_in = D("mw_in", [32, 1024, 2048])
    mb_inT = D("mb_inT", [128, 32, 16])
    mw_out = D("mw_out", [32, 1024, 1024])
    mb_out = D("mb_out", [32, 1024])
    b_inT = S.carve("b_inT", [32, 16], F32)
    S.dma(b_inT[:], mb_inT[:], [], R(b_inT))
    S.ts(b_inT[:, :, 8:16], b_inT[:, :, 8:16], 1.0, None, ALU.add, None, R(b_inT), R(b_inT))
    p4 = S.mark()
    tblocks = [([(0, 512), (512, 512)], [0, 0]), ([(1024, 512), (1536, 512)], [0, 0])]
    for chunks, cols in tblocks:
        acc = S.carve("acc", [8, sum(n for _, n in chunks)], F32, nreg=8)
        m = S.mark()
        offs = moe_block(S, C, XT, gateT, chunks, mw_in, mw_out, b_inT, mb_out, acc)
        S.reset(m)
        ln2_out(S, C, acc, offs, chunks, cols, x1d, xout, None, None)
        S.reset(p4)
    S.finish(R(xout))


def build_fused():
    nc = bass.Bass("TRN2", target_bir_lowering=False)
    es = ExitStack()
    with es:
        S = Sched(nc, es)
        nochk = lambda n: None
        S.prefix = "a_"
        xown = _build(S, nc, nochk, fused=True)
        S.prefix = ""
        xall = S.dram("xall", [2 * 2176, 1024], F32, "Internal")
        for k in range(17):
            S.op("pool", lambda e, k=k: e.collective_compute("AllGather", ALU.bypass,
                                                             replica_groups=[[0, 1], [2, 3], [4, 5], [6, 7]],
                                                             ins=[xown[k * 128:(k + 1) * 128, :]],
                                                             outs=[xall[k * 256:(k + 1) * 256, :]]),
                 R(xown), R(xall), dma=True, inc=1)
        S.reset(0)
        S.prefix = "b_"
        _build1(S, nc, nochk, fused=True, xown=xown, xall=xall)
        S.emit()
    return nc


def consts():
    c = np.zeros((128, 1024), np.float32)
    c[:, 0:128] = np.eye(128)
    c[:, 128:256] = 1.0
    k = np.arange(128)[:, None]; l = np.arange(128)[None, :]
    c[:, 256:384] = (k <= l)
    c[:, 384:512] = (k >= l)
    c[:, 512:640] = np.where(k > l, -30000.0, 0.0)
    c[:, 640:768] = np.where(k < l, -30000.0, 0.0)
    c[0:32, 768:800] = np.eye(32)
    return c

def fm(v, nchunk):
    return np.ascontiguousarray(v.reshape(nchunk, 128).T)

def prep_l0(inp, b, half):
    f = np.float32
    flip = half == 1
    ctx_ = inp['ctx'][b][::-1] if flip else inp['ctx'][b]
    lat_ = inp['x'][b][::-1] if flip else inp['x'][b]
    m = {}
    m['xs'] = np.ascontiguousarray(np.concatenate([ctx_, lat_], 0), dtype=f)
    cT = np.zeros((128, 8, 2), f)
    cT[:, :, 0] = fm(inp['c'][b], 8); cT[:, :, 1] = fm(inp['c_ctx'], 8)
    m['cT'] = cT
    m['mod_w'] = np.ascontiguousarray(inp['mod_w'][0])
    m['mod_bT'] = fm(inp['mod_b'][0], 48)
    m['lnp'] = np.ascontiguousarray(np.stack([fm(inp['ln1_g'][0], 8), fm(inp['ln1_b'][0], 8), fm(inp['ln2_g'][0], 8), fm(inp['ln2_b'][0], 8)], 1))
    W = inp['ssd_w_in'][0]
    dA = 1 if flip else 0; dB = 1 - dA
    win = np.zeros((8, 1024, 776), f)
    convp = np.zeros((128, 8, 4, 6), f)
    ssdp = np.zeros((1, 160), f)
    cw = inp['ssd_conv_w'][0]; cb = inp['ssd_conv_b'][0]
    if flip: cw = cw[::-1]
    for g in range(8):
        win[g] = np.concatenate([W[:, 256*g:256*g+256], W[:, 2048+256*g:2048+256*g+256], W[:, 4096+128*g:4096+128*g+128],
                                 W[:, 5120+128*g:5120+128*g+128], W[:, 6144+dA*32+4*g:6144+dA*32+4*g+4], W[:, 6144+dB*32+4*g:6144+dB*32+4*g+4]], 1)
        for ch, c0 in enumerate([256*g, 256*g+128, 2048+128*g, 3072+128*g]):
            convp[:, g, ch, 0:5] = cw[:, c0:c0+128].T
            convp[:, g, ch, 5] = cb[c0:c0+128]
        ssdp[0, g*20:g*20+20] = np.concatenate([inp['ssd_dt_bias'][0][dA, 4*g:4*g+4], inp['ssd_dt_bias'][0][dB, 4*g:4*g+4],
                                               inp['ssd_a_log'][0][dA, 4*g:4*g+4], inp['ssd_a_log'][0][dB, 4*g:4*g+4], inp['ssd_d'][0][4*g:4*g+4]])
    m['win'] = win; m['convp'] = convp; m['ssdp'] = ssdp
    m['normw'] = fm(inp['ssd_norm_w'][0], 16)
    m['wout'] = np.ascontiguousarray(inp['ssd_w_out'][0])
    add_moe(m, inp, 0)
    m['cst'] = consts()
    return m

def add_moe(m, inp, i):
    f = np.float32
    m['rw'] = np.ascontiguousarray(inp['router_w'][i].reshape(8, 128, 32).transpose(1, 0, 2))
    m['rb'] = np.ascontiguousarray(inp['router_b'][i].reshape(1, 32))
    wi = inp['moe_w_in'][i]
    m['mw_in'] = np.ascontiguousarray(np.concatenate([wi[:, :, 0:512], wi[:, :, 1024:1536], wi[:, :, 512:1024], wi[:, :, 1536:2048]], 2))
    m['mb_inT'] = np.ascontiguousarray(inp['moe_b_in'][i].reshape(32, 16, 128).transpose(2, 0, 1))
    m['mw_out'] = np.ascontiguousarray(inp['moe_w_out'][i])
    m['mb_out'] = np.ascontiguousarray(inp['moe_b_out'][i])

def gather_l0(results, B=4):
    x1 = np.zeros((B, 4096, 1024), np.float32); ctx1 = np.zeros((B, 256, 1024), np.float32)
    for cid, r in enumerate(results):
        b, half = cid // 2, cid % 2
        o = r['xout']
        if half == 0:
            ctx1[b, 0:128] = o[0:128]; x1[b, 0:2048] = o[128:]
        else:
            ctx1[b, 128:256] = o[0:128][::-1]; x1[b, 2048:4096] = o[128:][::-1]
    return x1, ctx1


def rope_consts():
    f = np.float32
    t = np.arange(4096)
    row = (t // 64).astype(f); col = (t % 64).astype(f)
    inv = (f(10000.0) ** (-np.arange(16, dtype=f) / f(16))).astype(f)
    ang = np.stack([row[:, None] * inv, col[:, None] * inv], axis=1).astype(f)
    cos, sin = np.cos(ang).astype(f), np.sin(ang).astype(f)
    cosT = np.zeros((64, 4096), f); sinT = np.zeros((64, 4096), f)
    for a in range(2):
        for hf in range(2):
            cosT[a * 32 + hf * 16:a * 32 + hf * 16 + 16] = cos[:, a, :].T
            sinT[a * 32 + hf * 16:a * 32 + hf * 16 + 16] = sin[:, a, :].T
    rm = np.zeros((64, 64), f)
    for a in range(2):
        for k in range(16):
            i1 = a * 32 + k; i2 = a * 32 + 16 + k
            rm[i2, i1] = -1.0
            rm[i1, i2] = 1.0
    return cosT, sinT, rm

def prep_l1(inp, x1, ctx1, b, half, rc=None):
    f = np.float32
    cosT, sinT, rm = rc if rc is not None else rope_consts()
    own = slice(half * 2048, (half + 1) * 2048)
    oth = slice((1 - half) * 2048, (2 - half) * 2048)
    m = {}
    m['xs'] = np.ascontiguousarray(np.concatenate([x1[b, own], x1[b, oth], ctx1[b]], 0), dtype=f)
    cT = np.zeros((128, 8, 2), f)
    cT[:, :, 0] = fm(inp['c'][b], 8); cT[:, :, 1] = fm(inp['c_ctx'], 8)
    m['cT'] = cT
    m['mod_w'] = np.ascontiguousarray(inp['mod_w'][1])
    m['mod_bT'] = fm(inp['mod_b'][1], 48)
    m['lnp'] = np.ascontiguousarray(np.stack([fm(inp['ln1_g'][1], 8), fm(inp['ln1_b'][1], 8), fm(inp['ln2_g'][1], 8), fm(inp['ln2_b'][1], 8)], 1))
    m['wa'] = np.ascontiguousarray(inp['mla_w_a'][0])
    m['qnorm'] = fm(inp['mla_q_norm'][0], 3)
    m['kvnorm'] = fm(inp['mla_kv_norm'][0], 2)
    wq = inp['mla_w_qb'][0]
    m['wqb'] = np.ascontiguousarray(np.concatenate([wq[:, h * 192:h * 192 + 128] for h in range(8)] +
                                                  [wq[:, h * 192 + 128:h * 192 + 192] for h in range(8)], 1))
    wk = inp['mla_w_kvb'][0]
    m['wkvb'] = np.ascontiguousarray(np.concatenate([wk[:, h * 256:h * 256 + 128] for h in range(8)] +
                                                   [wk[:, h * 256 + 128:h * 256 + 256] for h in range(8)], 1))
    m['wo'] = np.ascontiguousarray(inp['mla_w_o'][0])
    m['ropeq'] = np.ascontiguousarray(np.stack([cosT[:, own], sinT[:, own]], 1))
    ck = np.concatenate([cosT[:, own], cosT[:, oth], np.ones((64, 256), f)], 1)
    sk = np.concatenate([sinT[:, own], sinT[:, oth], np.zeros((64, 256), f)], 1)
    m['ropek'] = np.ascontiguousarray(np.stack([ck, sk], 1))
    m['rm'] = rm
    add_moe(m, inp, 1)
    m['cst'] = consts()
    return m

def gather_l1(results, B=4):
    out = np.zeros((B, 4096, 1024), np.float32)
    for cid, r in enumerate(results):
        b, half = cid // 2, cid % 2
        out[b, half * 2048:(half + 1) * 2048] = r['xout']
    return out


def prep_fused(inp, b, half, rc):
    f = np.float32
    cosT, sinT, rm = rc
    m0 = prep_l0(inp, b, half)
    m = {("cst" if k == "cst" else "a_" + k): v for k, v in m0.items()}
    idx1 = 4095 - np.arange(2048)
    ck = np.ones((64, 4352), f)
    sk = np.zeros((64, 4352), f)
    for t in range(2, 34):
        k, r = t // 2, t % 2
        base = (k - 1) * 128 + np.arange(128)
        lat = base if r == 0 else 4095 - base
        ck[:, t * 128:(t + 1) * 128] = cosT[:, lat]
        sk[:, t * 128:(t + 1) * 128] = sinT[:, lat]
    qi = np.arange(2048) if half == 0 else idx1
    m1 = {}
    cT = np.zeros((128, 8, 2), f)
    cT[:, :, 0] = fm(inp['c'][b], 8); cT[:, :, 1] = fm(inp['c_ctx'], 8)
    m1['cT'] = cT
    m1['mod_w'] = np.ascontiguousarray(inp['mod_w'][1])
    m1['mod_bT'] = fm(inp['mod_b'][1], 48)
    m1['lnp'] = np.ascontiguousarray(np.stack([fm(inp['ln1_g'][1], 8), fm(inp['ln1_b'][1], 8), fm(inp['ln2_g'][1], 8), fm(inp['ln2_b'][1], 8)], 1))
    m1['wa'] = np.ascontiguousarray(inp['mla_w_a'][0])
    m1['qnorm'] = fm(inp['mla_q_norm'][0], 3)
    m1['kvnorm'] = fm(inp['mla_kv_norm'][0], 2)
    wq = inp['mla_w_qb'][0]
    m1['wqb'] = np.ascontiguousarray(np.concatenate([wq[:, h * 192:h * 192 + 128] for h in range(8)] +
                                                   [wq[:, h * 192 + 128:h * 192 + 192] for h in range(8)], 1))
    wk = inp['mla_w_kvb'][0]
    m1['wkvb'] = np.ascontiguousarray(np.concatenate([wk[:, h * 256:h * 256 + 128] for h in range(8)] +
                                                    [wk[:, h * 256 + 128:h * 256 + 256] for h in range(8)], 1))
    m1['wo'] = np.ascontiguousarray(inp['mla_w_o'][0])
    m1['ropeq'] = np.ascontiguousarray(np.stack([cosT[:, qi], sinT[:, qi]], 1))
    m1['ropek'] = np.ascontiguousarray(np.stack([ck, sk], 1))
    m1['rm'] = rm
    add_moe(m1, inp, 1)
    for k, v in m1.items():
        m["b_" + k] = v
    return m

def gather_fused(results, B=4):
    out = np.zeros((B, 4096, 1024), np.float32)
    for cid, r in enumerate(results):
        b, half = cid // 2, cid % 2
        o = r['b_xout']
        if half == 0:
            out[b, 0:2048] = o
        else:
            out[b, 2048:4096] = o[::-1]
    return out


def kernel(**inputs):
    inp = {k: np.asarray(v) for k, v in inputs.items()}
    nc = build_fused()
    rc = rope_consts()
    maps = []
    for cid in range(8):
        m = prep_fused(inp, cid // 2, cid % 2, rc)
        maps.append({k: m[k] for k in nc._in_names})
    res = run_bass_kernel_spmd(nc, maps, core_ids=list(range(8)))
    return gather_fused(res.results)
```

```python
import numpy as np
import concourse.bass as bass
import concourse.mybir as mybir
from concourse.bass_utils import run_bass_kernel_spmd
from contextlib import ExitStack

F32 = mybir.dt.float32
BF16 = mybir.dt.bfloat16
U32 = mybir.dt.uint32
I32 = mybir.dt.int32
AF = mybir.ActivationFunctionType
ALU = mybir.AluOpType
AX = mybir.AxisListType


class Tile:
    def __init__(self, name, h, nreg=1):
        self.name = name
        self.h = h
        self.nreg = nreg

    def k(self, i=0):
        return (self.name, i)

    def all(self):
        return [(self.name, i) for i in range(self.nreg)]

    def __getitem__(self, idx):
        return self.h[idx]


class Sched:
    ENGS = ("pe", "act", "dve", "pool", "sp")

    def __init__(self, nc, es, n_dma_sems=8, sync_same=True):
        self.nc = nc
        self.es = es
        self.sync_same = sync_same
        self.q = {e: [] for e in self.ENGS}
        self.csem = {e: es.enter_context(nc.semaphore("c_" + e)) for e in self.ENGS}
        self.ccount = {e: 0 for e in self.ENGS}
        self.dq = ("sp", "pool", "act")
        self.dsems = {e: [es.enter_context(nc.semaphore("d_%s_%d" % (e, i))) for i in range(n_dma_sems)]
                      for e in self.dq}
        self.duse = {e: [0] * n_dma_sems for e in self.dq}
        self.dnext = {e: 0 for e in self.dq}
        self.lastw = {}
        self.readers = {}
        self.known = {e: {} for e in self.ENGS}
        self.ntile = 0

    def sb(self, name, shape, dtype, nreg=1):
        h = self.es.enter_context(self.nc.sbuf_tensor(name, list(shape), dtype))
        return Tile(name, h, nreg)

    def ps(self, name, shape, dtype, nreg=1):
        h = self.es.enter_context(self.nc.psum_tensor(name, list(shape), dtype))
        return Tile(name, h, nreg)

    def dram(self, name, shape, dtype, kind, nreg=1):
        name = getattr(self, "prefix", "") + name
        if not hasattr(self.nc, "_in_names"):
            self.nc._in_names = []
        if kind == "ExternalInput":
            self.nc._in_names.append(name)
        h = self.nc.dram_tensor(name, list(shape), dtype, kind=kind)
        return Tile(name, h.ap(), nreg)

    def arena(self, nfloats):
        self.ar = self.sb("arena", [128, nfloats], F32)
        self.ar_n = nfloats
        self.ar_off = 0
        self.ar_base = 0

    def carve(self, name, shape, dtype, nreg=1, parts=128):
        n = 1
        for s in shape:
            n *= s
        nf = n if dtype == F32 else (n + 1) // 2
        nf = (nf + 7) // 8 * 8
        assert self.ar_off + nf <= self.ar_n, ("arena overflow", name, self.ar_off, nf, self.ar_n)
        ap = self.ar.h[0:parts, self.ar_off:self.ar_off + nf]
        if dtype != F32:
            ap = ap.bitcast(dtype)
        ap = ap[:, 0:n]
        if len(shape) == 2:
            ap = ap.rearrange("p (a b) -> p a b", b=shape[1])
        elif len(shape) == 3:
            ap = ap.rearrange("p (a b c) -> p a b c", b=shape[1], c=shape[2])
        self.ar_off += nf
        self.ntile += 1
        return Tile("%s#%d" % (name, self.ntile), ap, nreg)

    def mark(self):
        return self.ar_off

    def reset(self, mark):
        self.barrier()
        self.ar_off = mark

    def barrier(self):
        allt = {}
        for e in self.ENGS:
            if self.ccount[e] > 0:
                allt[("c", e)] = self.ccount[e]
        for e in self.dq:
            for i, u in enumerate(self.duse[e]):
                if u > 0:
                    allt[("d", e, i)] = u
        self.pending = {e: dict(allt) for e in self.ENGS}

    def op(self, eng, fn, reads=(), writes=(), dma=False, same=None, inc=16):
        deps = {}
        pend = getattr(self, "pending", None)
        if pend and pend.get(eng):
            for sk, v in pend[eng].items():
                if deps.get(sk, 0) < v:
                    deps[sk] = v
            pend[eng] = None

        def add(tok):
            sk, v = tok
            if deps.get(sk, 0) < v:
                deps[sk] = v

        for k in reads:
            if k in self.lastw:
                add(self.lastw[k])
        for k in writes:
            if k in self.lastw:
                add(self.lastw[k])
            for sk, v in self.readers.get(k, {}).items():
                add((sk, v))
        if dma:
            i = self.dnext[eng]
            self.dnext[eng] = (i + 1) % len(self.dsems[eng])
            prev = self.duse[eng][i]
            self.duse[eng][i] = prev + inc
            tok = (("d", eng, i), prev + inc)
            if prev > 0:
                add((("d", eng, i), prev))
        else:
            self.ccount[eng] += 1
            tok = (("c", eng), self.ccount[eng])
        same = self.sync_same if same is None else same
        waits = []
        for sk, v in deps.items():
            if sk == ("c", eng) and not same:
                continue
            if self.known[eng].get(sk, 0) >= v:
                continue
            self.known[eng][sk] = v
            waits.append((sk, v))
        self.q[eng].append((waits, fn, tok, inc if dma else 1))
        for k in reads:
            r = self.readers.setdefault(k, {})
            if r.get(tok[0], 0) < tok[1]:
                r[tok[0]] = tok[1]
        for k in writes:
            self.lastw[k] = tok
            self.readers[k] = {}
        return tok

    def dma(self, out, in_, reads, writes, eng="sp", **kw):
        return self.op(eng, lambda e: e.dma_start(out=out, in_=in_, **kw), reads, writes, dma=True)

    def mm(self, out, lhsT, rhs, start, stop, reads, writes):
        return self.op("pe", lambda e: e.matmul(out, lhsT, rhs, start=start, stop=stop), reads, writes, same=False)

    def tr(self, out, in_, ident, reads, writes):
        return self.op("pe", lambda e: e.transpose(out, in_, ident), reads, writes, same=False)

    def act(self, out, in_, func, reads, writes, eng="act", **kw):
        return self.op(eng, lambda e: e.activation(out=out, in_=in_, func=func, **kw), reads, writes)

    def tt(self, out, in0, in1, op, reads, writes, eng="dve"):
        return self.op(eng, lambda e: e.tensor_tensor(out=out, in0=in0, in1=in1, op=op), reads, writes)

    def ts(self, out, in0, s1, s2, op0, op1, reads, writes, eng="dve", **kw):
        if op1 is None:
            return self.op(eng, lambda e: e.tensor_scalar(out=out, in0=in0, scalar1=s1, scalar2=None, op0=op0, **kw),
                           reads, writes)
        return self.op(eng, lambda e: e.tensor_scalar(out=out, in0=in0, scalar1=s1, scalar2=s2, op0=op0, op1=op1, **kw),
                       reads, writes)

    def cp(self, out, in_, reads, writes, eng="dve"):
        if eng == "act":
            return self.op(eng, lambda e: e.copy(out=out, in_=in_), reads, writes)
        return self.op(eng, lambda e: e.tensor_copy(out=out, in_=in_), reads, writes)

    def stt(self, out, in0, scalar, in1, op0, op1, reads, writes):
        return self.op("dve", lambda e: e.scalar_tensor_tensor(out=out, in0=in0, scalar=scalar, in1=in1, op0=op0, op1=op1),
                       reads, writes)

    def recip(self, out, in_, reads, writes):
        return self.op("dve", lambda e: e.reciprocal(out=out, in_=in_), reads, writes)

    def max8(self, out, in_, reads, writes):
        return self.op("dve", lambda e: e.max(out=out, in_=in_), reads, writes)

    def reduce(self, out, in_, axis, op, reads, writes):
        return self.op("dve", lambda e: e.tensor_reduce(out=out, in_=in_, axis=axis, op=op), reads, writes)

    def memset(self, out, val, writes, eng="dve"):
        return self.op(eng, lambda e: e.memset(out, val), (), writes)

    def _semh(self, sk):
        if sk[0] == "c":
            return self.csem[sk[1]]
        return self.dsems[sk[1]][sk[2]]

    def finish(self, out_keys):
        waits = []
        for k in out_keys:
            if k in self.lastw:
                waits.append(self.lastw[k])
        self.final_waits = waits

    def emit(self):
        nc = self.nc
        counts = {e: len(self.q[e]) for e in self.ENGS}
        nw = {e: sum(len(w[0]) for w in self.q[e]) for e in self.ENGS}
        print("SCHED instr counts", counts, "waits", nw, flush=True)
        with nc.Block() as block:
            def mk(engname):
                def body(e):
                    for waits, fn, tok, inc_ in self.q[engname]:
                        for sk, v in waits:
                            e.wait_ge(self._semh(sk), v)
                        ins = fn(e)
                        ins.then_inc(self._semh(tok[0]), inc_)
                    if engname == "sp":
                        done = {}
                        for sk, v in getattr(self, "final_waits", []):
                            if done.get(sk, 0) < v:
                                done[sk] = v
                        for sk, v in done.items():
                            e.wait_ge(self._semh(sk), v)
                return body

            block.tensor(mk("pe"))
            block.scalar(mk("act"))
            block.vector(mk("dve"))
            block.gpsimd(mk("pool"))
            block.sync(mk("sp"))


ALPHA = 4.0 ** 0.25
LN_EPS = 1e-5
SH1, SC1, G1, SH2, SC2, G2 = 0, 8, 16, 24, 32, 40


def R(*tiles):
    out = []
    for t in tiles:
        out += t.all()
    return out


class Ctx:
    pass


def setup_common(S, C, layer_has_ctx):
    if hasattr(S, "_common"):
        C.__dict__.update(S._common.__dict__)
        return
    S._common = C
    C.PS = [S.ps("ps%d" % i, [128, 512], F32) for i in range(8)]
    pre, S.prefix = getattr(S, "prefix", ""), ""
    C.cst = S.dram("cst", [128, 1024], F32, "ExternalInput")
    S.prefix = pre
    C.cs = S.sb("cs", [128, 1024], F32)
    S.dma(C.cs[:], C.cst[:], [], R(C.cs))
    C.ident = C.cs[:, 0:128]
    C.ones = C.cs[:, 128:256]
    C.U = [C.cs[:, 256:384], C.cs[:, 384:512]]
    C.NEG1 = [C.cs[:, 512:640], C.cs[:, 640:768]]
    C.eye32 = C.cs[0:32, 768:800]
    C.identbf = S.sb("identbf", [128, 128], BF16)
    S.cp(C.identbf[:], C.ident, R(C.cs), R(C.identbf))


def modulation(S, C, mod_w, mod_bT, cT):
    PS = C.PS
    m0 = S.mark()
    cs = S.carve("cTs", [8, 2], F32)
    S.dma(cs[:], cT[:], [], R(cs))
    csl = S.carve("csl", [8, 2], F32)
    S.act(csl[:], cs[:], AF.Silu, R(cs), R(csl))
    wb = [S.carve("modw%d" % i, [8, 512], F32) for i in range(2)]
    mb = S.carve("modb", [48], F32)
    S.dma(mb[:], mod_bT[:], [], R(mb))
    mwv = mod_w.h.rearrange("(kc p) f -> p kc f", p=128)
    for fg in range(12):
        w = wb[fg % 2]
        S.dma(w[:], mwv[:, :, fg * 512:(fg + 1) * 512], [], R(w))
        for fl in range(4):
            fc = fg * 4 + fl
            for kc in range(8):
                S.mm(PS[7][:, fc * 2:fc * 2 + 2], w[:, kc, fl * 128:(fl + 1) * 128], csl[:, kc, :], kc == 0, kc == 7,
                     R(w, csl), R(PS[7]))
    S.tt(C.mod[:], PS[7][:, 0:96].rearrange("p (f j) -> p f j", j=2), mb[:].unsqueeze(2).to_broadcast([128, 48, 2]),
         ALU.add, R(PS[7], mb), R(C.mod))
    for base in (SC1, SC2):
        S.ts(C.mod[:, base:base + 8, :], C.mod[:, base:base + 8, :], 1.0, None, ALU.add, None, R(C.mod), R(C.mod))
    S.reset(m0)


def layer_norm_fm(S, C, r, n, gi, out, tmp):
    PS = C.PS
    sq, mean, msq, var, rs = tmp["sq"], tmp["mean"], tmp["msq"], tmp["var"], tmp["rs"]
    for c in range(8):
        S.mm(PS[6][:, 0:n], C.ones, r[:, c, 0:n], c == 0, c == 7, R(r, C.cs), R(PS[6]))
    S.tt(sq[:, :, 0:n], r[:, :, 0:n], r[:, :, 0:n], ALU.mult, R(r), R(sq), eng="pool")
    for c in range(8):
        S.mm(PS[7][:, 0:n], C.ones, sq[:, c, 0:n], c == 0, c == 7, R(sq, C.cs), R(PS[7]))
    S.act(mean[:, 0:n], PS[6][:, 0:n], AF.Identity, R(PS[6]), R(mean), scale=1.0 / 1024)
    S.tt(msq[:, 0:n], mean[:, 0:n], mean[:, 0:n], ALU.mult, R(mean), R(msq), eng="pool")
    S.stt(var[:, 0:n], PS[7][:, 0:n], 1.0 / 1024, msq[:, 0:n], ALU.mult, ALU.subtract, R(PS[7], msq), R(var))
    S.act(var[:, 0:n], var[:, 0:n], AF.Sqrt, R(var), R(var), bias=LN_EPS)
    S.recip(rs[:, 0:n], var[:, 0:n], R(var), R(rs))
    for c in range(8):
        S.tt(r[:, c, 0:n], r[:, c, 0:n], mean[:, 0:n], ALU.subtract, R(r, mean), R(r))
        S.tt(r[:, c, 0:n], r[:, c, 0:n], rs[:, 0:n], ALU.mult, R(r, rs), R(r), eng="pool")
        S.act(out(c), r[:, c, 0:n], AF.Identity, R(r, C.lnp), tmp["outkeys"],
              scale=C.lnp[:, gi, c:c + 1], bias=C.lnp[:, gi + 1, c:c + 1])


def ln_tmp(S, nmax, outkeys):
    return {"sq": S.carve("lnsq", [8, nmax], F32), "mean": S.carve("lnmean", [nmax], F32),
            "msq": S.carve("lnmsq", [nmax], F32), "var": S.carve("lnvar", [nmax], F32),
            "rs": S.carve("lnrs", [nmax], F32), "outkeys": outkeys}


def router(S, C, Xf, n, o0, rw, rb, gateT, rt):
    PS = C.PS
    for i in range(n // 128):
        for kc in range(8):
            S.mm(PS[5][:, 0:32], Xf[:, kc, i * 128:(i + 1) * 128], rw[:, kc, :], kc == 0, kc == 7, R(Xf, rw), R(PS[5]))
        lg, m8, msk, ex, sm = rt["lg"], rt["m8"], rt["msk"], rt["ex"], rt["sm"]
        S.tt(lg[:], PS[5][:, 0:32], rb[:], ALU.add, R(PS[5], rb), R(lg))
        S.max8(m8[:], lg[:], R(lg), R(m8))
        S.ts(msk[:], lg[:], m8[:, 3:4], None, ALU.is_ge, None, R(lg, m8), R(msk))
        S.ts(sm[:, 0:1], m8[:, 0:1], -1.0, None, ALU.mult, None, R(m8), R(sm))
        S.act(ex[:], lg[:], AF.Exp, R(lg, sm), R(ex), bias=sm[:, 0:1])
        S.tt(ex[:], ex[:], msk[:], ALU.mult, R(ex, msk), R(ex))
        S.reduce(sm[:, 1:2], ex[:], AX.X, ALU.add, R(ex), R(sm))
        S.recip(sm[:, 2:3], sm[:, 1:2], R(sm), R(sm))
        S.ts(ex[:], ex[:], sm[:, 2:3], None, ALU.mult, None, R(ex, sm), R(ex))
        S.tr(PS[5][0:32, 128:256], ex[:], C.ident, R(ex, C.cs), R(PS[5]))
        S.cp(gateT[0:32, o0 + i * 128:o0 + (i + 1) * 128], PS[5][0:32, 128:256], R(PS[5]), R(gateT), eng="act")


def router_tmp(S):
    return {"lg": S.carve("rlg", [32], F32), "m8": S.carve("rm8", [8], F32), "msk": S.carve("rmsk", [32], F32),
            "ex": S.carve("rex", [32], F32), "sm": S.carve("rsm", [4], F32)}


def moe_block(S, C, XT, gateT, chunks, w_in, w_out, b_inT, b_out, acc):
    PS = C.PS
    tot = sum(n for _, n in chunks)
    offs = []
    o = 0
    for lo, n in chunks:
        offs.append(o)
        o += n
    wi = S.carve("wi", [8, 2048], BF16, nreg=2)
    wo = S.carve("wo", [8, 1024], BF16)
    actT = S.carve("actT", [8, tot], BF16, nreg=8)
    Gb = S.carve("Gb", [tot], F32)
    ge = S.carve("ge", [tot], F32, parts=32)
    bo = S.carve("bo", [1024], F32, parts=32)
    S.dma(bo[:], b_out[:], [], R(bo))
    tg = [S.carve("tg%d" % i, [512], F32) for i in range(2)]
    tsg = [S.carve("tsg%d" % i, [512], F32) for i in range(2)]
    tl = [S.carve("tl%d" % i, [512], F32) for i in range(2)]
    it = 0
    for dc in range(8):
        for ci, (lo, n) in enumerate(chunks):
            p = PS[4 + it % 2]
            it += 1
            S.mm(p[:, 0:n], bo[:, dc * 128:(dc + 1) * 128], gateT[0:32, lo:lo + n], True, True, R(bo, gateT), R(p))
            S.cp(acc[:, dc, offs[ci]:offs[ci] + n], p[:, 0:n], R(p), [acc.k(dc)], eng="act")
    wiv = w_in.h.rearrange("e (kc p) f -> e p kc f", p=128)
    wov = w_out.h.rearrange("e (kc p) f -> e p kc f", p=128)

    def load_wi(e, h):
        S.dma(wi[:, :, h * 1024:(h + 1) * 1024], wiv[e, :, :, h * 1024:(h + 1) * 1024], [], [wi.k(h)], eng="pool")

    load_wi(0, 0)
    load_wi(0, 1)
    it = 0
    for e in range(32):
        S.dma(wo[:], wov[e], [], R(wo), eng="pool")
        for ci, (lo, n) in enumerate(chunks):
            S.ts(ge[0:32, offs[ci]:offs[ci] + n], gateT[0:32, lo:lo + n], C.eye32[:, e:e + 1], None, ALU.mult, None,
                 R(gateT, C.cs), R(ge))
            S.mm(PS[6][:, 0:n], C.ones[0:32, :], ge[0:32, offs[ci]:offs[ci] + n], True, True, R(ge, C.cs), R(PS[6]))
            S.cp(Gb[:, offs[ci]:offs[ci] + n], PS[6][:, 0:n], R(PS[6]), R(Gb), eng="act")
        for j in range(8):
            hh, jj = j // 4, j % 4
            cg = hh * 1024 + jj * 128
            cl = hh * 1024 + 512 + jj * 128
            for ci, (lo, n) in enumerate(chunks):
                pa, pb = (PS[0], PS[1]) if it % 2 == 0 else (PS[2], PS[3])
                q = it % 2
                it += 1
                for kc in range(8):
                    S.mm(pa[:, 0:n], wi[:, kc, cg:cg + 128], XT[:, kc, lo:lo + n], kc == 0, kc == 7,
                         [wi.k(hh)] + R(XT), R(pa))
                for kc in range(8):
                    S.mm(pb[:, 0:n], wi[:, kc, cl:cl + 128], XT[:, kc, lo:lo + n], kc == 0, kc == 7,
                         [wi.k(hh)] + R(XT), R(pb))
                g, sg, l = tg[q], tsg[q], tl[q]
                S.ts(g[:, 0:n], pa[:, 0:n], b_inT[:, e, j:j + 1], 7.0, ALU.add, ALU.min, R(pa, b_inT), R(g))
                S.act(sg[:, 0:n], g[:, 0:n], AF.Sigmoid, R(g), R(sg), scale=1.702)
                S.ts(l[:, 0:n], pb[:, 0:n], b_inT[:, e, 8 + j:9 + j], 8.0, ALU.add, ALU.min, R(pb, b_inT), R(l))
                S.stt(l[:, 0:n], l[:, 0:n], -6.0, Gb[:, offs[ci]:offs[ci] + n], ALU.max, ALU.mult, R(l, Gb), R(l))
                S.tt(g[:, 0:n], g[:, 0:n], sg[:, 0:n], ALU.mult, R(g, sg), R(g), eng="pool")
                S.tt(actT[:, j, offs[ci]:offs[ci] + n], g[:, 0:n], l[:, 0:n], ALU.mult, R(g, l), [actT.k(j)])
            if e + 1 < 32 and j in (3, 7):
                load_wi(e + 1, j // 4)
        for dc in range(8):
            for ci, (lo, n) in enumerate(chunks):
                p = PS[4 + it % 2]
                it += 1
                for j in range(8):
                    S.mm(p[:, 0:n], wo[:, j, dc * 128:(dc + 1) * 128], actT[:, j, offs[ci]:offs[ci] + n], j == 0, j == 7,
                         R(wo) + [actT.k(j)], R(p))
                S.tt(acc[:, dc, offs[ci]:offs[ci] + n], acc[:, dc, offs[ci]:offs[ci] + n], p[:, 0:n], ALU.add,
                     R(p) + [acc.k(dc)], [acc.k(dc)])
    return offs


def ln2_out(S, C, acc, offs, chunks, cols, x1d, xout, tmp, mk):
    PS = C.PS
    x1b = S.carve("x1b", [8, 512], F32)
    r = S.carve("r2", [8, 512], F32)
    x2 = S.carve("x2", [8, 512], F32)
    tA = S.carve("tA2", [512], F32)
    orow = [S.carve("orow%d" % i, [1024], F32) for i in range(2)]
    lt = ln_tmp(S, 512, R(x2))
    it = 0
    for ci, (lo, n) in enumerate(chunks):
        col = cols[ci]
        S.dma(x1b[:, :, 0:n], x1d[:, :, lo:lo + n], R(x1d), R(x1b))
        for dc in range(8):
            S.act(tA[:, 0:n], acc[:, dc, offs[ci]:offs[ci] + n], AF.Identity, [acc.k(dc)] + R(C.mod), R(tA),
                  scale=C.mod[:, G2 + dc, col:col + 1])
            S.stt(r[:, dc, 0:n], x1b[:, dc, 0:n], ALPHA, tA[:, 0:n], ALU.mult, ALU.add, R(x1b, tA), R(r))
        layer_norm_fm(S, C, r, n, 2, lambda c: x2[:, c, 0:n], lt)
        for i in range(n // 128):
            for dc in range(8):
                p = PS[dc // 4]
                S.tr(p[:, (dc % 4) * 128:(dc % 4 + 1) * 128], x2[:, dc, i * 128:(i + 1) * 128], C.ident, R(x2, C.cs), R(p))
            ot = orow[it % 2]
            it += 1
            S.cp(ot[:, 0:512], PS[0][:, :], R(PS[0]), R(ot), eng="act")
            S.cp(ot[:, 512:1024], PS[1][:, :], R(PS[1]), R(ot))
            S.dma(xout[lo + i * 128:lo + (i + 1) * 128, :], ot[:], R(ot), R(xout))


NT = 34
TOK = 4352
OWN = [0] + list(range(2, 18))
NOWN = 2176


def own_segments(t0, t1):
    segs = []
    for lo, hi, base in ((0, 128, 0), (256, 2304, 128)):
        a, b = max(t0, lo), min(t1, hi)
        if a < b:
            segs.append((a, b - a, base + a - lo))
    return segs


class _Stop(Exception):
    pass


def build_l0(debug=False):
    import os
    stop = int(os.environ.get("STOP", "99"))

    def chk(n):
        if stop == n:
            raise _Stop()

    nc = bass.Bass("TRN2", target_bir_lowering=False)
    es = ExitStack()
    with es:
      S = Sched(nc, es)
      try:
        _build(S, nc, chk)
      except _Stop:
        print("STOPPED at", stop)
        S.finish([])
      S.emit()
    return nc


def _build(S, nc, chk, fused=False):
    if True:
        C = Ctx()
        D = lambda name, shape, dt=F32, kind="ExternalInput": S.dram(name, shape, dt, kind)
        xs = D("xs", [TOK, 1024])
        cT = D("cT", [128, 8, 2])
        mod_w = D("mod_w", [1024, 6144])
        mod_bT = D("mod_bT", [128, 48])
        lnp_d = D("lnp", [128, 4, 8])
        win = D("win", [8, 1024, 776])
        convp_d = D("convp", [128, 8, 4, 6])
        ssdp_d = D("ssdp", [1, 160])
        normw_d = D("normw", [128, 16])
        wout_d = D("wout", [2048, 1024])
        rw_d = D("rw", [128, 8, 32])
        rb_d = D("rb", [1, 32])
        xout = D("xout", [NOWN, 1024], F32, "Internal" if fused else "ExternalOutput")
        hTd = D("hTd", [128, 8, TOK], BF16, "Internal")
        ygd = D("ygd", [128, 16, NOWN], BF16, "Internal")
        x1d = D("x1d", [128, 8, NOWN], F32, "Internal")
        setup_common(S, C, True)
        PS = C.PS
        if not hasattr(S, "ar"):
            S.arena(49800)
        C.mod = S.carve("mod", [48, 2], F32)
        C.lnp = S.carve("lnp", [4, 8], F32)
        S.dma(C.lnp[:], lnp_d[:], [], R(C.lnp))
        modulation(S, C, mod_w, mod_bT, cT)
        base = S.mark()
        chk(0)

        xr = [S.carve("xr%d" % i, [1024], F32) for i in range(2)]
        hb = [S.carve("hb%d" % i, [8, 512], BF16) for i in range(2)]
        for blk in range(9):
            t0 = blk * 4
            nt = min(4, NT - t0)
            h = hb[blk % 2]
            for i in range(nt):
                t = t0 + i
                col = 1 if t < 2 else 0
                x = xr[t % 2]
                S.dma(x[:], xs[t * 128:(t + 1) * 128, :], [], R(x))
                for kc in range(8):
                    p = PS[kc // 4 + 2 * (t % 2)]
                    S.tr(p[:, (kc % 4) * 128:(kc % 4 + 1) * 128], x[:, kc * 128:(kc + 1) * 128], C.ident, R(x, C.cs), R(p))
                for kc in range(8):
                    p = PS[kc // 4 + 2 * (t % 2)]
                    S.act(h[:, kc, i * 128:(i + 1) * 128], p[:, (kc % 4) * 128:(kc % 4 + 1) * 128], AF.Identity,
                          R(p, C.mod), R(h), scale=C.mod[:, SC1 + kc, col:col + 1], bias=C.mod[:, SH1 + kc, col:col + 1])
            S.dma(hTd[:, :, t0 * 128:(t0 + nt) * 128], h[:, :, 0:nt * 128], R(h), R(hTd))
        S.reset(base)
        chk(1)

        convp = S.carve("convp", [8, 4, 6], F32)
        S.dma(convp[:], convp_d[:], [], R(convp))
        ssdp = S.carve("ssdp", [8, 20], F32)
        S.dma(ssdp[:], ssdp_d.h.partition_broadcast(128).rearrange("p o (g k) -> p (o g) k", k=20), [], R(ssdp))
        normw = S.carve("normw", [16], F32)
        S.dma(normw[:], normw_d[:], [], R(normw))
        wg = [S.carve("wg%d" % i, [8, 776], BF16) for i in range(2)]
        hblk = [S.carve("hblk%d" % i, [8, 512], BF16) for i in range(2)]
        raw = [S.carve("raw%d" % i, [4358], BF16) for i in range(2)]
        for r_ in raw:
            S.memset(r_[:], 0.0, R(r_))
        cv = [S.carve("cv%d" % i, [1024], F32) for i in range(2)]
        fm = S.carve("fm", [TOK], BF16)
        bT = S.carve("bT", [TOK], BF16)
        cTt = S.carve("cTt", [2304], BF16)
        zT = S.carve("zT", [2, NOWN], BF16)
        xtok = S.carve("xtok", [NT, 256], BF16)
        btok = S.carve("btok", [NT, 128], BF16)
        yacc = S.carve("yacc", [17, 256], F32, nreg=17)
        ygs = S.carve("ygs", [2, NOWN], BF16)
        dtv = S.carve("dtv", [2, NT, 4], F32)
        dte = S.carve("dte", [2, NT, 4], F32)
        lndt = S.carve("lndt", [2, NT, 4], F32)
        la = S.carve("la", [2, NT, 4], F32)
        biasL = S.carve("biasL", [2, NT, 4], F32)
        ecum = S.carve("ecum", [2, NT, 4], F32)
        wdec = S.carve("wdec", [2, NT, 4], F32)
        cdec = S.carve("cdec", [2, NT, 4], F32)
        aneg = S.carve("aneg", [8], F32)
        cumS = S.carve("cumS", [2, NT, 4], F32)
        hl_t = S.carve("hl_t", [2, NT, 4], F32)
        cum_hi = S.carve("cum_hi", [2, NT, 4], BF16)
        cum_lo = S.carve("cum_lo", [2, NT, 4], BF16)
        bl_hi = S.carve("bl_hi", [2, NT, 4], BF16)
        bl_lo = S.carve("bl_lo", [2, NT, 4], BF16)
        NEG4 = [S.carve("NEG4%d" % i, [4, 128], BF16) for i in range(2)]
        for d in range(2):
            S.cp(NEG4[d][:], C.NEG1[d].unsqueeze(1).to_broadcast([128, 4, 128]), R(C.cs), R(NEG4[d]))
        Lp = [S.carve("Lp%d" % i, [4, 128], F32) for i in range(2)]
        Mp = [S.carve("Mp%d" % i, [4, 128], BF16) for i in range(2)]
        t1 = [S.carve("t1%d" % i, [4, 64], F32) for i in range(2)]
        t2 = [S.carve("t2%d" % i, [4, 64], F32) for i in range(2)]
        xdec = [S.carve("xdec%d" % i, [4, 64], BF16) for i in range(2)]
        st = [S.carve("st%d" % i, [4, 64], F32) for i in range(2)]
        stbf = [S.carve("stbf%d" % i, [4, 64], BF16) for i in range(2)]
        yz = S.carve("yz", [2, 512], F32)
        sqg = S.carve("sqg", [2, 512], F32)
        rsg = S.carve("rsg", [512], F32)
        hv = hTd.h
        blocks = [(b * 512, min(512, TOK - b * 512)) for b in range(9)]
        orderA = list(range(NT))
        orderB = [1, 0] + list(range(NT - 1, 1, -1))
        flat = lambda t: t[:].rearrange("p a b -> p (a b)")

        for g in range(8):
            w = wg[g % 2]
            S.dma(w[:], win.h[g].rearrange("(kc p) f -> p kc f", p=128), [], R(w), eng="pool")

            def inproj_pass(chunks, evac, with_dt):
                for bi, (b0, n) in enumerate(blocks):
                    hbk = hblk[bi % 2]
                    S.dma(hbk[:, :, 0:n], hv[:, :, b0:b0 + n], R(hTd), R(hbk))
                    for ci, off in enumerate(chunks):
                        p = PS[ci % 2]
                        for kc in range(8):
                            S.mm(p[:, 0:n], w[:, kc, off:off + 128], hbk[:, kc, 0:n], kc == 0, kc == 7, R(w, hbk), R(p))
                        evac(ci, p, b0, n)
                    if with_dt:
                        for i in range(n // 128):
                            t = b0 // 128 + i
                            for kc in range(8):
                                S.mm(PS[2][:, t * 8:(t + 1) * 8], hbk[:, kc, i * 128:(i + 1) * 128], w[:, kc, 768:776],
                                     kc == 0, kc == 7, R(w, hbk), R(PS[2]))

            def evac_raw(ci, p, b0, n):
                r_ = raw[ci]
                for lo, hi, sh in ((0, 256, 2), (256, TOK, 4)):
                    a, b = max(b0, lo), min(b0 + n, hi)
                    if a < b:
                        S.cp(r_[:, a + sh:b + sh], p[:, a - b0:b - b0], R(p), R(r_), eng="act")

            def conv_silu(slot, ch, dst, limit=TOK):
                r_ = raw[slot]
                segs = [(0, 256, 0)] + [(256 + 1024 * k, 1024, 258 + 1024 * k) for k in range(4)]
                segs = [sg for sg in segs if sg[0] < limit]
                for si, (olo, n, rlo) in enumerate(segs):
                    c_ = cv[si % 2]
                    S.ts(c_[:, 0:n], r_[:, rlo:rlo + n], convp[:, g, ch, 0:1], convp[:, g, ch, 5:6], ALU.mult, ALU.add,
                         R(r_, convp), R(c_))
                    for j in range(1, 5):
                        S.stt(c_[:, 0:n], r_[:, rlo + j:rlo + j + n], convp[:, g, ch, j:j + 1], c_[:, 0:n], ALU.mult, ALU.add,
                              R(r_, convp, c_), R(c_))
                    S.act(dst[:, olo:olo + n], c_[:, 0:n], AF.Silu, R(c_), R(dst))

            if g == 0: chk(20)
            inproj_pass([256, 384], evac_raw, False)
            if g == 0: chk(21)
            for xi in range(2):
                conv_silu(xi, xi, fm)
                for t0 in range(0, NT, 4):
                    nt = min(4, NT - t0)
                    pb = PS[3 + (t0 // 4) % 2][:, 0:256].bitcast(BF16)
                    pk = PS[3 + (t0 // 4) % 2]
                    for i in range(nt):
                        S.tr(pb[:, i * 128:(i + 1) * 128], fm[:, (t0 + i) * 128:(t0 + i + 1) * 128], C.identbf[:],
                             R(fm, C.identbf), R(pk))
                    S.cp(xtok[:, t0:t0 + nt, xi * 128:(xi + 1) * 128],
                         pb[:, 0:nt * 128].rearrange("p (a b) -> p a b", b=128), R(pk), R(xtok), eng="act")
            if g == 0: chk(22)
            inproj_pass([512, 640], evac_raw, False)
            conv_silu(0, 2, bT)
            conv_silu(1, 3, cTt, 2304)
            for t0 in range(0, NT, 4):
                nt = min(4, NT - t0)
                pb = PS[3 + (t0 // 4) % 2][:, 0:256].bitcast(BF16)
                pk = PS[3 + (t0 // 4) % 2]
                for i in range(nt):
                    S.tr(pb[:, i * 128:(i + 1) * 128], bT[:, (t0 + i) * 128:(t0 + i + 1) * 128], C.identbf[:],
                         R(bT, C.identbf), R(pk))
                S.cp(btok[:, t0:t0 + nt, :], pb[:, 0:nt * 128].rearrange("p (a b) -> p a b", b=128), R(pk), R(btok), eng="act")

            if g == 0: chk(23)
            def evac_z(ci, p, b0, n):
                for a, m, o in own_segments(b0, b0 + n):
                    S.act(zT[:, ci, o:o + m], p[:, a - b0:a - b0 + m], AF.Silu, R(p), R(zT))

            inproj_pass([0, 128], evac_z, True)
            if g == 0: chk(24)
            dtb = ssdp[:, g, 0:8].rearrange("p (d h) -> p d h", h=4).unsqueeze(2).to_broadcast([128, 2, NT, 4])
            if g == 0: chk(240)
            S.tt(dtv[:], PS[2][:, 0:NT * 8].rearrange("p (c d h) -> p d c h", d=2, h=4), dtb, ALU.add, R(PS[2], ssdp), R(dtv))
            if g == 0: chk(241)
            S.act(dte[:], dtv[:], AF.Exp, R(dtv), R(dte))
            if g == 0: chk(242)
            S.act(dtv[:], dte[:], AF.Ln, R(dte), R(dtv), bias=1.0)
            if g == 0: chk(243)
            S.act(lndt[:], dtv[:], AF.Ln, R(dtv), R(lndt))
            if g == 0: chk(244)
            S.act(aneg[:], ssdp[:, g, 8:16], AF.Exp, R(ssdp), R(aneg))
            if g == 0: chk(245)
            S.ts(aneg[:], aneg[:], -1.0, None, ALU.mult, None, R(aneg), R(aneg))
            if g == 0: chk(246)
            S.tt(la[:], dtv[:], aneg[:].rearrange("p (d h) -> p d h", h=4).unsqueeze(2).to_broadcast([128, 2, NT, 4]),
                 ALU.mult, R(dtv, aneg), R(la))
            laf = la[:].rearrange("p d c h -> p (d c h)")
            if g == 0: chk(247)
            for d in range(2):
                S.mm(PS[5][:, d * 136:(d + 1) * 136], C.U[d], laf[:, d * 136:(d + 1) * 136], True, True, R(la, C.cs), R(PS[5]))
            if g == 0: chk(248)
            S.mm(PS[6][:, 0:272], C.ones, laf, True, True, R(la, C.cs), R(PS[6]))
            fl = lambda t: t[:].rearrange("p d c h -> p (d c h)")
            if g == 0: chk(249)
            S.tt(fl(biasL), fl(lndt), PS[5][:, 0:272], ALU.subtract, R(lndt, PS[5]), R(biasL))
            if g == 0: chk(250)
            S.cp(fl(cumS), PS[5][:, 0:272], R(PS[5]), R(cumS))
            S.act(fl(ecum), fl(cumS), AF.Exp, R(cumS), R(ecum))
            for src, hi, lo in ((cumS, cum_hi, cum_lo), (biasL, bl_hi, bl_lo)):
                S.cp(fl(hi), fl(src), R(src), R(hi))
                S.tt(fl(hl_t), fl(src), fl(hi), ALU.subtract, R(src, hi), R(hl_t))
                S.cp(fl(lo), fl(hl_t), R(hl_t), R(lo))
            if g == 0: chk(251)
            S.tt(fl(wdec), fl(biasL), PS[6][:, 0:272], ALU.add, R(biasL, PS[6]), R(wdec))
            if g == 0: chk(252)
            S.act(fl(wdec), fl(wdec), AF.Exp, R(wdec), R(wdec))
            if g == 0: chk(253)
            S.cp(fl(cdec), PS[6][:, 0:272], R(PS[6]), R(cdec))
            S.act(fl(cdec), fl(cdec), AF.Exp, R(cdec), R(cdec))

            if g == 0: chk(25)
            Dh = ssdp[:, g, 16:20].unsqueeze(2).to_broadcast([128, 4, 64])
            for oc, c in enumerate(OWN):
                S.tt(yacc[:, oc, :].rearrange("p (h q) -> p h q", q=64), xtok[:, c, :].rearrange("p (h q) -> p h q", q=64),
                     Dh, ALU.mult, R(xtok, ssdp), [yacc.k(oc)], eng="pool")
            for d in range(2):
                S.memset(st[d][:], 0.0, R(st[d]))
                S.memset(stbf[d][:], 0.0, R(stbf[d]))
            step = 0
            for i in range(NT):
                for d, order in ((0, orderA), (1, orderB)):
                    c = order[i]
                    par = step % 2
                    step += 1
                    tc0 = c * 128
                    own = c in OWN
                    pCB, pseg, pY = PS[0], PS[1 + par], PS[3 + par]
                    if own:
                        oc = OWN.index(c)
                        S.mm(pseg[:, :], C.identbf[:], flat(NEG4[d]), True, False, R(NEG4[d], C.identbf), R(pseg))
                        for h in range(4):
                            ps_h = pseg[:, h * 128:(h + 1) * 128]
                            for ti, t_ in enumerate((cum_hi, cum_lo)):
                                S.mm(ps_h, t_[:, d, c, h:h + 1].to_broadcast([128, 128]), C.identbf[:], False, False,
                                     R(t_, C.identbf), R(pseg))
                            for ti, t_ in enumerate((bl_hi, bl_lo)):
                                S.mm(ps_h, C.identbf[:], t_[:, d, c, h:h + 1].to_broadcast([128, 128]), False,
                                     h == 3 and ti == 1, R(t_, C.identbf), R(pseg))
                        S.mm(pCB[:, 0:128], bT[:, tc0:tc0 + 128], cTt[:, tc0:tc0 + 128], True, True, R(bT, cTt), R(pCB))
                        S.act(flat(Lp[par]), pseg[:, :], AF.Exp, R(pseg), R(Lp[par]))
                        S.tt(Mp[par][:], Lp[par][:], pCB[:, 0:128].unsqueeze(1).to_broadcast([128, 4, 128]), ALU.mult,
                             R(Lp[par], pCB), R(Mp[par]))
                        S.mm(pY[:, 256:512], cTt[:, tc0:tc0 + 128], flat(stbf[d]), True, True, R(cTt, stbf[d]), R(pY))
                    if any(cc in OWN for cc in order[i + 1:]):
                        pds = PS[5 + par]
                        S.tt(xdec[par][:], xtok[:, c, :].rearrange("p (h q) -> p h q", q=64),
                             wdec[:, d, c, :].unsqueeze(2).to_broadcast([128, 4, 64]), ALU.mult, R(xtok, wdec), R(xdec[par]),
                             eng="pool")
                        S.mm(pds[:, 0:256], btok[:, c, :], flat(xdec[par]), True, True, R(btok, xdec[par]), R(pds))
                        S.tt(st[d][:], st[d][:], cdec[:, d, c, :].unsqueeze(2).to_broadcast([128, 4, 64]), ALU.mult,
                             R(st[d], cdec), R(st[d]), eng="pool")
                        S.tt(flat(st[d]), flat(st[d]), pds[:, 0:256], ALU.add, R(st[d], pds), R(st[d]))
                        S.cp(stbf[d][:], st[d][:], R(st[d]), R(stbf[d]), eng="act")
                    if own:
                        for h in range(4):
                            S.mm(pY[:, h * 64:(h + 1) * 64], Mp[par][:, h, :], xtok[:, c, h * 64:(h + 1) * 64], True, True,
                                 R(Mp[par], xtok), R(pY))
                        S.tt(t1[par][:], pY[:, 256:512].rearrange("p (h q) -> p h q", q=64),
                             ecum[:, d, c, :].unsqueeze(2).to_broadcast([128, 4, 64]), ALU.mult, R(pY, ecum), R(t1[par]))
                        S.tt(flat(t2[par]), flat(t1[par]), pY[:, 0:256], ALU.add, R(t1[par], pY), R(t2[par]))
                        S.tt(yacc[:, oc, :], yacc[:, oc, :], flat(t2[par]), ALU.add, R(t2[par]) + [yacc.k(oc)], [yacc.k(oc)],
                             eng="pool")

            if g == 0: chk(26)
            for o0 in range(0, 17, 4):
                nt = min(4, 17 - o0)
                n = nt * 128
                for i in range(nt):
                    for j in range(2):
                        S.tr(PS[5 + j][:, i * 128:(i + 1) * 128], yacc[:, o0 + i, j * 128:(j + 1) * 128], C.ident,
                             [yacc.k(o0 + i)] + R(C.cs), R(PS[5 + j]))
                for j in range(2):
                    S.tt(yz[:, j, 0:n], PS[5 + j][:, 0:n], zT[:, j, o0 * 128:o0 * 128 + n], ALU.mult, R(PS[5 + j], zT), R(yz))
                S.tt(sqg[:, :, 0:n], yz[:, :, 0:n], yz[:, :, 0:n], ALU.mult, R(yz), R(sqg), eng="pool")
                for j in range(2):
                    S.mm(PS[7][:, 0:n], C.ones, sqg[:, j, 0:n], j == 0, j == 1, R(sqg, C.cs), R(PS[7]))
                S.act(rsg[:, 0:n], PS[7][:, 0:n], AF.Sqrt, R(PS[7]), R(rsg), scale=1.0 / 256, bias=1e-5)
                S.recip(rsg[:, 0:n], rsg[:, 0:n], R(rsg), R(rsg))
                S.tt(yz[:, :, 0:n], yz[:, :, 0:n], rsg[:, 0:n].unsqueeze(1).to_broadcast([128, 2, n]), ALU.mult,
                     R(yz, rsg), R(yz))
                for j in range(2):
                    S.act(ygs[:, j, o0 * 128:o0 * 128 + n], yz[:, j, 0:n], AF.Identity, R(yz, normw), R(ygs),
                          scale=normw[:, 2 * g + j:2 * g + j + 1])
            S.dma(ygd[:, 2 * g:2 * g + 2, :], ygs[:], R(ygs), R(ygd))
            if g == 0: chk(27)
        S.reset(base)
        chk(2)

        XT = S.carve("XT", [8, NOWN], BF16)
        gateT = S.carve("gateT", [NOWN], F32, parts=32)
        p3 = S.mark()
        wout = S.carve("wout", [16, 1024], BF16)
        for h in range(2):
            S.dma(wout[:, h * 8:(h + 1) * 8, :], wout_d.h.rearrange("(kc p) f -> p kc f", p=128)[:, h * 8:(h + 1) * 8, :],
                  [], R(wout), eng="pool")
        rw = S.carve("rw", [8, 32], F32)
        S.dma(rw[:], rw_d[:], [], R(rw))
        rb = S.carve("rb", [32], F32)
        S.dma(rb[:], rb_d.h.partition_broadcast(128).rearrange("p o k -> p (o k)"), [], R(rb))
        ygb = S.carve("ygb", [16, 512], BF16)
        xrw = S.carve("xrw", [4, 1024], F32)
        r = S.carve("r1", [8, 512], F32)
        x1t = S.carve("x1t", [8, 512], F32)
        Xf = S.carve("Xf", [8, 512], F32)
        tA = S.carve("tA", [512], F32)
        lt = ln_tmp(S, 512, R(x1t))
        rt = router_tmp(S)
        chunks3 = [(0, 128, 1)] + [(128 + 512 * k, 512, 0) for k in range(4)]
        for (o0, n, col) in chunks3:
            row0 = o0 if o0 < 128 else o0 + 128
            S.dma(ygb[:, :, 0:n], ygd[:, :, o0:o0 + n], R(ygd), R(ygb))
            S.dma(xrw[:, 0:n // 128, :], xs[row0:row0 + n, :].rearrange("(i p) f -> p i f", p=128), [], R(xrw))
            for dc in range(8):
                py, px = PS[dc % 2], PS[2 + dc % 2]
                for kc in range(16):
                    S.mm(py[:, 0:n], wout[:, kc, dc * 128:(dc + 1) * 128], ygb[:, kc, 0:n], kc == 0, kc == 15, R(wout, ygb), R(py))
                for i in range(n // 128):
                    S.tr(px[:, i * 128:(i + 1) * 128], xrw[:, i, dc * 128:(dc + 1) * 128], C.ident, R(xrw, C.cs), R(px))
                S.act(tA[:, 0:n], py[:, 0:n], AF.Identity, R(py, C.mod), R(tA), scale=C.mod[:, G1 + dc, col:col + 1])
                S.stt(r[:, dc, 0:n], px[:, 0:n], ALPHA, tA[:, 0:n], ALU.mult, ALU.add, R(px, tA), R(r))
            layer_norm_fm(S, C, r, n, 0, lambda c: x1t[:, c, 0:n], lt)
            S.dma(x1d[:, :, o0:o0 + n], x1t[:, :, 0:n], R(x1t), R(x1d))
            for dc in range(8):
                S.act(Xf[:, dc, 0:n], x1t[:, dc, 0:n], AF.Identity, R(x1t, C.mod), R(Xf),
                      scale=C.mod[:, SC2 + dc, col:col + 1], bias=C.mod[:, SH2 + dc, col:col + 1])
            S.cp(XT[:, :, o0:o0 + n], Xf[:, :, 0:n], R(Xf), R(XT), eng="pool")
            router(S, C, Xf, n, o0, rw, rb, gateT, rt)
        S.reset(p3)
        chk(3)

        mw_in = D("mw_in", [32, 1024, 2048])
        mb_inT = D("mb_inT", [128, 32, 16])
        mw_out = D("mw_out", [32, 1024, 1024])
        mb_out = D("mb_out", [32, 1024])
        b_inT = S.carve("b_inT", [32, 16], F32)
        S.dma(b_inT[:], mb_inT[:], [], R(b_inT))
        S.ts(b_inT[:, :, 8:16], b_inT[:, :, 8:16], 1.0, None, ALU.add, None, R(b_inT), R(b_inT))
        p4 = S.mark()
        tblocks = [([(0, 128), (128, 512), (640, 512)], [1, 0, 0]), ([(1152, 512), (1664, 512)], [0, 0])]
        for chunks, cols in tblocks:
            acc = S.carve("acc", [8, sum(n for _, n in chunks)], F32, nreg=8)
            m = S.mark()
            offs = moe_block(S, C, XT, gateT, chunks, mw_in, mw_out, b_inT, mb_out, acc)
            S.reset(m)
            ln2_out(S, C, acc, offs, chunks, cols, x1d, xout, None, None)
            S.reset(p4)
        if not fused:
            S.finish(R(xout))
        return xout


TOK1 = 4352
NQ = 2048
NT1 = 34
MLA_SCALE = 192.0 ** -0.5
RMS_EPS = 1e-6


class _Stop1(Exception):
    pass


def build_l1():
    import os
    stop = int(os.environ.get("STOP", "99"))

    def chk(n):
        if stop == n:
            raise _Stop1()

    nc = bass.Bass("TRN2", target_bir_lowering=False)
    es = ExitStack()
    with es:
        S = Sched(nc, es)
        try:
            _build1(S, nc, chk)
        except _Stop1:
            print("STOPPED at", stop)
            S.finish([])
        S.emit()
    return nc


def _build1(S, nc, chk, fused=False, xown=None, xall=None):
    C = Ctx()
    D = lambda name, shape, dt=F32, kind="ExternalInput": S.dram(name, shape, dt, kind)
    xs = None if fused else D("xs", [TOK1, 1024])
    NTQ = 16 if fused else 0
    cT = D("cT", [128, 8, 2])
    mod_w = D("mod_w", [1024, 6144])
    mod_bT = D("mod_bT", [128, 48])
    lnp_d = D("lnp", [128, 4, 8])
    wa_d = D("wa", [1024, 704])
    qnorm_d = D("qnorm", [128, 3])
    kvnorm_d = D("kvnorm", [128, 2])
    wqb_d = D("wqb", [384, 1536])
    wkvb_d = D("wkvb", [256, 2048])
    wo_d = D("wo", [1024, 1024])
    ropeq_d = D("ropeq", [64, 2, NQ])
    ropek_d = D("ropek", [64, 2, TOK1])
    rm_d = D("rm", [64, 64])
    rw_d = D("rw", [128, 8, 32])
    rb_d = D("rb", [1, 32])
    xout = D("xout", [NQ, 1024], F32, "ExternalOutput")
    hTd = D("hTd", [128, 8, TOK1 + NTQ * 128], BF16, "Internal")
    x1d = D("x1d", [128, 8, NQ], F32, "Internal")
    setup_common(S, C, True)
    PS = C.PS
    if not hasattr(S, "ar"):
        S.arena(49800)
    C.mod = S.carve("mod", [48, 2], F32)
    C.lnp = S.carve("lnp", [4, 8], F32)
    S.dma(C.lnp[:], lnp_d[:], [], R(C.lnp))
    onesbf = S.carve("onesbf", [128], BF16)
    S.cp(onesbf[:], C.ones, R(C.cs), R(onesbf))
    c128 = S.carve("c128", [128], BF16)
    S.ts(c128[:], C.ones, 1.0 / 128, None, ALU.mult, None, R(C.cs), R(c128))
    rm = S.carve("rm", [64], F32, parts=64)
    S.dma(rm[:], rm_d[:], [], R(rm))
    modulation(S, C, mod_w, mod_bT, cT)
    XT = S.carve("XT", [8, NQ], BF16)
    gateT = S.carve("gateT", [NQ], F32, parts=32)
    markA = S.mark()
    oT = S.carve("oT", [8, NQ], BF16, nreg=8)
    markB = S.mark()
    chk(0)

    xr = [S.carve("xr%d" % i, [1024], F32) for i in range(2)]
    hb = [S.carve("hb%d" % i, [8, 512], BF16) for i in range(2)]
    ntl = NT1 + NTQ
    for blk in range((ntl + 3) // 4):
        t0 = blk * 4
        nt = min(4, ntl - t0)
        h = hb[blk % 2]
        for i in range(nt):
            t = t0 + i
            x = xr[t % 2]
            if not fused:
                col = 1 if t >= 32 else 0
                S.dma(x[:], xs[t * 128:(t + 1) * 128, :], [], R(x))
            elif t < NT1:
                col = 1 if t in (0, 1) else 0
                S.dma(x[:], xall[t * 128:(t + 1) * 128, :], R(xall), R(x))
            else:
                col = 0
                S.dma(x[:], xown[128 + (t - NT1) * 128:128 + (t - NT1 + 1) * 128, :], R(xown), R(x))
            for kc in range(8):
                p = PS[kc // 4 + 2 * (t % 2)]
                S.tr(p[:, (kc % 4) * 128:(kc % 4 + 1) * 128], x[:, kc * 128:(kc + 1) * 128], C.ident, R(x, C.cs), R(p))
            for kc in range(8):
                p = PS[kc // 4 + 2 * (t % 2)]
                S.act(h[:, kc, i * 128:(i + 1) * 128], p[:, (kc % 4) * 128:(kc % 4 + 1) * 128], AF.Identity,
                      R(p, C.mod), R(h), scale=C.mod[:, SC1 + kc, col:col + 1], bias=C.mod[:, SH1 + kc, col:col + 1])
        S.dma(hTd[:, :, t0 * 128:(t0 + nt) * 128], h[:, :, 0:nt * 128], R(h), R(hTd))
    S.reset(markB)
    chk(1)

    kvn = S.carve("kvn", [2, TOK1], BF16)
    qln = S.carve("qln", [3, NQ], BF16)
    KrT = S.carve("KrT", [TOK1], BF16)
    S.memset(KrT[64:128, :], 0.0, R(KrT))
    markC = S.mark()
    wa = S.carve("wa", [8, 704], BF16)
    S.dma(wa[:], wa_d.h.rearrange("(kc p) f -> p kc f", p=128), [], R(wa), eng="pool")
    qnorm = S.carve("qnorm", [3], F32)
    S.dma(qnorm[:], qnorm_d[:], [], R(qnorm))
    kvnorm = S.carve("kvnorm", [2], F32)
    S.dma(kvnorm[:], kvnorm_d[:], [], R(kvnorm))
    hblk = [S.carve("hblk%d" % i, [8, 512], BF16) for i in range(2)]
    kvl = S.carve("kvl", [2, 512], F32)
    ql = S.carve("ql", [3, 512], F32)
    krl = S.carve("krl", [512], F32)
    sq = S.carve("sq", [3, 512], F32)
    rs = S.carve("rs", [512], F32)
    rk = S.carve("rk", [2, 512], F32)
    t1 = S.carve("t1", [512], F32)
    t2 = S.carve("t2", [512], F32)
    blocks = [(b * 512, min(512, TOK1 - b * 512)) for b in range(9)]

    def rms_fm(src, nch, width, g, dst, d0, n):
        S.tt(sq[:, 0:nch, 0:n], src[:, 0:nch, 0:n], src[:, 0:nch, 0:n], ALU.mult, R(src), R(sq), eng="pool")
        for c in range(nch):
            S.mm(PS[6][:, 0:n], C.ones, sq[:, c, 0:n], c == 0, c == nch - 1, R(sq, C.cs), R(PS[6]))
        S.cp(rs[:, 0:n], PS[6][:, 0:n], R(PS[6]), R(rs))
        S.act(rs[:, 0:n], rs[:, 0:n], AF.Sqrt, R(rs), R(rs), scale=1.0 / width, bias=RMS_EPS)
        S.recip(rs[:, 0:n], rs[:, 0:n], R(rs), R(rs))
        S.tt(src[:, 0:nch, 0:n], src[:, 0:nch, 0:n], rs[:, 0:n].unsqueeze(1).to_broadcast([128, nch, n]), ALU.mult,
             R(src, rs), R(src))
        for c in range(nch):
            S.act(dst[:, c, d0:d0 + n], src[:, c, 0:n], AF.Identity, R(src, g), R(dst), scale=g[:, c:c + 1])

    def rope(src, tab, dst_ap, n, dstkeys):
        S.mm(PS[7][0:64, 0:n], rm[0:64, 0:64], src[0:64, 0:n], True, True, R(src, rm), R(PS[7]))
        S.tt(t1[0:64, 0:n], src[0:64, 0:n], tab[0:64, 0, 0:n], ALU.mult, R(src, tab), R(t1))
        S.tt(t2[0:64, 0:n], PS[7][0:64, 0:n], tab[0:64, 1, 0:n], ALU.mult, R(PS[7], tab), R(t2))
        S.tt(dst_ap, t1[0:64, 0:n], t2[0:64, 0:n], ALU.add, R(t1, t2), dstkeys, eng="pool")

    for bi, (b0, n) in enumerate(blocks):
        hbk = hblk[bi % 2]
        S.dma(hbk[:, :, 0:n], hTd.h[:, :, b0:b0 + n], R(hTd), R(hbk))
        S.dma(rk[0:64, :, 0:n], ropek_d[:, :, b0:b0 + n], [], R(rk))
        it = 0
        for c in range(2):
            p = PS[it % 2]
            it += 1
            for kc in range(8):
                S.mm(p[:, 0:n], wa[:, kc, 384 + c * 128:384 + (c + 1) * 128], hbk[:, kc, 0:n], kc == 0, kc == 7, R(wa, hbk), R(p))
            S.cp(kvl[:, c, 0:n], p[:, 0:n], R(p), R(kvl))
        p = PS[2]
        for kc in range(8):
            S.mm(p[0:64, 0:n], wa[:, kc, 640:704], hbk[:, kc, 0:n], kc == 0, kc == 7, R(wa, hbk), R(p))
        S.cp(krl[0:64, 0:n], p[0:64, 0:n], R(p), R(krl))
        if b0 < NQ and not fused:
            for c in range(3):
                p = PS[it % 2]
                it += 1
                for kc in range(8):
                    S.mm(p[:, 0:n], wa[:, kc, c * 128:(c + 1) * 128], hbk[:, kc, 0:n], kc == 0, kc == 7, R(wa, hbk), R(p))
                S.cp(ql[:, c, 0:n], p[:, 0:n], R(p), R(ql))
            rms_fm(ql, 3, 384, qnorm, qln, b0, n)
        rms_fm(kvl, 2, 256, kvnorm, kvn, b0, n)
        rope(krl, rk, KrT[0:64, b0:b0 + n], n, R(KrT))
    if fused:
        for qi in range(4):
            hbk = hblk[qi % 2]
            n = 512
            b0 = qi * 512
            S.dma(hbk[:, :, 0:n], hTd.h[:, :, TOK1 + b0:TOK1 + b0 + n], R(hTd), R(hbk))
            for c in range(3):
                p = PS[c % 2]
                for kc in range(8):
                    S.mm(p[:, 0:n], wa[:, kc, c * 128:(c + 1) * 128], hbk[:, kc, 0:n], kc == 0, kc == 7, R(wa, hbk), R(p))
                S.cp(ql[:, c, 0:n], p[:, 0:n], R(p), R(ql))
            rms_fm(ql, 3, 384, qnorm, qln, b0, n)
    S.reset(markC)
    chk(2)

    wqb = S.carve("wqb", [3, 1536], BF16)
    S.dma(wqb[:], wqb_d.h.rearrange("(kc p) f -> p kc f", p=128), [], R(wqb), eng="pool")
    wkvb = S.carve("wkvb", [2, 2048], BF16)
    S.dma(wkvb[:], wkvb_d.h.rearrange("(kc p) f -> p kc f", p=128), [], R(wkvb), eng="pool")
    rq = S.carve("rq", [2, 512], F32)
    KnT = S.carve("KnT", [TOK1], BF16)
    V = S.carve("V", [NT1, 128], BF16)
    QnT = S.carve("QnT", [NQ], BF16)
    QrT = S.carve("QrT", [NQ], BF16)
    S.memset(QrT[64:128, :], 0.0, R(QrT))
    negm = S.carve("negm", [NQ], BF16)
    pT = [S.carve("pT%d" % i, [512], BF16) for i in range(3)]
    sqb = S.carve("sqb", [512], BF16)
    sqr = S.carve("sqr", [512], BF16)
    qrl = S.carve("qrl", [512], F32)
    t1 = S.carve("t1b", [512], F32)
    t2 = S.carve("t2b", [512], F32)
    tq = S.carve("tq", [512], F32)
    rden = S.carve("rden", [512], F32)
    mx = S.carve("mx", [16], F32)
    kmax = S.carve("kmax", [4], F32)
    for bi, (b0, n) in enumerate(blocks):
        S.tt(sqr[0:64, 0:n], KrT[0:64, b0:b0 + n], KrT[0:64, b0:b0 + n], ALU.mult, R(KrT), R(sqr), eng="pool")
        S.mm(PS[6][:, 0:n], onesbf[0:64, :], sqr[0:64, 0:n], True, True, R(sqr, onesbf), R(PS[6]))
        S.reduce(mx[:, bi:bi + 1], PS[6][:, 0:n], AX.X, ALU.max, R(PS[6]), R(mx))
    S.reduce(kmax[:, 0:1], mx[:, 0:9], AX.X, ALU.max, R(mx), R(kmax))
    qblocks = [(q * 512, 512) for q in range(4)]
    step = 0
    for h in range(8):
        for bi, (b0, n) in enumerate(blocks):
            p = PS[bi % 2]
            for kc in range(2):
                S.mm(p[:, 0:n], wkvb[:, kc, h * 128:(h + 1) * 128], kvn[:, kc, b0:b0 + n], kc == 0, kc == 1, R(wkvb, kvn), R(p))
            S.cp(KnT[:, b0:b0 + n], p[:, 0:n], R(p), R(KnT), eng="act")
            S.tt(sqb[:, 0:n], KnT[:, b0:b0 + n], KnT[:, b0:b0 + n], ALU.mult, R(KnT), R(sqb), eng="pool")
            S.mm(PS[6][:, 0:n], onesbf[:], sqb[:, 0:n], True, True, R(sqb, onesbf), R(PS[6]))
            S.reduce(mx[:, bi:bi + 1], PS[6][:, 0:n], AX.X, ALU.max, R(PS[6]), R(mx))
        S.reduce(kmax[:, 1:2], mx[:, 0:9], AX.X, ALU.max, R(mx), R(kmax))
        S.tt(kmax[:, 2:3], kmax[:, 0:1], kmax[:, 1:2], ALU.add, R(kmax), R(kmax))
        for t0 in range(0, NT1, 4):
            nt = min(4, NT1 - t0)
            p = PS[2 + (t0 // 4) % 2]
            for i in range(nt):
                for kc in range(2):
                    S.mm(p[:, i * 128:(i + 1) * 128], kvn[:, kc, (t0 + i) * 128:(t0 + i + 1) * 128],
                         wkvb[:, kc, 1024 + h * 128:1024 + (h + 1) * 128], kc == 0, kc == 1, R(wkvb, kvn), R(p))
            S.cp(V[:, t0:t0 + nt, :], p[:, 0:nt * 128].rearrange("p (a b) -> p a b", b=128), R(p), R(V), eng="act")
        for (q0, n) in qblocks:
            p = PS[0]
            for kc in range(3):
                S.mm(p[:, 0:n], wqb[:, kc, h * 128:(h + 1) * 128], qln[:, kc, q0:q0 + n], kc == 0, kc == 2, R(wqb, qln), R(p))
            S.cp(QnT[:, q0:q0 + n], p[:, 0:n], R(p), R(QnT), eng="act")
            p = PS[1]
            for kc in range(3):
                S.mm(p[0:64, 0:n], wqb[:, kc, 1024 + h * 64:1024 + (h + 1) * 64], qln[:, kc, q0:q0 + n], kc == 0, kc == 2,
                     R(wqb, qln), R(p))
            S.cp(qrl[0:64, 0:n], p[0:64, 0:n], R(p), R(qrl))
            S.dma(rq[0:64, :, 0:n], ropeq_d[:, :, q0:q0 + n], [], R(rq))
            S.mm(PS[7][0:64, 0:n], rm[0:64, 0:64], qrl[0:64, 0:n], True, True, R(qrl, rm), R(PS[7]))
            S.tt(t1[0:64, 0:n], qrl[0:64, 0:n], rq[0:64, 0, 0:n], ALU.mult, R(qrl, rq), R(t1))
            S.tt(t2[0:64, 0:n], PS[7][0:64, 0:n], rq[0:64, 1, 0:n], ALU.mult, R(PS[7], rq), R(t2))
            S.tt(QrT[0:64, q0:q0 + n], t1[0:64, 0:n], t2[0:64, 0:n], ALU.add, R(t1, t2), R(QrT), eng="pool")
            S.tt(sqb[:, 0:n], QnT[:, q0:q0 + n], QnT[:, q0:q0 + n], ALU.mult, R(QnT), R(sqb), eng="pool")
            S.tt(sqr[0:64, 0:n], QrT[0:64, q0:q0 + n], QrT[0:64, q0:q0 + n], ALU.mult, R(QrT), R(sqr), eng="pool")
            S.mm(PS[6][:, 0:n], onesbf[:], sqb[:, 0:n], True, False, R(sqb, onesbf), R(PS[6]))
            S.mm(PS[6][:, 0:n], onesbf[0:64, :], sqr[0:64, 0:n], False, True, R(sqr, onesbf), R(PS[6]))
            S.cp(tq[:, 0:n], PS[6][:, 0:n], R(PS[6]), R(tq))
            S.act(tq[:, 0:n], tq[:, 0:n], AF.Sqrt, R(tq, kmax), R(tq), scale=kmax[:, 2:3])
            S.ts(negm[:, q0:q0 + n], tq[:, 0:n], -1.0, None, ALU.mult, None, R(tq), R(negm))
        for (q0, n) in qblocks:
            po, pd = PS[4], PS[5]
            def scores(kt):
                par = kt % 3
                p = PS[par]
                k0 = kt * 128
                S.mm(p[:, 0:n], KnT[:, k0:k0 + 128], QnT[:, q0:q0 + n], True, False, R(KnT, QnT), R(p))
                S.mm(p[:, 0:n], KrT[:, k0:k0 + 128], QrT[:, q0:q0 + n], False, False, R(KrT, QrT), R(p))
                S.mm(p[:, 0:n], c128[:], negm[:, q0:q0 + n], False, True, R(c128, negm), R(p))
                S.act(pT[par][:, 0:n], p[:, 0:n], AF.Exp, R(p), R(pT[par]), scale=MLA_SCALE)

            scores(0)
            scores(1)
            for kt in range(NT1):
                if kt + 2 < NT1:
                    scores(kt + 2)
                par = kt % 3
                S.mm(po[:, 0:n], V[:, kt, :], pT[par][:, 0:n], kt == 0, kt == NT1 - 1, R(V, pT[par]), R(po))
                S.mm(pd[:, 0:n], onesbf[:], pT[par][:, 0:n], kt == 0, kt == NT1 - 1, R(onesbf, pT[par]), R(pd))
            S.recip(rden[:, 0:n], pd[:, 0:n], R(pd), R(rden))
            S.tt(oT[:, h, q0:q0 + n], po[:, 0:n], rden[:, 0:n], ALU.mult, R(po, rden), [oT.k(h)])
        if h == 0:
            chk(20)
    S.reset(markB)
    chk(3)

    wo = S.carve("wo", [8, 1024], BF16)
    S.dma(wo[:], wo_d.h.rearrange("(kc p) f -> p kc f", p=128), [], R(wo), eng="pool")
    rw = S.carve("rw", [8, 32], F32)
    S.dma(rw[:], rw_d[:], [], R(rw))
    rb = S.carve("rb", [32], F32)
    S.dma(rb[:], rb_d.h.partition_broadcast(128).rearrange("p o k -> p (o k)"), [], R(rb))
    xrw = S.carve("xrw", [4, 1024], F32)
    r = S.carve("r1", [8, 512], F32)
    x1t = S.carve("x1t", [8, 512], F32)
    Xf = S.carve("Xf", [8, 512], F32)
    tA = S.carve("tA", [512], F32)
    lt = ln_tmp(S, 512, R(x1t))
    rt = router_tmp(S)
    for (o0, n) in qblocks:
        if fused:
            S.dma(xrw[:, 0:n // 128, :], xown[128 + o0:128 + o0 + n, :].rearrange("(i p) f -> p i f", p=128), R(xown), R(xrw))
        else:
            S.dma(xrw[:, 0:n // 128, :], xs[o0:o0 + n, :].rearrange("(i p) f -> p i f", p=128), [], R(xrw))
        for dc in range(8):
            py, px = PS[dc % 2], PS[2 + dc % 2]
            for kc in range(8):
                S.mm(py[:, 0:n], wo[:, kc, dc * 128:(dc + 1) * 128], oT[:, kc, o0:o0 + n], kc == 0, kc == 7, R(wo, oT), R(py))
            for i in range(n // 128):
                S.tr(px[:, i * 128:(i + 1) * 128], xrw[:, i, dc * 128:(dc + 1) * 128], C.ident, R(xrw, C.cs), R(px))
            S.act(tA[:, 0:n], py[:, 0:n], AF.Identity, R(py, C.mod), R(tA), scale=C.mod[:, G1 + dc, 0:1])
            S.stt(r[:, dc, 0:n], px[:, 0:n], ALPHA, tA[:, 0:n], ALU.mult, ALU.add, R(px, tA), R(r))
        layer_norm_fm(S, C, r, n, 0, lambda c: x1t[:, c, 0:n], lt)
        S.dma(x1d[:, :, o0:o0 + n], x1t[:, :, 0:n], R(x1t), R(x1d))
        for dc in range(8):
            S.act(Xf[:, dc, 0:n], x1t[:, dc, 0:n], AF.Identity, R(x1t, C.mod), R(Xf),
                  scale=C.mod[:, SC2 + dc, 0:1], bias=C.mod[:, SH2 + dc, 0:1])
        S.cp(XT[:, :, o0:o0 + n], Xf[:, :, 0:n], R(Xf), R(XT), eng="pool")
        router(S, C, Xf, n, o0, rw, rb, gateT, rt)
    S.reset(markA)
    chk(4)

    mw_in = D("mw_in", [32, 1024, 2048])
    mb_inT = D("mb_inT", [128, 32, 16])
    mw_out = D("mw_out", [32, 1024, 1024])
    mb_out = D("mb_out", [32, 1024])
    b_inT = S.carve("b_inT", [32, 16], F32)
    S.dma(b_inT[:], mb_inT[:], [], R(b_inT))
    S.ts(b_inT[:, :, 8:16], b_inT[:, :, 8:16], 1.0, None, ALU.add, None, R(b_inT), R(b_inT))
    p4 = S.mark()
    tblocks = [([(0, 512), (512, 512)], [0, 0]), ([(1024, 512), (1536, 512)], [0, 0])]
    for chunks, cols in tblocks:
        acc = S.carve("acc", [8, sum(n for _, n in chunks)], F32, nreg=8)
        m = S.mark()
        offs = moe_block(S, C, XT, gateT, chunks, mw_in, mw_out, b_inT, mb_out, acc)
        S.reset(m)
        ln2_out(S, C, acc, offs, chunks, cols, x1d, xout, None, None)
        S.reset(p4)
    S.finish(R(xout))


def build_fused():
    nc = bass.Bass("TRN2", target_bir_lowering=False)
    es = ExitStack()
    with es:
        S = Sched(nc, es)
        nochk = lambda n: None
        S.prefix = "a_"
        xown = _build(S, nc, nochk, fused=True)
        S.prefix = ""
        xall = S.dram("xall", [2 * 2176, 1024], F32, "Internal")
        for k in range(17):
            S.op("pool", lambda e, k=k: e.collective_compute("AllGather", ALU.bypass,
                                                             replica_groups=[[0, 1], [2, 3], [4, 5], [6, 7]],
                                                             ins=[xown[k * 128:(k + 1) * 128, :]],
                                                             outs=[xall[k * 256:(k + 1) * 256, :]]),
                 R(xown), R(xall), dma=True, inc=1)
        S.reset(0)
        S.prefix = "b_"
        _build1(S, nc, nochk, fused=True, xown=xown, xall=xall)
        S.emit()
    return nc


def consts():
    c = np.zeros((128, 1024), np.float32)
    c[:, 0:128] = np.eye(128)
    c[:, 128:256] = 1.0
    k = np.arange(128)[:, None]; l = np.arange(128)[None, :]
    c[:, 256:384] = (k <= l)
    c[:, 384:512] = (k >= l)
    c[:, 512:640] = np.where(k > l, -30000.0, 0.0)
    c[:, 640:768] = np.where(k < l, -30000.0, 0.0)
    c[0:32, 768:800] = np.eye(32)
    return c

def fm(v, nchunk):
    return np.ascontiguousarray(v.reshape(nchunk, 128).T)

def prep_l0(inp, b, half):
    f = np.float32
    flip = half == 1
    ctx_ = inp['ctx'][b][::-1] if flip else inp['ctx'][b]
    lat_ = inp['x'][b][::-1] if flip else inp['x'][b]
    m = {}
    m['xs'] = np.ascontiguousarray(np.concatenate([ctx_, lat_], 0), dtype=f)
    cT = np.zeros((128, 8, 2), f)
    cT[:, :, 0] = fm(inp['c'][b], 8); cT[:, :, 1] = fm(inp['c_ctx'], 8)
    m['cT'] = cT
    m['mod_w'] = np.ascontiguousarray(inp['mod_w'][0])
    m['mod_bT'] = fm(inp['mod_b'][0], 48)
    m['lnp'] = np.ascontiguousarray(np.stack([fm(inp['ln1_g'][0], 8), fm(inp['ln1_b'][0], 8), fm(inp['ln2_g'][0], 8), fm(inp['ln2_b'][0], 8)], 1))
    W = inp['ssd_w_in'][0]
    dA = 1 if flip else 0; dB = 1 - dA
    win = np.zeros((8, 1024, 776), f)
    convp = np.zeros((128, 8, 4, 6), f)
    ssdp = np.zeros((1, 160), f)
    cw = inp['ssd_conv_w'][0]; cb = inp['ssd_conv_b'][0]
    if flip: cw = cw[::-1]
    for g in range(8):
        win[g] = np.concatenate([W[:, 256*g:256*g+256], W[:, 2048+256*g:2048+256*g+256], W[:, 4096+128*g:4096+128*g+128],
                                 W[:, 5120+128*g:5120+128*g+128], W[:, 6144+dA*32+4*g:6144+dA*32+4*g+4], W[:, 6144+dB*32+4*g:6144+dB*32+4*g+4]], 1)
        for ch, c0 in enumerate([256*g, 256*g+128, 2048+128*g, 3072+128*g]):
            convp[:, g, ch, 0:5] = cw[:, c0:c0+128].T
            convp[:, g, ch, 5] = cb[c0:c0+128]
        ssdp[0, g*20:g*20+20] = np.concatenate([inp['ssd_dt_bias'][0][dA, 4*g:4*g+4], inp['ssd_dt_bias'][0][dB, 4*g:4*g+4],
                                               inp['ssd_a_log'][0][dA, 4*g:4*g+4], inp['ssd_a_log'][0][dB, 4*g:4*g+4], inp['ssd_d'][0][4*g:4*g+4]])
    m['win'] = win; m['convp'] = convp; m['ssdp'] = ssdp
    m['normw'] = fm(inp['ssd_norm_w'][0], 16)
    m['wout'] = np.ascontiguousarray(inp['ssd_w_out'][0])
    add_moe(m, inp, 0)
    m['cst'] = consts()
    return m

def add_moe(m, inp, i):
    f = np.float32
    m['rw'] = np.ascontiguousarray(inp['router_w'][i].reshape(8, 128, 32).transpose(1, 0, 2))
    m['rb'] = np.ascontiguousarray(inp['router_b'][i].reshape(1, 32))
    wi = inp['moe_w_in'][i]
    m['mw_in'] = np.ascontiguousarray(np.concatenate([wi[:, :, 0:512], wi[:, :, 1024:1536], wi[:, :, 512:1024], wi[:, :, 1536:2048]], 2))
    m['mb_inT'] = np.ascontiguousarray(inp['moe_b_in'][i].reshape(32, 16, 128).transpose(2, 0, 1))
    m['mw_out'] = np.ascontiguousarray(inp['moe_w_out'][i])
    m['mb_out'] = np.ascontiguousarray(inp['moe_b_out'][i])

def gather_l0(results, B=4):
    x1 = np.zeros((B, 4096, 1024), np.float32); ctx1 = np.zeros((B, 256, 1024), np.float32)
    for cid, r in enumerate(results):
        b, half = cid // 2, cid % 2
        o = r['xout']
        if half == 0:
            ctx1[b, 0:128] = o[0:128]; x1[b, 0:2048] = o[128:]
        else:
            ctx1[b, 128:256] = o[0:128][::-1]; x1[b, 2048:4096] = o[128:][::-1]
    return x1, ctx1


def rope_consts():
    f = np.float32
    t = np.arange(4096)
    row = (t // 64).astype(f); col = (t % 64).astype(f)
    inv = (f(10000.0) ** (-np.arange(16, dtype=f) / f(16))).astype(f)
    ang = np.stack([row[:, None] * inv, col[:, None] * inv], axis=1).astype(f)
    cos, sin = np.cos(ang).astype(f), np.sin(ang).astype(f)
    cosT = np.zeros((64, 4096), f); sinT = np.zeros((64, 4096), f)
    for a in range(2):
        for hf in range(2):
            cosT[a * 32 + hf * 16:a * 32 + hf * 16 + 16] = cos[:, a, :].T
            sinT[a * 32 + hf * 16:a * 32 + hf * 16 + 16] = sin[:, a, :].T
    rm = np.zeros((64, 64), f)
    for a in range(2):
        for k in range(16):
            i1 = a * 32 + k; i2 = a * 32 + 16 + k
            rm[i2, i1] = -1.0
            rm[i1, i2] = 1.0
    return cosT, sinT, rm

def prep_l1(inp, x1, ctx1, b, half, rc=None):
    f = np.float32
    cosT, sinT, rm = rc if rc is not None else rope_consts()
    own = slice(half * 2048, (half + 1) * 2048)
    oth = slice((1 - half) * 2048, (2 - half) * 2048)
    m = {}
    m['xs'] = np.ascontiguousarray(np.concatenate([x1[b, own], x1[b, oth], ctx1[b]], 0), dtype=f)
    cT = np.zeros((128, 8, 2), f)
    cT[:, :, 0] = fm(inp['c'][b], 8); cT[:, :, 1] = fm(inp['c_ctx'], 8)
    m['cT'] = cT
    m['mod_w'] = np.ascontiguousarray(inp['mod_w'][1])
    m['mod_bT'] = fm(inp['mod_b'][1], 48)
    m['lnp'] = np.ascontiguousarray(np.stack([fm(inp['ln1_g'][1], 8), fm(inp['ln1_b'][1], 8), fm(inp['ln2_g'][1], 8), fm(inp['ln2_b'][1], 8)], 1))
    m['wa'] = np.ascontiguousarray(inp['mla_w_a'][0])
    m['qnorm'] = fm(inp['mla_q_norm'][0], 3)
    m['kvnorm'] = fm(inp['mla_kv_norm'][0], 2)
    wq = inp['mla_w_qb'][0]
    m['wqb'] = np.ascontiguousarray(np.concatenate([wq[:, h * 192:h * 192 + 128] for h in range(8)] +
                                                  [wq[:, h * 192 + 128:h * 192 + 192] for h in range(8)], 1))
    wk = inp['mla_w_kvb'][0]
    m['wkvb'] = np.ascontiguousarray(np.concatenate([wk[:, h * 256:h * 256 + 128] for h in range(8)] +
                                                   [wk[:, h * 256 + 128:h * 256 + 256] for h in range(8)], 1))
    m['wo'] = np.ascontiguousarray(inp['mla_w_o'][0])
    m['ropeq'] = np.ascontiguousarray(np.stack([cosT[:, own], sinT[:, own]], 1))
    ck = np.concatenate([cosT[:, own], cosT[:, oth], np.ones((64, 256), f)], 1)
    sk = np.concatenate([sinT[:, own], sinT[:, oth], np.zeros((64, 256), f)], 1)
    m['ropek'] = np.ascontiguousarray(np.stack([ck, sk], 1))
    m['rm'] = rm
    add_moe(m, inp, 1)
    m['cst'] = consts()
    return m

def gather_l1(results, B=4):
    out = np.zeros((B, 4096, 1024), np.float32)
    for cid, r in enumerate(results):
        b, half = cid // 2, cid % 2
        out[b, half * 2048:(half + 1) * 2048] = r['xout']
    return out


def prep_fused(inp, b, half, rc):
    f = np.float32
    cosT, sinT, rm = rc
    m0 = prep_l0(inp, b, half)
    m = {("cst" if k == "cst" else "a_" + k): v for k, v in m0.items()}
    idx1 = 4095 - np.arange(2048)
    ck = np.ones((64, 4352), f)
    sk = np.zeros((64, 4352), f)
    for t in range(2, 34):
        k, r = t // 2, t % 2
        base = (k - 1) * 128 + np.arange(128)
        lat = base if r == 0 else 4095 - base
        ck[:, t * 128:(t + 1) * 128] = cosT[:, lat]
        sk[:, t * 128:(t + 1) * 128] = sinT[:, lat]
    qi = np.arange(2048) if half == 0 else idx1
    m1 = {}
    cT = np.zeros((128, 8, 2), f)
    cT[:, :, 0] = fm(inp['c'][b], 8); cT[:, :, 1] = fm(inp['c_ctx'], 8)
    m1['cT'] = cT
    m1['mod_w'] = np.ascontiguousarray(inp['mod_w'][1])
    m1['mod_bT'] = fm(inp['mod_b'][1], 48)
    m1['lnp'] = np.ascontiguousarray(np.stack([fm(inp['ln1_g'][1], 8), fm(inp['ln1_b'][1], 8), fm(inp['ln2_g'][1], 8), fm(inp['ln2_b'][1], 8)], 1))
    m1['wa'] = np.ascontiguousarray(inp['mla_w_a'][0])
    m1['qnorm'] = fm(inp['mla_q_norm'][0], 3)
    m1['kvnorm'] = fm(inp['mla_kv_norm'][0], 2)
    wq = inp['mla_w_qb'][0]
    m1['wqb'] = np.ascontiguousarray(np.concatenate([wq[:, h * 192:h * 192 + 128] for h in range(8)] +
                                                   [wq[:, h * 192 + 128:h * 192 + 192] for h in range(8)], 1))
    wk = inp['mla_w_kvb'][0]
    m1['wkvb'] = np.ascontiguousarray(np.concatenate([wk[:, h * 256:h * 256 + 128] for h in range(8)] +
                                                    [wk[:, h * 256 + 128:h * 256 + 256] for h in range(8)], 1))
    m1['wo'] = np.ascontiguousarray(inp['mla_w_o'][0])
    m1['ropeq'] = np.ascontiguousarray(np.stack([cosT[:, qi], sinT[:, qi]], 1))
    m1['ropek'] = np.ascontiguousarray(np.stack([ck, sk], 1))
    m1['rm'] = rm
    add_moe(m1, inp, 1)
    for k, v in m1.items():
        m["b_" + k] = v
    return m

def gather_fused(results, B=4):
    out = np.zeros((B, 4096, 1024), np.float32)
    for cid, r in enumerate(results):
        b, half = cid // 2, cid % 2
        o = r['b_xout']
        if half == 0:
            out[b, 0:2048] = o
        else:
            out[b, 2048:4096] = o[::-1]
    return out


def kernel(**inputs):
    inp = {k: np.asarray(v) for k, v in inputs.items()}
    nc = build_fused()
    rc = rope_consts()
    maps = []
    for cid in range(8):
        m = prep_fused(inp, cid // 2, cid % 2, rc)
        maps.append({k: m[k] for k in nc._in_names})
    res = run_bass_kernel_spmd(nc, maps, core_ids=list(range(8)))
    return gather_fused(res.results)
```

```python
import numpy as np
import concourse.bass as bass
import concourse.mybir as mybir
from concourse.bass_utils import run_bass_kernel_spmd
from contextlib import ExitStack

F32 = mybir.dt.float32
BF16 = mybir.dt.bfloat16
U32 = mybir.dt.uint32
I32 = mybir.dt.int32
AF = mybir.ActivationFunctionType
ALU = mybir.AluOpType
AX = mybir.AxisListType


class Tile:
    def __init__(self, name, h, nreg=1):
        self.name = name
        self.h = h
        self.nreg = nreg

    def k(self, i=0):
        return (self.name, i)

    def all(self):
        return [(self.name, i) for i in range(self.nreg)]

    def __getitem__(self, idx):
        return self.h[idx]


class Sched:
    ENGS = ("pe", "act", "dve", "pool", "sp")

    def __init__(self, nc, es, n_dma_sems=8, sync_same=True):
        self.nc = nc
        self.es = es
        self.sync_same = sync_same
        self.q = {e: [] for e in self.ENGS}
        self.csem = {e: es.enter_context(nc.semaphore("c_" + e)) for e in self.ENGS}
        self.ccount = {e: 0 for e in self.ENGS}
        self.dq = ("sp", "pool", "act")
        self.dsems = {e: [es.enter_context(nc.semaphore("d_%s_%d" % (e, i))) for i in range(n_dma_sems)]
                      for e in self.dq}
        self.duse = {e: [0] * n_dma_sems for e in self.dq}
        self.dnext = {e: 0 for e in self.dq}
        self.lastw = {}
        self.readers = {}
        self.known = {e: {} for e in self.ENGS}
        self.ntile = 0

    def sb(self, name, shape, dtype, nreg=1):
        h = self.es.enter_context(self.nc.sbuf_tensor(name, list(shape), dtype))
        return Tile(name, h, nreg)

    def ps(self, name, shape, dtype, nreg=1):
        h = self.es.enter_context(self.nc.psum_tensor(name, list(shape), dtype))
        return Tile(name, h, nreg)

    def dram(self, name, shape, dtype, kind, nreg=1):
        name = getattr(self, "prefix", "") + name
        if not hasattr(self.nc, "_in_names"):
            self.nc._in_names = []
        if kind == "ExternalInput":
            self.nc._in_names.append(name)
        h = self.nc.dram_tensor(name, list(shape), dtype, kind=kind)
        return Tile(name, h.ap(), nreg)

    def arena(self, nfloats):
        self.ar = self.sb("arena", [128, nfloats], F32)
        self.ar_n = nfloats
        self.ar_off = 0
        self.ar_base = 0

    def carve(self, name, shape, dtype, nreg=1, parts=128):
        n = 1
        for s in shape:
            n *= s
        nf = n if dtype == F32 else (n + 1) // 2
        nf = (nf + 7) // 8 * 8
        assert self.ar_off + nf <= self.ar_n, ("arena overflow", name, self.ar_off, nf, self.ar_n)
        ap = self.ar.h[0:parts, self.ar_off:self.ar_off + nf]
        if dtype != F32:
            ap = ap.bitcast(dtype)
        ap = ap[:, 0:n]
        if len(shape) == 2:
            ap = ap.rearrange("p (a b) -> p a b", b=shape[1])
        elif len(shape) == 3:
            ap = ap.rearrange("p (a b c) -> p a b c", b=shape[1], c=shape[2])
        self.ar_off += nf
        self.ntile += 1
        return Tile("%s#%d" % (name, self.ntile), ap, nreg)

    def mark(self):
        return self.ar_off

    def reset(self, mark):
        self.barrier()
        self.ar_off = mark

    def barrier(self):
        allt = {}
        for e in self.ENGS:
            if self.ccount[e] > 0:
                allt[("c", e)] = self.ccount[e]
        for e in self.dq:
            for i, u in enumerate(self.duse[e]):
                if u > 0:
                    allt[("d", e, i)] = u
        self.pending = {e: dict(allt) for e in self.ENGS}

    def op(self, eng, fn, reads=(), writes=(), dma=False, same=None, inc=16):
        deps = {}
        pend = getattr(self, "pending", None)
        if pend and pend.get(eng):
            for sk, v in pend[eng].items():
                if deps.get(sk, 0) < v:
                    deps[sk] = v
            pend[eng] = None

        def add(tok):
            sk, v = tok
            if deps.get(sk, 0) < v:
                deps[sk] = v

        for k in reads:
            if k in self.lastw:
                add(self.lastw[k])
        for k in writes:
            if k in self.lastw:
                add(self.lastw[k])
            for sk, v in self.readers.get(k, {}).items():
                add((sk, v))
        if dma:
            i = self.dnext[eng]
            self.dnext[eng] = (i + 1) % len(self.dsems[eng])
            prev = self.duse[eng][i]
            self.duse[eng][i] = prev + inc
            tok = (("d", eng, i), prev + inc)
            if prev > 0:
                add((("d", eng, i), prev))
        else:
            self.ccount[eng] += 1
            tok = (("c", eng), self.ccount[eng])
        same = self.sync_same if same is None else same
        waits = []
        for sk, v in deps.items():
            if sk == ("c", eng) and not same:
                continue
            if self.known[eng].get(sk, 0) >= v:
                continue
            self.known[eng][sk] = v
            waits.append((sk, v))
        self.q[eng].append((waits, fn, tok, inc if dma else 1))
        for k in reads:
            r = self.readers.setdefault(k, {})
            if r.get(tok[0], 0) < tok[1]:
                r[tok[0]] = tok[1]
        for k in writes:
            self.lastw[k] = tok
            self.readers[k] = {}
        return tok

    def dma(self, out, in_, reads, writes, eng="sp", **kw):
        return self.op(eng, lambda e: e.dma_start(out=out, in_=in_, **kw), reads, writes, dma=True)

    def mm(self, out, lhsT, rhs, start, stop, reads, writes):
        return self.op("pe", lambda e: e.matmul(out, lhsT, rhs, start=start, stop=stop), reads, writes, same=False)

    def tr(self, out, in_, ident, reads, writes):
        return self.op("pe", lambda e: e.transpose(out, in_, ident), reads, writes, same=False)

    def act(self, out, in_, func, reads, writes, eng="act", **kw):
        return self.op(eng, lambda e: e.activation(out=out, in_=in_, func=func, **kw), reads, writes)

    def tt(self, out, in0, in1, op, reads, writes, eng="dve"):
        return self.op(eng, lambda e: e.tensor_tensor(out=out, in0=in0, in1=in1, op=op), reads, writes)

    def ts(self, out, in0, s1, s2, op0, op1, reads, writes, eng="dve", **kw):
        if op1 is None:
            return self.op(eng, lambda e: e.tensor_scalar(out=out, in0=in0, scalar1=s1, scalar2=None, op0=op0, **kw),
                           reads, writes)
        return self.op(eng, lambda e: e.tensor_scalar(out=out, in0=in0, scalar1=s1, scalar2=s2, op0=op0, op1=op1, **kw),
                       reads, writes)

    def cp(self, out, in_, reads, writes, eng="dve"):
        if eng == "act":
            return self.op(eng, lambda e: e.copy(out=out, in_=in_), reads, writes)
        return self.op(eng, lambda e: e.tensor_copy(out=out, in_=in_), reads, writes)

    def stt(self, out, in0, scalar, in1, op0, op1, reads, writes):
        return self.op("dve", lambda e: e.scalar_tensor_tensor(out=out, in0=in0, scalar=scalar, in1=in1, op0=op0, op1=op1),
                       reads, writes)

    def recip(self, out, in_, reads, writes):
        return self.op("dve", lambda e: e.reciprocal(out=out, in_=in_), reads, writes)

    def max8(self, out, in_, reads, writes):
        return self.op("dve", lambda e: e.max(out=out, in_=in_), reads, writes)

    def reduce(self, out, in_, axis, op, reads, writes):
        return self.op("dve", lambda e: e.tensor_reduce(out=out, in_=in_, axis=axis, op=op), reads, writes)

    def memset(self, out, val, writes, eng="dve"):
        return self.op(eng, lambda e: e.memset(out, val), (), writes)

    def _semh(self, sk):
        if sk[0] == "c":
            return self.csem[sk[1]]
        return self.dsems[sk[1]][sk[2]]

    def finish(self, out_keys):
        waits = []
        for k in out_keys:
            if k in self.lastw:
                waits.append(self.lastw[k])
        self.final_waits = waits

    def emit(self):
        nc = self.nc
        counts = {e: len(self.q[e]) for e in self.ENGS}
        nw = {e: sum(len(w[0]) for w in self.q[e]) for e in self.ENGS}
        print("SCHED instr counts", counts, "waits", nw, flush=True)
        with nc.Block() as block:
            def mk(engname):
                def body(e):
                    for waits, fn, tok, inc_ in self.q[engname]:
                        for sk, v in waits:
                            e.wait_ge(self._semh(sk), v)
                        ins = fn(e)
                        ins.then_inc(self._semh(tok[0]), inc_)
                    if engname == "sp":
                        done = {}
                        for sk, v in getattr(self, "final_waits", []):
                            if done.get(sk, 0) < v:
                                done[sk] = v
                        for sk, v in done.items():
                            e.wait_ge(self._semh(sk), v)
                return body

            block.tensor(mk("pe"))
            block.scalar(mk("act"))
            block.vector(mk("dve"))
            block.gpsimd(mk("pool"))
            block.sync(mk("sp"))


ALPHA = 4.0 ** 0.25
LN_EPS = 1e-5
SH1, SC1, G1, SH2, SC2, G2 = 0, 8, 16, 24, 32, 40


def R(*tiles):
    out = []
    for t in tiles:
        out += t.all()
    return out


class Ctx:
    pass


XCHUNKS = [(0, 512), (512, 512), (1024, 128), (1152, 512), (1664, 512)]


def xall_tile_map():
    out = []
    for r0, n in XCHUNKS:
        for r in range(2):
            for i in range(n // 128):
                out.append((r, r0 // 128 + i))
    return out


def setup_common(S, C, layer_has_ctx):
    if hasattr(S, "_common"):
        C.__dict__.update(S._common.__dict__)
        return
    S._common = C
    C.PS = [S.ps("ps%d" % i, [128, 512], F32) for i in range(8)]
    pre, S.prefix = getattr(S, "prefix", ""), ""
    C.cst = S.dram("cst", [128, 1024], F32, "ExternalInput")
    S.prefix = pre
    C.cs = S.sb("cs", [128, 1024], F32)
    S.dma(C.cs[:], C.cst[:], [], R(C.cs))
    C.ident = C.cs[:, 0:128]
    C.ones = C.cs[:, 128:256]
    C.U = [C.cs[:, 256:384], C.cs[:, 384:512]]
    C.NEG1 = [C.cs[:, 512:640], C.cs[:, 640:768]]
    C.eye32 = C.cs[0:32, 768:800]
    C.identbf = S.sb("identbf", [128, 128], BF16)
    S.cp(C.identbf[:], C.ident, R(C.cs), R(C.identbf))


def modulation(S, C, mod_w, mod_bT, cT):
    PS = C.PS
    m0 = S.mark()
    cs = S.carve("cTs", [8, 2], F32)
    S.dma(cs[:], cT[:], [], R(cs))
    csl = S.carve("csl", [8, 2], F32)
    S.act(csl[:], cs[:], AF.Silu, R(cs), R(csl))
    wb = [S.carve("modw%d" % i, [8, 512], F32) for i in range(2)]
    mb = S.carve("modb", [48], F32)
    S.dma(mb[:], mod_bT[:], [], R(mb))
    mwv = mod_w.h.rearrange("(kc p) f -> p kc f", p=128)
    for fg in range(12):
        w = wb[fg % 2]
        S.dma(w[:], mwv[:, :, fg * 512:(fg + 1) * 512], [], R(w))
        for fl in range(4):
            fc = fg * 4 + fl
            for kc in range(8):
                S.mm(PS[7][:, fc * 2:fc * 2 + 2], w[:, kc, fl * 128:(fl + 1) * 128], csl[:, kc, :], kc == 0, kc == 7,
                     R(w, csl), R(PS[7]))
    S.tt(C.mod[:], PS[7][:, 0:96].rearrange("p (f j) -> p f j", j=2), mb[:].unsqueeze(2).to_broadcast([128, 48, 2]),
         ALU.add, R(PS[7], mb), R(C.mod))
    for base in (SC1, SC2):
        S.ts(C.mod[:, base:base + 8, :], C.mod[:, base:base + 8, :], 1.0, None, ALU.add, None, R(C.mod), R(C.mod))
    S.reset(m0)


def layer_norm_fm(S, C, r, n, gi, out, tmp):
    PS = C.PS
    sq, mean, msq, var, rs = tmp["sq"], tmp["mean"], tmp["msq"], tmp["var"], tmp["rs"]
    for c in range(8):
        S.mm(PS[6][:, 0:n], C.ones, r[:, c, 0:n], c == 0, c == 7, R(r, C.cs), R(PS[6]))
    S.tt(sq[:, :, 0:n], r[:, :, 0:n], r[:, :, 0:n], ALU.mult, R(r), R(sq), eng="pool")
    for c in range(8):
        S.mm(PS[7][:, 0:n], C.ones, sq[:, c, 0:n], c == 0, c == 7, R(sq, C.cs), R(PS[7]))
    S.act(mean[:, 0:n], PS[6][:, 0:n], AF.Identity, R(PS[6]), R(mean), scale=1.0 / 1024)
    S.tt(msq[:, 0:n], mean[:, 0:n], mean[:, 0:n], ALU.mult, R(mean), R(msq), eng="pool")
    S.stt(var[:, 0:n], PS[7][:, 0:n], 1.0 / 1024, msq[:, 0:n], ALU.mult, ALU.subtract, R(PS[7], msq), R(var))
    S.act(var[:, 0:n], var[:, 0:n], AF.Sqrt, R(var), R(var), bias=LN_EPS)
    S.recip(rs[:, 0:n], var[:, 0:n], R(var), R(rs))
    for c in range(8):
        S.tt(r[:, c, 0:n], r[:, c, 0:n], mean[:, 0:n], ALU.subtract, R(r, mean), R(r))
        S.tt(r[:, c, 0:n], r[:, c, 0:n], rs[:, 0:n], ALU.mult, R(r, rs), R(r), eng="pool")
        S.act(out(c), r[:, c, 0:n], AF.Identity, R(r, C.lnp), tmp["outkeys"],
              scale=C.lnp[:, gi, c:c + 1], bias=C.lnp[:, gi + 1, c:c + 1])


def ln_tmp(S, nmax, outkeys):
    return {"sq": S.carve("lnsq", [8, nmax], F32), "mean": S.carve("lnmean", [nmax], F32),
            "msq": S.carve("lnmsq", [nmax], F32), "var": S.carve("lnvar", [nmax], F32),
            "rs": S.carve("lnrs", [nmax], F32), "outkeys": outkeys}


def router(S, C, Xf, n, o0, rw, rb, gateT, rt):
    PS = C.PS
    for i in range(n // 128):
        for kc in range(8):
            S.mm(PS[5][:, 0:32], Xf[:, kc, i * 128:(i + 1) * 128], rw[:, kc, :], kc == 0, kc == 7, R(Xf, rw), R(PS[5]))
        lg, m8, msk, ex, sm = rt["lg"], rt["m8"], rt["msk"], rt["ex"], rt["sm"]
        S.tt(lg[:], PS[5][:, 0:32], rb[:], ALU.add, R(PS[5], rb), R(lg))
        S.max8(m8[:], lg[:], R(lg), R(m8))
        S.ts(msk[:], lg[:], m8[:, 3:4], None, ALU.is_ge, None, R(lg, m8), R(msk))
        S.ts(sm[:, 0:1], m8[:, 0:1], -1.0, None, ALU.mult, None, R(m8), R(sm))
        S.act(ex[:], lg[:], AF.Exp, R(lg, sm), R(ex), bias=sm[:, 0:1])
        S.tt(ex[:], ex[:], msk[:], ALU.mult, R(ex, msk), R(ex))
        S.reduce(sm[:, 1:2], ex[:], AX.X, ALU.add, R(ex), R(sm))
        S.recip(sm[:, 2:3], sm[:, 1:2], R(sm), R(sm))
        S.ts(ex[:], ex[:], sm[:, 2:3], None, ALU.mult, None, R(ex, sm), R(ex))
        S.tr(PS[5][0:32, 128:256], ex[:], C.ident, R(ex, C.cs), R(PS[5]))
        S.cp(gateT[0:32, o0 + i * 128:o0 + (i + 1) * 128], PS[5][0:32, 128:256], R(PS[5]), R(gateT), eng="act")


def router_tmp(S):
    return {"lg": S.carve("rlg", [32], F32), "m8": S.carve("rm8", [8], F32), "msk": S.carve("rmsk", [32], F32),
            "ex": S.carve("rex", [32], F32), "sm": S.carve("rsm", [4], F32)}


def moe_block(S, C, XT, gateT, chunks, w_in, w_out, b_inT, b_out, acc):
    PS = C.PS
    tot = sum(n for _, n in chunks)
    offs = []
    o = 0
    for lo, n in chunks:
        offs.append(o)
        o += n
    wi = S.carve("wi", [8, 2048], BF16, nreg=2)
    wo = S.carve("wo", [8, 1024], BF16)
    actT = S.carve("actT", [8, tot], BF16, nreg=8)
    Gbs = [S.carve("Gb%d" % i, [tot], F32) for i in range(2)]
    ge = S.carve("ge", [tot], F32, parts=32)
    gT_hi = S.carve("gT_hi", [tot], BF16, parts=32)
    gT_lo = S.carve("gT_lo", [tot], BF16, parts=32)
    eyebf = S.carve("eyebf", [32], BF16, parts=32)
    S.cp(eyebf[:], C.eye32, R(C.cs), R(eyebf))
    for ci, (lo, n) in enumerate(chunks):
        sl = slice(offs[ci], offs[ci] + n)
        S.cp(gT_hi[0:32, sl], gateT[0:32, lo:lo + n], R(gateT), R(gT_hi))
        S.tt(ge[0:32, sl], gateT[0:32, lo:lo + n], gT_hi[0:32, sl], ALU.subtract, R(gateT, gT_hi), R(ge))
        S.cp(gT_lo[0:32, sl], ge[0:32, sl], R(ge), R(gT_lo))
    bo = S.carve("bo", [1024], F32, parts=32)
    S.dma(bo[:], b_out[:], [], R(bo))
    tg = [S.carve("tg%d" % i, [512], F32) for i in range(2)]
    tsg = [S.carve("tsg%d" % i, [512], F32) for i in range(2)]
    tl = [S.carve("tl%d" % i, [512], F32) for i in range(2)]
    it = 0
    for dc in range(8):
        for ci, (lo, n) in enumerate(chunks):
            p = PS[4 + it % 2]
            it += 1
            S.mm(p[:, 0:n], bo[:, dc * 128:(dc + 1) * 128], gateT[0:32, lo:lo + n], True, True, R(bo, gateT), R(p))
            S.cp(acc[:, dc, offs[ci]:offs[ci] + n], p[:, 0:n], R(p), [acc.k(dc)], eng="act")
    wiv = w_in.h.rearrange("e (kc p) f -> e p kc f", p=128)
    wov = w_out.h.rearrange("e (kc p) f -> e p kc f", p=128)

    def load_wi(e, h):
        S.dma(wi[:, :, h * 1024:(h + 1) * 1024], wiv[e, :, :, h * 1024:(h + 1) * 1024], [], [wi.k(h)], eng="pool")

    def prep_gate(e):
        Gb_ = Gbs[e % 2]
        sel = eyebf[0:32, e:e + 1].to_broadcast([32, 128])
        for ci, (lo, n) in enumerate(chunks):
            sl = slice(offs[ci], offs[ci] + n)
            pg = PS[6 + ci % 2]
            S.mm(pg[:, 0:n], sel, gT_hi[0:32, sl], True, False, R(gT_hi, eyebf), R(pg))
            S.mm(pg[:, 0:n], sel, gT_lo[0:32, sl], False, True, R(gT_lo, eyebf), R(pg))
            S.cp(Gb_[:, sl], pg[:, 0:n], R(pg), R(Gb_), eng="act")

    load_wi(0, 0)
    load_wi(0, 1)
    prep_gate(0)
    it = 0
    for e in range(32):
        Gb = Gbs[e % 2]
        S.dma(wo[:], wov[e], [], R(wo), eng="pool")
        for j in range(8):
            hh, jj = j // 4, j % 4
            cg = hh * 1024 + jj * 128
            cl = hh * 1024 + 512 + jj * 128
            for ci, (lo, n) in enumerate(chunks):
                pa, pb = (PS[0], PS[1]) if it % 2 == 0 else (PS[2], PS[3])
                q = it % 2
                it += 1
                for kc in range(8):
                    S.mm(pa[:, 0:n], wi[:, kc, cg:cg + 128], XT[:, kc, lo:lo + n], kc == 0, kc == 7,
                         [wi.k(hh)] + R(XT), R(pa))
                for kc in range(8):
                    S.mm(pb[:, 0:n], wi[:, kc, cl:cl + 128], XT[:, kc, lo:lo + n], kc == 0, kc == 7,
                         [wi.k(hh)] + R(XT), R(pb))
                g, sg, l = tg[q], tsg[q], tl[q]
                S.ts(g[:, 0:n], pa[:, 0:n], b_inT[:, e, j:j + 1], 7.0, ALU.add, ALU.min, R(pa, b_inT), R(g))
                S.act(sg[:, 0:n], g[:, 0:n], AF.Sigmoid, R(g), R(sg), scale=1.702)
                S.ts(l[:, 0:n], pb[:, 0:n], b_inT[:, e, 8 + j:9 + j], 8.0, ALU.add, ALU.min, R(pb, b_inT), R(l))
                S.stt(l[:, 0:n], l[:, 0:n], -6.0, Gb[:, offs[ci]:offs[ci] + n], ALU.max, ALU.mult, R(l, Gb), R(l))
                S.tt(g[:, 0:n], g[:, 0:n], sg[:, 0:n], ALU.mult, R(g, sg), R(g), eng="pool")
                S.tt(actT[:, j, offs[ci]:offs[ci] + n], g[:, 0:n], l[:, 0:n], ALU.mult, R(g, l), [actT.k(j)])
            if e + 1 < 32 and j in (3, 7):
                load_wi(e + 1, j // 4)
            if e + 1 < 32 and j == 5:
                prep_gate(e + 1)
        for dc in range(8):
            for ci, (lo, n) in enumerate(chunks):
                p = PS[4 + it % 2]
                it += 1
                for j in range(8):
                    S.mm(p[:, 0:n], wo[:, j, dc * 128:(dc + 1) * 128], actT[:, j, offs[ci]:offs[ci] + n], j == 0, j == 7,
                         R(wo) + [actT.k(j)], R(p))
                S.tt(acc[:, dc, offs[ci]:offs[ci] + n], acc[:, dc, offs[ci]:offs[ci] + n], p[:, 0:n], ALU.add,
                     R(p) + [acc.k(dc)], [acc.k(dc)])
    return offs


def ln2_out(S, C, acc, offs, chunks, cols, x1d, xout, tmp, mk):
    PS = C.PS
    W = 256
    sets = []
    for i in range(2):
        x2 = S.carve("x2_%d" % i, [8, W], F32)
        sets.append({"x1b": S.carve("x1b%d" % i, [8, W], F32), "r": S.carve("r2_%d" % i, [8, W], F32), "x2": x2,
                     "tA": S.carve("tA2_%d" % i, [W], F32), "lt": ln_tmp(S, W, R(x2))})
    orow = [S.carve("orow%d" % i, [1024], F32) for i in range(2)]
    subs = []
    for ci, (lo, n) in enumerate(chunks):
        for s0 in range(0, n, W):
            subs.append((lo + s0, min(W, n - s0), offs[ci] + s0, cols[ci]))
    it = 0
    for si, (lo, n, ao, col) in enumerate(subs):
        st_ = sets[si % 2]
        x1b, r, x2, tA, lt = st_["x1b"], st_["r"], st_["x2"], st_["tA"], st_["lt"]
        S.dma(x1b[:, :, 0:n], x1d[:, :, lo:lo + n], R(x1d), R(x1b))
        for dc in range(8):
            S.act(tA[:, 0:n], acc[:, dc, ao:ao + n], AF.Identity, [acc.k(dc)] + R(C.mod), R(tA),
                  scale=C.mod[:, G2 + dc, col:col + 1])
            S.stt(r[:, dc, 0:n], x1b[:, dc, 0:n], ALPHA, tA[:, 0:n], ALU.mult, ALU.add, R(x1b, tA), R(r))
        layer_norm_fm(S, C, r, n, 2, lambda c, x2=x2, n=n: x2[:, c, 0:n], lt)
        for i in range(n // 128):
            pp = (PS[0], PS[1]) if it % 2 == 0 else (PS[2], PS[3])
            for dc in range(8):
                p = pp[dc // 4]
                S.tr(p[:, (dc % 4) * 128:(dc % 4 + 1) * 128], x2[:, dc, i * 128:(i + 1) * 128], C.ident, R(x2, C.cs), R(p))
            ot = orow[it % 2]
            it += 1
            S.cp(ot[:, 0:512], pp[0][:, :], R(pp[0]), R(ot), eng="act")
            S.cp(ot[:, 512:1024], pp[1][:, :], R(pp[1]), R(ot))
            okey = [xout.k((lo + i * 128) // 128)] if xout.nreg > 1 else R(xout)
            S.dma(xout[lo + i * 128:lo + (i + 1) * 128, :], ot[:], R(ot), okey)


NT = 34
TOK = 4352
OWN = [0] + list(range(2, 18))
NOWN = 2176


def own_segments(t0, t1):
    segs = []
    for lo, hi, base in ((0, 128, 0), (256, 2304, 128)):
        a, b = max(t0, lo), min(t1, hi)
        if a < b:
            segs.append((a, b - a, base + a - lo))
    return segs


class _Stop(Exception):
    pass


def build_l0(debug=False):
    import os
    stop = int(os.environ.get("STOP", "99"))

    def chk(n):
        if stop == n:
            raise _Stop()

    nc = bass.Bass("TRN2", target_bir_lowering=False)
    es = ExitStack()
    with es:
      S = Sched(nc, es)
      try:
        _build(S, nc, chk)
      except _Stop:
        print("STOPPED at", stop)
        S.finish([])
      S.emit()
    return nc


def _build(S, nc, chk, fused=False, after_block=None):
    if True:
        C = Ctx()
        D = lambda name, shape, dt=F32, kind="ExternalInput": S.dram(name, shape, dt, kind)
        xs = D("xs", [TOK, 1024])
        cT = D("cT", [128, 8, 2])
        mod_w = D("mod_w", [1024, 6144])
        mod_bT = D("mod_bT", [128, 48])
        lnp_d = D("lnp", [128, 4, 8])
        win = D("win", [8, 1024, 776])
        convp_d = D("convp", [128, 8, 4, 6])
        ssdp_d = D("ssdp", [1, 160])
        normw_d = D("normw", [128, 16])
        wout_d = D("wout", [2048, 1024])
        rw_d = D("rw", [128, 8, 32])
        rb_d = D("rb", [1, 32])
        xout = S.dram("xout", [NOWN, 1024], F32, "Internal" if fused else "ExternalOutput", nreg=17 if fused else 1)
        hTd = D("hTd", [128, 8, TOK], BF16, "Internal")
        ygd = D("ygd", [128, 16, NOWN], BF16, "Internal")
        x1d = D("x1d", [128, 8, NOWN], F32, "Internal")
        setup_common(S, C, True)
        PS = C.PS
        if not hasattr(S, "ar"):
            S.arena(49800)
        C.mod = S.carve("mod", [48, 2], F32)
        C.lnp = S.carve("lnp", [4, 8], F32)
        S.dma(C.lnp[:], lnp_d[:], [], R(C.lnp))
        modulation(S, C, mod_w, mod_bT, cT)
        base = S.mark()
        chk(0)

        xr = [S.carve("xr%d" % i, [1024], F32) for i in range(2)]
        hb = [S.carve("hb%d" % i, [8, 512], BF16) for i in range(2)]
        for blk in range(9):
            t0 = blk * 4
            nt = min(4, NT - t0)
            h = hb[blk % 2]
            for i in range(nt):
                t = t0 + i
                col = 1 if t < 2 else 0
                x = xr[t % 2]
                S.dma(x[:], xs[t * 128:(t + 1) * 128, :], [], R(x))
                for kc in range(8):
                    p = PS[kc // 4 + 2 * (t % 2)]
                    S.tr(p[:, (kc % 4) * 128:(kc % 4 + 1) * 128], x[:, kc * 128:(kc + 1) * 128], C.ident, R(x, C.cs), R(p))
                for kc in range(8):
                    p = PS[kc // 4 + 2 * (t % 2)]
                    S.act(h[:, kc, i * 128:(i + 1) * 128], p[:, (kc % 4) * 128:(kc % 4 + 1) * 128], AF.Identity,
                          R(p, C.mod), R(h), scale=C.mod[:, SC1 + kc, col:col + 1], bias=C.mod[:, SH1 + kc, col:col + 1])
            S.dma(hTd[:, :, t0 * 128:(t0 + nt) * 128], h[:, :, 0:nt * 128], R(h), R(hTd))
        S.reset(base)
        chk(1)

        convp = S.carve("convp", [8, 4, 6], F32)
        S.dma(convp[:], convp_d[:], [], R(convp))
        ssdp = S.carve("ssdp", [8, 20], F32)
        S.dma(ssdp[:], ssdp_d.h.partition_broadcast(128).rearrange("p o (g k) -> p (o g) k", k=20), [], R(ssdp))
        normw = S.carve("normw", [16], F32)
        S.dma(normw[:], normw_d[:], [], R(normw))
        wg = [S.carve("wg%d" % i, [8, 776], BF16) for i in range(2)]
        hblk = [S.carve("hblk%d" % i, [8, 512], BF16) for i in range(2)]
        raw = [S.carve("raw%d" % i, [4358], BF16) for i in range(2)]
        for r_ in raw:
            S.memset(r_[:], 0.0, R(r_))
        dgw = S.carve("dgw", [5, 128], BF16)
        fm = S.carve("fm", [TOK], BF16)
        bT = S.carve("bT", [TOK], BF16)
        cTt = S.carve("cTt", [2304], BF16)
        zT = S.carve("zT", [2, NOWN], BF16)
        xtok = S.carve("xtok", [NT, 256], BF16)
        btok = S.carve("btok", [NT, 128], BF16)
        yacc = S.carve("yacc", [17, 256], F32, nreg=17)
        ygs = S.carve("ygs", [2, NOWN], BF16)
        dtv = S.carve("dtv", [2, NT, 4], F32)
        dte = S.carve("dte", [2, NT, 4], F32)
        lndt = S.carve("lndt", [2, NT, 4], F32)
        la = S.carve("la", [2, NT, 4], F32)
        biasL = S.carve("biasL", [2, NT, 4], F32)
        ecum = S.carve("ecum", [2, NT, 4], F32)
        wdec = S.carve("wdec", [2, NT, 4], F32)
        cdec = S.carve("cdec", [2, NT, 4], F32)
        aneg = S.carve("aneg", [8], F32)
        cumS = S.carve("cumS", [2, NT, 4], F32)
        hl_t = S.carve("hl_t", [2, NT, 4], F32)
        cum_hi = S.carve("cum_hi", [2, NT, 4], BF16)
        cum_lo = S.carve("cum_lo", [2, NT, 4], BF16)
        bl_hi = S.carve("bl_hi", [2, NT, 4], BF16)
        bl_lo = S.carve("bl_lo", [2, NT, 4], BF16)
        NEG4 = [S.carve("NEG4%d" % i, [4, 128], BF16) for i in range(2)]
        for d in range(2):
            S.cp(NEG4[d][:], C.NEG1[d].unsqueeze(1).to_broadcast([128, 4, 128]), R(C.cs), R(NEG4[d]))
        Lp = [S.carve("Lp%d" % i, [4, 128], F32) for i in range(2)]
        Mp = [S.carve("Mp%d" % i, [4, 128], BF16) for i in range(2)]
        t1 = [S.carve("t1%d" % i, [4, 64], F32) for i in range(2)]
        t2 = [S.carve("t2%d" % i, [4, 64], F32) for i in range(2)]
        xdec = [S.carve("xdec%d" % i, [4, 64], BF16) for i in range(2)]
        st = [S.carve("st%d" % i, [4, 64], F32) for i in range(2)]
        stbf = [S.carve("stbf%d" % i, [4, 64], BF16) for i in range(2)]
        yz = S.carve("yz", [2, 512], F32)
        sqg = S.carve("sqg", [2, 512], F32)
        rsg = S.carve("rsg", [512], F32)
        hv = hTd.h
        blocks = [(b * 512, min(512, TOK - b * 512)) for b in range(9)]
        orderA = list(range(NT))
        orderB = [1, 0] + list(range(NT - 1, 1, -1))
        flat = lambda t: t[:].rearrange("p a b -> p (a b)")

        for g in range(8):
            w = wg[g % 2]
            S.dma(w[:], win.h[g].rearrange("(kc p) f -> p kc f", p=128), [], R(w), eng="pool")

            def inproj_pass(chunks, evac, with_dt):
                for bi, (b0, n) in enumerate(blocks):
                    hbk = hblk[bi % 2]
                    S.dma(hbk[:, :, 0:n], hv[:, :, b0:b0 + n], R(hTd), R(hbk))
                    for ci, off in enumerate(chunks):
                        p = PS[ci % 2]
                        for kc in range(8):
                            S.mm(p[:, 0:n], w[:, kc, off:off + 128], hbk[:, kc, 0:n], kc == 0, kc == 7, R(w, hbk), R(p))
                        evac(ci, p, b0, n)
                    if with_dt:
                        for i in range(n // 128):
                            t = b0 // 128 + i
                            for kc in range(8):
                                S.mm(PS[2][:, t * 8:(t + 1) * 8], hbk[:, kc, i * 128:(i + 1) * 128], w[:, kc, 768:776],
                                     kc == 0, kc == 7, R(w, hbk), R(PS[2]))

            def evac_raw(ci, p, b0, n):
                r_ = raw[ci]
                for lo, hi, sh in ((0, 256, 2), (256, TOK, 4)):
                    a, b = max(b0, lo), min(b0 + n, hi)
                    if a < b:
                        S.cp(r_[:, a + sh:b + sh], p[:, a - b0:b - b0], R(p), R(r_), eng="act")

            def conv_silu(slot, ch, dst, limit=TOK):
                r_ = raw[slot]
                for j in range(5):
                    S.ts(dgw[:, j, :], C.ident, convp[:, g, ch, j:j + 1], None, ALU.mult, None, R(C.cs, convp), R(dgw))
                segs = [(0, 256, 0)] + [(256 + 512 * k, 512, 258 + 512 * k) for k in range(8)]
                segs = [sg for sg in segs if sg[0] < limit]
                for si, (olo, n, rlo) in enumerate(segs):
                    p = PS[si % 2]
                    for j in range(5):
                        S.mm(p[:, 0:n], dgw[:, j, :], r_[:, rlo + j:rlo + j + n], j == 0, j == 4, R(dgw, r_), R(p))
                    S.act(dst[:, olo:olo + n], p[:, 0:n], AF.Silu, R(p, convp), R(dst), bias=convp[:, g, ch, 5:6])

            if g == 0: chk(20)
            inproj_pass([256, 384], evac_raw, False)
            if g == 0: chk(21)
            for xi in range(2):
                conv_silu(xi, xi, fm)
                for t0 in range(0, NT, 4):
                    nt = min(4, NT - t0)
                    pb = PS[3 + (t0 // 4) % 2][:, 0:256].bitcast(BF16)
                    pk = PS[3 + (t0 // 4) % 2]
                    for i in range(nt):
                        S.tr(pb[:, i * 128:(i + 1) * 128], fm[:, (t0 + i) * 128:(t0 + i + 1) * 128], C.identbf[:],
                             R(fm, C.identbf), R(pk))
                    S.cp(xtok[:, t0:t0 + nt, xi * 128:(xi + 1) * 128],
                         pb[:, 0:nt * 128].rearrange("p (a b) -> p a b", b=128), R(pk), R(xtok), eng="act")
            if g == 0: chk(22)
            inproj_pass([512, 640], evac_raw, False)
            conv_silu(0, 2, bT)
            conv_silu(1, 3, cTt, 2304)
            for t0 in range(0, NT, 4):
                nt = min(4, NT - t0)
                pb = PS[3 + (t0 // 4) % 2][:, 0:256].bitcast(BF16)
                pk = PS[3 + (t0 // 4) % 2]
                for i in range(nt):
                    S.tr(pb[:, i * 128:(i + 1) * 128], bT[:, (t0 + i) * 128:(t0 + i + 1) * 128], C.identbf[:],
                         R(bT, C.identbf), R(pk))
                S.cp(btok[:, t0:t0 + nt, :], pb[:, 0:nt * 128].rearrange("p (a b) -> p a b", b=128), R(pk), R(btok), eng="act")

            if g == 0: chk(23)
            def evac_z(ci, p, b0, n):
                for a, m, o in own_segments(b0, b0 + n):
                    S.act(zT[:, ci, o:o + m], p[:, a - b0:a - b0 + m], AF.Silu, R(p), R(zT))

            inproj_pass([0, 128], evac_z, True)
            if g == 0: chk(24)
            dtb = ssdp[:, g, 0:8].rearrange("p (d h) -> p d h", h=4).unsqueeze(2).to_broadcast([128, 2, NT, 4])
            if g == 0: chk(240)
            S.tt(dtv[:], PS[2][:, 0:NT * 8].rearrange("p (c d h) -> p d c h", d=2, h=4), dtb, ALU.add, R(PS[2], ssdp), R(dtv))
            if g == 0: chk(241)
            S.act(dte[:], dtv[:], AF.Exp, R(dtv), R(dte))
            if g == 0: chk(242)
            S.act(dtv[:], dte[:], AF.Ln, R(dte), R(dtv), bias=1.0)
            if g == 0: chk(243)
            S.act(lndt[:], dtv[:], AF.Ln, R(dtv), R(lndt))
            if g == 0: chk(244)
            S.act(aneg[:], ssdp[:, g, 8:16], AF.Exp, R(ssdp), R(aneg))
            if g == 0: chk(245)
            S.ts(aneg[:], aneg[:], -1.0, None, ALU.mult, None, R(aneg), R(aneg))
            if g == 0: chk(246)
            S.tt(la[:], dtv[:], aneg[:].rearrange("p (d h) -> p d h", h=4).unsqueeze(2).to_broadcast([128, 2, NT, 4]),
                 ALU.mult, R(dtv, aneg), R(la))
            laf = la[:].rearrange("p d c h -> p (d c h)")
            if g == 0: chk(247)
            for d in range(2):
                S.mm(PS[5][:, d * 136:(d + 1) * 136], C.U[d], laf[:, d * 136:(d + 1) * 136], True, True, R(la, C.cs), R(PS[5]))
            if g == 0: chk(248)
            S.mm(PS[6][:, 0:272], C.ones, laf, True, True, R(la, C.cs), R(PS[6]))
            fl = lambda t: t[:].rearrange("p d c h -> p (d c h)")
            if g == 0: chk(249)
            S.tt(fl(biasL), fl(lndt), PS[5][:, 0:272], ALU.subtract, R(lndt, PS[5]), R(biasL))
            if g == 0: chk(250)
            S.cp(fl(cumS), PS[5][:, 0:272], R(PS[5]), R(cumS))
            S.act(fl(ecum), fl(cumS), AF.Exp, R(cumS), R(ecum))
            for src, hi, lo in ((cumS, cum_hi, cum_lo), (biasL, bl_hi, bl_lo)):
                S.cp(fl(hi), fl(src), R(src), R(hi))
                S.tt(fl(hl_t), fl(src), fl(hi), ALU.subtract, R(src, hi), R(hl_t))
                S.cp(fl(lo), fl(hl_t), R(hl_t), R(lo))
            if g == 0: chk(251)
            S.tt(fl(wdec), fl(biasL), PS[6][:, 0:272], ALU.add, R(biasL, PS[6]), R(wdec))
            if g == 0: chk(252)
            S.act(fl(wdec), fl(wdec), AF.Exp, R(wdec), R(wdec))
            if g == 0: chk(253)
            S.cp(fl(cdec), PS[6][:, 0:272], R(PS[6]), R(cdec))
            S.act(fl(cdec), fl(cdec), AF.Exp, R(cdec), R(cdec))

            if g == 0: chk(25)
            Dh = ssdp[:, g, 16:20].unsqueeze(2).to_broadcast([128, 4, 64])
            for oc, c in enumerate(OWN):
                S.tt(yacc[:, oc, :].rearrange("p (h q) -> p h q", q=64), xtok[:, c, :].rearrange("p (h q) -> p h q", q=64),
                     Dh, ALU.mult, R(xtok, ssdp), [yacc.k(oc)], eng="pool")
            for d in range(2):
                S.memset(st[d][:], 0.0, R(st[d]))
                S.memset(stbf[d][:], 0.0, R(stbf[d]))
            step = 0
            for i in range(NT):
                for d, order in ((0, orderA), (1, orderB)):
                    c = order[i]
                    par = step % 2
                    step += 1
                    tc0 = c * 128
                    own = c in OWN
                    pCB, pseg, pY = PS[0], PS[1 + par], PS[3 + par]
                    if own:
                        oc = OWN.index(c)
                        S.mm(pseg[:, :], C.identbf[:], flat(NEG4[d]), True, False, R(NEG4[d], C.identbf), R(pseg))
                        for h in range(4):
                            ps_h = pseg[:, h * 128:(h + 1) * 128]
                            for ti, t_ in enumerate((cum_hi, cum_lo)):
                                S.mm(ps_h, t_[:, d, c, h:h + 1].to_broadcast([128, 128]), C.identbf[:], False, False,
                                     R(t_, C.identbf), R(pseg))
                            for ti, t_ in enumerate((bl_hi, bl_lo)):
                                S.mm(ps_h, C.identbf[:], t_[:, d, c, h:h + 1].to_broadcast([128, 128]), False,
                                     h == 3 and ti == 1, R(t_, C.identbf), R(pseg))
                        S.mm(pCB[:, 0:128], bT[:, tc0:tc0 + 128], cTt[:, tc0:tc0 + 128], True, True, R(bT, cTt), R(pCB))
                        S.act(flat(Lp[par]), pseg[:, :], AF.Exp, R(pseg), R(Lp[par]))
                        S.tt(Mp[par][:], Lp[par][:], pCB[:, 0:128].unsqueeze(1).to_broadcast([128, 4, 128]), ALU.mult,
                             R(Lp[par], pCB), R(Mp[par]))
                        S.mm(pY[:, 256:512], cTt[:, tc0:tc0 + 128], flat(stbf[d]), True, True, R(cTt, stbf[d]), R(pY))
                    if any(cc in OWN for cc in order[i + 1:]):
                        pds = PS[5 + par]
                        S.tt(xdec[par][:], xtok[:, c, :].rearrange("p (h q) -> p h q", q=64),
                             wdec[:, d, c, :].unsqueeze(2).to_broadcast([128, 4, 64]), ALU.mult, R(xtok, wdec), R(xdec[par]),
                             eng="pool")
                        S.mm(pds[:, 0:256], btok[:, c, :], flat(xdec[par]), True, True, R(btok, xdec[par]), R(pds))
                        S.tt(st[d][:], st[d][:], cdec[:, d, c, :].unsqueeze(2).to_broadcast([128, 4, 64]), ALU.mult,
                             R(st[d], cdec), R(st[d]), eng="pool")
                        S.tt(flat(st[d]), flat(st[d]), pds[:, 0:256], ALU.add, R(st[d], pds), R(st[d]))
                        S.cp(stbf[d][:], st[d][:], R(st[d]), R(stbf[d]), eng="act")
                    if own:
                        for h in range(4):
                            S.mm(pY[:, h * 64:(h + 1) * 64], Mp[par][:, h, :], xtok[:, c, h * 64:(h + 1) * 64], True, True,
                                 R(Mp[par], xtok), R(pY))
                        S.tt(t1[par][:], pY[:, 256:512].rearrange("p (h q) -> p h q", q=64),
                             ecum[:, d, c, :].unsqueeze(2).to_broadcast([128, 4, 64]), ALU.mult, R(pY, ecum), R(t1[par]))
                        S.tt(flat(t2[par]), flat(t1[par]), pY[:, 0:256], ALU.add, R(t1[par], pY), R(t2[par]))
                        S.tt(yacc[:, oc, :], yacc[:, oc, :], flat(t2[par]), ALU.add, R(t2[par]) + [yacc.k(oc)], [yacc.k(oc)],
                             eng="pool")

            if g == 0: chk(26)
            for o0 in range(0, 17, 4):
                nt = min(4, 17 - o0)
                n = nt * 128
                for i in range(nt):
                    for j in range(2):
                        S.tr(PS[5 + j][:, i * 128:(i + 1) * 128], yacc[:, o0 + i, j * 128:(j + 1) * 128], C.ident,
                             [yacc.k(o0 + i)] + R(C.cs), R(PS[5 + j]))
                for j in range(2):
                    S.tt(yz[:, j, 0:n], PS[5 + j][:, 0:n], zT[:, j, o0 * 128:o0 * 128 + n], ALU.mult, R(PS[5 + j], zT), R(yz))
                S.tt(sqg[:, :, 0:n], yz[:, :, 0:n], yz[:, :, 0:n], ALU.mult, R(yz), R(sqg), eng="pool")
                for j in range(2):
                    S.mm(PS[7][:, 0:n], C.ones, sqg[:, j, 0:n], j == 0, j == 1, R(sqg, C.cs), R(PS[7]))
                S.act(rsg[:, 0:n], PS[7][:, 0:n], AF.Sqrt, R(PS[7]), R(rsg), scale=1.0 / 256, bias=1e-5)
                S.recip(rsg[:, 0:n], rsg[:, 0:n], R(rsg), R(rsg))
                S.tt(yz[:, :, 0:n], yz[:, :, 0:n], rsg[:, 0:n].unsqueeze(1).to_broadcast([128, 2, n]), ALU.mult,
                     R(yz, rsg), R(yz))
                for j in range(2):
                    S.act(ygs[:, j, o0 * 128:o0 * 128 + n], yz[:, j, 0:n], AF.Identity, R(yz, normw), R(ygs),
                          scale=normw[:, 2 * g + j:2 * g + j + 1])
            S.dma(ygd[:, 2 * g:2 * g + 2, :], ygs[:], R(ygs), R(ygd))
            if g == 0: chk(27)
        S.reset(base)
        chk(2)

        XT = S.carve("XT", [8, NOWN], BF16)
        gateT = S.carve("gateT", [NOWN], F32, parts=32)
        p3 = S.mark()
        wout = S.carve("wout", [16, 1024], BF16)
        for h in range(2):
            S.dma(wout[:, h * 8:(h + 1) * 8, :], wout_d.h.rearrange("(kc p) f -> p kc f", p=128)[:, h * 8:(h + 1) * 8, :],
                  [], R(wout), eng="pool")
        rw = S.carve("rw", [8, 32], F32)
        S.dma(rw[:], rw_d[:], [], R(rw))
        rb = S.carve("rb", [32], F32)
        S.dma(rb[:], rb_d.h.partition_broadcast(128).rearrange("p o k -> p (o k)"), [], R(rb))
        ygb = S.carve("ygb", [16, 512], BF16)
        xrw = S.carve("xrw", [4, 1024], F32)
        r = S.carve("r1", [8, 512], F32)
        x1t = S.carve("x1t", [8, 512], F32)
        Xf = S.carve("Xf", [8, 512], F32)
        tA = S.carve("tA", [512], F32)
        lt = ln_tmp(S, 512, R(x1t))
        rt = router_tmp(S)
        chunks3 = [(0, 128, 1)] + [(128 + 512 * k, 512, 0) for k in range(4)]
        for (o0, n, col) in chunks3:
            row0 = o0 if o0 < 128 else o0 + 128
            S.dma(ygb[:, :, 0:n], ygd[:, :, o0:o0 + n], R(ygd), R(ygb))
            S.dma(xrw[:, 0:n // 128, :], xs[row0:row0 + n, :].rearrange("(i p) f -> p i f", p=128), [], R(xrw))
            for dc in range(8):
                py, px = PS[dc % 2], PS[2 + dc % 2]
                for kc in range(16):
                    S.mm(py[:, 0:n], wout[:, kc, dc * 128:(dc + 1) * 128], ygb[:, kc, 0:n], kc == 0, kc == 15, R(wout, ygb), R(py))
                for i in range(n // 128):
                    S.tr(px[:, i * 128:(i + 1) * 128], xrw[:, i, dc * 128:(dc + 1) * 128], C.ident, R(xrw, C.cs), R(px))
                S.act(tA[:, 0:n], py[:, 0:n], AF.Identity, R(py, C.mod), R(tA), scale=C.mod[:, G1 + dc, col:col + 1])
                S.stt(r[:, dc, 0:n], px[:, 0:n], ALPHA, tA[:, 0:n], ALU.mult, ALU.add, R(px, tA), R(r))
            layer_norm_fm(S, C, r, n, 0, lambda c: x1t[:, c, 0:n], lt)
            S.dma(x1d[:, :, o0:o0 + n], x1t[:, :, 0:n], R(x1t), R(x1d))
            for dc in range(8):
                S.act(Xf[:, dc, 0:n], x1t[:, dc, 0:n], AF.Identity, R(x1t, C.mod), R(Xf),
                      scale=C.mod[:, SC2 + dc, col:col + 1], bias=C.mod[:, SH2 + dc, col:col + 1])
            S.cp(XT[:, :, o0:o0 + n], Xf[:, :, 0:n], R(Xf), R(XT), eng="pool")
            router(S, C, Xf, n, o0, rw, rb, gateT, rt)
        S.reset(p3)
        chk(3)

        mw_in = D("mw_in", [32, 1024, 2048])
        mb_inT = D("mb_inT", [128, 32, 16])
        mw_out = D("mw_out", [32, 1024, 1024])
        mb_out = D("mb_out", [32, 1024])
        b_inT = S.carve("b_inT", [32, 16], F32)
        S.dma(b_inT[:], mb_inT[:], [], R(b_inT))
        S.ts(b_inT[:, :, 8:16], b_inT[:, :, 8:16], 1.0, None, ALU.add, None, R(b_inT), R(b_inT))
        p4 = S.mark()
        tblocks = [([(0, 128), (128, 512), (640, 512)], [1, 0, 0]), ([(1152, 512), (1664, 512)], [0, 0])]
        for chunks, cols in tblocks:
            acc = S.carve("acc", [8, sum(n for _, n in chunks)], F32, nreg=8)
            m = S.mark()
            offs = moe_block(S, C, XT, gateT, chunks, mw_in, mw_out, b_inT, mb_out, acc)
            S.reset(m)
            ln2_out(S, C, acc, offs, chunks, cols, x1d, xout, None, None)
            if after_block is not None:
                after_block(xout, chunks[0][0], chunks[-1][0] + chunks[-1][1])
            S.reset(p4)
        if not fused:
            S.finish(R(xout))
        return xout


TOK1 = 4352
NQ = 2048
NT1 = 34
MLA_SCALE = 192.0 ** -0.5
RMS_EPS = 1e-6


class _Stop1(Exception):
    pass


def build_l1():
    import os
    stop = int(os.environ.get("STOP", "99"))

    def chk(n):
        if stop == n:
            raise _Stop1()

    nc = bass.Bass("TRN2", target_bir_lowering=False)
    es = ExitStack()
    with es:
        S = Sched(nc, es)
        try:
            _build1(S, nc, chk)
        except _Stop1:
            print("STOPPED at", stop)
            S.finish([])
        S.emit()
    return nc


def _build1(S, nc, chk, fused=False, xown=None, xall=None):
    C = Ctx()
    D = lambda name, shape, dt=F32, kind="ExternalInput": S.dram(name, shape, dt, kind)
    xs = None if fused else D("xs", [TOK1, 1024])
    NTQ = 16 if fused else 0
    cT = D("cT", [128, 8, 2])
    mod_w = D("mod_w", [1024, 6144])
    mod_bT = D("mod_bT", [128, 48])
    lnp_d = D("lnp", [128, 4, 8])
    wa_d = D("wa", [1024, 704])
    qnorm_d = D("qnorm", [128, 3])
    kvnorm_d = D("kvnorm", [128, 2])
    wqb_d = D("wqb", [384, 1536])
    wkvb_d = D("wkvb", [256, 2048])
    wo_d = D("wo", [1024, 1024])
    ropeq_d = D("ropeq", [64, 2, NQ])
    ropek_d = D("ropek", [64, 2, TOK1])
    rm_d = D("rm", [64, 64])
    rw_d = D("rw", [128, 8, 32])
    rb_d = D("rb", [1, 32])
    xout = D("xout", [NQ, 1024], F32, "ExternalOutput")
    hTd = D("hTd", [128, 8, TOK1 + NTQ * 128], BF16, "Internal")
    x1d = D("x1d", [128, 8, NQ], F32, "Internal")
    setup_common(S, C, True)
    PS = C.PS
    if not hasattr(S, "ar"):
        S.arena(49800)
    C.mod = S.carve("mod", [48, 2], F32)
    C.lnp = S.carve("lnp", [4, 8], F32)
    S.dma(C.lnp[:], lnp_d[:], [], R(C.lnp))
    onesbf = S.carve("onesbf", [128], BF16)
    S.cp(onesbf[:], C.ones, R(C.cs), R(onesbf))
    c128 = S.carve("c128", [128], BF16)
    S.ts(c128[:], C.ones, 1.0 / 128, None, ALU.mult, None, R(C.cs), R(c128))
    rm = S.carve("rm", [64], F32, parts=64)
    S.dma(rm[:], rm_d[:], [], R(rm))
    modulation(S, C, mod_w, mod_bT, cT)
    XT = S.carve("XT", [8, NQ], BF16)
    gateT = S.carve("gateT", [NQ], F32, parts=32)
    markA = S.mark()
    oT = S.carve("oT", [8, NQ], BF16, nreg=8)
    markB = S.mark()
    chk(0)

    xr = [S.carve("xr%d" % i, [1024], F32) for i in range(2)]
    hb = [S.carve("hb%d" % i, [8, 512], BF16) for i in range(2)]
    ntl = NT1 + NTQ
    tmap = xall_tile_map()
    tchunk = []
    for k, (r0_, n_) in enumerate(XCHUNKS):
        tchunk += [k] * (2 * n_ // 128)
    for blk in range((ntl + 3) // 4):
        t0 = blk * 4
        nt = min(4, ntl - t0)
        h = hb[blk % 2]
        for i in range(nt):
            t = t0 + i
            x = xr[t % 2]
            if not fused:
                col = 1 if t >= 32 else 0
                S.dma(x[:], xs[t * 128:(t + 1) * 128, :], [], R(x))
            elif t < NT1:
                col = 1 if tmap[t][1] == 0 else 0
                S.dma(x[:], xall[t * 128:(t + 1) * 128, :], [xall.k(tchunk[t])], R(x))
            else:
                col = 0
                S.dma(x[:], xown[128 + (t - NT1) * 128:128 + (t - NT1 + 1) * 128, :], [xown.k(1 + t - NT1)], R(x))
            for kc in range(8):
                p = PS[kc // 4 + 2 * (t % 2)]
                S.tr(p[:, (kc % 4) * 128:(kc % 4 + 1) * 128], x[:, kc * 128:(kc + 1) * 128], C.ident, R(x, C.cs), R(p))
            for kc in range(8):
                p = PS[kc // 4 + 2 * (t % 2)]
                S.act(h[:, kc, i * 128:(i + 1) * 128], p[:, (kc % 4) * 128:(kc % 4 + 1) * 128], AF.Identity,
                      R(p, C.mod), R(h), scale=C.mod[:, SC1 + kc, col:col + 1], bias=C.mod[:, SH1 + kc, col:col + 1])
        S.dma(hTd[:, :, t0 * 128:(t0 + nt) * 128], h[:, :, 0:nt * 128], R(h), R(hTd))
    S.reset(markB)
    chk(1)

    kvn = S.carve("kvn", [2, TOK1], BF16)
    qln = S.carve("qln", [3, NQ], BF16)
    KrT = S.carve("KrT", [TOK1], BF16)
    S.memset(KrT[64:128, :], 0.0, R(KrT))
    markC = S.mark()
    wa = S.carve("wa", [8, 704], BF16)
    S.dma(wa[:], wa_d.h.rearrange("(kc p) f -> p kc f", p=128), [], R(wa), eng="pool")
    qnorm = S.carve("qnorm", [3], F32)
    S.dma(qnorm[:], qnorm_d[:], [], R(qnorm))
    kvnorm = S.carve("kvnorm", [2], F32)
    S.dma(kvnorm[:], kvnorm_d[:], [], R(kvnorm))
    hblk = [S.carve("hblk%d" % i, [8, 512], BF16) for i in range(2)]
    kvl = S.carve("kvl", [2, 512], F32)
    ql = S.carve("ql", [3, 512], F32)
    krl = S.carve("krl", [512], F32)
    sq = S.carve("sq", [3, 512], F32)
    rs = S.carve("rs", [512], F32)
    rk = S.carve("rk", [2, 512], F32)
    t1 = S.carve("t1", [512], F32)
    t2 = S.carve("t2", [512], F32)
    blocks = [(b * 512, min(512, TOK1 - b * 512)) for b in range(9)]

    def rms_fm(src, nch, width, g, dst, d0, n):
        S.tt(sq[:, 0:nch, 0:n], src[:, 0:nch, 0:n], src[:, 0:nch, 0:n], ALU.mult, R(src), R(sq), eng="pool")
        for c in range(nch):
            S.mm(PS[6][:, 0:n], C.ones, sq[:, c, 0:n], c == 0, c == nch - 1, R(sq, C.cs), R(PS[6]))
        S.cp(rs[:, 0:n], PS[6][:, 0:n], R(PS[6]), R(rs))
        S.act(rs[:, 0:n], rs[:, 0:n], AF.Sqrt, R(rs), R(rs), scale=1.0 / width, bias=RMS_EPS)
        S.recip(rs[:, 0:n], rs[:, 0:n], R(rs), R(rs))
        S.tt(src[:, 0:nch, 0:n], src[:, 0:nch, 0:n], rs[:, 0:n].unsqueeze(1).to_broadcast([128, nch, n]), ALU.mult,
             R(src, rs), R(src))
        for c in range(nch):
            S.act(dst[:, c, d0:d0 + n], src[:, c, 0:n], AF.Identity, R(src, g), R(dst), scale=g[:, c:c + 1])

    def rope(src, tab, dst_ap, n, dstkeys):
        S.mm(PS[7][0:64, 0:n], rm[0:64, 0:64], src[0:64, 0:n], True, True, R(src, rm), R(PS[7]))
        S.tt(t1[0:64, 0:n], src[0:64, 0:n], tab[0:64, 0, 0:n], ALU.mult, R(src, tab), R(t1))
        S.tt(t2[0:64, 0:n], PS[7][0:64, 0:n], tab[0:64, 1, 0:n], ALU.mult, R(PS[7], tab), R(t2))
        S.tt(dst_ap, t1[0:64, 0:n], t2[0:64, 0:n], ALU.add, R(t1, t2), dstkeys, eng="pool")

    for bi, (b0, n) in enumerate(blocks):
        hbk = hblk[bi % 2]
        S.dma(hbk[:, :, 0:n], hTd.h[:, :, b0:b0 + n], R(hTd), R(hbk))
        S.dma(rk[0:64, :, 0:n], ropek_d[:, :, b0:b0 + n], [], R(rk))
        it = 0
        for c in range(2):
            p = PS[it % 2]
            it += 1
            for kc in range(8):
                S.mm(p[:, 0:n], wa[:, kc, 384 + c * 128:384 + (c + 1) * 128], hbk[:, kc, 0:n], kc == 0, kc == 7, R(wa, hbk), R(p))
            S.cp(kvl[:, c, 0:n], p[:, 0:n], R(p), R(kvl))
        p = PS[2]
        for kc in range(8):
            S.mm(p[0:64, 0:n], wa[:, kc, 640:704], hbk[:, kc, 0:n], kc == 0, kc == 7, R(wa, hbk), R(p))
        S.cp(krl[0:64, 0:n], p[0:64, 0:n], R(p), R(krl))
        if b0 < NQ and not fused:
            for c in range(3):
                p = PS[it % 2]
                it += 1
                for kc in range(8):
                    S.mm(p[:, 0:n], wa[:, kc, c * 128:(c + 1) * 128], hbk[:, kc, 0:n], kc == 0, kc == 7, R(wa, hbk), R(p))
                S.cp(ql[:, c, 0:n], p[:, 0:n], R(p), R(ql))
            rms_fm(ql, 3, 384, qnorm, qln, b0, n)
        rms_fm(kvl, 2, 256, kvnorm, kvn, b0, n)
        rope(krl, rk, KrT[0:64, b0:b0 + n], n, R(KrT))
    if fused:
        for qi in range(4):
            hbk = hblk[qi % 2]
            n = 512
            b0 = qi * 512
            S.dma(hbk[:, :, 0:n], hTd.h[:, :, TOK1 + b0:TOK1 + b0 + n], R(hTd), R(hbk))
            for c in range(3):
                p = PS[c % 2]
                for kc in range(8):
                    S.mm(p[:, 0:n], wa[:, kc, c * 128:(c + 1) * 128], hbk[:, kc, 0:n], kc == 0, kc == 7, R(wa, hbk), R(p))
                S.cp(ql[:, c, 0:n], p[:, 0:n], R(p), R(ql))
            rms_fm(ql, 3, 384, qnorm, qln, b0, n)
    S.reset(markC)
    chk(2)

    wqb = S.carve("wqb", [3, 1536], BF16)
    S.dma(wqb[:], wqb_d.h.rearrange("(kc p) f -> p kc f", p=128), [], R(wqb), eng="pool")
    wkvb = S.carve("wkvb", [2, 2048], BF16)
    S.dma(wkvb[:], wkvb_d.h.rearrange("(kc p) f -> p kc f", p=128), [], R(wkvb), eng="pool")
    rq = S.carve("rq", [2, 512], F32)
    KnT = S.carve("KnT", [TOK1], BF16)
    V = S.carve("V", [NT1, 128], BF16)
    QnT = S.carve("QnT", [NQ], BF16)
    QrT = S.carve("QrT", [NQ], BF16)
    S.memset(QrT[64:128, :], 0.0, R(QrT))
    negm = S.carve("negm", [NQ], BF16)
    pT = [S.carve("pT%d" % i, [512], BF16) for i in range(3)]
    sqb = S.carve("sqb", [512], BF16)
    sqr = S.carve("sqr", [512], BF16)
    qrl = S.carve("qrl", [512], F32)
    t1 = S.carve("t1b", [512], F32)
    t2 = S.carve("t2b", [512], F32)
    tq = S.carve("tq", [512], F32)
    rden = S.carve("rden", [512], F32)
    mx = S.carve("mx", [16], F32)
    kmax = S.carve("kmax", [4], F32)
    for bi, (b0, n) in enumerate(blocks):
        S.tt(sqr[0:64, 0:n], KrT[0:64, b0:b0 + n], KrT[0:64, b0:b0 + n], ALU.mult, R(KrT), R(sqr), eng="pool")
        S.mm(PS[6][:, 0:n], onesbf[0:64, :], sqr[0:64, 0:n], True, True, R(sqr, onesbf), R(PS[6]))
        S.reduce(mx[:, bi:bi + 1], PS[6][:, 0:n], AX.X, ALU.max, R(PS[6]), R(mx))
    S.reduce(kmax[:, 0:1], mx[:, 0:9], AX.X, ALU.max, R(mx), R(kmax))
    qblocks = [(q * 512, 512) for q in range(4)]
    step = 0
    for h in range(8):
        for bi, (b0, n) in enumerate(blocks):
            p = PS[bi % 2]
            for kc in range(2):
                S.mm(p[:, 0:n], wkvb[:, kc, h * 128:(h + 1) * 128], kvn[:, kc, b0:b0 + n], kc == 0, kc == 1, R(wkvb, kvn), R(p))
            S.cp(KnT[:, b0:b0 + n], p[:, 0:n], R(p), R(KnT), eng="act")
            S.tt(sqb[:, 0:n], KnT[:, b0:b0 + n], KnT[:, b0:b0 + n], ALU.mult, R(KnT), R(sqb), eng="pool")
            S.mm(PS[6][:, 0:n], onesbf[:], sqb[:, 0:n], True, True, R(sqb, onesbf), R(PS[6]))
            S.reduce(mx[:, bi:bi + 1], PS[6][:, 0:n], AX.X, ALU.max, R(PS[6]), R(mx))
        S.reduce(kmax[:, 1:2], mx[:, 0:9], AX.X, ALU.max, R(mx), R(kmax))
        S.tt(kmax[:, 2:3], kmax[:, 0:1], kmax[:, 1:2], ALU.add, R(kmax), R(kmax))
        for t0 in range(0, NT1, 4):
            nt = min(4, NT1 - t0)
            p = PS[2 + (t0 // 4) % 2]
            for i in range(nt):
                for kc in range(2):
                    S.mm(p[:, i * 128:(i + 1) * 128], kvn[:, kc, (t0 + i) * 128:(t0 + i + 1) * 128],
                         wkvb[:, kc, 1024 + h * 128:1024 + (h + 1) * 128], kc == 0, kc == 1, R(wkvb, kvn), R(p))
            S.cp(V[:, t0:t0 + nt, :], p[:, 0:nt * 128].rearrange("p (a b) -> p a b", b=128), R(p), R(V), eng="act")
        for (q0, n) in qblocks:
            p = PS[0]
            for kc in range(3):
                S.mm(p[:, 0:n], wqb[:, kc, h * 128:(h + 1) * 128], qln[:, kc, q0:q0 + n], kc == 0, kc == 2, R(wqb, qln), R(p))
            S.cp(QnT[:, q0:q0 + n], p[:, 0:n], R(p), R(QnT), eng="act")
            p = PS[1]
            for kc in range(3):
                S.mm(p[0:64, 0:n], wqb[:, kc, 1024 + h * 64:1024 + (h + 1) * 64], qln[:, kc, q0:q0 + n], kc == 0, kc == 2,
                     R(wqb, qln), R(p))
            S.cp(qrl[0:64, 0:n], p[0:64, 0:n], R(p), R(qrl))
            S.dma(rq[0:64, :, 0:n], ropeq_d[:, :, q0:q0 + n], [], R(rq))
            S.mm(PS[7][0:64, 0:n], rm[0:64, 0:64], qrl[0:64, 0:n], True, True, R(qrl, rm), R(PS[7]))
            S.tt(t1[0:64, 0:n], qrl[0:64, 0:n], rq[0:64, 0, 0:n], ALU.mult, R(qrl, rq), R(t1))
            S.tt(t2[0:64, 0:n], PS[7][0:64, 0:n], rq[0:64, 1, 0:n], ALU.mult, R(PS[7], rq), R(t2))
            S.tt(QrT[0:64, q0:q0 + n], t1[0:64, 0:n], t2[0:64, 0:n], ALU.add, R(t1, t2), R(QrT), eng="pool")
            S.tt(sqb[:, 0:n], QnT[:, q0:q0 + n], QnT[:, q0:q0 + n], ALU.mult, R(QnT), R(sqb), eng="pool")
            S.tt(sqr[0:64, 0:n], QrT[0:64, q0:q0 + n], QrT[0:64, q0:q0 + n], ALU.mult, R(QrT), R(sqr), eng="pool")
            S.mm(PS[6][:, 0:n], onesbf[:], sqb[:, 0:n], True, False, R(sqb, onesbf), R(PS[6]))
            S.mm(PS[6][:, 0:n], onesbf[0:64, :], sqr[0:64, 0:n], False, True, R(sqr, onesbf), R(PS[6]))
            S.cp(tq[:, 0:n], PS[6][:, 0:n], R(PS[6]), R(tq))
            S.act(tq[:, 0:n], tq[:, 0:n], AF.Sqrt, R(tq, kmax), R(tq), scale=kmax[:, 2:3])
            S.ts(negm[:, q0:q0 + n], tq[:, 0:n], -1.0, None, ALU.mult, None, R(tq), R(negm))
        for (q0, n) in qblocks:
            po, pd = PS[4], PS[5]
            def scores(kt):
                par = kt % 3
                p = PS[par]
                k0 = kt * 128
                S.mm(p[:, 0:n], KnT[:, k0:k0 + 128], QnT[:, q0:q0 + n], True, False, R(KnT, QnT), R(p))
                S.mm(p[:, 0:n], KrT[:, k0:k0 + 128], QrT[:, q0:q0 + n], False, False, R(KrT, QrT), R(p))
                S.mm(p[:, 0:n], c128[:], negm[:, q0:q0 + n], False, True, R(c128, negm), R(p))
                S.act(pT[par][:, 0:n], p[:, 0:n], AF.Exp, R(p), R(pT[par]), scale=MLA_SCALE)

            scores(0)
            scores(1)
            for kt in range(NT1):
                if kt + 2 < NT1:
                    scores(kt + 2)
                par = kt % 3
                S.mm(po[:, 0:n], V[:, kt, :], pT[par][:, 0:n], kt == 0, kt == NT1 - 1, R(V, pT[par]), R(po))
                S.mm(pd[:, 0:n], onesbf[:], pT[par][:, 0:n], kt == 0, kt == NT1 - 1, R(onesbf, pT[par]), R(pd))
            S.recip(rden[:, 0:n], pd[:, 0:n], R(pd), R(rden))
            S.tt(oT[:, h, q0:q0 + n], po[:, 0:n], rden[:, 0:n], ALU.mult, R(po, rden), [oT.k(h)])
        if h == 0:
            chk(20)
    S.reset(markB)
    chk(3)

    wo = S.carve("wo", [8, 1024], BF16)
    S.dma(wo[:], wo_d.h.rearrange("(kc p) f -> p kc f", p=128), [], R(wo), eng="pool")
    rw = S.carve("rw", [8, 32], F32)
    S.dma(rw[:], rw_d[:], [], R(rw))
    rb = S.carve("rb", [32], F32)
    S.dma(rb[:], rb_d.h.partition_broadcast(128).rearrange("p o k -> p (o k)"), [], R(rb))
    xrw = S.carve("xrw", [4, 1024], F32)
    r = S.carve("r1", [8, 512], F32)
    x1t = S.carve("x1t", [8, 512], F32)
    Xf = S.carve("Xf", [8, 512], F32)
    tA = S.carve("tA", [512], F32)
    lt = ln_tmp(S, 512, R(x1t))
    rt = router_tmp(S)
    for (o0, n) in qblocks:
        if fused:
            S.dma(xrw[:, 0:n // 128, :], xown[128 + o0:128 + o0 + n, :].rearrange("(i p) f -> p i f", p=128), R(xown), R(xrw))
        else:
            S.dma(xrw[:, 0:n // 128, :], xs[o0:o0 + n, :].rearrange("(i p) f -> p i f", p=128), [], R(xrw))
        for dc in range(8):
            py, px = PS[dc % 2], PS[2 + dc % 2]
            for kc in range(8):
                S.mm(py[:, 0:n], wo[:, kc, dc * 128:(dc + 1) * 128], oT[:, kc, o0:o0 + n], kc == 0, kc == 7, R(wo, oT), R(py))
            for i in range(n // 128):
                S.tr(px[:, i * 128:(i + 1) * 128], xrw[:, i, dc * 128:(dc + 1) * 128], C.ident, R(xrw, C.cs), R(px))
            S.act(tA[:, 0:n], py[:, 0:n], AF.Identity, R(py, C.mod), R(tA), scale=C.mod[:, G1 + dc, 0:1])
            S.stt(r[:, dc, 0:n], px[:, 0:n], ALPHA, tA[:, 0:n], ALU.mult, ALU.add, R(px, tA), R(r))
        layer_norm_fm(S, C, r, n, 0, lambda c: x1t[:, c, 0:n], lt)
        S.dma(x1d[:, :, o0:o0 + n], x1t[:, :, 0:n], R(x1t), R(x1d))
        for dc in range(8):
            S.act(Xf[:, dc, 0:n], x1t[:, dc, 0:n], AF.Identity, R(x1t, C.mod), R(Xf),
                  scale=C.mod[:, SC2 + dc, 0:1], bias=C.mod[:, SH2 + dc, 0:1])
        S.cp(XT[:, :, o0:o0 + n], Xf[:, :, 0:n], R(Xf), R(XT), eng="pool")
        router(S, C, Xf, n, o0, rw, rb, gateT, rt)
    S.reset(markA)
    chk(4)

    mw_in = D("mw_in", [32, 1024, 2048])
    mb_inT = D("mb_inT", [128, 32, 16])
    mw_out = D("mw_out", [32, 1024, 1024])
    mb_out = D("mb_out", [32, 1024])
    b_inT = S.carve("b_inT", [32, 16], F32)
    S.dma(b_inT[:], mb_inT[:], [], R(b_inT))
    S.ts(b_inT[:, :, 8:16], b_inT[:, :, 8:16], 1.0, None, ALU.add, None, R(b_inT), R(b_inT))
    p4 = S.mark()
    tblocks = [([(0, 512), (512, 512)], [0, 0]), ([(1024, 512), (1536, 512)], [0, 0])]
    for chunks, cols in tblocks:
        acc = S.carve("acc", [8, sum(n for _, n in chunks)], F32, nreg=8)
        m = S.mark()
        offs = moe_block(S, C, XT, gateT, chunks, mw_in, mw_out, b_inT, mb_out, acc)
        S.reset(m)
        ln2_out(S, C, acc, offs, chunks, cols, x1d, xout, None, None)
        S.reset(p4)
    S.finish(R(xout))


def build_fused():
    nc = bass.Bass("TRN2", target_bir_lowering=False)
    es = ExitStack()
    with es:
        S = Sched(nc, es)
        nochk = lambda n: None
        S.prefix = ""
        xall = S.dram("xall", [2 * 2176, 1024], F32, "Internal", nreg=len(XCHUNKS))

        def exchange(xown, row_lo, row_hi):
            for k, (r0, n) in enumerate(XCHUNKS):
                if row_lo <= r0 < row_hi:
                    S.op("pool", lambda e, r0=r0, n=n: e.collective_compute(
                        "AllGather", ALU.bypass, replica_groups=[[0, 1], [2, 3], [4, 5], [6, 7]],
                        ins=[xown[r0:r0 + n, :]], outs=[xall[2 * r0:2 * r0 + 2 * n, :]]),
                        [xown.k(t) for t in range(r0 // 128, (r0 + n) // 128)], [xall.k(k)], dma=True, inc=1)

        S.prefix = "a_"
        xown = _build(S, nc, nochk, fused=True, after_block=exchange)
        S.reset(0)
        S.prefix = "b_"
        _build1(S, nc, nochk, fused=True, xown=xown, xall=xall)
        S.emit()
    return nc


def consts():
    c = np.zeros((128, 1024), np.float32)
    c[:, 0:128] = np.eye(128)
    c[:, 128:256] = 1.0
    k = np.arange(128)[:, None]; l = np.arange(128)[None, :]
    c[:, 256:384] = (k <= l)
    c[:, 384:512] = (k >= l)
    c[:, 512:640] = np.where(k > l, -30000.0, 0.0)
    c[:, 640:768] = np.where(k < l, -30000.0, 0.0)
    c[0:32, 768:800] = np.eye(32)
    return c

def fm(v, nchunk):
    return np.ascontiguousarray(v.reshape(nchunk, 128).T)

def prep_l0(inp, b, half):
    f = np.float32
    flip = half == 1
    ctx_ = inp['ctx'][b][::-1] if flip else inp['ctx'][b]
    lat_ = inp['x'][b][::-1] if flip else inp['x'][b]
    m = {}
    m['xs'] = np.ascontiguousarray(np.concatenate([ctx_, lat_], 0), dtype=f)
    cT = np.zeros((128, 8, 2), f)
    cT[:, :, 0] = fm(inp['c'][b], 8); cT[:, :, 1] = fm(inp['c_ctx'], 8)
    m['cT'] = cT
    m['mod_w'] = np.ascontiguousarray(inp['mod_w'][0])
    m['mod_bT'] = fm(inp['mod_b'][0], 48)
    m['lnp'] = np.ascontiguousarray(np.stack([fm(inp['ln1_g'][0], 8), fm(inp['ln1_b'][0], 8), fm(inp['ln2_g'][0], 8), fm(inp['ln2_b'][0], 8)], 1))
    W = inp['ssd_w_in'][0]
    dA = 1 if flip else 0; dB = 1 - dA
    win = np.zeros((8, 1024, 776), f)
    convp = np.zeros((128, 8, 4, 6), f)
    ssdp = np.zeros((1, 160), f)
    cw = inp['ssd_conv_w'][0]; cb = inp['ssd_conv_b'][0]
    if flip: cw = cw[::-1]
    for g in range(8):
        win[g] = np.concatenate([W[:, 256*g:256*g+256], W[:, 2048+256*g:2048+256*g+256], W[:, 4096+128*g:4096+128*g+128],
                                 W[:, 5120+128*g:5120+128*g+128], W[:, 6144+dA*32+4*g:6144+dA*32+4*g+4], W[:, 6144+dB*32+4*g:6144+dB*32+4*g+4]], 1)
        for ch, c0 in enumerate([256*g, 256*g+128, 2048+128*g, 3072+128*g]):
            convp[:, g, ch, 0:5] = cw[:, c0:c0+128].T
            convp[:, g, ch, 5] = cb[c0:c0+128]
        ssdp[0, g*20:g*20+20] = np.concatenate([inp['ssd_dt_bias'][0][dA, 4*g:4*g+4], inp['ssd_dt_bias'][0][dB, 4*g:4*g+4],
                                               inp['ssd_a_log'][0][dA, 4*g:4*g+4], inp['ssd_a_log'][0][dB, 4*g:4*g+4], inp['ssd_d'][0][4*g:4*g+4]])
    m['win'] = win; m['convp'] = convp; m['ssdp'] = ssdp
    m['normw'] = fm(inp['ssd_norm_w'][0], 16)
    m['wout'] = np.ascontiguousarray(inp['ssd_w_out'][0])
    add_moe(m, inp, 0)
    m['cst'] = consts()
    return m

def add_moe(m, inp, i):
    f = np.float32
    m['rw'] = np.ascontiguousarray(inp['router_w'][i].reshape(8, 128, 32).transpose(1, 0, 2))
    m['rb'] = np.ascontiguousarray(inp['router_b'][i].reshape(1, 32))
    wi = inp['moe_w_in'][i]
    m['mw_in'] = np.ascontiguousarray(np.concatenate([wi[:, :, 0:512], wi[:, :, 1024:1536], wi[:, :, 512:1024], wi[:, :, 1536:2048]], 2))
    m['mb_inT'] = np.ascontiguousarray(inp['moe_b_in'][i].reshape(32, 16, 128).transpose(2, 0, 1))
    m['mw_out'] = np.ascontiguousarray(inp['moe_w_out'][i])
    m['mb_out'] = np.ascontiguousarray(inp['moe_b_out'][i])

def gather_l0(results, B=4):
    x1 = np.zeros((B, 4096, 1024), np.float32); ctx1 = np.zeros((B, 256, 1024), np.float32)
    for cid, r in enumerate(results):
        b, half = cid // 2, cid % 2
        o = r['xout']
        if half == 0:
            ctx1[b, 0:128] = o[0:128]; x1[b, 0:2048] = o[128:]
        else:
            ctx1[b, 128:256] = o[0:128][::-1]; x1[b, 2048:4096] = o[128:][::-1]
    return x1, ctx1


def rope_consts():
    f = np.float32
    t = np.arange(4096)
    row = (t // 64).astype(f); col = (t % 64).astype(f)
    inv = (f(10000.0) ** (-np.arange(16, dtype=f) / f(16))).astype(f)
    ang = np.stack([row[:, None] * inv, col[:, None] * inv], axis=1).astype(f)
    cos, sin = np.cos(ang).astype(f), np.sin(ang).astype(f)
    cosT = np.zeros((64, 4096), f); sinT = np.zeros((64, 4096), f)
    for a in range(2):
        for hf in range(2):
            cosT[a * 32 + hf * 16:a * 32 + hf * 16 + 16] = cos[:, a, :].T
            sinT[a * 32 + hf * 16:a * 32 + hf * 16 + 16] = sin[:, a, :].T
    rm = np.zeros((64, 64), f)
    for a in range(2):
        for k in range(16):
            i1 = a * 32 + k; i2 = a * 32 + 16 + k
            rm[i2, i1] = -1.0
            rm[i1, i2] = 1.0
    return cosT, sinT, rm

def prep_l1(inp, x1, ctx1, b, half, rc=None):
    f = np.float32
    cosT, sinT, rm = rc if rc is not None else rope_consts()
    own = slice(half * 2048, (half + 1) * 2048)
    oth = slice((1 - half) * 2048, (2 - half) * 2048)
    m = {}
    m['xs'] = np.ascontiguousarray(np.concatenate([x1[b, own], x1[b, oth], ctx1[b]], 0), dtype=f)
    cT = np.zeros((128, 8, 2), f)
    cT[:, :, 0] = fm(inp['c'][b], 8); cT[:, :, 1] = fm(inp['c_ctx'], 8)
    m['cT'] = cT
    m['mod_w'] = np.ascontiguousarray(inp['mod_w'][1])
    m['mod_bT'] = fm(inp['mod_b'][1], 48)
    m['lnp'] = np.ascontiguousarray(np.stack([fm(inp['ln1_g'][1], 8), fm(inp['ln1_b'][1], 8), fm(inp['ln2_g'][1], 8), fm(inp['ln2_b'][1], 8)], 1))
    m['wa'] = np.ascontiguousarray(inp['mla_w_a'][0])
    m['qnorm'] = fm(inp['mla_q_norm'][0], 3)
    m['kvnorm'] = fm(inp['mla_kv_norm'][0], 2)
    wq = inp['mla_w_qb'][0]
    m['wqb'] = np.ascontiguousarray(np.concatenate([wq[:, h * 192:h * 192 + 128] for h in range(8)] +
                                                  [wq[:, h * 192 + 128:h * 192 + 192] for h in range(8)], 1))
    wk = inp['mla_w_kvb'][0]
    m['wkvb'] = np.ascontiguousarray(np.concatenate([wk[:, h * 256:h * 256 + 128] for h in range(8)] +
                                                   [wk[:, h * 256 + 128:h * 256 + 256] for h in range(8)], 1))
    m['wo'] = np.ascontiguousarray(inp['mla_w_o'][0])
    m['ropeq'] = np.ascontiguousarray(np.stack([cosT[:, own], sinT[:, own]], 1))
    ck = np.concatenate([cosT[:, own], cosT[:, oth], np.ones((64, 256), f)], 1)
    sk = np.concatenate([sinT[:, own], sinT[:, oth], np.zeros((64, 256), f)], 1)
    m['ropek'] = np.ascontiguousarray(np.stack([ck, sk], 1))
    m['rm'] = rm
    add_moe(m, inp, 1)
    m['cst'] = consts()
    return m

def gather_l1(results, B=4):
    out = np.zeros((B, 4096, 1024), np.float32)
    for cid, r in enumerate(results):
        b, half = cid // 2, cid % 2
        out[b, half * 2048:(half + 1) * 2048] = r['xout']
    return out


def prep_fused(inp, b, half, rc):
    f = np.float32
    cosT, sinT, rm = rc
    m0 = prep_l0(inp, b, half)
    m = {("cst" if k == "cst" else "a_" + k): v for k, v in m0.items()}
    idx1 = 4095 - np.arange(2048)
    ck = np.ones((64, 4352), f)
    sk = np.zeros((64, 4352), f)
    for t, (r, mt) in enumerate(xall_tile_map()):
        if mt == 0:
            continue
        base = (mt - 1) * 128 + np.arange(128)
        lat = base if r == 0 else 4095 - base
        ck[:, t * 128:(t + 1) * 128] = cosT[:, lat]
        sk[:, t * 128:(t + 1) * 128] = sinT[:, lat]
    qi = np.arange(2048) if half == 0 else idx1
    m1 = {}
    cT = np.zeros((128, 8, 2), f)
    cT[:, :, 0] = fm(inp['c'][b], 8); cT[:, :, 1] = fm(inp['c_ctx'], 8)
    m1['cT'] = cT
    m1['mod_w'] = np.ascontiguousarray(inp['mod_w'][1])
    m1['mod_bT'] = fm(inp['mod_b'][1], 48)
    m1['lnp'] = np.ascontiguousarray(np.stack([fm(inp['ln1_g'][1], 8), fm(inp['ln1_b'][1], 8), fm(inp['ln2_g'][1], 8), fm(inp['ln2_b'][1], 8)], 1))
    m1['wa'] = np.ascontiguousarray(inp['mla_w_a'][0])
    m1['qnorm'] = fm(inp['mla_q_norm'][0], 3)
    m1['kvnorm'] = fm(inp['mla_kv_norm'][0], 2)
    wq = inp['mla_w_qb'][0]
    m1['wqb'] = np.ascontiguousarray(np.concatenate([wq[:, h * 192:h * 192 + 128] for h in range(8)] +
                                                   [wq[:, h * 192 + 128:h * 192 + 192] for h in range(8)], 1))
    wk = inp['mla_w_kvb'][0]
    m1['wkvb'] = np.ascontiguousarray(np.concatenate([wk[:, h * 256:h * 256 + 128] for h in range(8)] +
                                                    [wk[:, h * 256 + 128:h * 256 + 256] for h in range(8)], 1))
    m1['wo'] = np.ascontiguousarray(inp['mla_w_o'][0])
    m1['ropeq'] = np.ascontiguousarray(np.stack([cosT[:, qi], sinT[:, qi]], 1))
    m1['ropek'] = np.ascontiguousarray(np.stack([ck, sk], 1))
    m1['rm'] = rm
    add_moe(m1, inp, 1)
    for k, v in m1.items():
        m["b_" + k] = v
    return m

def gather_fused(results, B=4):
    out = np.zeros((B, 4096, 1024), np.float32)
    for cid, r in enumerate(results):
        b, half = cid // 2, cid % 2
        o = r['b_xout']
        if half == 0:
            out[b, 0:2048] = o
        else:
            out[b, 2048:4096] = o[::-1]
    return out


def kernel(**inputs):
    inp = {k: np.asarray(v) for k, v in inputs.items()}
    nc = build_fused()
    rc = rope_consts()
    maps = []
    for cid in range(8):
        m = prep_fused(inp, cid // 2, cid % 2, rc)
        maps.append({k: m[k] for k in nc._in_names})
    res = run_bass_kernel_spmd(nc, maps, core_ids=list(range(8)))
    return gather_fused(res.results)
```

```python
import numpy as np
import concourse.bass as bass
import concourse.mybir as mybir
from concourse.bass_utils import run_bass_kernel_spmd
from contextlib import ExitStack

F32 = mybir.dt.float32
BF16 = mybir.dt.bfloat16
U32 = mybir.dt.uint32
I32 = mybir.dt.int32
AF = mybir.ActivationFunctionType
ALU = mybir.AluOpType
AX = mybir.AxisListType


class Tile:
    def __init__(self, name, h, nreg=1):
        self.name = name
        self.h = h
        self.nreg = nreg

    def k(self, i=0):
        return (self.name, i)

    def all(self):
        return [(self.name, i) for i in range(self.nreg)]

    def __getitem__(self, idx):
        return self.h[idx]


class Sched:
    ENGS = ("pe", "act", "dve", "pool", "sp")

    def __init__(self, nc, es, n_dma_sems=8, sync_same=True):
        self.nc = nc
        self.es = es
        self.sync_same = sync_same
        self.q = {e: [] for e in self.ENGS}
        self.csem = {e: es.enter_context(nc.semaphore("c_" + e)) for e in self.ENGS}
        self.ccount = {e: 0 for e in self.ENGS}
        self.dq = ("sp", "pool", "act")
        self.dsems = {e: [es.enter_context(nc.semaphore("d_%s_%d" % (e, i))) for i in range(n_dma_sems)]
                      for e in self.dq}
        self.duse = {e: [0] * n_dma_sems for e in self.dq}
        self.dnext = {e: 0 for e in self.dq}
        self.lastw = {}
        self.readers = {}
        self.known = {e: {} for e in self.ENGS}
        self.ntile = 0

    def sb(self, name, shape, dtype, nreg=1):
        h = self.es.enter_context(self.nc.sbuf_tensor(name, list(shape), dtype))
        return Tile(name, h, nreg)

    def ps(self, name, shape, dtype, nreg=1):
        h = self.es.enter_context(self.nc.psum_tensor(name, list(shape), dtype))
        return Tile(name, h, nreg)

    def dram(self, name, shape, dtype, kind, nreg=1):
        name = getattr(self, "prefix", "") + name
        if not hasattr(self.nc, "_in_names"):
            self.nc._in_names = []
        if kind == "ExternalInput":
            self.nc._in_names.append(name)
        h = self.nc.dram_tensor(name, list(shape), dtype, kind=kind)
        return Tile(name, h.ap(), nreg)

    def arena(self, nfloats):
        self.ar = self.sb("arena", [128, nfloats], F32)
        self.ar_n = nfloats
        self.ar_off = 0
        self.ar_base = 0

    def carve(self, name, shape, dtype, nreg=1, parts=128):
        n = 1
        for s in shape:
            n *= s
        nf = n if dtype == F32 else (n + 1) // 2
        nf = (nf + 7) // 8 * 8
        assert self.ar_off + nf <= self.ar_n, ("arena overflow", name, self.ar_off, nf, self.ar_n)
        ap = self.ar.h[0:parts, self.ar_off:self.ar_off + nf]
        if dtype != F32:
            ap = ap.bitcast(dtype)
        ap = ap[:, 0:n]
        if len(shape) == 2:
            ap = ap.rearrange("p (a b) -> p a b", b=shape[1])
        elif len(shape) == 3:
            ap = ap.rearrange("p (a b c) -> p a b c", b=shape[1], c=shape[2])
        self.ar_off += nf
        self.ntile += 1
        return Tile("%s#%d" % (name, self.ntile), ap, nreg)

    def mark(self):
        return self.ar_off

    def reset(self, mark):
        self.barrier()
        self.ar_off = mark

    def barrier(self):
        allt = {}
        for e in self.ENGS:
            if self.ccount[e] > 0:
                allt[("c", e)] = self.ccount[e]
        for e in self.dq:
            for i, u in enumerate(self.duse[e]):
                if u > 0:
                    allt[("d", e, i)] = u
        self.pending = {e: dict(allt) for e in self.ENGS}

    def op(self, eng, fn, reads=(), writes=(), dma=False, same=None, inc=16):
        deps = {}
        pend = getattr(self, "pending", None)
        if pend and pend.get(eng):
            for sk, v in pend[eng].items():
                if deps.get(sk, 0) < v:
                    deps[sk] = v
            pend[eng] = None

        def add(tok):
            sk, v = tok
            if deps.get(sk, 0) < v:
                deps[sk] = v

        for k in reads:
            if k in self.lastw:
                add(self.lastw[k])
        for k in writes:
            if k in self.lastw:
                add(self.lastw[k])
            for sk, v in self.readers.get(k, {}).items():
                add((sk, v))
        if dma:
            i = self.dnext[eng]
            self.dnext[eng] = (i + 1) % len(self.dsems[eng])
            prev = self.duse[eng][i]
            self.duse[eng][i] = prev + inc
            tok = (("d", eng, i), prev + inc)
            if prev > 0:
                add((("d", eng, i), prev))
        else:
            self.ccount[eng] += 1
            tok = (("c", eng), self.ccount[eng])
        same = self.sync_same if same is None else same
        waits = []
        for sk, v in deps.items():
            if sk == ("c", eng) and not same:
                continue
            if self.known[eng].get(sk, 0) >= v:
                continue
            self.known[eng][sk] = v
            waits.append((sk, v))
        self.q[eng].append((waits, fn, tok, inc if dma else 1))
        for k in reads:
            r = self.readers.setdefault(k, {})
            if r.get(tok[0], 0) < tok[1]:
                r[tok[0]] = tok[1]
        for k in writes:
            self.lastw[k] = tok
            self.readers[k] = {}
        return tok

    def dma(self, out, in_, reads, writes, eng="sp", **kw):
        return self.op(eng, lambda e: e.dma_start(out=out, in_=in_, **kw), reads, writes, dma=True)

    def mm(self, out, lhsT, rhs, start, stop, reads, writes):
        return self.op("pe", lambda e: e.matmul(out, lhsT, rhs, start=start, stop=stop), reads, writes, same=False)

    def tr(self, out, in_, ident, reads, writes):
        return self.op("pe", lambda e: e.transpose(out, in_, ident), reads, writes, same=False)

    def act(self, out, in_, func, reads, writes, eng="act", **kw):
        return self.op(eng, lambda e: e.activation(out=out, in_=in_, func=func, **kw), reads, writes)

    def tt(self, out, in0, in1, op, reads, writes, eng="dve"):
        return self.op(eng, lambda e: e.tensor_tensor(out=out, in0=in0, in1=in1, op=op), reads, writes)

    def ts(self, out, in0, s1, s2, op0, op1, reads, writes, eng="dve", **kw):
        if op1 is None:
            return self.op(eng, lambda e: e.tensor_scalar(out=out, in0=in0, scalar1=s1, scalar2=None, op0=op0, **kw),
                           reads, writes)
        return self.op(eng, lambda e: e.tensor_scalar(out=out, in0=in0, scalar1=s1, scalar2=s2, op0=op0, op1=op1, **kw),
                       reads, writes)

    def cp(self, out, in_, reads, writes, eng="dve"):
        if eng == "act":
            return self.op(eng, lambda e: e.copy(out=out, in_=in_), reads, writes)
        return self.op(eng, lambda e: e.tensor_copy(out=out, in_=in_), reads, writes)

    def stt(self, out, in0, scalar, in1, op0, op1, reads, writes):
        return self.op("dve", lambda e: e.scalar_tensor_tensor(out=out, in0=in0, scalar=scalar, in1=in1, op0=op0, op1=op1),
                       reads, writes)

    def recip(self, out, in_, reads, writes):
        return self.op("dve", lambda e: e.reciprocal(out=out, in_=in_), reads, writes)

    def max8(self, out, in_, reads, writes):
        return self.op("dve", lambda e: e.max(out=out, in_=in_), reads, writes)

    def reduce(self, out, in_, axis, op, reads, writes):
        return self.op("dve", lambda e: e.tensor_reduce(out=out, in_=in_, axis=axis, op=op), reads, writes)

    def memset(self, out, val, writes, eng="dve"):
        return self.op(eng, lambda e: e.memset(out, val), (), writes)

    def _semh(self, sk):
        if sk[0] == "c":
            return self.csem[sk[1]]
        return self.dsems[sk[1]][sk[2]]

    def finish(self, out_keys):
        waits = []
        for k in out_keys:
            if k in self.lastw:
                waits.append(self.lastw[k])
        self.final_waits = waits

    def emit(self):
        nc = self.nc
        counts = {e: len(self.q[e]) for e in self.ENGS}
        nw = {e: sum(len(w[0]) for w in self.q[e]) for e in self.ENGS}
        print("SCHED instr counts", counts, "waits", nw, flush=True)
        with nc.Block() as block:
            def mk(engname):
                def body(e):
                    for waits, fn, tok, inc_ in self.q[engname]:
                        for sk, v in waits:
                            e.wait_ge(self._semh(sk), v)
                        ins = fn(e)
                        ins.then_inc(self._semh(tok[0]), inc_)
                    if engname == "sp":
                        done = {}
                        for sk, v in getattr(self, "final_waits", []):
                            if done.get(sk, 0) < v:
                                done[sk] = v
                        for sk, v in done.items():
                            e.wait_ge(self._semh(sk), v)
                return body

            block.tensor(mk("pe"))
            block.scalar(mk("act"))
            block.vector(mk("dve"))
            block.gpsimd(mk("pool"))
            block.sync(mk("sp"))


ALPHA = 4.0 ** 0.25
LN_EPS = 1e-5
SH1, SC1, G1, SH2, SC2, G2 = 0, 8, 16, 24, 32, 40


def R(*tiles):
    out = []
    for t in tiles:
        out += t.all()
    return out


class Ctx:
    pass


XCHUNKS = [(0, 512), (512, 512), (1024, 128), (1152, 512), (1664, 512)]


def xall_tile_map():
    out = []
    for r0, n in XCHUNKS:
        for r in range(2):
            for i in range(n // 128):
                out.append((r, r0 // 128 + i))
    return out


def setup_common(S, C, layer_has_ctx):
    if hasattr(S, "_common"):
        C.__dict__.update(S._common.__dict__)
        return
    S._common = C
    C.PS = [S.ps("ps%d" % i, [128, 512], F32) for i in range(8)]
    pre, S.prefix = getattr(S, "prefix", ""), ""
    C.cst = S.dram("cst", [128, 1024], F32, "ExternalInput")
    S.prefix = pre
    C.cs = S.sb("cs", [128, 1024], F32)
    S.dma(C.cs[:], C.cst[:], [], R(C.cs))
    C.ident = C.cs[:, 0:128]
    C.ones = C.cs[:, 128:256]
    C.U = [C.cs[:, 256:384], C.cs[:, 384:512]]
    C.NEG1 = [C.cs[:, 512:640], C.cs[:, 640:768]]
    C.eye32 = C.cs[0:32, 768:800]
    C.identbf = S.sb("identbf", [128, 128], BF16)
    S.cp(C.identbf[:], C.ident, R(C.cs), R(C.identbf))


def modulation(S, C, mod_w, mod_bT, cT):
    PS = C.PS
    m0 = S.mark()
    cs = S.carve("cTs", [8, 2], F32)
    S.dma(cs[:], cT[:], [], R(cs))
    csl = S.carve("csl", [8, 2], F32)
    S.act(csl[:], cs[:], AF.Silu, R(cs), R(csl))
    wb = [S.carve("modw%d" % i, [8, 512], F32) for i in range(2)]
    mb = S.carve("modb", [48], F32)
    S.dma(mb[:], mod_bT[:], [], R(mb))
    mwv = mod_w.h.rearrange("(kc p) f -> p kc f", p=128)
    for fg in range(12):
        w = wb[fg % 2]
        S.dma(w[:], mwv[:, :, fg * 512:(fg + 1) * 512], [], R(w))
        for fl in range(4):
            fc = fg * 4 + fl
            for kc in range(8):
                S.mm(PS[7][:, fc * 2:fc * 2 + 2], w[:, kc, fl * 128:(fl + 1) * 128], csl[:, kc, :], kc == 0, kc == 7,
                     R(w, csl), R(PS[7]))
    S.tt(C.mod[:], PS[7][:, 0:96].rearrange("p (f j) -> p f j", j=2), mb[:].unsqueeze(2).to_broadcast([128, 48, 2]),
         ALU.add, R(PS[7], mb), R(C.mod))
    for base in (SC1, SC2):
        S.ts(C.mod[:, base:base + 8, :], C.mod[:, base:base + 8, :], 1.0, None, ALU.add, None, R(C.mod), R(C.mod))
    S.reset(m0)


def layer_norm_fm(S, C, r, n, gi, out, tmp):
    PS = C.PS
    sq, mean, msq, var, rs = tmp["sq"], tmp["mean"], tmp["msq"], tmp["var"], tmp["rs"]
    for c in range(8):
        S.mm(PS[6][:, 0:n], C.ones, r[:, c, 0:n], c == 0, c == 7, R(r, C.cs), R(PS[6]))
    S.tt(sq[:, :, 0:n], r[:, :, 0:n], r[:, :, 0:n], ALU.mult, R(r), R(sq), eng="pool")
    for c in range(8):
        S.mm(PS[7][:, 0:n], C.ones, sq[:, c, 0:n], c == 0, c == 7, R(sq, C.cs), R(PS[7]))
    S.act(mean[:, 0:n], PS[6][:, 0:n], AF.Identity, R(PS[6]), R(mean), scale=1.0 / 1024)
    S.tt(msq[:, 0:n], mean[:, 0:n], mean[:, 0:n], ALU.mult, R(mean), R(msq), eng="pool")
    S.stt(var[:, 0:n], PS[7][:, 0:n], 1.0 / 1024, msq[:, 0:n], ALU.mult, ALU.subtract, R(PS[7], msq), R(var))
    S.act(var[:, 0:n], var[:, 0:n], AF.Sqrt, R(var), R(var), bias=LN_EPS)
    S.recip(rs[:, 0:n], var[:, 0:n], R(var), R(rs))
    for c in range(8):
        S.tt(r[:, c, 0:n], r[:, c, 0:n], mean[:, 0:n], ALU.subtract, R(r, mean), R(r))
        S.tt(r[:, c, 0:n], r[:, c, 0:n], rs[:, 0:n], ALU.mult, R(r, rs), R(r), eng="pool")
        S.act(out(c), r[:, c, 0:n], AF.Identity, R(r, C.lnp), tmp["outkeys"],
              scale=C.lnp[:, gi, c:c + 1], bias=C.lnp[:, gi + 1, c:c + 1])


def ln_tmp(S, nmax, outkeys):
    return {"sq": S.carve("lnsq", [8, nmax], F32), "mean": S.carve("lnmean", [nmax], F32),
            "msq": S.carve("lnmsq", [nmax], F32), "var": S.carve("lnvar", [nmax], F32),
            "rs": S.carve("lnrs", [nmax], F32), "outkeys": outkeys}


def router(S, C, Xf, n, o0, rw, rb, gateT, rt):
    PS = C.PS
    for i in range(n // 128):
        for kc in range(8):
            S.mm(PS[5][:, 0:32], Xf[:, kc, i * 128:(i + 1) * 128], rw[:, kc, :], kc == 0, kc == 7, R(Xf, rw), R(PS[5]))
        lg, m8, msk, ex, sm = rt["lg"], rt["m8"], rt["msk"], rt["ex"], rt["sm"]
        S.tt(lg[:], PS[5][:, 0:32], rb[:], ALU.add, R(PS[5], rb), R(lg))
        S.max8(m8[:], lg[:], R(lg), R(m8))
        S.ts(msk[:], lg[:], m8[:, 3:4], None, ALU.is_ge, None, R(lg, m8), R(msk))
        S.ts(sm[:, 0:1], m8[:, 0:1], -1.0, None, ALU.mult, None, R(m8), R(sm))
        S.act(ex[:], lg[:], AF.Exp, R(lg, sm), R(ex), bias=sm[:, 0:1])
        S.tt(ex[:], ex[:], msk[:], ALU.mult, R(ex, msk), R(ex))
        S.reduce(sm[:, 1:2], ex[:], AX.X, ALU.add, R(ex), R(sm))
        S.recip(sm[:, 2:3], sm[:, 1:2], R(sm), R(sm))
        S.ts(ex[:], ex[:], sm[:, 2:3], None, ALU.mult, None, R(ex, sm), R(ex))
        S.tr(PS[5][0:32, 128:256], ex[:], C.ident, R(ex, C.cs), R(PS[5]))
        S.cp(gateT[0:32, o0 + i * 128:o0 + (i + 1) * 128], PS[5][0:32, 128:256], R(PS[5]), R(gateT), eng="act")


def phase3(S, C, subs, nkc, wmat, rhs_of, load_rows, x1d, XT, gateT, rw, rb, W=512, nbuf=1):
    PS = C.PS
    sets = []
    for i in range(nbuf):
        x1t = S.carve("x1t%d" % i, [8, W], F32)
        sets.append({"xrw": S.carve("xrw%d" % i, [W // 128, 1024], F32), "r": S.carve("r1_%d" % i, [8, W], F32), "x1t": x1t,
                     "Xf": S.carve("Xf%d" % i, [8, W], F32), "tA": S.carve("tA%d" % i, [W], F32), "lt": ln_tmp(S, W, R(x1t))})
    rt = router_tmp(S)
    for si, (o0, n, col) in enumerate(subs):
        st_ = sets[si % nbuf]
        xrw, r, x1t, Xf, tA, lt = st_["xrw"], st_["r"], st_["x1t"], st_["Xf"], st_["tA"], st_["lt"]
        rhs, rkeys = rhs_of(si, o0, n)
        load_rows(o0, n, xrw)
        for dc in range(8):
            py, px = PS[dc % 2], PS[2 + dc % 2]
            for kc in range(nkc):
                S.mm(py[:, 0:n], wmat[:, kc, dc * 128:(dc + 1) * 128], rhs(kc), kc == 0, kc == nkc - 1, R(wmat) + rkeys, R(py))
            for i in range(n // 128):
                S.tr(px[:, i * 128:(i + 1) * 128], xrw[:, i, dc * 128:(dc + 1) * 128], C.ident, R(xrw, C.cs), R(px))
            S.act(tA[:, 0:n], py[:, 0:n], AF.Identity, R(py, C.mod), R(tA), scale=C.mod[:, G1 + dc, col:col + 1])
            S.stt(r[:, dc, 0:n], px[:, 0:n], ALPHA, tA[:, 0:n], ALU.mult, ALU.add, R(px, tA), R(r))
        layer_norm_fm(S, C, r, n, 0, lambda c, x1t=x1t, n=n: x1t[:, c, 0:n], lt)
        S.dma(x1d[:, :, o0:o0 + n], x1t[:, :, 0:n], R(x1t), R(x1d))
        for dc in range(8):
            S.act(Xf[:, dc, 0:n], x1t[:, dc, 0:n], AF.Identity, R(x1t, C.mod), R(Xf),
                  scale=C.mod[:, SC2 + dc, col:col + 1], bias=C.mod[:, SH2 + dc, col:col + 1])
        S.cp(XT[:, :, o0:o0 + n], Xf[:, :, 0:n], R(Xf), R(XT), eng="pool")
        router(S, C, Xf, n, o0, rw, rb, gateT, rt)


def router_tmp(S):
    return {"lg": S.carve("rlg", [32], F32), "m8": S.carve("rm8", [8], F32), "msk": S.carve("rmsk", [32], F32),
            "ex": S.carve("rex", [32], F32), "sm": S.carve("rsm", [4], F32)}


def moe_block(S, C, XT, gateT, chunks, w_in, w_out, b_inT, b_out, acc):
    PS = C.PS
    tot = sum(n for _, n in chunks)
    offs = []
    o = 0
    for lo, n in chunks:
        offs.append(o)
        o += n
    wi = S.carve("wi", [8, 2048], BF16, nreg=2)
    wo = S.carve("wo", [8, 1024], BF16)
    actT = S.carve("actT", [8, tot], BF16, nreg=8)
    Gbs = [S.carve("Gb%d" % i, [tot], F32) for i in range(2)]
    ge = S.carve("ge", [tot], F32, parts=32)
    gT_hi = S.carve("gT_hi", [tot], BF16, parts=32)
    gT_lo = S.carve("gT_lo", [tot], BF16, parts=32)
    eyebf = S.carve("eyebf", [32], BF16, parts=32)
    S.cp(eyebf[:], C.eye32, R(C.cs), R(eyebf))
    for ci, (lo, n) in enumerate(chunks):
        sl = slice(offs[ci], offs[ci] + n)
        S.cp(gT_hi[0:32, sl], gateT[0:32, lo:lo + n], R(gateT), R(gT_hi))
        S.tt(ge[0:32, sl], gateT[0:32, lo:lo + n], gT_hi[0:32, sl], ALU.subtract, R(gateT, gT_hi), R(ge))
        S.cp(gT_lo[0:32, sl], ge[0:32, sl], R(ge), R(gT_lo))
    bo = S.carve("bo", [1024], F32, parts=32)
    S.dma(bo[:], b_out[:], [], R(bo))
    tg = [S.carve("tg%d" % i, [512], F32) for i in range(2)]
    tsg = [S.carve("tsg%d" % i, [512], F32) for i in range(2)]
    tl = [S.carve("tl%d" % i, [512], F32) for i in range(2)]
    it = 0
    for dc in range(8):
        for ci, (lo, n) in enumerate(chunks):
            p = PS[4 + it % 2]
            it += 1
            S.mm(p[:, 0:n], bo[:, dc * 128:(dc + 1) * 128], gateT[0:32, lo:lo + n], True, True, R(bo, gateT), R(p))
            S.cp(acc[:, dc, offs[ci]:offs[ci] + n], p[:, 0:n], R(p), [acc.k(dc)], eng="act")
    wiv = w_in.h.rearrange("e (kc p) f -> e p kc f", p=128)
    wov = w_out.h.rearrange("e (kc p) f -> e p kc f", p=128)

    def load_wi(e, h):
        S.dma(wi[:, :, h * 1024:(h + 1) * 1024], wiv[e, :, :, h * 1024:(h + 1) * 1024], [], [wi.k(h)], eng="pool")

    def prep_gate(e):
        Gb_ = Gbs[e % 2]
        sel = eyebf[0:32, e:e + 1].to_broadcast([32, 128])
        for ci, (lo, n) in enumerate(chunks):
            sl = slice(offs[ci], offs[ci] + n)
            pg = PS[6 + ci % 2]
            S.mm(pg[:, 0:n], sel, gT_hi[0:32, sl], True, False, R(gT_hi, eyebf), R(pg))
            S.mm(pg[:, 0:n], sel, gT_lo[0:32, sl], False, True, R(gT_lo, eyebf), R(pg))
            S.cp(Gb_[:, sl], pg[:, 0:n], R(pg), R(Gb_), eng="act")

    load_wi(0, 0)
    load_wi(0, 1)
    prep_gate(0)
    it = 0
    for e in range(32):
        Gb = Gbs[e % 2]
        S.dma(wo[:], wov[e], [], R(wo), eng="pool")
        for j in range(8):
            hh, jj = j // 4, j % 4
            cg = hh * 1024 + jj * 128
            cl = hh * 1024 + 512 + jj * 128
            for ci, (lo, n) in enumerate(chunks):
                pa, pb = (PS[0], PS[1]) if it % 2 == 0 else (PS[2], PS[3])
                q = it % 2
                it += 1
                for kc in range(8):
                    S.mm(pa[:, 0:n], wi[:, kc, cg:cg + 128], XT[:, kc, lo:lo + n], kc == 0, kc == 7,
                         [wi.k(hh)] + R(XT), R(pa))
                for kc in range(8):
                    S.mm(pb[:, 0:n], wi[:, kc, cl:cl + 128], XT[:, kc, lo:lo + n], kc == 0, kc == 7,
                         [wi.k(hh)] + R(XT), R(pb))
                g, sg, l = tg[q], tsg[q], tl[q]
                S.ts(g[:, 0:n], pa[:, 0:n], b_inT[:, e, j:j + 1], 7.0, ALU.add, ALU.min, R(pa, b_inT), R(g))
                S.act(sg[:, 0:n], g[:, 0:n], AF.Sigmoid, R(g), R(sg), scale=1.702)
                S.ts(l[:, 0:n], pb[:, 0:n], b_inT[:, e, 8 + j:9 + j], 8.0, ALU.add, ALU.min, R(pb, b_inT), R(l))
                S.stt(l[:, 0:n], l[:, 0:n], -6.0, Gb[:, offs[ci]:offs[ci] + n], ALU.max, ALU.mult, R(l, Gb), R(l))
                S.tt(g[:, 0:n], g[:, 0:n], sg[:, 0:n], ALU.mult, R(g, sg), R(g), eng="pool")
                S.tt(actT[:, j, offs[ci]:offs[ci] + n], g[:, 0:n], l[:, 0:n], ALU.mult, R(g, l), [actT.k(j)])
            if e + 1 < 32 and j in (3, 7):
                load_wi(e + 1, j // 4)
            if e + 1 < 32 and j == 5:
                prep_gate(e + 1)
        for dc in range(8):
            for ci, (lo, n) in enumerate(chunks):
                p = PS[4 + it % 2]
                it += 1
                for j in range(8):
                    S.mm(p[:, 0:n], wo[:, j, dc * 128:(dc + 1) * 128], actT[:, j, offs[ci]:offs[ci] + n], j == 0, j == 7,
                         R(wo) + [actT.k(j)], R(p))
                S.tt(acc[:, dc, offs[ci]:offs[ci] + n], acc[:, dc, offs[ci]:offs[ci] + n], p[:, 0:n], ALU.add,
                     R(p) + [acc.k(dc)], [acc.k(dc)])
    return offs


def ln2_out(S, C, acc, offs, chunks, cols, x1d, xout, tmp, mk):
    PS = C.PS
    W = 256
    sets = []
    for i in range(2):
        x2 = S.carve("x2_%d" % i, [8, W], F32)
        sets.append({"x1b": S.carve("x1b%d" % i, [8, W], F32), "r": S.carve("r2_%d" % i, [8, W], F32), "x2": x2,
                     "tA": S.carve("tA2_%d" % i, [W], F32), "lt": ln_tmp(S, W, R(x2))})
    orow = [S.carve("orow%d" % i, [1024], F32) for i in range(2)]
    subs = []
    for ci, (lo, n) in enumerate(chunks):
        for s0 in range(0, n, W):
            subs.append((lo + s0, min(W, n - s0), offs[ci] + s0, cols[ci]))
    it = 0
    for si, (lo, n, ao, col) in enumerate(subs):
        st_ = sets[si % 2]
        x1b, r, x2, tA, lt = st_["x1b"], st_["r"], st_["x2"], st_["tA"], st_["lt"]
        S.dma(x1b[:, :, 0:n], x1d[:, :, lo:lo + n], R(x1d), R(x1b))
        for dc in range(8):
            S.act(tA[:, 0:n], acc[:, dc, ao:ao + n], AF.Identity, [acc.k(dc)] + R(C.mod), R(tA),
                  scale=C.mod[:, G2 + dc, col:col + 1])
            S.stt(r[:, dc, 0:n], x1b[:, dc, 0:n], ALPHA, tA[:, 0:n], ALU.mult, ALU.add, R(x1b, tA), R(r))
        layer_norm_fm(S, C, r, n, 2, lambda c, x2=x2, n=n: x2[:, c, 0:n], lt)
        for i in range(n // 128):
            pp = (PS[0], PS[1]) if it % 2 == 0 else (PS[2], PS[3])
            for dc in range(8):
                p = pp[dc // 4]
                S.tr(p[:, (dc % 4) * 128:(dc % 4 + 1) * 128], x2[:, dc, i * 128:(i + 1) * 128], C.ident, R(x2, C.cs), R(p))
            ot = orow[it % 2]
            it += 1
            S.cp(ot[:, 0:512], pp[0][:, :], R(pp[0]), R(ot), eng="act")
            S.cp(ot[:, 512:1024], pp[1][:, :], R(pp[1]), R(ot))
            okey = [xout.k((lo + i * 128) // 128)] if xout.nreg > 1 else R(xout)
            S.dma(xout[lo + i * 128:lo + (i + 1) * 128, :], ot[:], R(ot), okey)


NT = 34
TOK = 4352
OWN = [0] + list(range(2, 18))
NOWN = 2176


def own_segments(t0, t1):
    segs = []
    for lo, hi, base in ((0, 128, 0), (256, 2304, 128)):
        a, b = max(t0, lo), min(t1, hi)
        if a < b:
            segs.append((a, b - a, base + a - lo))
    return segs


class _Stop(Exception):
    pass


def build_l0(debug=False):
    import os
    stop = int(os.environ.get("STOP", "99"))

    def chk(n):
        if stop == n:
            raise _Stop()

    nc = bass.Bass("TRN2", target_bir_lowering=False)
    es = ExitStack()
    with es:
      S = Sched(nc, es)
      try:
        _build(S, nc, chk)
      except _Stop:
        print("STOPPED at", stop)
        S.finish([])
      S.emit()
    return nc


def _build(S, nc, chk, fused=False, after_block=None):
    if True:
        C = Ctx()
        D = lambda name, shape, dt=F32, kind="ExternalInput": S.dram(name, shape, dt, kind)
        xs = D("xs", [TOK, 1024])
        cT = D("cT", [128, 8, 2])
        mod_w = D("mod_w", [1024, 6144])
        mod_bT = D("mod_bT", [128, 48])
        lnp_d = D("lnp", [128, 4, 8])
        win = D("win", [8, 1024, 776])
        convp_d = D("convp", [128, 8, 4, 6])
        ssdp_d = D("ssdp", [1, 160])
        normw_d = D("normw", [128, 16])
        wout_d = D("wout", [2048, 1024])
        rw_d = D("rw", [128, 8, 32])
        rb_d = D("rb", [1, 32])
        xout = S.dram("xout", [NOWN, 1024], F32, "Internal" if fused else "ExternalOutput", nreg=17 if fused else 1)
        hTd = D("hTd", [128, 8, TOK], BF16, "Internal")
        ygd = D("ygd", [128, 16, NOWN], BF16, "Internal")
        x1d = D("x1d", [128, 8, NOWN], F32, "Internal")
        setup_common(S, C, True)
        PS = C.PS
        if not hasattr(S, "ar"):
            S.arena(49800)
        C.mod = S.carve("mod", [48, 2], F32)
        C.lnp = S.carve("lnp", [4, 8], F32)
        S.dma(C.lnp[:], lnp_d[:], [], R(C.lnp))
        modulation(S, C, mod_w, mod_bT, cT)
        base = S.mark()
        chk(0)

        xr = [S.carve("xr%d" % i, [1024], F32) for i in range(2)]
        hb = [S.carve("hb%d" % i, [8, 512], BF16) for i in range(2)]
        for blk in range(9):
            t0 = blk * 4
            nt = min(4, NT - t0)
            h = hb[blk % 2]
            for i in range(nt):
                t = t0 + i
                col = 1 if t < 2 else 0
                x = xr[t % 2]
                S.dma(x[:], xs[t * 128:(t + 1) * 128, :], [], R(x))
                for kc in range(8):
                    p = PS[kc // 4 + 2 * (t % 2)]
                    S.tr(p[:, (kc % 4) * 128:(kc % 4 + 1) * 128], x[:, kc * 128:(kc + 1) * 128], C.ident, R(x, C.cs), R(p))
                for kc in range(8):
                    p = PS[kc // 4 + 2 * (t % 2)]
                    S.act(h[:, kc, i * 128:(i + 1) * 128], p[:, (kc % 4) * 128:(kc % 4 + 1) * 128], AF.Identity,
                          R(p, C.mod), R(h), scale=C.mod[:, SC1 + kc, col:col + 1], bias=C.mod[:, SH1 + kc, col:col + 1])
            S.dma(hTd[:, :, t0 * 128:(t0 + nt) * 128], h[:, :, 0:nt * 128], R(h), R(hTd))
        S.reset(base)
        chk(1)

        convp = S.carve("convp", [8, 4, 6], F32)
        S.dma(convp[:], convp_d[:], [], R(convp))
        ssdp = S.carve("ssdp", [8, 20], F32)
        S.dma(ssdp[:], ssdp_d.h.partition_broadcast(128).rearrange("p o (g k) -> p (o g) k", k=20), [], R(ssdp))
        normw = S.carve("normw", [16], F32)
        S.dma(normw[:], normw_d[:], [], R(normw))
        wg = [S.carve("wg%d" % i, [8, 776], BF16) for i in range(2)]
        hblk = [S.carve("hblk%d" % i, [8, 512], BF16) for i in range(2)]
        raw = [S.carve("raw%d" % i, [4358], BF16) for i in range(2)]
        for r_ in raw:
            S.memset(r_[:], 0.0, R(r_))
        dgw = S.carve("dgw", [5, 128], BF16)
        fm = S.carve("fm", [TOK], BF16)
        bT = S.carve("bT", [TOK], BF16)
        cTt = S.carve("cTt", [2304], BF16)
        zT = S.carve("zT", [2, NOWN], BF16)
        xtok = S.carve("xtok", [NT, 256], BF16)
        btok = S.carve("btok", [NT, 128], BF16)
        yacc = S.carve("yacc", [17, 256], F32, nreg=17)
        ygs = S.carve("ygs", [2, NOWN], BF16)
        dtv = S.carve("dtv", [2, NT, 4], F32)
        dte = S.carve("dte", [2, NT, 4], F32)
        lndt = S.carve("lndt", [2, NT, 4], F32)
        la = S.carve("la", [2, NT, 4], F32)
        biasL = S.carve("biasL", [2, NT, 4], F32)
        ecum = S.carve("ecum", [2, NT, 4], F32)
        wdec = S.carve("wdec", [2, NT, 4], F32)
        cdec = S.carve("cdec", [2, NT, 4], F32)
        aneg = S.carve("aneg", [8], F32)
        cumS = S.carve("cumS", [2, NT, 4], F32)
        hl_t = S.carve("hl_t", [2, NT, 4], F32)
        cum_hi = S.carve("cum_hi", [2, NT, 4], BF16)
        cum_lo = S.carve("cum_lo", [2, NT, 4], BF16)
        bl_hi = S.carve("bl_hi", [2, NT, 4], BF16)
        bl_lo = S.carve("bl_lo", [2, NT, 4], BF16)
        NEG4 = [S.carve("NEG4%d" % i, [4, 128], BF16) for i in range(2)]
        for d in range(2):
            S.cp(NEG4[d][:], C.NEG1[d].unsqueeze(1).to_broadcast([128, 4, 128]), R(C.cs), R(NEG4[d]))
        Lp = [S.carve("Lp%d" % i, [4, 128], F32) for i in range(2)]
        Mp = [S.carve("Mp%d" % i, [4, 128], BF16) for i in range(2)]
        t1 = [S.carve("t1%d" % i, [4, 64], F32) for i in range(2)]
        t2 = [S.carve("t2%d" % i, [4, 64], F32) for i in range(2)]
        xdec = [S.carve("xdec%d" % i, [4, 64], BF16) for i in range(2)]
        st = [S.carve("st%d" % i, [4, 64], F32) for i in range(2)]
        stbf = [S.carve("stbf%d" % i, [4, 64], BF16) for i in range(2)]
        yz = S.carve("yz", [2, 512], F32)
        sqg = S.carve("sqg", [2, 512], F32)
        rsg = S.carve("rsg", [512], F32)
        hv = hTd.h
        blocks = [(b * 512, min(512, TOK - b * 512)) for b in range(9)]
        orderA = list(range(NT))
        orderB = [1, 0] + list(range(NT - 1, 1, -1))
        flat = lambda t: t[:].rearrange("p a b -> p (a b)")

        for g in range(8):
            w = wg[g % 2]
            S.dma(w[:], win.h[g].rearrange("(kc p) f -> p kc f", p=128), [], R(w), eng="pool")

            def inproj_pass(chunks, evac, with_dt):
                for bi, (b0, n) in enumerate(blocks):
                    hbk = hblk[bi % 2]
                    S.dma(hbk[:, :, 0:n], hv[:, :, b0:b0 + n], R(hTd), R(hbk))
                    for ci, off in enumerate(chunks):
                        p = PS[ci % 2]
                        for kc in range(8):
                            S.mm(p[:, 0:n], w[:, kc, off:off + 128], hbk[:, kc, 0:n], kc == 0, kc == 7, R(w, hbk), R(p))
                        evac(ci, p, b0, n)
                    if with_dt:
                        for i in range(n // 128):
                            t = b0 // 128 + i
                            for kc in range(8):
                                S.mm(PS[2][:, t * 8:(t + 1) * 8], hbk[:, kc, i * 128:(i + 1) * 128], w[:, kc, 768:776],
                                     kc == 0, kc == 7, R(w, hbk), R(PS[2]))

            def evac_raw(ci, p, b0, n):
                r_ = raw[ci]
                for lo, hi, sh in ((0, 256, 2), (256, TOK, 4)):
                    a, b = max(b0, lo), min(b0 + n, hi)
                    if a < b:
                        S.cp(r_[:, a + sh:b + sh], p[:, a - b0:b - b0], R(p), R(r_), eng="act")

            def conv_silu(slot, ch, dst, limit=TOK):
                r_ = raw[slot]
                for j in range(5):
                    S.ts(dgw[:, j, :], C.ident, convp[:, g, ch, j:j + 1], None, ALU.mult, None, R(C.cs, convp), R(dgw))
                segs = [(0, 256, 0)] + [(256 + 512 * k, 512, 258 + 512 * k) for k in range(8)]
                segs = [sg for sg in segs if sg[0] < limit]
                for si, (olo, n, rlo) in enumerate(segs):
                    p = PS[si % 2]
                    for j in range(5):
                        S.mm(p[:, 0:n], dgw[:, j, :], r_[:, rlo + j:rlo + j + n], j == 0, j == 4, R(dgw, r_), R(p))
                    S.act(dst[:, olo:olo + n], p[:, 0:n], AF.Silu, R(p, convp), R(dst), bias=convp[:, g, ch, 5:6])

            if g == 0: chk(20)
            inproj_pass([256, 384], evac_raw, False)
            if g == 0: chk(21)
            for xi in range(2):
                conv_silu(xi, xi, fm)
                for t0 in range(0, NT, 4):
                    nt = min(4, NT - t0)
                    pb = PS[3 + (t0 // 4) % 2][:, 0:256].bitcast(BF16)
                    pk = PS[3 + (t0 // 4) % 2]
                    for i in range(nt):
                        S.tr(pb[:, i * 128:(i + 1) * 128], fm[:, (t0 + i) * 128:(t0 + i + 1) * 128], C.identbf[:],
                             R(fm, C.identbf), R(pk))
                    S.cp(xtok[:, t0:t0 + nt, xi * 128:(xi + 1) * 128],
                         pb[:, 0:nt * 128].rearrange("p (a b) -> p a b", b=128), R(pk), R(xtok), eng="act")
            if g == 0: chk(22)
            inproj_pass([512, 640], evac_raw, False)
            conv_silu(0, 2, bT)
            conv_silu(1, 3, cTt, 2304)
            for t0 in range(0, NT, 4):
                nt = min(4, NT - t0)
                pb = PS[3 + (t0 // 4) % 2][:, 0:256].bitcast(BF16)
                pk = PS[3 + (t0 // 4) % 2]
                for i in range(nt):
                    S.tr(pb[:, i * 128:(i + 1) * 128], bT[:, (t0 + i) * 128:(t0 + i + 1) * 128], C.identbf[:],
                         R(bT, C.identbf), R(pk))
                S.cp(btok[:, t0:t0 + nt, :], pb[:, 0:nt * 128].rearrange("p (a b) -> p a b", b=128), R(pk), R(btok), eng="act")

            if g == 0: chk(23)
            def evac_z(ci, p, b0, n):
                for a, m, o in own_segments(b0, b0 + n):
                    S.act(zT[:, ci, o:o + m], p[:, a - b0:a - b0 + m], AF.Silu, R(p), R(zT))

            inproj_pass([0, 128], evac_z, True)
            if g == 0: chk(24)
            dtb = ssdp[:, g, 0:8].rearrange("p (d h) -> p d h", h=4).unsqueeze(2).to_broadcast([128, 2, NT, 4])
            if g == 0: chk(240)
            S.tt(dtv[:], PS[2][:, 0:NT * 8].rearrange("p (c d h) -> p d c h", d=2, h=4), dtb, ALU.add, R(PS[2], ssdp), R(dtv))
            if g == 0: chk(241)
            S.act(dte[:], dtv[:], AF.Exp, R(dtv), R(dte))
            if g == 0: chk(242)
            S.act(dtv[:], dte[:], AF.Ln, R(dte), R(dtv), bias=1.0)
            if g == 0: chk(243)
            S.act(lndt[:], dtv[:], AF.Ln, R(dtv), R(lndt))
            if g == 0: chk(244)
            S.act(aneg[:], ssdp[:, g, 8:16], AF.Exp, R(ssdp), R(aneg))
            if g == 0: chk(245)
            S.ts(aneg[:], aneg[:], -1.0, None, ALU.mult, None, R(aneg), R(aneg))
            if g == 0: chk(246)
            S.tt(la[:], dtv[:], aneg[:].rearrange("p (d h) -> p d h", h=4).unsqueeze(2).to_broadcast([128, 2, NT, 4]),
                 ALU.mult, R(dtv, aneg), R(la))
            laf = la[:].rearrange("p d c h -> p (d c h)")
            if g == 0: chk(247)
            for d in range(2):
                S.mm(PS[5][:, d * 136:(d + 1) * 136], C.U[d], laf[:, d * 136:(d + 1) * 136], True, True, R(la, C.cs), R(PS[5]))
            if g == 0: chk(248)
            S.mm(PS[6][:, 0:272], C.ones, laf, True, True, R(la, C.cs), R(PS[6]))
            fl = lambda t: t[:].rearrange("p d c h -> p (d c h)")
            if g == 0: chk(249)
            S.tt(fl(biasL), fl(lndt), PS[5][:, 0:272], ALU.subtract, R(lndt, PS[5]), R(biasL))
            if g == 0: chk(250)
            S.cp(fl(cumS), PS[5][:, 0:272], R(PS[5]), R(cumS))
            S.act(fl(ecum), fl(cumS), AF.Exp, R(cumS), R(ecum))
            for src, hi, lo in ((cumS, cum_hi, cum_lo), (biasL, bl_hi, bl_lo)):
                S.cp(fl(hi), fl(src), R(src), R(hi))
                S.tt(fl(hl_t), fl(src), fl(hi), ALU.subtract, R(src, hi), R(hl_t))
                S.cp(fl(lo), fl(hl_t), R(hl_t), R(lo))
            if g == 0: chk(251)
            S.tt(fl(wdec), fl(biasL), PS[6][:, 0:272], ALU.add, R(biasL, PS[6]), R(wdec))
            if g == 0: chk(252)
            S.act(fl(wdec), fl(wdec), AF.Exp, R(wdec), R(wdec))
            if g == 0: chk(253)
            S.cp(fl(cdec), PS[6][:, 0:272], R(PS[6]), R(cdec))
            S.act(fl(cdec), fl(cdec), AF.Exp, R(cdec), R(cdec))

            if g == 0: chk(25)
            Dh = ssdp[:, g, 16:20].unsqueeze(2).to_broadcast([128, 4, 64])
            for oc, c in enumerate(OWN):
                S.tt(yacc[:, oc, :].rearrange("p (h q) -> p h q", q=64), xtok[:, c, :].rearrange("p (h q) -> p h q", q=64),
                     Dh, ALU.mult, R(xtok, ssdp), [yacc.k(oc)], eng="pool")
            for d in range(2):
                S.memset(st[d][:], 0.0, R(st[d]))
                S.memset(stbf[d][:], 0.0, R(stbf[d]))
            step = 0
            for i in range(NT):
                for d, order in ((0, orderA), (1, orderB)):
                    c = order[i]
                    par = step % 2
                    step += 1
                    tc0 = c * 128
                    own = c in OWN
                    pCB, pseg, pY = PS[0], PS[1 + par], PS[3 + par]
                    if own:
                        oc = OWN.index(c)
                        S.mm(pseg[:, :], C.identbf[:], flat(NEG4[d]), True, False, R(NEG4[d], C.identbf), R(pseg))
                        for h in range(4):
                            ps_h = pseg[:, h * 128:(h + 1) * 128]
                            for ti, t_ in enumerate((cum_hi, cum_lo)):
                                S.mm(ps_h, t_[:, d, c, h:h + 1].to_broadcast([128, 128]), C.identbf[:], False, False,
                                     R(t_, C.identbf), R(pseg))
                            for ti, t_ in enumerate((bl_hi, bl_lo)):
                                S.mm(ps_h, C.identbf[:], t_[:, d, c, h:h + 1].to_broadcast([128, 128]), False,
                                     h == 3 and ti == 1, R(t_, C.identbf), R(pseg))
                        S.mm(pCB[:, 0:128], bT[:, tc0:tc0 + 128], cTt[:, tc0:tc0 + 128], True, True, R(bT, cTt), R(pCB))
                        S.act(flat(Lp[par]), pseg[:, :], AF.Exp, R(pseg), R(Lp[par]))
                        S.tt(Mp[par][:], Lp[par][:], pCB[:, 0:128].unsqueeze(1).to_broadcast([128, 4, 128]), ALU.mult,
                             R(Lp[par], pCB), R(Mp[par]))
                        S.mm(pY[:, 256:512], cTt[:, tc0:tc0 + 128], flat(stbf[d]), True, True, R(cTt, stbf[d]), R(pY))
                    if any(cc in OWN for cc in order[i + 1:]):
                        pds = PS[5 + par]
                        S.tt(xdec[par][:], xtok[:, c, :].rearrange("p (h q) -> p h q", q=64),
                             wdec[:, d, c, :].unsqueeze(2).to_broadcast([128, 4, 64]), ALU.mult, R(xtok, wdec), R(xdec[par]),
                             eng="pool")
                        S.mm(pds[:, 0:256], btok[:, c, :], flat(xdec[par]), True, True, R(btok, xdec[par]), R(pds))
                        S.tt(st[d][:], st[d][:], cdec[:, d, c, :].unsqueeze(2).to_broadcast([128, 4, 64]), ALU.mult,
                             R(st[d], cdec), R(st[d]), eng="pool")
                        S.tt(flat(st[d]), flat(st[d]), pds[:, 0:256], ALU.add, R(st[d], pds), R(st[d]))
                        S.cp(stbf[d][:], st[d][:], R(st[d]), R(stbf[d]), eng="act")
                    if own:
                        for h in range(4):
                            S.mm(pY[:, h * 64:(h + 1) * 64], Mp[par][:, h, :], xtok[:, c, h * 64:(h + 1) * 64], True, True,
                                 R(Mp[par], xtok), R(pY))
                        S.tt(t1[par][:], pY[:, 256:512].rearrange("p (h q) -> p h q", q=64),
                             ecum[:, d, c, :].unsqueeze(2).to_broadcast([128, 4, 64]), ALU.mult, R(pY, ecum), R(t1[par]))
                        S.tt(flat(t2[par]), flat(t1[par]), pY[:, 0:256], ALU.add, R(t1[par], pY), R(t2[par]))
                        S.tt(yacc[:, oc, :], yacc[:, oc, :], flat(t2[par]), ALU.add, R(t2[par]) + [yacc.k(oc)], [yacc.k(oc)],
                             eng="pool")

            if g == 0: chk(26)
            for o0 in range(0, 17, 4):
                nt = min(4, 17 - o0)
                n = nt * 128
                for i in range(nt):
                    for j in range(2):
                        S.tr(PS[5 + j][:, i * 128:(i + 1) * 128], yacc[:, o0 + i, j * 128:(j + 1) * 128], C.ident,
                             [yacc.k(o0 + i)] + R(C.cs), R(PS[5 + j]))
                for j in range(2):
                    S.tt(yz[:, j, 0:n], PS[5 + j][:, 0:n], zT[:, j, o0 * 128:o0 * 128 + n], ALU.mult, R(PS[5 + j], zT), R(yz))
                S.tt(sqg[:, :, 0:n], yz[:, :, 0:n], yz[:, :, 0:n], ALU.mult, R(yz), R(sqg), eng="pool")
                for j in range(2):
                    S.mm(PS[7][:, 0:n], C.ones, sqg[:, j, 0:n], j == 0, j == 1, R(sqg, C.cs), R(PS[7]))
                S.act(rsg[:, 0:n], PS[7][:, 0:n], AF.Sqrt, R(PS[7]), R(rsg), scale=1.0 / 256, bias=1e-5)
                S.recip(rsg[:, 0:n], rsg[:, 0:n], R(rsg), R(rsg))
                S.tt(yz[:, :, 0:n], yz[:, :, 0:n], rsg[:, 0:n].unsqueeze(1).to_broadcast([128, 2, n]), ALU.mult,
                     R(yz, rsg), R(yz))
                for j in range(2):
                    S.act(ygs[:, j, o0 * 128:o0 * 128 + n], yz[:, j, 0:n], AF.Identity, R(yz, normw), R(ygs),
                          scale=normw[:, 2 * g + j:2 * g + j + 1])
            S.dma(ygd[:, 2 * g:2 * g + 2, :], ygs[:], R(ygs), R(ygd))
            if g == 0: chk(27)
        S.reset(base)
        chk(2)

        XT = S.carve("XT", [8, NOWN], BF16)
        gateT = S.carve("gateT", [NOWN], F32, parts=32)
        p3 = S.mark()
        wout = S.carve("wout", [16, 1024], BF16)
        for h in range(2):
            S.dma(wout[:, h * 8:(h + 1) * 8, :], wout_d.h.rearrange("(kc p) f -> p kc f", p=128)[:, h * 8:(h + 1) * 8, :],
                  [], R(wout), eng="pool")
        rw = S.carve("rw", [8, 32], F32)
        S.dma(rw[:], rw_d[:], [], R(rw))
        rb = S.carve("rb", [32], F32)
        S.dma(rb[:], rb_d.h.partition_broadcast(128).rearrange("p o k -> p (o k)"), [], R(rb))
        ygbs = [S.carve("ygb%d" % i, [16, 512], BF16) for i in range(1)]
        subs3 = [(0, 128, 1)] + [(128 + 512 * k, 512, 0) for k in range(4)]

        def rhs_of(si, o0, n):
            yb = ygbs[0]
            S.dma(yb[:, :, 0:n], ygd[:, :, o0:o0 + n], R(ygd), R(yb))
            return (lambda kc: yb[:, kc, 0:n]), R(yb)

        def load_rows(o0, n, xrw):
            row0 = o0 if o0 < 128 else o0 + 128
            S.dma(xrw[:, 0:n // 128, :], xs[row0:row0 + n, :].rearrange("(i p) f -> p i f", p=128), [], R(xrw))

        phase3(S, C, subs3, 16, wout, rhs_of, load_rows, x1d, XT, gateT, rw, rb)
        S.reset(p3)
        chk(3)

        mw_in = D("mw_in", [32, 1024, 2048])
        mb_inT = D("mb_inT", [128, 32, 16])
        mw_out = D("mw_out", [32, 1024, 1024])
        mb_out = D("mb_out", [32, 1024])
        b_inT = S.carve("b_inT", [32, 16], F32)
        S.dma(b_inT[:], mb_inT[:], [], R(b_inT))
        S.ts(b_inT[:, :, 8:16], b_inT[:, :, 8:16], 1.0, None, ALU.add, None, R(b_inT), R(b_inT))
        p4 = S.mark()
        tblocks = [([(0, 384), (384, 384), (768, 384)], [(0, 128), (128, 512), (640, 512)], [1, 0, 0]),
                   ([(1152, 512), (1664, 512)], [(1152, 512), (1664, 512)], [0, 0])]
        for mchunks, chunks, cols in tblocks:
            acc = S.carve("acc", [8, sum(n for _, n in chunks)], F32, nreg=8)
            m = S.mark()
            moe_block(S, C, XT, gateT, mchunks, mw_in, mw_out, b_inT, mb_out, acc)
            offs = [lo - chunks[0][0] for lo, _ in chunks]
            S.reset(m)
            ln2_out(S, C, acc, offs, chunks, cols, x1d, xout, None, None)
            if after_block is not None:
                after_block(xout, chunks[0][0], chunks[-1][0] + chunks[-1][1])
            S.reset(p4)
        if not fused:
            S.finish(R(xout))
        return xout


TOK1 = 4352
NQ = 2048
NT1 = 34
MLA_SCALE = 192.0 ** -0.5
RMS_EPS = 1e-6


class _Stop1(Exception):
    pass


def build_l1():
    import os
    stop = int(os.environ.get("STOP", "99"))

    def chk(n):
        if stop == n:
            raise _Stop1()

    nc = bass.Bass("TRN2", target_bir_lowering=False)
    es = ExitStack()
    with es:
        S = Sched(nc, es)
        try:
            _build1(S, nc, chk)
        except _Stop1:
            print("STOPPED at", stop)
            S.finish([])
        S.emit()
    return nc


def _build1(S, nc, chk, fused=False, xown=None, xall=None):
    C = Ctx()
    D = lambda name, shape, dt=F32, kind="ExternalInput": S.dram(name, shape, dt, kind)
    xs = None if fused else D("xs", [TOK1, 1024])
    NTQ = 16 if fused else 0
    cT = D("cT", [128, 8, 2])
    mod_w = D("mod_w", [1024, 6144])
    mod_bT = D("mod_bT", [128, 48])
    lnp_d = D("lnp", [128, 4, 8])
    wa_d = D("wa", [1024, 704])
    qnorm_d = D("qnorm", [128, 3])
    kvnorm_d = D("kvnorm", [128, 2])
    wqb_d = D("wqb", [384, 1536])
    wkvb_d = D("wkvb", [256, 2048])
    wo_d = D("wo", [1024, 1024])
    ropeq_d = D("ropeq", [64, 2, NQ])
    ropek_d = D("ropek", [64, 2, TOK1])
    rm_d = D("rm", [64, 64])
    rw_d = D("rw", [128, 8, 32])
    rb_d = D("rb", [1, 32])
    xout = D("xout", [NQ, 1024], F32, "ExternalOutput")
    hTd = D("hTd", [128, 8, TOK1 + NTQ * 128], BF16, "Internal")
    x1d = D("x1d", [128, 8, NQ], F32, "Internal")
    setup_common(S, C, True)
    PS = C.PS
    if not hasattr(S, "ar"):
        S.arena(49800)
    C.mod = S.carve("mod", [48, 2], F32)
    C.lnp = S.carve("lnp", [4, 8], F32)
    S.dma(C.lnp[:], lnp_d[:], [], R(C.lnp))
    onesbf = S.carve("onesbf", [128], BF16)
    S.cp(onesbf[:], C.ones, R(C.cs), R(onesbf))
    c128 = S.carve("c128", [128], BF16)
    S.ts(c128[:], C.ones, 1.0 / 128, None, ALU.mult, None, R(C.cs), R(c128))
    rm = S.carve("rm", [64], F32, parts=64)
    S.dma(rm[:], rm_d[:], [], R(rm))
    modulation(S, C, mod_w, mod_bT, cT)
    XT = S.carve("XT", [8, NQ], BF16)
    gateT = S.carve("gateT", [NQ], F32, parts=32)
    markA = S.mark()
    oT = S.carve("oT", [8, NQ], BF16, nreg=8)
    markB = S.mark()
    chk(0)

    xr = [S.carve("xr%d" % i, [1024], F32) for i in range(2)]
    hb = [S.carve("hb%d" % i, [8, 512], BF16) for i in range(2)]
    ntl = NT1 + NTQ
    tmap = xall_tile_map()
    tchunk = []
    for k, (r0_, n_) in enumerate(XCHUNKS):
        tchunk += [k] * (2 * n_ // 128)
    for blk in range((ntl + 3) // 4):
        t0 = blk * 4
        nt = min(4, ntl - t0)
        h = hb[blk % 2]
        for i in range(nt):
            t = t0 + i
            x = xr[t % 2]
            if not fused:
                col = 1 if t >= 32 else 0
                S.dma(x[:], xs[t * 128:(t + 1) * 128, :], [], R(x))
            elif t < NT1:
                col = 1 if tmap[t][1] == 0 else 0
                S.dma(x[:], xall[t * 128:(t + 1) * 128, :], [xall.k(tchunk[t])], R(x))
            else:
                col = 0
                S.dma(x[:], xown[128 + (t - NT1) * 128:128 + (t - NT1 + 1) * 128, :], [xown.k(1 + t - NT1)], R(x))
            for kc in range(8):
                p = PS[kc // 4 + 2 * (t % 2)]
                S.tr(p[:, (kc % 4) * 128:(kc % 4 + 1) * 128], x[:, kc * 128:(kc + 1) * 128], C.ident, R(x, C.cs), R(p))
            for kc in range(8):
                p = PS[kc // 4 + 2 * (t % 2)]
                S.act(h[:, kc, i * 128:(i + 1) * 128], p[:, (kc % 4) * 128:(kc % 4 + 1) * 128], AF.Identity,
                      R(p, C.mod), R(h), scale=C.mod[:, SC1 + kc, col:col + 1], bias=C.mod[:, SH1 + kc, col:col + 1])
        S.dma(hTd[:, :, t0 * 128:(t0 + nt) * 128], h[:, :, 0:nt * 128], R(h), R(hTd))
    S.reset(markB)
    chk(1)

    kvn = S.carve("kvn", [2, TOK1], BF16)
    qln = S.carve("qln", [3, NQ], BF16)
    KrT = S.carve("KrT", [TOK1], BF16)
    S.memset(KrT[64:128, :], 0.0, R(KrT))
    markC = S.mark()
    wa = S.carve("wa", [8, 704], BF16)
    S.dma(wa[:], wa_d.h.rearrange("(kc p) f -> p kc f", p=128), [], R(wa), eng="pool")
    qnorm = S.carve("qnorm", [3], F32)
    S.dma(qnorm[:], qnorm_d[:], [], R(qnorm))
    kvnorm = S.carve("kvnorm", [2], F32)
    S.dma(kvnorm[:], kvnorm_d[:], [], R(kvnorm))
    hblk = [S.carve("hblk%d" % i, [8, 512], BF16) for i in range(2)]
    kvl = S.carve("kvl", [2, 512], F32)
    ql = S.carve("ql", [3, 512], F32)
    krl = S.carve("krl", [512], F32)
    sq = S.carve("sq", [3, 512], F32)
    rs = S.carve("rs", [512], F32)
    rk = S.carve("rk", [2, 512], F32)
    t1 = S.carve("t1", [512], F32)
    t2 = S.carve("t2", [512], F32)
    blocks = [(b * 512, min(512, TOK1 - b * 512)) for b in range(9)]

    def rms_fm(src, nch, width, g, dst, d0, n):
        S.tt(sq[:, 0:nch, 0:n], src[:, 0:nch, 0:n], src[:, 0:nch, 0:n], ALU.mult, R(src), R(sq), eng="pool")
        for c in range(nch):
            S.mm(PS[6][:, 0:n], C.ones, sq[:, c, 0:n], c == 0, c == nch - 1, R(sq, C.cs), R(PS[6]))
        S.cp(rs[:, 0:n], PS[6][:, 0:n], R(PS[6]), R(rs))
        S.act(rs[:, 0:n], rs[:, 0:n], AF.Sqrt, R(rs), R(rs), scale=1.0 / width, bias=RMS_EPS)
        S.recip(rs[:, 0:n], rs[:, 0:n], R(rs), R(rs))
        S.tt(src[:, 0:nch, 0:n], src[:, 0:nch, 0:n], rs[:, 0:n].unsqueeze(1).to_broadcast([128, nch, n]), ALU.mult,
             R(src, rs), R(src))
        for c in range(nch):
            S.act(dst[:, c, d0:d0 + n], src[:, c, 0:n], AF.Identity, R(src, g), R(dst), scale=g[:, c:c + 1])

    def rope(src, tab, dst_ap, n, dstkeys):
        S.mm(PS[7][0:64, 0:n], rm[0:64, 0:64], src[0:64, 0:n], True, True, R(src, rm), R(PS[7]))
        S.tt(t1[0:64, 0:n], src[0:64, 0:n], tab[0:64, 0, 0:n], ALU.mult, R(src, tab), R(t1))
        S.tt(t2[0:64, 0:n], PS[7][0:64, 0:n], tab[0:64, 1, 0:n], ALU.mult, R(PS[7], tab), R(t2))
        S.tt(dst_ap, t1[0:64, 0:n], t2[0:64, 0:n], ALU.add, R(t1, t2), dstkeys, eng="pool")

    for bi, (b0, n) in enumerate(blocks):
        hbk = hblk[bi % 2]
        S.dma(hbk[:, :, 0:n], hTd.h[:, :, b0:b0 + n], R(hTd), R(hbk))
        S.dma(rk[0:64, :, 0:n], ropek_d[:, :, b0:b0 + n], [], R(rk))
        it = 0
        for c in range(2):
            p = PS[it % 2]
            it += 1
            for kc in range(8):
                S.mm(p[:, 0:n], wa[:, kc, 384 + c * 128:384 + (c + 1) * 128], hbk[:, kc, 0:n], kc == 0, kc == 7, R(wa, hbk), R(p))
            S.cp(kvl[:, c, 0:n], p[:, 0:n], R(p), R(kvl))
        p = PS[2]
        for kc in range(8):
            S.mm(p[0:64, 0:n], wa[:, kc, 640:704], hbk[:, kc, 0:n], kc == 0, kc == 7, R(wa, hbk), R(p))
        S.cp(krl[0:64, 0:n], p[0:64, 0:n], R(p), R(krl))
        if b0 < NQ and not fused:
            for c in range(3):
                p = PS[it % 2]
                it += 1
                for kc in range(8):
                    S.mm(p[:, 0:n], wa[:, kc, c * 128:(c + 1) * 128], hbk[:, kc, 0:n], kc == 0, kc == 7, R(wa, hbk), R(p))
                S.cp(ql[:, c, 0:n], p[:, 0:n], R(p), R(ql))
            rms_fm(ql, 3, 384, qnorm, qln, b0, n)
        rms_fm(kvl, 2, 256, kvnorm, kvn, b0, n)
        rope(krl, rk, KrT[0:64, b0:b0 + n], n, R(KrT))
    if fused:
        for qi in range(4):
            hbk = hblk[qi % 2]
            n = 512
            b0 = qi * 512
            S.dma(hbk[:, :, 0:n], hTd.h[:, :, TOK1 + b0:TOK1 + b0 + n], R(hTd), R(hbk))
            for c in range(3):
                p = PS[c % 2]
                for kc in range(8):
                    S.mm(p[:, 0:n], wa[:, kc, c * 128:(c + 1) * 128], hbk[:, kc, 0:n], kc == 0, kc == 7, R(wa, hbk), R(p))
                S.cp(ql[:, c, 0:n], p[:, 0:n], R(p), R(ql))
            rms_fm(ql, 3, 384, qnorm, qln, b0, n)
    S.reset(markC)
    chk(2)

    wqb = S.carve("wqb", [3, 1536], BF16)
    S.dma(wqb[:], wqb_d.h.rearrange("(kc p) f -> p kc f", p=128), [], R(wqb), eng="pool")
    wkvb = S.carve("wkvb", [2, 2048], BF16)
    S.dma(wkvb[:], wkvb_d.h.rearrange("(kc p) f -> p kc f", p=128), [], R(wkvb), eng="pool")
    rq = S.carve("rq", [2, 512], F32)
    KnT = S.carve("KnT", [TOK1], BF16)
    V = S.carve("V", [NT1, 128], BF16)
    QnT = S.carve("QnT", [NQ], BF16)
    QrT = S.carve("QrT", [NQ], BF16)
    S.memset(QrT[64:128, :], 0.0, R(QrT))
    negm = S.carve("negm", [NQ], BF16)
    pT = [S.carve("pT%d" % i, [512], BF16) for i in range(3)]
    sqb = S.carve("sqb", [512], BF16)
    sqr = S.carve("sqr", [512], BF16)
    qrl = S.carve("qrl", [512], F32)
    t1 = S.carve("t1b", [512], F32)
    t2 = S.carve("t2b", [512], F32)
    tq = S.carve("tq", [512], F32)
    rden = S.carve("rden", [512], F32)
    mx = S.carve("mx", [16], F32)
    kmax = S.carve("kmax", [4], F32)
    for bi, (b0, n) in enumerate(blocks):
        S.tt(sqr[0:64, 0:n], KrT[0:64, b0:b0 + n], KrT[0:64, b0:b0 + n], ALU.mult, R(KrT), R(sqr), eng="pool")
        S.mm(PS[6][:, 0:n], onesbf[0:64, :], sqr[0:64, 0:n], True, True, R(sqr, onesbf), R(PS[6]))
        S.reduce(mx[:, bi:bi + 1], PS[6][:, 0:n], AX.X, ALU.max, R(PS[6]), R(mx))
    S.reduce(kmax[:, 0:1], mx[:, 0:9], AX.X, ALU.max, R(mx), R(kmax))
    qblocks = [(q * 512, 512) for q in range(4)]
    step = 0
    for h in range(8):
        for bi, (b0, n) in enumerate(blocks):
            p = PS[bi % 2]
            for kc in range(2):
                S.mm(p[:, 0:n], wkvb[:, kc, h * 128:(h + 1) * 128], kvn[:, kc, b0:b0 + n], kc == 0, kc == 1, R(wkvb, kvn), R(p))
            S.cp(KnT[:, b0:b0 + n], p[:, 0:n], R(p), R(KnT), eng="act")
            S.tt(sqb[:, 0:n], KnT[:, b0:b0 + n], KnT[:, b0:b0 + n], ALU.mult, R(KnT), R(sqb), eng="pool")
            S.mm(PS[6][:, 0:n], onesbf[:], sqb[:, 0:n], True, True, R(sqb, onesbf), R(PS[6]))
            S.reduce(mx[:, bi:bi + 1], PS[6][:, 0:n], AX.X, ALU.max, R(PS[6]), R(mx))
        S.reduce(kmax[:, 1:2], mx[:, 0:9], AX.X, ALU.max, R(mx), R(kmax))
        S.tt(kmax[:, 2:3], kmax[:, 0:1], kmax[:, 1:2], ALU.add, R(kmax), R(kmax))
        for t0 in range(0, NT1, 4):
            nt = min(4, NT1 - t0)
            p = PS[2 + (t0 // 4) % 2]
            for i in range(nt):
                for kc in range(2):
                    S.mm(p[:, i * 128:(i + 1) * 128], kvn[:, kc, (t0 + i) * 128:(t0 + i + 1) * 128],
                         wkvb[:, kc, 1024 + h * 128:1024 + (h + 1) * 128], kc == 0, kc == 1, R(wkvb, kvn), R(p))
            S.cp(V[:, t0:t0 + nt, :], p[:, 0:nt * 128].rearrange("p (a b) -> p a b", b=128), R(p), R(V), eng="act")
        for (q0, n) in qblocks:
            p = PS[0]
            for kc in range(3):
                S.mm(p[:, 0:n], wqb[:, kc, h * 128:(h + 1) * 128], qln[:, kc, q0:q0 + n], kc == 0, kc == 2, R(wqb, qln), R(p))
            S.cp(QnT[:, q0:q0 + n], p[:, 0:n], R(p), R(QnT), eng="act")
            p = PS[1]
            for kc in range(3):
                S.mm(p[0:64, 0:n], wqb[:, kc, 1024 + h * 64:1024 + (h + 1) * 64], qln[:, kc, q0:q0 + n], kc == 0, kc == 2,
                     R(wqb, qln), R(p))
            S.cp(qrl[0:64, 0:n], p[0:64, 0:n], R(p), R(qrl))
            S.dma(rq[0:64, :, 0:n], ropeq_d[:, :, q0:q0 + n], [], R(rq))
            S.mm(PS[7][0:64, 0:n], rm[0:64, 0:64], qrl[0:64, 0:n], True, True, R(qrl, rm), R(PS[7]))
            S.tt(t1[0:64, 0:n], qrl[0:64, 0:n], rq[0:64, 0, 0:n], ALU.mult, R(qrl, rq), R(t1))
            S.tt(t2[0:64, 0:n], PS[7][0:64, 0:n], rq[0:64, 1, 0:n], ALU.mult, R(PS[7], rq), R(t2))
            S.tt(QrT[0:64, q0:q0 + n], t1[0:64, 0:n], t2[0:64, 0:n], ALU.add, R(t1, t2), R(QrT), eng="pool")
            S.tt(sqb[:, 0:n], QnT[:, q0:q0 + n], QnT[:, q0:q0 + n], ALU.mult, R(QnT), R(sqb), eng="pool")
            S.tt(sqr[0:64, 0:n], QrT[0:64, q0:q0 + n], QrT[0:64, q0:q0 + n], ALU.mult, R(QrT), R(sqr), eng="pool")
            S.mm(PS[6][:, 0:n], onesbf[:], sqb[:, 0:n], True, False, R(sqb, onesbf), R(PS[6]))
            S.mm(PS[6][:, 0:n], onesbf[0:64, :], sqr[0:64, 0:n], False, True, R(sqr, onesbf), R(PS[6]))
            S.cp(tq[:, 0:n], PS[6][:, 0:n], R(PS[6]), R(tq))
            S.act(tq[:, 0:n], tq[:, 0:n], AF.Sqrt, R(tq, kmax), R(tq), scale=kmax[:, 2:3])
            S.ts(negm[:, q0:q0 + n], tq[:, 0:n], -1.0, None, ALU.mult, None, R(tq), R(negm))
        for (q0, n) in qblocks:
            po, pd = PS[4], PS[5]
            def scores(kt):
                par = kt % 3
                p = PS[par]
                k0 = kt * 128
                S.mm(p[:, 0:n], KnT[:, k0:k0 + 128], QnT[:, q0:q0 + n], True, False, R(KnT, QnT), R(p))
                S.mm(p[:, 0:n], KrT[:, k0:k0 + 128], QrT[:, q0:q0 + n], False, False, R(KrT, QrT), R(p))
                S.mm(p[:, 0:n], c128[:], negm[:, q0:q0 + n], False, True, R(c128, negm), R(p))
                S.act(pT[par][:, 0:n], p[:, 0:n], AF.Exp, R(p), R(pT[par]), scale=MLA_SCALE)

            scores(0)
            scores(1)
            for kt in range(NT1):
                if kt + 2 < NT1:
                    scores(kt + 2)
                par = kt % 3
                S.mm(po[:, 0:n], V[:, kt, :], pT[par][:, 0:n], kt == 0, kt == NT1 - 1, R(V, pT[par]), R(po))
                S.mm(pd[:, 0:n], onesbf[:], pT[par][:, 0:n], kt == 0, kt == NT1 - 1, R(onesbf, pT[par]), R(pd))
            S.recip(rden[:, 0:n], pd[:, 0:n], R(pd), R(rden))
            S.tt(oT[:, h, q0:q0 + n], po[:, 0:n], rden[:, 0:n], ALU.mult, R(po, rden), [oT.k(h)])
        if h == 0:
            chk(20)
    S.reset(markB)
    chk(3)

    wo = S.carve("wo", [8, 1024], BF16)
    S.dma(wo[:], wo_d.h.rearrange("(kc p) f -> p kc f", p=128), [], R(wo), eng="pool")
    rw = S.carve("rw", [8, 32], F32)
    S.dma(rw[:], rw_d[:], [], R(rw))
    rb = S.carve("rb", [32], F32)
    S.dma(rb[:], rb_d.h.partition_broadcast(128).rearrange("p o k -> p (o k)"), [], R(rb))
    subs3 = [(512 * k, 512, 0) for k in range(4)]

    def rhs_of(si, o0, n):
        return (lambda kc: oT[:, kc, o0:o0 + n]), R(oT)

    def load_rows(o0, n, xrw):
        if fused:
            S.dma(xrw[:, 0:n // 128, :], xown[128 + o0:128 + o0 + n, :].rearrange("(i p) f -> p i f", p=128), R(xown), R(xrw))
        else:
            S.dma(xrw[:, 0:n // 128, :], xs[o0:o0 + n, :].rearrange("(i p) f -> p i f", p=128), [], R(xrw))

    phase3(S, C, subs3, 8, wo, rhs_of, load_rows, x1d, XT, gateT, rw, rb)
    S.reset(markA)
    chk(4)

    mw_in = D("mw_in", [32, 1024, 2048])
    mb_inT = D("mb_inT", [128, 32, 16])
    mw_out = D("mw_out", [32, 1024, 1024])
    mb_out = D("mb_out", [32, 1024])
    b_inT = S.carve("b_inT", [32, 16], F32)
    S.dma(b_inT[:], mb_inT[:], [], R(b_inT))
    S.ts(b_inT[:, :, 8:16], b_inT[:, :, 8:16], 1.0, None, ALU.add, None, R(b_inT), R(b_inT))
    p4 = S.mark()
    tblocks = [([(0, 512), (512, 512)], [0, 0]), ([(1024, 512), (1536, 512)], [0, 0])]
    for chunks, cols in tblocks:
        acc = S.carve("acc", [8, sum(n for _, n in chunks)], F32, nreg=8)
        m = S.mark()
        offs = moe_block(S, C, XT, gateT, chunks, mw_in, mw_out, b_inT, mb_out, acc)
        S.reset(m)
        ln2_out(S, C, acc, offs, chunks, cols, x1d, xout, None, None)
        S.reset(p4)
    S.finish(R(xout))


def build_fused():
    nc = bass.Bass("TRN2", target_bir_lowering=False)
    es = ExitStack()
    with es:
        S = Sched(nc, es)
        nochk = lambda n: None
        S.prefix = ""
        xall = S.dram("xall", [2 * 2176, 1024], F32, "Internal", nreg=len(XCHUNKS))

        def exchange(xown, row_lo, row_hi):
            for k, (r0, n) in enumerate(XCHUNKS):
                if row_lo <= r0 < row_hi:
                    S.op("pool", lambda e, r0=r0, n=n: e.collective_compute(
                        "AllGather", ALU.bypass, replica_groups=[[0, 1], [2, 3], [4, 5], [6, 7]],
                        ins=[xown[r0:r0 + n, :]], outs=[xall[2 * r0:2 * r0 + 2 * n, :]]),
                        [xown.k(t) for t in range(r0 // 128, (r0 + n) // 128)], [xall.k(k)], dma=True, inc=1)

        S.prefix = "a_"
        xown = _build(S, nc, nochk, fused=True, after_block=exchange)
        S.reset(0)
        S.prefix = "b_"
        _build1(S, nc, nochk, fused=True, xown=xown, xall=xall)
        S.emit()
    return nc


def consts():
    c = np.zeros((128, 1024), np.float32)
    c[:, 0:128] = np.eye(128)
    c[:, 128:256] = 1.0
    k = np.arange(128)[:, None]; l = np.arange(128)[None, :]
    c[:, 256:384] = (k <= l)
    c[:, 384:512] = (k >= l)
    c[:, 512:640] = np.where(k > l, -30000.0, 0.0)
    c[:, 640:768] = np.where(k < l, -30000.0, 0.0)
    c[0:32, 768:800] = np.eye(32)
    return c

def fm(v, nchunk):
    return np.ascontiguousarray(v.reshape(nchunk, 128).T)

def prep_l0(inp, b, half):
    f = np.float32
    flip = half == 1
    ctx_ = inp['ctx'][b][::-1] if flip else inp['ctx'][b]
    lat_ = inp['x'][b][::-1] if flip else inp['x'][b]
    m = {}
    m['xs'] = np.ascontiguousarray(np.concatenate([ctx_, lat_], 0), dtype=f)
    cT = np.zeros((128, 8, 2), f)
    cT[:, :, 0] = fm(inp['c'][b], 8); cT[:, :, 1] = fm(inp['c_ctx'], 8)
    m['cT'] = cT
    m['mod_w'] = np.ascontiguousarray(inp['mod_w'][0])
    m['mod_bT'] = fm(inp['mod_b'][0], 48)
    m['lnp'] = np.ascontiguousarray(np.stack([fm(inp['ln1_g'][0], 8), fm(inp['ln1_b'][0], 8), fm(inp['ln2_g'][0], 8), fm(inp['ln2_b'][0], 8)], 1))
    W = inp['ssd_w_in'][0]
    dA = 1 if flip else 0; dB = 1 - dA
    win = np.zeros((8, 1024, 776), f)
    convp = np.zeros((128, 8, 4, 6), f)
    ssdp = np.zeros((1, 160), f)
    cw = inp['ssd_conv_w'][0]; cb = inp['ssd_conv_b'][0]
    if flip: cw = cw[::-1]
    for g in range(8):
        win[g] = np.concatenate([W[:, 256*g:256*g+256], W[:, 2048+256*g:2048+256*g+256], W[:, 4096+128*g:4096+128*g+128],
                                 W[:, 5120+128*g:5120+128*g+128], W[:, 6144+dA*32+4*g:6144+dA*32+4*g+4], W[:, 6144+dB*32+4*g:6144+dB*32+4*g+4]], 1)
        for ch, c0 in enumerate([256*g, 256*g+128, 2048+128*g, 3072+128*g]):
            convp[:, g, ch, 0:5] = cw[:, c0:c0+128].T
            convp[:, g, ch, 5] = cb[c0:c0+128]
        ssdp[0, g*20:g*20+20] = np.concatenate([inp['ssd_dt_bias'][0][dA, 4*g:4*g+4], inp['ssd_dt_bias'][0][dB, 4*g:4*g+4],
                                               inp['ssd_a_log'][0][dA, 4*g:4*g+4], inp['ssd_a_log'][0][dB, 4*g:4*g+4], inp['ssd_d'][0][4*g:4*g+4]])
    m['win'] = win; m['convp'] = convp; m['ssdp'] = ssdp
    m['normw'] = fm(inp['ssd_norm_w'][0], 16)
    m['wout'] = np.ascontiguousarray(inp['ssd_w_out'][0])
    add_moe(m, inp, 0)
    m['cst'] = consts()
    return m

def add_moe(m, inp, i):
    f = np.float32
    m['rw'] = np.ascontiguousarray(inp['router_w'][i].reshape(8, 128, 32).transpose(1, 0, 2))
    m['rb'] = np.ascontiguousarray(inp['router_b'][i].reshape(1, 32))
    wi = inp['moe_w_in'][i]
    m['mw_in'] = np.ascontiguousarray(np.concatenate([wi[:, :, 0:512], wi[:, :, 1024:1536], wi[:, :, 512:1024], wi[:, :, 1536:2048]], 2))
    m['mb_inT'] = np.ascontiguousarray(inp['moe_b_in'][i].reshape(32, 16, 128).transpose(2, 0, 1))
    m['mw_out'] = np.ascontiguousarray(inp['moe_w_out'][i])
    m['mb_out'] = np.ascontiguousarray(inp['moe_b_out'][i])

def gather_l0(results, B=4):
    x1 = np.zeros((B, 4096, 1024), np.float32); ctx1 = np.zeros((B, 256, 1024), np.float32)
    for cid, r in enumerate(results):
        b, half = cid // 2, cid % 2
        o = r['xout']
        if half == 0:
            ctx1[b, 0:128] = o[0:128]; x1[b, 0:2048] = o[128:]
        else:
            ctx1[b, 128:256] = o[0:128][::-1]; x1[b, 2048:4096] = o[128:][::-1]
    return x1, ctx1


def rope_consts():
    f = np.float32
    t = np.arange(4096)
    row = (t // 64).astype(f); col = (t % 64).astype(f)
    inv = (f(10000.0) ** (-np.arange(16, dtype=f) / f(16))).astype(f)
    ang = np.stack([row[:, None] * inv, col[:, None] * inv], axis=1).astype(f)
    cos, sin = np.cos(ang).astype(f), np.sin(ang).astype(f)
    cosT = np.zeros((64, 4096), f); sinT = np.zeros((64, 4096), f)
    for a in range(2):
        for hf in range(2):
            cosT[a * 32 + hf * 16:a * 32 + hf * 16 + 16] = cos[:, a, :].T
            sinT[a * 32 + hf * 16:a * 32 + hf * 16 + 16] = sin[:, a, :].T
    rm = np.zeros((64, 64), f)
    for a in range(2):
        for k in range(16):
            i1 = a * 32 + k; i2 = a * 32 + 16 + k
            rm[i2, i1] = -1.0
            rm[i1, i2] = 1.0
    return cosT, sinT, rm

def prep_l1(inp, x1, ctx1, b, half, rc=None):
    f = np.float32
    cosT, sinT, rm = rc if rc is not None else rope_consts()
    own = slice(half * 2048, (half + 1) * 2048)
    oth = slice((1 - half) * 2048, (2 - half) * 2048)
    m = {}
    m['xs'] = np.ascontiguousarray(np.concatenate([x1[b, own], x1[b, oth], ctx1[b]], 0), dtype=f)
    cT = np.zeros((128, 8, 2), f)
    cT[:, :, 0] = fm(inp['c'][b], 8); cT[:, :, 1] = fm(inp['c_ctx'], 8)
    m['cT'] = cT
    m['mod_w'] = np.ascontiguousarray(inp['mod_w'][1])
    m['mod_bT'] = fm(inp['mod_b'][1], 48)
    m['lnp'] = np.ascontiguousarray(np.stack([fm(inp['ln1_g'][1], 8), fm(inp['ln1_b'][1], 8), fm(inp['ln2_g'][1], 8), fm(inp['ln2_b'][1], 8)], 1))
    m['wa'] = np.ascontiguousarray(inp['mla_w_a'][0])
    m['qnorm'] = fm(inp['mla_q_norm'][0], 3)
    m['kvnorm'] = fm(inp['mla_kv_norm'][0], 2)
    wq = inp['mla_w_qb'][0]
    m['wqb'] = np.ascontiguousarray(np.concatenate([wq[:, h * 192:h * 192 + 128] for h in range(8)] +
                                                  [wq[:, h * 192 + 128:h * 192 + 192] for h in range(8)], 1))
    wk = inp['mla_w_kvb'][0]
    m['wkvb'] = np.ascontiguousarray(np.concatenate([wk[:, h * 256:h * 256 + 128] for h in range(8)] +
                                                   [wk[:, h * 256 + 128:h * 256 + 256] for h in range(8)], 1))
    m['wo'] = np.ascontiguousarray(inp['mla_w_o'][0])
    m['ropeq'] = np.ascontiguousarray(np.stack([cosT[:, own], sinT[:, own]], 1))
    ck = np.concatenate([cosT[:, own], cosT[:, oth], np.ones((64, 256), f)], 1)
    sk = np.concatenate([sinT[:, own], sinT[:, oth], np.zeros((64, 256), f)], 1)
    m['ropek'] = np.ascontiguousarray(np.stack([ck, sk], 1))
    m['rm'] = rm
    add_moe(m, inp, 1)
    m['cst'] = consts()
    return m

def gather_l1(results, B=4):
    out = np.zeros((B, 4096, 1024), np.float32)
    for cid, r in enumerate(results):
        b, half = cid // 2, cid % 2
        out[b, half * 2048:(half + 1) * 2048] = r['xout']
    return out


def prep_fused(inp, b, half, rc):
    f = np.float32
    cosT, sinT, rm = rc
    m0 = prep_l0(inp, b, half)
    m = {("cst" if k == "cst" else "a_" + k): v for k, v in m0.items()}
    idx1 = 4095 - np.arange(2048)
    ck = np.ones((64, 4352), f)
    sk = np.zeros((64, 4352), f)
    for t, (r, mt) in enumerate(xall_tile_map()):
        if mt == 0:
            continue
        base = (mt - 1) * 128 + np.arange(128)
        lat = base if r == 0 else 4095 - base
        ck[:, t * 128:(t + 1) * 128] = cosT[:, lat]
        sk[:, t * 128:(t + 1) * 128] = sinT[:, lat]
    qi = np.arange(2048) if half == 0 else idx1
    m1 = {}
    cT = np.zeros((128, 8, 2), f)
    cT[:, :, 0] = fm(inp['c'][b], 8); cT[:, :, 1] = fm(inp['c_ctx'], 8)
    m1['cT'] = cT
    m1['mod_w'] = np.ascontiguousarray(inp['mod_w'][1])
    m1['mod_bT'] = fm(inp['mod_b'][1], 48)
    m1['lnp'] = np.ascontiguousarray(np.stack([fm(inp['ln1_g'][1], 8), fm(inp['ln1_b'][1], 8), fm(inp['ln2_g'][1], 8), fm(inp['ln2_b'][1], 8)], 1))
    m1['wa'] = np.ascontiguousarray(inp['mla_w_a'][0])
    m1['qnorm'] = fm(inp['mla_q_norm'][0], 3)
    m1['kvnorm'] = fm(inp['mla_kv_norm'][0], 2)
    wq = inp['mla_w_qb'][0]
    m1['wqb'] = np.ascontiguousarray(np.concatenate([wq[:, h * 192:h * 192 + 128] for h in range(8)] +
                                                   [wq[:, h * 192 + 128:h * 192 + 192] for h in range(8)], 1))
    wk = inp['mla_w_kvb'][0]
    m1['wkvb'] = np.ascontiguousarray(np.concatenate([wk[:, h * 256:h * 256 + 128] for h in range(8)] +
                                                    [wk[:, h * 256 + 128:h * 256 + 256] for h in range(8)], 1))
    m1['wo'] = np.ascontiguousarray(inp['mla_w_o'][0])
    m1['ropeq'] = np.ascontiguousarray(np.stack([cosT[:, qi], sinT[:, qi]], 1))
    m1['ropek'] = np.ascontiguousarray(np.stack([ck, sk], 1))
    m1['rm'] = rm
    add_moe(m1, inp, 1)
    for k, v in m1.items():
        m["b_" + k] = v
    return m

def gather_fused(results, B=4):
    out = np.zeros((B, 4096, 1024), np.float32)
    for cid, r in enumerate(results):
        b, half = cid // 2, cid % 2
        o = r['b_xout']
        if half == 0:
            out[b, 0:2048] = o
        else:
            out[b, 2048:4096] = o[::-1]
    return out


def kernel(**inputs):
    inp = {k: np.asarray(v) for k, v in inputs.items()}
    nc = build_fused()
    rc = rope_consts()
    maps = []
    for cid in range(8):
        m = prep_fused(inp, cid // 2, cid % 2, rc)
        maps.append({k: m[k] for k in nc._in_names})
    res = run_bass_kernel_spmd(nc, maps, core_ids=list(range(8)))
    return gather_fused(res.results)
```

```python
import numpy as np
import concourse.bass as bass
import concourse.mybir as mybir
from concourse.bass_utils import run_bass_kernel_spmd
from contextlib import ExitStack

F32 = mybir.dt.float32
BF16 = mybir.dt.bfloat16
U32 = mybir.dt.uint32
I32 = mybir.dt.int32
AF = mybir.ActivationFunctionType
ALU = mybir.AluOpType
AX = mybir.AxisListType


class Tile:
    def __init__(self, name, h, nreg=1):
        self.name = name
        self.h = h
        self.nreg = nreg

    def k(self, i=0):
        return (self.name, i)

    def all(self):
        return [(self.name, i) for i in range(self.nreg)]

    def __getitem__(self, idx):
        return self.h[idx]


class Sched:
    ENGS = ("pe", "act", "dve", "pool", "sp")

    def __init__(self, nc, es, n_dma_sems=8, sync_same=True):
        self.nc = nc
        self.es = es
        self.sync_same = sync_same
        self.q = {e: [] for e in self.ENGS}
        self.csem = {e: es.enter_context(nc.semaphore("c_" + e)) for e in self.ENGS}
        self.ccount = {e: 0 for e in self.ENGS}
        self.dq = ("sp", "pool", "act")
        self.dsems = {e: [es.enter_context(nc.semaphore("d_%s_%d" % (e, i))) for i in range(n_dma_sems)]
                      for e in self.dq}
        self.duse = {e: [0] * n_dma_sems for e in self.dq}
        self.dnext = {e: 0 for e in self.dq}
        self.lastw = {}
        self.readers = {}
        self.known = {e: {} for e in self.ENGS}
        self.ntile = 0

    def sb(self, name, shape, dtype, nreg=1):
        h = self.es.enter_context(self.nc.sbuf_tensor(name, list(shape), dtype))
        return Tile(name, h, nreg)

    def ps(self, name, shape, dtype, nreg=1):
        h = self.es.enter_context(self.nc.psum_tensor(name, list(shape), dtype))
        return Tile(name, h, nreg)

    def dram(self, name, shape, dtype, kind, nreg=1):
        name = getattr(self, "prefix", "") + name
        if not hasattr(self.nc, "_in_names"):
            self.nc._in_names = []
        if kind == "ExternalInput":
            self.nc._in_names.append(name)
        h = self.nc.dram_tensor(name, list(shape), dtype, kind=kind)
        return Tile(name, h.ap(), nreg)

    def arena(self, nfloats):
        self.ar = self.sb("arena", [128, nfloats], F32)
        self.ar_n = nfloats
        self.ar_off = 0
        self.ar_base = 0

    def carve(self, name, shape, dtype, nreg=1, parts=128):
        n = 1
        for s in shape:
            n *= s
        nf = n if dtype == F32 else (n + 1) // 2
        nf = (nf + 7) // 8 * 8
        assert self.ar_off + nf <= self.ar_n, ("arena overflow", name, self.ar_off, nf, self.ar_n)
        ap = self.ar.h[0:parts, self.ar_off:self.ar_off + nf]
        if dtype != F32:
            ap = ap.bitcast(dtype)
        ap = ap[:, 0:n]
        if len(shape) == 2:
            ap = ap.rearrange("p (a b) -> p a b", b=shape[1])
        elif len(shape) == 3:
            ap = ap.rearrange("p (a b c) -> p a b c", b=shape[1], c=shape[2])
        self.ar_off += nf
        self.ntile += 1
        return Tile("%s#%d" % (name, self.ntile), ap, nreg)

    def mark(self):
        return self.ar_off

    def reset(self, mark):
        self.barrier()
        self.ar_off = mark

    def barrier(self):
        allt = {}
        for e in self.ENGS:
            if self.ccount[e] > 0:
                allt[("c", e)] = self.ccount[e]
        for e in self.dq:
            for i, u in enumerate(self.duse[e]):
                if u > 0:
                    allt[("d", e, i)] = u
        self.pending = {e: dict(allt) for e in self.ENGS}

    def op(self, eng, fn, reads=(), writes=(), dma=False, same=None, inc=16):
        deps = {}
        pend = getattr(self, "pending", None)
        if pend and pend.get(eng):
            for sk, v in pend[eng].items():
                if deps.get(sk, 0) < v:
                    deps[sk] = v
            pend[eng] = None

        def add(tok):
            sk, v = tok
            if deps.get(sk, 0) < v:
                deps[sk] = v

        for k in reads:
            if k in self.lastw:
                add(self.lastw[k])
        for k in writes:
            if k in self.lastw:
                add(self.lastw[k])
            for sk, v in self.readers.get(k, {}).items():
                add((sk, v))
        if dma:
            i = self.dnext[eng]
            self.dnext[eng] = (i + 1) % len(self.dsems[eng])
            prev = self.duse[eng][i]
            self.duse[eng][i] = prev + inc
            tok = (("d", eng, i), prev + inc)
            if prev > 0:
                add((("d", eng, i), prev))
        else:
            self.ccount[eng] += 1
            tok = (("c", eng), self.ccount[eng])
        same = self.sync_same if same is None else same
        waits = []
        for sk, v in deps.items():
            if sk == ("c", eng) and not same:
                continue
            if self.known[eng].get(sk, 0) >= v:
                continue
            self.known[eng][sk] = v
            waits.append((sk, v))
        self.q[eng].append((waits, fn, tok, inc if dma else 1))
        for k in reads:
            r = self.readers.setdefault(k, {})
            if r.get(tok[0], 0) < tok[1]:
                r[tok[0]] = tok[1]
        for k in writes:
            self.lastw[k] = tok
            self.readers[k] = {}
        return tok

    def dma(self, out, in_, reads, writes, eng="sp", **kw):
        return self.op(eng, lambda e: e.dma_start(out=out, in_=in_, **kw), reads, writes, dma=True)

    def mm(self, out, lhsT, rhs, start, stop, reads, writes):
        return self.op("pe", lambda e: e.matmul(out, lhsT, rhs, start=start, stop=stop), reads, writes, same=False)

    def tr(self, out, in_, ident, reads, writes):
        return self.op("pe", lambda e: e.transpose(out, in_, ident), reads, writes, same=False)

    def act(self, out, in_, func, reads, writes, eng="act", **kw):
        return self.op(eng, lambda e: e.activation(out=out, in_=in_, func=func, **kw), reads, writes)

    def tt(self, out, in0, in1, op, reads, writes, eng="dve"):
        return self.op(eng, lambda e: e.tensor_tensor(out=out, in0=in0, in1=in1, op=op), reads, writes)

    def ts(self, out, in0, s1, s2, op0, op1, reads, writes, eng="dve", **kw):
        if op1 is None:
            return self.op(eng, lambda e: e.tensor_scalar(out=out, in0=in0, scalar1=s1, scalar2=None, op0=op0, **kw),
                           reads, writes)
        return self.op(eng, lambda e: e.tensor_scalar(out=out, in0=in0, scalar1=s1, scalar2=s2, op0=op0, op1=op1, **kw),
                       reads, writes)

    def cp(self, out, in_, reads, writes, eng="dve"):
        if eng == "act":
            return self.op(eng, lambda e: e.copy(out=out, in_=in_), reads, writes)
        return self.op(eng, lambda e: e.tensor_copy(out=out, in_=in_), reads, writes)

    def stt(self, out, in0, scalar, in1, op0, op1, reads, writes):
        return self.op("dve", lambda e: e.scalar_tensor_tensor(out=out, in0=in0, scalar=scalar, in1=in1, op0=op0, op1=op1),
                       reads, writes)

    def recip(self, out, in_, reads, writes):
        return self.op("dve", lambda e: e.reciprocal(out=out, in_=in_), reads, writes)

    def max8(self, out, in_, reads, writes):
        return self.op("dve", lambda e: e.max(out=out, in_=in_), reads, writes)

    def reduce(self, out, in_, axis, op, reads, writes):
        return self.op("dve", lambda e: e.tensor_reduce(out=out, in_=in_, axis=axis, op=op), reads, writes)

    def memset(self, out, val, writes, eng="dve"):
        return self.op(eng, lambda e: e.memset(out, val), (), writes)

    def _semh(self, sk):
        if sk[0] == "c":
            return self.csem[sk[1]]
        return self.dsems[sk[1]][sk[2]]

    def finish(self, out_keys):
        waits = []
        for k in out_keys:
            if k in self.lastw:
                waits.append(self.lastw[k])
        self.final_waits = waits

    def emit(self):
        nc = self.nc
        counts = {e: len(self.q[e]) for e in self.ENGS}
        nw = {e: sum(len(w[0]) for w in self.q[e]) for e in self.ENGS}
        print("SCHED instr counts", counts, "waits", nw, flush=True)
        with nc.Block() as block:
            def mk(engname):
                def body(e):
                    for waits, fn, tok, inc_ in self.q[engname]:
                        for sk, v in waits:
                            e.wait_ge(self._semh(sk), v)
                        ins = fn(e)
                        ins.then_inc(self._semh(tok[0]), inc_)
                    if engname == "sp":
                        done = {}
                        for sk, v in getattr(self, "final_waits", []):
                            if done.get(sk, 0) < v:
                                done[sk] = v
                        for sk, v in done.items():
                            e.wait_ge(self._semh(sk), v)
                return body

            block.tensor(mk("pe"))
            block.scalar(mk("act"))
            block.vector(mk("dve"))
            block.gpsimd(mk("pool"))
            block.sync(mk("sp"))


ALPHA = 4.0 ** 0.25
LN_EPS = 1e-5
SH1, SC1, G1, SH2, SC2, G2 = 0, 8, 16, 24, 32, 40


def R(*tiles):
    out = []
    for t in tiles:
        out += t.all()
    return out


class Ctx:
    pass


XCHUNKS = [(0, 512), (512, 512), (1024, 128), (1152, 512), (1664, 512)]


def xall_tile_map():
    out = []
    for r0, n in XCHUNKS:
        for r in range(2):
            for i in range(n // 128):
                out.append((r, r0 // 128 + i))
    return out


def setup_common(S, C, layer_has_ctx):
    if hasattr(S, "_common"):
        C.__dict__.update(S._common.__dict__)
        return
    S._common = C
    C.PS = [S.ps("ps%d" % i, [128, 512], F32) for i in range(8)]
    pre, S.prefix = getattr(S, "prefix", ""), ""
    C.cst = S.dram("cst", [128, 1024], F32, "ExternalInput")
    S.prefix = pre
    C.cs = S.sb("cs", [128, 1024], F32)
    S.dma(C.cs[:], C.cst[:], [], R(C.cs))
    C.ident = C.cs[:, 0:128]
    C.ones = C.cs[:, 128:256]
    C.U = [C.cs[:, 256:384], C.cs[:, 384:512]]
    C.NEG1 = [C.cs[:, 512:640], C.cs[:, 640:768]]
    C.eye32 = C.cs[0:32, 768:800]
    C.identbf = S.sb("identbf", [128, 128], BF16)
    S.cp(C.identbf[:], C.ident, R(C.cs), R(C.identbf))


def modulation(S, C, mod_w, mod_bT, cT):
    PS = C.PS
    m0 = S.mark()
    cs = S.carve("cTs", [8, 2], F32)
    S.dma(cs[:], cT[:], [], R(cs))
    csl = S.carve("csl", [8, 2], F32)
    S.act(csl[:], cs[:], AF.Silu, R(cs), R(csl))
    wb = [S.carve("modw%d" % i, [8, 512], F32) for i in range(2)]
    mb = S.carve("modb", [48], F32)
    S.dma(mb[:], mod_bT[:], [], R(mb))
    mwv = mod_w.h.rearrange("(kc p) f -> p kc f", p=128)
    for fg in range(12):
        w = wb[fg % 2]
        S.dma(w[:], mwv[:, :, fg * 512:(fg + 1) * 512], [], R(w))
        for fl in range(4):
            fc = fg * 4 + fl
            for kc in range(8):
                S.mm(PS[7][:, fc * 2:fc * 2 + 2], w[:, kc, fl * 128:(fl + 1) * 128], csl[:, kc, :], kc == 0, kc == 7,
                     R(w, csl), R(PS[7]))
    S.tt(C.mod[:], PS[7][:, 0:96].rearrange("p (f j) -> p f j", j=2), mb[:].unsqueeze(2).to_broadcast([128, 48, 2]),
         ALU.add, R(PS[7], mb), R(C.mod))
    for base in (SC1, SC2):
        S.ts(C.mod[:, base:base + 8, :], C.mod[:, base:base + 8, :], 1.0, None, ALU.add, None, R(C.mod), R(C.mod))
    S.reset(m0)


def layer_norm_fm(S, C, r, n, gi, out, tmp):
    PS = C.PS
    sq, mean, msq, var, rs = tmp["sq"], tmp["mean"], tmp["msq"], tmp["var"], tmp["rs"]
    for c in range(8):
        S.mm(PS[6][:, 0:n], C.ones, r[:, c, 0:n], c == 0, c == 7, R(r, C.cs), R(PS[6]))
    S.tt(sq[:, :, 0:n], r[:, :, 0:n], r[:, :, 0:n], ALU.mult, R(r), R(sq), eng="pool")
    for c in range(8):
        S.mm(PS[7][:, 0:n], C.ones, sq[:, c, 0:n], c == 0, c == 7, R(sq, C.cs), R(PS[7]))
    S.act(mean[:, 0:n], PS[6][:, 0:n], AF.Identity, R(PS[6]), R(mean), scale=1.0 / 1024)
    S.tt(msq[:, 0:n], mean[:, 0:n], mean[:, 0:n], ALU.mult, R(mean), R(msq), eng="pool")
    S.stt(var[:, 0:n], PS[7][:, 0:n], 1.0 / 1024, msq[:, 0:n], ALU.mult, ALU.subtract, R(PS[7], msq), R(var))
    S.act(var[:, 0:n], var[:, 0:n], AF.Sqrt, R(var), R(var), bias=LN_EPS)
    S.recip(rs[:, 0:n], var[:, 0:n], R(var), R(rs))
    for c in range(8):
        S.tt(r[:, c, 0:n], r[:, c, 0:n], mean[:, 0:n], ALU.subtract, R(r, mean), R(r))
        S.tt(r[:, c, 0:n], r[:, c, 0:n], rs[:, 0:n], ALU.mult, R(r, rs), R(r), eng="pool")
        S.act(out(c), r[:, c, 0:n], AF.Identity, R(r, C.lnp), tmp["outkeys"],
              scale=C.lnp[:, gi, c:c + 1], bias=C.lnp[:, gi + 1, c:c + 1])


def ln_tmp(S, nmax, outkeys):
    return {"sq": S.carve("lnsq", [8, nmax], F32), "mean": S.carve("lnmean", [nmax], F32),
            "msq": S.carve("lnmsq", [nmax], F32), "var": S.carve("lnvar", [nmax], F32),
            "rs": S.carve("lnrs", [nmax], F32), "outkeys": outkeys}


def router(S, C, Xf, n, o0, rw, rb, gateT, rt):
    PS = C.PS
    for i in range(n // 128):
        for kc in range(8):
            S.mm(PS[5][:, 0:32], Xf[:, kc, i * 128:(i + 1) * 128], rw[:, kc, :], kc == 0, kc == 7, R(Xf, rw), R(PS[5]))
        lg, m8, msk, ex, sm = rt["lg"], rt["m8"], rt["msk"], rt["ex"], rt["sm"]
        S.tt(lg[:], PS[5][:, 0:32], rb[:], ALU.add, R(PS[5], rb), R(lg))
        S.max8(m8[:], lg[:], R(lg), R(m8))
        S.ts(msk[:], lg[:], m8[:, 3:4], None, ALU.is_ge, None, R(lg, m8), R(msk))
        S.ts(sm[:, 0:1], m8[:, 0:1], -1.0, None, ALU.mult, None, R(m8), R(sm))
        S.act(ex[:], lg[:], AF.Exp, R(lg, sm), R(ex), bias=sm[:, 0:1])
        S.tt(ex[:], ex[:], msk[:], ALU.mult, R(ex, msk), R(ex))
        S.reduce(sm[:, 1:2], ex[:], AX.X, ALU.add, R(ex), R(sm))
        S.recip(sm[:, 2:3], sm[:, 1:2], R(sm), R(sm))
        S.ts(ex[:], ex[:], sm[:, 2:3], None, ALU.mult, None, R(ex, sm), R(ex))
        S.tr(PS[5][0:32, 128:256], ex[:], C.ident, R(ex, C.cs), R(PS[5]))
        S.cp(gateT[0:32, o0 + i * 128:o0 + (i + 1) * 128], PS[5][0:32, 128:256], R(PS[5]), R(gateT), eng="act")


def phase3(S, C, subs, nkc, wmat, rhs_of, load_rows, x1d, XT, gateT, rw, rb, W=512, nbuf=1):
    PS = C.PS
    sets = []
    for i in range(nbuf):
        x1t = S.carve("x1t%d" % i, [8, W], F32)
        sets.append({"xrw": S.carve("xrw%d" % i, [W // 128, 1024], F32), "r": S.carve("r1_%d" % i, [8, W], F32), "x1t": x1t,
                     "Xf": S.carve("Xf%d" % i, [8, W], F32), "tA": S.carve("tA%d" % i, [W], F32), "lt": ln_tmp(S, W, R(x1t))})
    rt = router_tmp(S)
    for si, (o0, n, col) in enumerate(subs):
        st_ = sets[si % nbuf]
        xrw, r, x1t, Xf, tA, lt = st_["xrw"], st_["r"], st_["x1t"], st_["Xf"], st_["tA"], st_["lt"]
        rhs, rkeys = rhs_of(si, o0, n)
        load_rows(o0, n, xrw)
        for dc in range(8):
            py, px = PS[dc % 2], PS[2 + dc % 2]
            for kc in range(nkc):
                S.mm(py[:, 0:n], wmat[:, kc, dc * 128:(dc + 1) * 128], rhs(kc), kc == 0, kc == nkc - 1, R(wmat) + rkeys, R(py))
            for i in range(n // 128):
                S.tr(px[:, i * 128:(i + 1) * 128], xrw[:, i, dc * 128:(dc + 1) * 128], C.ident, R(xrw, C.cs), R(px))
            S.act(tA[:, 0:n], py[:, 0:n], AF.Identity, R(py, C.mod), R(tA), scale=C.mod[:, G1 + dc, col:col + 1])
            S.stt(r[:, dc, 0:n], px[:, 0:n], ALPHA, tA[:, 0:n], ALU.mult, ALU.add, R(px, tA), R(r))
        layer_norm_fm(S, C, r, n, 0, lambda c, x1t=x1t, n=n: x1t[:, c, 0:n], lt)
        S.dma(x1d[:, :, o0:o0 + n], x1t[:, :, 0:n], R(x1t), R(x1d))
        for dc in range(8):
            S.act(Xf[:, dc, 0:n], x1t[:, dc, 0:n], AF.Identity, R(x1t, C.mod), R(Xf),
                  scale=C.mod[:, SC2 + dc, col:col + 1], bias=C.mod[:, SH2 + dc, col:col + 1])
        S.cp(XT[:, :, o0:o0 + n], Xf[:, :, 0:n], R(Xf), R(XT), eng="pool")
        router(S, C, Xf, n, o0, rw, rb, gateT, rt)


def router_tmp(S):
    return {"lg": S.carve("rlg", [32], F32), "m8": S.carve("rm8", [8], F32), "msk": S.carve("rmsk", [32], F32),
            "ex": S.carve("rex", [32], F32), "sm": S.carve("rsm", [4], F32)}


def moe_block(S, C, XT, gateT, chunks, w_in, w_out, b_inT, b_out, acc):
    PS = C.PS
    tot = sum(n for _, n in chunks)
    offs = []
    o = 0
    for lo, n in chunks:
        offs.append(o)
        o += n
    wi = S.carve("wi", [8, 2048], BF16, nreg=2)
    wo = S.carve("wo", [8, 1024], BF16)
    nch = len(chunks)
    actT = S.carve("actT", [8, tot], BF16, nreg=8 * nch)
    Gbs = [S.carve("Gb%d" % i, [tot], F32) for i in range(2)]
    ge = S.carve("ge", [tot], F32, parts=32)
    gT_hi = S.carve("gT_hi", [tot], BF16, parts=32)
    gT_lo = S.carve("gT_lo", [tot], BF16, parts=32)
    eyebf = S.carve("eyebf", [32], BF16, parts=32)
    S.cp(eyebf[:], C.eye32, R(C.cs), R(eyebf))
    for ci, (lo, n) in enumerate(chunks):
        sl = slice(offs[ci], offs[ci] + n)
        S.cp(gT_hi[0:32, sl], gateT[0:32, lo:lo + n], R(gateT), R(gT_hi))
        S.tt(ge[0:32, sl], gateT[0:32, lo:lo + n], gT_hi[0:32, sl], ALU.subtract, R(gateT, gT_hi), R(ge))
        S.cp(gT_lo[0:32, sl], ge[0:32, sl], R(ge), R(gT_lo))
    bo = S.carve("bo", [1024], F32, parts=32)
    S.dma(bo[:], b_out[:], [], R(bo))
    tg = [S.carve("tg%d" % i, [512], F32) for i in range(2)]
    tsg = [S.carve("tsg%d" % i, [512], F32) for i in range(2)]
    tl = [S.carve("tl%d" % i, [512], F32) for i in range(2)]
    it = 0
    for dc in range(8):
        for ci, (lo, n) in enumerate(chunks):
            p = PS[4 + it % 2]
            it += 1
            S.mm(p[:, 0:n], bo[:, dc * 128:(dc + 1) * 128], gateT[0:32, lo:lo + n], True, True, R(bo, gateT), R(p))
            S.cp(acc[:, dc, offs[ci]:offs[ci] + n], p[:, 0:n], R(p), [acc.k(dc)], eng="act")
    wiv = w_in.h.rearrange("e (kc p) f -> e p kc f", p=128)
    wov = w_out.h.rearrange("e (kc p) f -> e p kc f", p=128)

    def load_wi(e, h):
        S.dma(wi[:, :, h * 1024:(h + 1) * 1024], wiv[e, :, :, h * 1024:(h + 1) * 1024], [], [wi.k(h)], eng="pool")

    def prep_gate(e):
        Gb_ = Gbs[e % 2]
        sel = eyebf[0:32, e:e + 1].to_broadcast([32, 128])
        for ci, (lo, n) in enumerate(chunks):
            sl = slice(offs[ci], offs[ci] + n)
            pg = PS[6 + ci % 2]
            S.mm(pg[:, 0:n], sel, gT_hi[0:32, sl], True, False, R(gT_hi, eyebf), R(pg))
            S.mm(pg[:, 0:n], sel, gT_lo[0:32, sl], False, True, R(gT_lo, eyebf), R(pg))
            S.cp(Gb_[:, sl], pg[:, 0:n], R(pg), R(Gb_), eng="act")

    load_wi(0, 0)
    load_wi(0, 1)
    prep_gate(0)
    it = 0
    for e in range(32):
        Gb = Gbs[e % 2]
        S.dma(wo[:], wov[e], [], R(wo), eng="pool")
        for j in range(8):
            hh, jj = j // 4, j % 4
            cg = hh * 1024 + jj * 128
            cl = hh * 1024 + 512 + jj * 128
            for ci, (lo, n) in enumerate(chunks):
                pa, pb = (PS[0], PS[1]) if it % 2 == 0 else (PS[2], PS[3])
                q = it % 2
                it += 1
                for kc in range(8):
                    S.mm(pa[:, 0:n], wi[:, kc, cg:cg + 128], XT[:, kc, lo:lo + n], kc == 0, kc == 7,
                         [wi.k(hh)] + R(XT), R(pa))
                for kc in range(8):
                    S.mm(pb[:, 0:n], wi[:, kc, cl:cl + 128], XT[:, kc, lo:lo + n], kc == 0, kc == 7,
                         [wi.k(hh)] + R(XT), R(pb))
                g, sg, l = tg[q], tsg[q], tl[q]
                S.ts(g[:, 0:n], pa[:, 0:n], b_inT[:, e, j:j + 1], 7.0, ALU.add, ALU.min, R(pa, b_inT), R(g))
                S.act(sg[:, 0:n], g[:, 0:n], AF.Sigmoid, R(g), R(sg), scale=1.702)
                S.ts(l[:, 0:n], pb[:, 0:n], b_inT[:, e, 8 + j:9 + j], 8.0, ALU.add, ALU.min, R(pb, b_inT), R(l))
                S.stt(l[:, 0:n], l[:, 0:n], -6.0, Gb[:, offs[ci]:offs[ci] + n], ALU.max, ALU.mult, R(l, Gb), R(l))
                S.tt(g[:, 0:n], g[:, 0:n], sg[:, 0:n], ALU.mult, R(g, sg), R(g), eng="pool")
                S.tt(actT[:, j, offs[ci]:offs[ci] + n], g[:, 0:n], l[:, 0:n], ALU.mult, R(g, l), [actT.k(j * nch + ci)])
            if e + 1 < 32 and j in (3, 7):
                load_wi(e + 1, j // 4)
            if e + 1 < 32 and j == 5:
                prep_gate(e + 1)
        for ci, (lo, n) in enumerate(chunks):
            for dc in range(8):
                p = PS[4 + it % 2]
                it += 1
                for j in range(8):
                    S.mm(p[:, 0:n], wo[:, j, dc * 128:(dc + 1) * 128], actT[:, j, offs[ci]:offs[ci] + n], j == 0, j == 7,
                         R(wo) + [actT.k(j * nch + ci)], R(p))
                S.tt(acc[:, dc, offs[ci]:offs[ci] + n], acc[:, dc, offs[ci]:offs[ci] + n], p[:, 0:n], ALU.add,
                     R(p) + [acc.k(dc)], [acc.k(dc)])
    return offs


def ln2_out(S, C, acc, offs, chunks, cols, x1d, xout, tmp, mk):
    PS = C.PS
    W = 256
    sets = []
    for i in range(2):
        x2 = S.carve("x2_%d" % i, [8, W], F32)
        sets.append({"x1b": S.carve("x1b%d" % i, [8, W], F32), "r": S.carve("r2_%d" % i, [8, W], F32), "x2": x2,
                     "tA": S.carve("tA2_%d" % i, [W], F32), "lt": ln_tmp(S, W, R(x2))})
    orow = [S.carve("orow%d" % i, [1024], F32) for i in range(2)]
    subs = []
    for ci, (lo, n) in enumerate(chunks):
        for s0 in range(0, n, W):
            subs.append((lo + s0, min(W, n - s0), offs[ci] + s0, cols[ci]))
    it = 0
    for si, (lo, n, ao, col) in enumerate(subs):
        st_ = sets[si % 2]
        x1b, r, x2, tA, lt = st_["x1b"], st_["r"], st_["x2"], st_["tA"], st_["lt"]
        S.dma(x1b[:, :, 0:n], x1d[:, :, lo:lo + n], R(x1d), R(x1b))
        for dc in range(8):
            S.act(tA[:, 0:n], acc[:, dc, ao:ao + n], AF.Identity, [acc.k(dc)] + R(C.mod), R(tA),
                  scale=C.mod[:, G2 + dc, col:col + 1])
            S.stt(r[:, dc, 0:n], x1b[:, dc, 0:n], ALPHA, tA[:, 0:n], ALU.mult, ALU.add, R(x1b, tA), R(r))
        layer_norm_fm(S, C, r, n, 2, lambda c, x2=x2, n=n: x2[:, c, 0:n], lt)
        for i in range(n // 128):
            pp = (PS[0], PS[1]) if it % 2 == 0 else (PS[2], PS[3])
            for dc in range(8):
                p = pp[dc // 4]
                S.tr(p[:, (dc % 4) * 128:(dc % 4 + 1) * 128], x2[:, dc, i * 128:(i + 1) * 128], C.ident, R(x2, C.cs), R(p))
            ot = orow[it % 2]
            it += 1
            S.cp(ot[:, 0:512], pp[0][:, :], R(pp[0]), R(ot), eng="act")
            S.cp(ot[:, 512:1024], pp[1][:, :], R(pp[1]), R(ot))
            okey = [xout.k((lo + i * 128) // 128)] if xout.nreg > 1 else R(xout)
            S.dma(xout[lo + i * 128:lo + (i + 1) * 128, :], ot[:], R(ot), okey)


NT = 34
TOK = 4352
OWN = [0] + list(range(2, 18))
NOWN = 2176


def own_segments(t0, t1):
    segs = []
    for lo, hi, base in ((0, 128, 0), (256, 2304, 128)):
        a, b = max(t0, lo), min(t1, hi)
        if a < b:
            segs.append((a, b - a, base + a - lo))
    return segs


class _Stop(Exception):
    pass


def build_l0(debug=False):
    import os
    stop = int(os.environ.get("STOP", "99"))

    def chk(n):
        if stop == n:
            raise _Stop()

    nc = bass.Bass("TRN2", target_bir_lowering=False)
    es = ExitStack()
    with es:
      S = Sched(nc, es)
      try:
        _build(S, nc, chk)
      except _Stop:
        print("STOPPED at", stop)
        S.finish([])
      S.emit()
    return nc


def _build(S, nc, chk, fused=False, after_block=None):
    if True:
        C = Ctx()
        D = lambda name, shape, dt=F32, kind="ExternalInput": S.dram(name, shape, dt, kind)
        xs = D("xs", [TOK, 1024])
        cT = D("cT", [128, 8, 2])
        mod_w = D("mod_w", [1024, 6144])
        mod_bT = D("mod_bT", [128, 48])
        lnp_d = D("lnp", [128, 4, 8])
        win = D("win", [8, 1024, 776])
        convp_d = D("convp", [128, 8, 4, 6])
        ssdp_d = D("ssdp", [1, 160])
        normw_d = D("normw", [128, 16])
        wout_d = D("wout", [2048, 1024])
        rw_d = D("rw", [128, 8, 32])
        rb_d = D("rb", [1, 32])
        xout = S.dram("xout", [NOWN, 1024], F32, "Internal" if fused else "ExternalOutput", nreg=17 if fused else 1)
        hTd = D("hTd", [128, 8, TOK], BF16, "Internal")
        ygd = D("ygd", [128, 16, NOWN], BF16, "Internal")
        x1d = D("x1d", [128, 8, NOWN], F32, "Internal")
        setup_common(S, C, True)
        PS = C.PS
        if not hasattr(S, "ar"):
            S.arena(49800)
        C.mod = S.carve("mod", [48, 2], F32)
        C.lnp = S.carve("lnp", [4, 8], F32)
        S.dma(C.lnp[:], lnp_d[:], [], R(C.lnp))
        modulation(S, C, mod_w, mod_bT, cT)
        base = S.mark()
        chk(0)

        xr = [S.carve("xr%d" % i, [1024], F32) for i in range(2)]
        hb = [S.carve("hb%d" % i, [8, 512], BF16) for i in range(2)]
        for blk in range(9):
            t0 = blk * 4
            nt = min(4, NT - t0)
            h = hb[blk % 2]
            for i in range(nt):
                t = t0 + i
                col = 1 if t < 2 else 0
                x = xr[t % 2]
                S.dma(x[:], xs[t * 128:(t + 1) * 128, :], [], R(x))
                for kc in range(8):
                    p = PS[kc // 4 + 2 * (t % 2)]
                    S.tr(p[:, (kc % 4) * 128:(kc % 4 + 1) * 128], x[:, kc * 128:(kc + 1) * 128], C.ident, R(x, C.cs), R(p))
                for kc in range(8):
                    p = PS[kc // 4 + 2 * (t % 2)]
                    S.act(h[:, kc, i * 128:(i + 1) * 128], p[:, (kc % 4) * 128:(kc % 4 + 1) * 128], AF.Identity,
                          R(p, C.mod), R(h), scale=C.mod[:, SC1 + kc, col:col + 1], bias=C.mod[:, SH1 + kc, col:col + 1])
            S.dma(hTd[:, :, t0 * 128:(t0 + nt) * 128], h[:, :, 0:nt * 128], R(h), R(hTd))
        S.reset(base)
        chk(1)

        convp = S.carve("convp", [8, 4, 6], F32)
        S.dma(convp[:], convp_d[:], [], R(convp))
        ssdp = S.carve("ssdp", [8, 20], F32)
        S.dma(ssdp[:], ssdp_d.h.partition_broadcast(128).rearrange("p o (g k) -> p (o g) k", k=20), [], R(ssdp))
        normw = S.carve("normw", [16], F32)
        S.dma(normw[:], normw_d[:], [], R(normw))
        wg = [S.carve("wg%d" % i, [8, 776], BF16) for i in range(2)]
        hblk = [S.carve("hblk%d" % i, [8, 512], BF16) for i in range(2)]
        raw = [S.carve("raw%d" % i, [4358], BF16) for i in range(2)]
        for r_ in raw:
            S.memset(r_[:], 0.0, R(r_))
        dgw = S.carve("dgw", [5, 128], BF16)
        fm = S.carve("fm", [TOK], BF16)
        bT = S.carve("bT", [TOK], BF16)
        cTt = S.carve("cTt", [2304], BF16)
        zT = S.carve("zT", [2, NOWN], BF16)
        xtok = S.carve("xtok", [NT, 256], BF16)
        btok = S.carve("btok", [NT, 128], BF16)
        yacc = S.carve("yacc", [17, 256], F32, nreg=17)
        ygs = S.carve("ygs", [2, NOWN], BF16)
        dtv = S.carve("dtv", [2, NT, 4], F32)
        dte = S.carve("dte", [2, NT, 4], F32)
        lndt = S.carve("lndt", [2, NT, 4], F32)
        la = S.carve("la", [2, NT, 4], F32)
        biasL = S.carve("biasL", [2, NT, 4], F32)
        ecum = S.carve("ecum", [2, NT, 4], F32)
        wdec = S.carve("wdec", [2, NT, 4], F32)
        cdec = S.carve("cdec", [2, NT, 4], F32)
        aneg = S.carve("aneg", [8], F32)
        cumS = S.carve("cumS", [2, NT, 4], F32)
        hl_t = S.carve("hl_t", [2, NT, 4], F32)
        cum_hi = S.carve("cum_hi", [2, NT, 4], BF16)
        cum_lo = S.carve("cum_lo", [2, NT, 4], BF16)
        bl_hi = S.carve("bl_hi", [2, NT, 4], BF16)
        bl_lo = S.carve("bl_lo", [2, NT, 4], BF16)
        NEG4 = [S.carve("NEG4%d" % i, [4, 128], BF16) for i in range(2)]
        for d in range(2):
            S.cp(NEG4[d][:], C.NEG1[d].unsqueeze(1).to_broadcast([128, 4, 128]), R(C.cs), R(NEG4[d]))
        Lp = [S.carve("Lp%d" % i, [4, 128], F32) for i in range(2)]
        Mp = [S.carve("Mp%d" % i, [4, 128], BF16) for i in range(2)]
        t1 = [S.carve("t1%d" % i, [4, 64], F32) for i in range(2)]
        t2 = [S.carve("t2%d" % i, [4, 64], F32) for i in range(2)]
        xdec = [S.carve("xdec%d" % i, [4, 64], BF16) for i in range(2)]
        st = [S.carve("st%d" % i, [4, 64], F32) for i in range(2)]
        stbf = [S.carve("stbf%d" % i, [4, 64], BF16) for i in range(2)]
        yz = S.carve("yz", [2, 512], F32)
        sqg = S.carve("sqg", [2, 512], F32)
        rsg = S.carve("rsg", [512], F32)
        hv = hTd.h
        blocks = [(b * 512, min(512, TOK - b * 512)) for b in range(9)]
        orderA = list(range(NT))
        orderB = [1, 0] + list(range(NT - 1, 1, -1))
        flat = lambda t: t[:].rearrange("p a b -> p (a b)")

        for g in range(8):
            w = wg[g % 2]
            S.dma(w[:], win.h[g].rearrange("(kc p) f -> p kc f", p=128), [], R(w), eng="pool")

            def inproj_pass(chunks, evac, with_dt):
                for bi, (b0, n) in enumerate(blocks):
                    hbk = hblk[bi % 2]
                    S.dma(hbk[:, :, 0:n], hv[:, :, b0:b0 + n], R(hTd), R(hbk))
                    for ci, off in enumerate(chunks):
                        p = PS[ci % 2]
                        for kc in range(8):
                            S.mm(p[:, 0:n], w[:, kc, off:off + 128], hbk[:, kc, 0:n], kc == 0, kc == 7, R(w, hbk), R(p))
                        evac(ci, p, b0, n)
                    if with_dt:
                        for i in range(n // 128):
                            t = b0 // 128 + i
                            for kc in range(8):
                                S.mm(PS[2][:, t * 8:(t + 1) * 8], hbk[:, kc, i * 128:(i + 1) * 128], w[:, kc, 768:776],
                                     kc == 0, kc == 7, R(w, hbk), R(PS[2]))

            def evac_raw(ci, p, b0, n):
                r_ = raw[ci]
                for lo, hi, sh in ((0, 256, 2), (256, TOK, 4)):
                    a, b = max(b0, lo), min(b0 + n, hi)
                    if a < b:
                        S.cp(r_[:, a + sh:b + sh], p[:, a - b0:b - b0], R(p), R(r_), eng="act")

            def conv_silu(slot, ch, dst, limit=TOK):
                r_ = raw[slot]
                for j in range(5):
                    S.ts(dgw[:, j, :], C.ident, convp[:, g, ch, j:j + 1], None, ALU.mult, None, R(C.cs, convp), R(dgw))
                segs = [(0, 256, 0)] + [(256 + 512 * k, 512, 258 + 512 * k) for k in range(8)]
                segs = [sg for sg in segs if sg[0] < limit]
                for si, (olo, n, rlo) in enumerate(segs):
                    p = PS[si % 2]
                    for j in range(5):
                        S.mm(p[:, 0:n], dgw[:, j, :], r_[:, rlo + j:rlo + j + n], j == 0, j == 4, R(dgw, r_), R(p))
                    S.act(dst[:, olo:olo + n], p[:, 0:n], AF.Silu, R(p, convp), R(dst), bias=convp[:, g, ch, 5:6])

            if g == 0: chk(20)
            inproj_pass([256, 384], evac_raw, False)
            if g == 0: chk(21)
            for xi in range(2):
                conv_silu(xi, xi, fm)
                for t0 in range(0, NT, 4):
                    nt = min(4, NT - t0)
                    pb = PS[3 + (t0 // 4) % 2][:, 0:256].bitcast(BF16)
                    pk = PS[3 + (t0 // 4) % 2]
                    for i in range(nt):
                        S.tr(pb[:, i * 128:(i + 1) * 128], fm[:, (t0 + i) * 128:(t0 + i + 1) * 128], C.identbf[:],
                             R(fm, C.identbf), R(pk))
                    S.cp(xtok[:, t0:t0 + nt, xi * 128:(xi + 1) * 128],
                         pb[:, 0:nt * 128].rearrange("p (a b) -> p a b", b=128), R(pk), R(xtok), eng="act")
            if g == 0: chk(22)
            inproj_pass([512, 640], evac_raw, False)
            conv_silu(0, 2, bT)
            conv_silu(1, 3, cTt, 2304)
            for t0 in range(0, NT, 4):
                nt = min(4, NT - t0)
                pb = PS[3 + (t0 // 4) % 2][:, 0:256].bitcast(BF16)
                pk = PS[3 + (t0 // 4) % 2]
                for i in range(nt):
                    S.tr(pb[:, i * 128:(i + 1) * 128], bT[:, (t0 + i) * 128:(t0 + i + 1) * 128], C.identbf[:],
                         R(bT, C.identbf), R(pk))
                S.cp(btok[:, t0:t0 + nt, :], pb[:, 0:nt * 128].rearrange("p (a b) -> p a b", b=128), R(pk), R(btok), eng="act")

            if g == 0: chk(23)
            def evac_z(ci, p, b0, n):
                for a, m, o in own_segments(b0, b0 + n):
                    S.act(zT[:, ci, o:o + m], p[:, a - b0:a - b0 + m], AF.Silu, R(p), R(zT))

            inproj_pass([0, 128], evac_z, True)
            if g == 0: chk(24)
            dtb = ssdp[:, g, 0:8].rearrange("p (d h) -> p d h", h=4).unsqueeze(2).to_broadcast([128, 2, NT, 4])
            if g == 0: chk(240)
            S.tt(dtv[:], PS[2][:, 0:NT * 8].rearrange("p (c d h) -> p d c h", d=2, h=4), dtb, ALU.add, R(PS[2], ssdp), R(dtv))
            if g == 0: chk(241)
            S.act(dte[:], dtv[:], AF.Exp, R(dtv), R(dte))
            if g == 0: chk(242)
            S.act(dtv[:], dte[:], AF.Ln, R(dte), R(dtv), bias=1.0)
            if g == 0: chk(243)
            S.act(lndt[:], dtv[:], AF.Ln, R(dtv), R(lndt))
            if g == 0: chk(244)
            S.act(aneg[:], ssdp[:, g, 8:16], AF.Exp, R(ssdp), R(aneg))
            if g == 0: chk(245)
            S.ts(aneg[:], aneg[:], -1.0, None, ALU.mult, None, R(aneg), R(aneg))
            if g == 0: chk(246)
            S.tt(la[:], dtv[:], aneg[:].rearrange("p (d h) -> p d h", h=4).unsqueeze(2).to_broadcast([128, 2, NT, 4]),
                 ALU.mult, R(dtv, aneg), R(la))
            laf = la[:].rearrange("p d c h -> p (d c h)")
            if g == 0: chk(247)
            for d in range(2):
                S.mm(PS[5][:, d * 136:(d + 1) * 136], C.U[d], laf[:, d * 136:(d + 1) * 136], True, True, R(la, C.cs), R(PS[5]))
            if g == 0: chk(248)
            S.mm(PS[6][:, 0:272], C.ones, laf, True, True, R(la, C.cs), R(PS[6]))
            fl = lambda t: t[:].rearrange("p d c h -> p (d c h)")
            if g == 0: chk(249)
            S.tt(fl(biasL), fl(lndt), PS[5][:, 0:272], ALU.subtract, R(lndt, PS[5]), R(biasL))
            if g == 0: chk(250)
            S.cp(fl(cumS), PS[5][:, 0:272], R(PS[5]), R(cumS))
            S.act(fl(ecum), fl(cumS), AF.Exp, R(cumS), R(ecum))
            for src, hi, lo in ((cumS, cum_hi, cum_lo), (biasL, bl_hi, bl_lo)):
                S.cp(fl(hi), fl(src), R(src), R(hi))
                S.tt(fl(hl_t), fl(src), fl(hi), ALU.subtract, R(src, hi), R(hl_t))
                S.cp(fl(lo), fl(hl_t), R(hl_t), R(lo))
            if g == 0: chk(251)
            S.tt(fl(wdec), fl(biasL), PS[6][:, 0:272], ALU.add, R(biasL, PS[6]), R(wdec))
            if g == 0: chk(252)
            S.act(fl(wdec), fl(wdec), AF.Exp, R(wdec), R(wdec))
            if g == 0: chk(253)
            S.cp(fl(cdec), PS[6][:, 0:272], R(PS[6]), R(cdec))
            S.act(fl(cdec), fl(cdec), AF.Exp, R(cdec), R(cdec))

            if g == 0: chk(25)
            Dh = ssdp[:, g, 16:20].unsqueeze(2).to_broadcast([128, 4, 64])
            for oc, c in enumerate(OWN):
                S.tt(yacc[:, oc, :].rearrange("p (h q) -> p h q", q=64), xtok[:, c, :].rearrange("p (h q) -> p h q", q=64),
                     Dh, ALU.mult, R(xtok, ssdp), [yacc.k(oc)], eng="pool")
            for d in range(2):
                S.memset(st[d][:], 0.0, R(st[d]))
                S.memset(stbf[d][:], 0.0, R(stbf[d]))
            step = 0
            for i in range(NT):
                for d, order in ((0, orderA), (1, orderB)):
                    c = order[i]
                    par = step % 2
                    step += 1
                    tc0 = c * 128
                    own = c in OWN
                    pCB, pseg, pY = PS[0], PS[1 + par], PS[3 + par]
                    if own:
                        oc = OWN.index(c)
                        S.mm(pseg[:, :], C.identbf[:], flat(NEG4[d]), True, False, R(NEG4[d], C.identbf), R(pseg))
                        for h in range(4):
                            ps_h = pseg[:, h * 128:(h + 1) * 128]
                            for ti, t_ in enumerate((cum_hi, cum_lo)):
                                S.mm(ps_h, t_[:, d, c, h:h + 1].to_broadcast([128, 128]), C.identbf[:], False, False,
                                     R(t_, C.identbf), R(pseg))
                            for ti, t_ in enumerate((bl_hi, bl_lo)):
                                S.mm(ps_h, C.identbf[:], t_[:, d, c, h:h + 1].to_broadcast([128, 128]), False,
                                     h == 3 and ti == 1, R(t_, C.identbf), R(pseg))
                        S.mm(pCB[:, 0:128], bT[:, tc0:tc0 + 128], cTt[:, tc0:tc0 + 128], True, True, R(bT, cTt), R(pCB))
                        S.act(flat(Lp[par]), pseg[:, :], AF.Exp, R(pseg), R(Lp[par]))
                        S.tt(Mp[par][:], Lp[par][:], pCB[:, 0:128].unsqueeze(1).to_broadcast([128, 4, 128]), ALU.mult,
                             R(Lp[par], pCB), R(Mp[par]))
                        S.mm(pY[:, 256:512], cTt[:, tc0:tc0 + 128], flat(stbf[d]), True, True, R(cTt, stbf[d]), R(pY))
                    if any(cc in OWN for cc in order[i + 1:]):
                        pds = PS[5 + par]
                        S.tt(xdec[par][:], xtok[:, c, :].rearrange("p (h q) -> p h q", q=64),
                             wdec[:, d, c, :].unsqueeze(2).to_broadcast([128, 4, 64]), ALU.mult, R(xtok, wdec), R(xdec[par]),
                             eng="pool")
                        S.mm(pds[:, 0:256], btok[:, c, :], flat(xdec[par]), True, True, R(btok, xdec[par]), R(pds))
                        S.tt(st[d][:], st[d][:], cdec[:, d, c, :].unsqueeze(2).to_broadcast([128, 4, 64]), ALU.mult,
                             R(st[d], cdec), R(st[d]), eng="pool")
                        S.tt(flat(st[d]), flat(st[d]), pds[:, 0:256], ALU.add, R(st[d], pds), R(st[d]))
                        S.cp(stbf[d][:], st[d][:], R(st[d]), R(stbf[d]), eng="act")
                    if own:
                        for h in range(4):
                            S.mm(pY[:, h * 64:(h + 1) * 64], Mp[par][:, h, :], xtok[:, c, h * 64:(h + 1) * 64], True, True,
                                 R(Mp[par], xtok), R(pY))
                        S.tt(t1[par][:], pY[:, 256:512].rearrange("p (h q) -> p h q", q=64),
                             ecum[:, d, c, :].unsqueeze(2).to_broadcast([128, 4, 64]), ALU.mult, R(pY, ecum), R(t1[par]))
                        S.tt(flat(t2[par]), flat(t1[par]), pY[:, 0:256], ALU.add, R(t1[par], pY), R(t2[par]))
                        S.tt(yacc[:, oc, :], yacc[:, oc, :], flat(t2[par]), ALU.add, R(t2[par]) + [yacc.k(oc)], [yacc.k(oc)],
                             eng="pool")

            if g == 0: chk(26)
            for o0 in range(0, 17, 4):
                nt = min(4, 17 - o0)
                n = nt * 128
                for i in range(nt):
                    for j in range(2):
                        S.tr(PS[5 + j][:, i * 128:(i + 1) * 128], yacc[:, o0 + i, j * 128:(j + 1) * 128], C.ident,
                             [yacc.k(o0 + i)] + R(C.cs), R(PS[5 + j]))
                for j in range(2):
                    S.tt(yz[:, j, 0:n], PS[5 + j][:, 0:n], zT[:, j, o0 * 128:o0 * 128 + n], ALU.mult, R(PS[5 + j], zT), R(yz))
                S.tt(sqg[:, :, 0:n], yz[:, :, 0:n], yz[:, :, 0:n], ALU.mult, R(yz), R(sqg), eng="pool")
                for j in range(2):
                    S.mm(PS[7][:, 0:n], C.ones, sqg[:, j, 0:n], j == 0, j == 1, R(sqg, C.cs), R(PS[7]))
                S.act(rsg[:, 0:n], PS[7][:, 0:n], AF.Sqrt, R(PS[7]), R(rsg), scale=1.0 / 256, bias=1e-5)
                S.recip(rsg[:, 0:n], rsg[:, 0:n], R(rsg), R(rsg))
                S.tt(yz[:, :, 0:n], yz[:, :, 0:n], rsg[:, 0:n].unsqueeze(1).to_broadcast([128, 2, n]), ALU.mult,
                     R(yz, rsg), R(yz))
                for j in range(2):
                    S.act(ygs[:, j, o0 * 128:o0 * 128 + n], yz[:, j, 0:n], AF.Identity, R(yz, normw), R(ygs),
                          scale=normw[:, 2 * g + j:2 * g + j + 1])
            S.dma(ygd[:, 2 * g:2 * g + 2, :], ygs[:], R(ygs), R(ygd))
            if g == 0: chk(27)
        S.reset(base)
        chk(2)

        XT = S.carve("XT", [8, NOWN], BF16)
        gateT = S.carve("gateT", [NOWN], F32, parts=32)
        p3 = S.mark()
        wout = S.carve("wout", [16, 1024], BF16)
        for h in range(2):
            S.dma(wout[:, h * 8:(h + 1) * 8, :], wout_d.h.rearrange("(kc p) f -> p kc f", p=128)[:, h * 8:(h + 1) * 8, :],
                  [], R(wout), eng="pool")
        rw = S.carve("rw", [8, 32], F32)
        S.dma(rw[:], rw_d[:], [], R(rw))
        rb = S.carve("rb", [32], F32)
        S.dma(rb[:], rb_d.h.partition_broadcast(128).rearrange("p o k -> p (o k)"), [], R(rb))
        ygbs = [S.carve("ygb%d" % i, [16, 512], BF16) for i in range(1)]
        subs3 = [(0, 128, 1)] + [(128 + 512 * k, 512, 0) for k in range(4)]

        def rhs_of(si, o0, n):
            yb = ygbs[0]
            S.dma(yb[:, :, 0:n], ygd[:, :, o0:o0 + n], R(ygd), R(yb))
            return (lambda kc: yb[:, kc, 0:n]), R(yb)

        def load_rows(o0, n, xrw):
            row0 = o0 if o0 < 128 else o0 + 128
            S.dma(xrw[:, 0:n // 128, :], xs[row0:row0 + n, :].rearrange("(i p) f -> p i f", p=128), [], R(xrw))

        phase3(S, C, subs3, 16, wout, rhs_of, load_rows, x1d, XT, gateT, rw, rb)
        S.reset(p3)
        chk(3)

        mw_in = D("mw_in", [32, 1024, 2048])
        mb_inT = D("mb_inT", [128, 32, 16])
        mw_out = D("mw_out", [32, 1024, 1024])
        mb_out = D("mb_out", [32, 1024])
        b_inT = S.carve("b_inT", [32, 16], F32)
        S.dma(b_inT[:], mb_inT[:], [], R(b_inT))
        S.ts(b_inT[:, :, 8:16], b_inT[:, :, 8:16], 1.0, None, ALU.add, None, R(b_inT), R(b_inT))
        p4 = S.mark()
        tblocks = [([(0, 384), (384, 384), (768, 384)], [(0, 128), (128, 512), (640, 512)], [1, 0, 0]),
                   ([(1152, 512), (1664, 512)], [(1152, 512), (1664, 512)], [0, 0])]
        for mchunks, chunks, cols in tblocks:
            acc = S.carve("acc", [8, sum(n for _, n in chunks)], F32, nreg=8)
            m = S.mark()
            moe_block(S, C, XT, gateT, mchunks, mw_in, mw_out, b_inT, mb_out, acc)
            offs = [lo - chunks[0][0] for lo, _ in chunks]
            S.reset(m)
            ln2_out(S, C, acc, offs, chunks, cols, x1d, xout, None, None)
            if after_block is not None:
                after_block(xout, chunks[0][0], chunks[-1][0] + chunks[-1][1])
            S.reset(p4)
        if not fused:
            S.finish(R(xout))
        return xout


TOK1 = 4352
NQ = 2048
NT1 = 34
MLA_SCALE = 192.0 ** -0.5
RMS_EPS = 1e-6


class _Stop1(Exception):
    pass


def build_l1():
    import os
    stop = int(os.environ.get("STOP", "99"))

    def chk(n):
        if stop == n:
            raise _Stop1()

    nc = bass.Bass("TRN2", target_bir_lowering=False)
    es = ExitStack()
    with es:
        S = Sched(nc, es)
        try:
            _build1(S, nc, chk)
        except _Stop1:
            print("STOPPED at", stop)
            S.finish([])
        S.emit()
    return nc


def _build1(S, nc, chk, fused=False, xown=None, xall=None):
    C = Ctx()
    D = lambda name, shape, dt=F32, kind="ExternalInput": S.dram(name, shape, dt, kind)
    xs = None if fused else D("xs", [TOK1, 1024])
    NTQ = 16 if fused else 0
    cT = D("cT", [128, 8, 2])
    mod_w = D("mod_w", [1024, 6144])
    mod_bT = D("mod_bT", [128, 48])
    lnp_d = D("lnp", [128, 4, 8])
    wa_d = D("wa", [1024, 704])
    qnorm_d = D("qnorm", [128, 3])
    kvnorm_d = D("kvnorm", [128, 2])
    wqb_d = D("wqb", [384, 1536])
    wkvb_d = D("wkvb", [256, 2048])
    wo_d = D("wo", [1024, 1024])
    ropeq_d = D("ropeq", [64, 2, NQ])
    ropek_d = D("ropek", [64, 2, TOK1])
    rm_d = D("rm", [64, 64])
    rw_d = D("rw", [128, 8, 32])
    rb_d = D("rb", [1, 32])
    xout = D("xout", [NQ, 1024], F32, "ExternalOutput")
    hTd = D("hTd", [128, 8, TOK1 + NTQ * 128], BF16, "Internal")
    x1d = D("x1d", [128, 8, NQ], F32, "Internal")
    setup_common(S, C, True)
    PS = C.PS
    if not hasattr(S, "ar"):
        S.arena(49800)
    C.mod = S.carve("mod", [48, 2], F32)
    C.lnp = S.carve("lnp", [4, 8], F32)
    S.dma(C.lnp[:], lnp_d[:], [], R(C.lnp))
    onesbf = S.carve("onesbf", [128], BF16)
    S.cp(onesbf[:], C.ones, R(C.cs), R(onesbf))
    c128 = S.carve("c128", [128], BF16)
    S.ts(c128[:], C.ones, 1.0 / 128, None, ALU.mult, None, R(C.cs), R(c128))
    rm = S.carve("rm", [64], F32, parts=64)
    S.dma(rm[:], rm_d[:], [], R(rm))
    modulation(S, C, mod_w, mod_bT, cT)
    XT = S.carve("XT", [8, NQ], BF16)
    gateT = S.carve("gateT", [NQ], F32, parts=32)
    markA = S.mark()
    oT = S.carve("oT", [8, NQ], BF16, nreg=8)
    markB = S.mark()
    chk(0)

    xr = [S.carve("xr%d" % i, [1024], F32) for i in range(2)]
    hb = [S.carve("hb%d" % i, [8, 512], BF16) for i in range(2)]
    ntl = NT1 + NTQ
    tmap = xall_tile_map()
    tchunk = []
    for k, (r0_, n_) in enumerate(XCHUNKS):
        tchunk += [k] * (2 * n_ // 128)
    for blk in range((ntl + 3) // 4):
        t0 = blk * 4
        nt = min(4, ntl - t0)
        h = hb[blk % 2]
        for i in range(nt):
            t = t0 + i
            x = xr[t % 2]
            if not fused:
                col = 1 if t >= 32 else 0
                S.dma(x[:], xs[t * 128:(t + 1) * 128, :], [], R(x))
            elif t < NT1:
                col = 1 if tmap[t][1] == 0 else 0
                S.dma(x[:], xall[t * 128:(t + 1) * 128, :], [xall.k(tchunk[t])], R(x))
            else:
                col = 0
                S.dma(x[:], xown[128 + (t - NT1) * 128:128 + (t - NT1 + 1) * 128, :], [xown.k(1 + t - NT1)], R(x))
            for kc in range(8):
                p = PS[kc // 4 + 2 * (t % 2)]
                S.tr(p[:, (kc % 4) * 128:(kc % 4 + 1) * 128], x[:, kc * 128:(kc + 1) * 128], C.ident, R(x, C.cs), R(p))
            for kc in range(8):
                p = PS[kc // 4 + 2 * (t % 2)]
                S.act(h[:, kc, i * 128:(i + 1) * 128], p[:, (kc % 4) * 128:(kc % 4 + 1) * 128], AF.Identity,
                      R(p, C.mod), R(h), scale=C.mod[:, SC1 + kc, col:col + 1], bias=C.mod[:, SH1 + kc, col:col + 1])
        S.dma(hTd[:, :, t0 * 128:(t0 + nt) * 128], h[:, :, 0:nt * 128], R(h), R(hTd))
    S.reset(markB)
    chk(1)

    kvn = S.carve("kvn", [2, TOK1], BF16)
    qln = S.carve("qln", [3, NQ], BF16)
    KrT = S.carve("KrT", [TOK1], BF16)
    S.memset(KrT[64:128, :], 0.0, R(KrT))
    markC = S.mark()
    wa = S.carve("wa", [8, 704], BF16)
    S.dma(wa[:], wa_d.h.rearrange("(kc p) f -> p kc f", p=128), [], R(wa), eng="pool")
    qnorm = S.carve("qnorm", [3], F32)
    S.dma(qnorm[:], qnorm_d[:], [], R(qnorm))
    kvnorm = S.carve("kvnorm", [2], F32)
    S.dma(kvnorm[:], kvnorm_d[:], [], R(kvnorm))
    hblk = [S.carve("hblk%d" % i, [8, 512], BF16) for i in range(2)]
    kvl = S.carve("kvl", [2, 512], F32)
    ql = S.carve("ql", [3, 512], F32)
    krl = S.carve("krl", [512], F32)
    sq = S.carve("sq", [3, 512], F32)
    rs = S.carve("rs", [512], F32)
    rk = S.carve("rk", [2, 512], F32)
    t1 = S.carve("t1", [512], F32)
    t2 = S.carve("t2", [512], F32)
    blocks = [(b * 512, min(512, TOK1 - b * 512)) for b in range(9)]

    def rms_fm(src, nch, width, g, dst, d0, n):
        S.tt(sq[:, 0:nch, 0:n], src[:, 0:nch, 0:n], src[:, 0:nch, 0:n], ALU.mult, R(src), R(sq), eng="pool")
        for c in range(nch):
            S.mm(PS[6][:, 0:n], C.ones, sq[:, c, 0:n], c == 0, c == nch - 1, R(sq, C.cs), R(PS[6]))
        S.cp(rs[:, 0:n], PS[6][:, 0:n], R(PS[6]), R(rs))
        S.act(rs[:, 0:n], rs[:, 0:n], AF.Sqrt, R(rs), R(rs), scale=1.0 / width, bias=RMS_EPS)
        S.recip(rs[:, 0:n], rs[:, 0:n], R(rs), R(rs))
        S.tt(src[:, 0:nch, 0:n], src[:, 0:nch, 0:n], rs[:, 0:n].unsqueeze(1).to_broadcast([128, nch, n]), ALU.mult,
             R(src, rs), R(src))
        for c in range(nch):
            S.act(dst[:, c, d0:d0 + n], src[:, c, 0:n], AF.Identity, R(src, g), R(dst), scale=g[:, c:c + 1])

    def rope(src, tab, dst_ap, n, dstkeys):
        S.mm(PS[7][0:64, 0:n], rm[0:64, 0:64], src[0:64, 0:n], True, True, R(src, rm), R(PS[7]))
        S.tt(t1[0:64, 0:n], src[0:64, 0:n], tab[0:64, 0, 0:n], ALU.mult, R(src, tab), R(t1))
        S.tt(t2[0:64, 0:n], PS[7][0:64, 0:n], tab[0:64, 1, 0:n], ALU.mult, R(PS[7], tab), R(t2))
        S.tt(dst_ap, t1[0:64, 0:n], t2[0:64, 0:n], ALU.add, R(t1, t2), dstkeys, eng="pool")

    for bi, (b0, n) in enumerate(blocks):
        hbk = hblk[bi % 2]
        S.dma(hbk[:, :, 0:n], hTd.h[:, :, b0:b0 + n], R(hTd), R(hbk))
        S.dma(rk[0:64, :, 0:n], ropek_d[:, :, b0:b0 + n], [], R(rk))
        it = 0
        for c in range(2):
            p = PS[it % 2]
            it += 1
            for kc in range(8):
                S.mm(p[:, 0:n], wa[:, kc, 384 + c * 128:384 + (c + 1) * 128], hbk[:, kc, 0:n], kc == 0, kc == 7, R(wa, hbk), R(p))
            S.cp(kvl[:, c, 0:n], p[:, 0:n], R(p), R(kvl))
        p = PS[2]
        for kc in range(8):
            S.mm(p[0:64, 0:n], wa[:, kc, 640:704], hbk[:, kc, 0:n], kc == 0, kc == 7, R(wa, hbk), R(p))
        S.cp(krl[0:64, 0:n], p[0:64, 0:n], R(p), R(krl))
        if b0 < NQ and not fused:
            for c in range(3):
                p = PS[it % 2]
                it += 1
                for kc in range(8):
                    S.mm(p[:, 0:n], wa[:, kc, c * 128:(c + 1) * 128], hbk[:, kc, 0:n], kc == 0, kc == 7, R(wa, hbk), R(p))
                S.cp(ql[:, c, 0:n], p[:, 0:n], R(p), R(ql))
            rms_fm(ql, 3, 384, qnorm, qln, b0, n)
        rms_fm(kvl, 2, 256, kvnorm, kvn, b0, n)
        rope(krl, rk, KrT[0:64, b0:b0 + n], n, R(KrT))
    if fused:
        for qi in range(4):
            hbk = hblk[qi % 2]
            n = 512
            b0 = qi * 512
            S.dma(hbk[:, :, 0:n], hTd.h[:, :, TOK1 + b0:TOK1 + b0 + n], R(hTd), R(hbk))
            for c in range(3):
                p = PS[c % 2]
                for kc in range(8):
                    S.mm(p[:, 0:n], wa[:, kc, c * 128:(c + 1) * 128], hbk[:, kc, 0:n], kc == 0, kc == 7, R(wa, hbk), R(p))
                S.cp(ql[:, c, 0:n], p[:, 0:n], R(p), R(ql))
            rms_fm(ql, 3, 384, qnorm, qln, b0, n)
    S.reset(markC)
    chk(2)

    wqb = S.carve("wqb", [3, 1536], BF16)
    S.dma(wqb[:], wqb_d.h.rearrange("(kc p) f -> p kc f", p=128), [], R(wqb), eng="pool")
    wkvb = S.carve("wkvb", [2, 2048], BF16)
    S.dma(wkvb[:], wkvb_d.h.rearrange("(kc p) f -> p kc f", p=128), [], R(wkvb), eng="pool")
    rq = S.carve("rq", [2, 512], F32)
    KnT = S.carve("KnT", [TOK1], BF16)
    V = S.carve("V", [NT1, 128], BF16)
    QnT = S.carve("QnT", [NQ], BF16)
    QrT = S.carve("QrT", [NQ], BF16)
    S.memset(QrT[64:128, :], 0.0, R(QrT))
    negm = S.carve("negm", [NQ], BF16)
    pT = [S.carve("pT%d" % i, [512], BF16) for i in range(3)]
    sqb = S.carve("sqb", [512], BF16)
    sqr = S.carve("sqr", [512], BF16)
    qrl = S.carve("qrl", [512], F32)
    t1 = S.carve("t1b", [512], F32)
    t2 = S.carve("t2b", [512], F32)
    tq = S.carve("tq", [512], F32)
    rden = S.carve("rden", [512], F32)
    mx = S.carve("mx", [16], F32)
    kmax = S.carve("kmax", [4], F32)
    for bi, (b0, n) in enumerate(blocks):
        S.tt(sqr[0:64, 0:n], KrT[0:64, b0:b0 + n], KrT[0:64, b0:b0 + n], ALU.mult, R(KrT), R(sqr), eng="pool")
        S.mm(PS[6][:, 0:n], onesbf[0:64, :], sqr[0:64, 0:n], True, True, R(sqr, onesbf), R(PS[6]))
        S.reduce(mx[:, bi:bi + 1], PS[6][:, 0:n], AX.X, ALU.max, R(PS[6]), R(mx))
    S.reduce(kmax[:, 0:1], mx[:, 0:9], AX.X, ALU.max, R(mx), R(kmax))
    qblocks = [(q * 512, 512) for q in range(4)]
    step = 0
    for h in range(8):
        for bi, (b0, n) in enumerate(blocks):
            p = PS[bi % 2]
            for kc in range(2):
                S.mm(p[:, 0:n], wkvb[:, kc, h * 128:(h + 1) * 128], kvn[:, kc, b0:b0 + n], kc == 0, kc == 1, R(wkvb, kvn), R(p))
            S.cp(KnT[:, b0:b0 + n], p[:, 0:n], R(p), R(KnT), eng="act")
            S.tt(sqb[:, 0:n], KnT[:, b0:b0 + n], KnT[:, b0:b0 + n], ALU.mult, R(KnT), R(sqb), eng="pool")
            S.mm(PS[6][:, 0:n], onesbf[:], sqb[:, 0:n], True, True, R(sqb, onesbf), R(PS[6]))
            S.reduce(mx[:, bi:bi + 1], PS[6][:, 0:n], AX.X, ALU.max, R(PS[6]), R(mx))
        S.reduce(kmax[:, 1:2], mx[:, 0:9], AX.X, ALU.max, R(mx), R(kmax))
        S.tt(kmax[:, 2:3], kmax[:, 0:1], kmax[:, 1:2], ALU.add, R(kmax), R(kmax))
        for t0 in range(0, NT1, 4):
            nt = min(4, NT1 - t0)
            p = PS[2 + (t0 // 4) % 2]
            for i in range(nt):
                for kc in range(2):
                    S.mm(p[:, i * 128:(i + 1) * 128], kvn[:, kc, (t0 + i) * 128:(t0 + i + 1) * 128],
                         wkvb[:, kc, 1024 + h * 128:1024 + (h + 1) * 128], kc == 0, kc == 1, R(wkvb, kvn), R(p))
            S.cp(V[:, t0:t0 + nt, :], p[:, 0:nt * 128].rearrange("p (a b) -> p a b", b=128), R(p), R(V), eng="act")
        for (q0, n) in qblocks:
            p = PS[0]
            for kc in range(3):
                S.mm(p[:, 0:n], wqb[:, kc, h * 128:(h + 1) * 128], qln[:, kc, q0:q0 + n], kc == 0, kc == 2, R(wqb, qln), R(p))
            S.cp(QnT[:, q0:q0 + n], p[:, 0:n], R(p), R(QnT), eng="act")
            p = PS[1]
            for kc in range(3):
                S.mm(p[0:64, 0:n], wqb[:, kc, 1024 + h * 64:1024 + (h + 1) * 64], qln[:, kc, q0:q0 + n], kc == 0, kc == 2,
                     R(wqb, qln), R(p))
            S.cp(qrl[0:64, 0:n], p[0:64, 0:n], R(p), R(qrl))
            S.dma(rq[0:64, :, 0:n], ropeq_d[:, :, q0:q0 + n], [], R(rq))
            S.mm(PS[7][0:64, 0:n], rm[0:64, 0:64], qrl[0:64, 0:n], True, True, R(qrl, rm), R(PS[7]))
            S.tt(t1[0:64, 0:n], qrl[0:64, 0:n], rq[0:64, 0, 0:n], ALU.mult, R(qrl, rq), R(t1))
            S.tt(t2[0:64, 0:n], PS[7][0:64, 0:n], rq[0:64, 1, 0:n], ALU.mult, R(PS[7], rq), R(t2))
            S.tt(QrT[0:64, q0:q0 + n], t1[0:64, 0:n], t2[0:64, 0:n], ALU.add, R(t1, t2), R(QrT), eng="pool")
            S.tt(sqb[:, 0:n], QnT[:, q0:q0 + n], QnT[:, q0:q0 + n], ALU.mult, R(QnT), R(sqb), eng="pool")
            S.tt(sqr[0:64, 0:n], QrT[0:64, q0:q0 + n], QrT[0:64, q0:q0 + n], ALU.mult, R(QrT), R(sqr), eng="pool")
            S.mm(PS[6][:, 0:n], onesbf[:], sqb[:, 0:n], True, False, R(sqb, onesbf), R(PS[6]))
            S.mm(PS[6][:, 0:n], onesbf[0:64, :], sqr[0:64, 0:n], False, True, R(sqr, onesbf), R(PS[6]))
            S.cp(tq[:, 0:n], PS[6][:, 0:n], R(PS[6]), R(tq))
            S.act(tq[:, 0:n], tq[:, 0:n], AF.Sqrt, R(tq, kmax), R(tq), scale=kmax[:, 2:3])
            S.ts(negm[:, q0:q0 + n], tq[:, 0:n], -1.0, None, ALU.mult, None, R(tq), R(negm))
        for (q0, n) in qblocks:
            po, pd = PS[4], PS[5]
            def scores(kt):
                par = kt % 3
                p = PS[par]
                k0 = kt * 128
                S.mm(p[:, 0:n], KnT[:, k0:k0 + 128], QnT[:, q0:q0 + n], True, False, R(KnT, QnT), R(p))
                S.mm(p[:, 0:n], KrT[:, k0:k0 + 128], QrT[:, q0:q0 + n], False, False, R(KrT, QrT), R(p))
                S.mm(p[:, 0:n], c128[:], negm[:, q0:q0 + n], False, True, R(c128, negm), R(p))
                S.act(pT[par][:, 0:n], p[:, 0:n], AF.Exp, R(p), R(pT[par]), scale=MLA_SCALE)

            scores(0)
            scores(1)
            for kt in range(NT1):
                if kt + 2 < NT1:
                    scores(kt + 2)
                par = kt % 3
                S.mm(po[:, 0:n], V[:, kt, :], pT[par][:, 0:n], kt == 0, kt == NT1 - 1, R(V, pT[par]), R(po))
                S.mm(pd[:, 0:n], onesbf[:], pT[par][:, 0:n], kt == 0, kt == NT1 - 1, R(onesbf, pT[par]), R(pd))
            S.recip(rden[:, 0:n], pd[:, 0:n], R(pd), R(rden))
            S.tt(oT[:, h, q0:q0 + n], po[:, 0:n], rden[:, 0:n], ALU.mult, R(po, rden), [oT.k(h)])
        if h == 0:
            chk(20)
    S.reset(markB)
    chk(3)

    wo = S.carve("wo", [8, 1024], BF16)
    S.dma(wo[:], wo_d.h.rearrange("(kc p) f -> p kc f", p=128), [], R(wo), eng="pool")
    rw = S.carve("rw", [8, 32], F32)
    S.dma(rw[:], rw_d[:], [], R(rw))
    rb = S.carve("rb", [32], F32)
    S.dma(rb[:], rb_d.h.partition_broadcast(128).rearrange("p o k -> p (o k)"), [], R(rb))
    subs3 = [(512 * k, 512, 0) for k in range(4)]

    def rhs_of(si, o0, n):
        return (lambda kc: oT[:, kc, o0:o0 + n]), R(oT)

    def load_rows(o0, n, xrw):
        if fused:
            S.dma(xrw[:, 0:n // 128, :], xown[128 + o0:128 + o0 + n, :].rearrange("(i p) f -> p i f", p=128), R(xown), R(xrw))
        else:
            S.dma(xrw[:, 0:n // 128, :], xs[o0:o0 + n, :].rearrange("(i p) f -> p i f", p=128), [], R(xrw))

    phase3(S, C, subs3, 8, wo, rhs_of, load_rows, x1d, XT, gateT, rw, rb)
    S.reset(markA)
    chk(4)

    mw_in = D("mw_in", [32, 1024, 2048])
    mb_inT = D("mb_inT", [128, 32, 16])
    mw_out = D("mw_out", [32, 1024, 1024])
    mb_out = D("mb_out", [32, 1024])
    b_inT = S.carve("b_inT", [32, 16], F32)
    S.dma(b_inT[:], mb_inT[:], [], R(b_inT))
    S.ts(b_inT[:, :, 8:16], b_inT[:, :, 8:16], 1.0, None, ALU.add, None, R(b_inT), R(b_inT))
    p4 = S.mark()
    tblocks = [([(0, 512), (512, 512)], [0, 0]), ([(1024, 512), (1536, 512)], [0, 0])]
    for chunks, cols in tblocks:
        acc = S.carve("acc", [8, sum(n for _, n in chunks)], F32, nreg=8)
        m = S.mark()
        offs = moe_block(S, C, XT, gateT, chunks, mw_in, mw_out, b_inT, mb_out, acc)
        S.reset(m)
        ln2_out(S, C, acc, offs, chunks, cols, x1d, xout, None, None)
        S.reset(p4)
    S.finish(R(xout))


def build_fused():
    nc = bass.Bass("TRN2", target_bir_lowering=False)
    es = ExitStack()
    with es:
        S = Sched(nc, es)
        nochk = lambda n: None
        S.prefix = ""
        xall = S.dram("xall", [2 * 2176, 1024], F32, "Internal", nreg=len(XCHUNKS))

        def exchange(xown, row_lo, row_hi):
            for k, (r0, n) in enumerate(XCHUNKS):
                if row_lo <= r0 < row_hi:
                    S.op("pool", lambda e, r0=r0, n=n: e.collective_compute(
                        "AllGather", ALU.bypass, replica_groups=[[0, 1], [2, 3], [4, 5], [6, 7]],
                        ins=[xown[r0:r0 + n, :]], outs=[xall[2 * r0:2 * r0 + 2 * n, :]]),
                        [xown.k(t) for t in range(r0 // 128, (r0 + n) // 128)], [xall.k(k)], dma=True, inc=1)

        S.prefix = "a_"
        xown = _build(S, nc, nochk, fused=True, after_block=exchange)
        S.reset(0)
        S.prefix = "b_"
        _build1(S, nc, nochk, fused=True, xown=xown, xall=xall)
        S.emit()
    return nc


def consts():
    c = np.zeros((128, 1024), np.float32)
    c[:, 0:128] = np.eye(128)
    c[:, 128:256] = 1.0
    k = np.arange(128)[:, None]; l = np.arange(128)[None, :]
    c[:, 256:384] = (k <= l)
    c[:, 384:512] = (k >= l)
    c[:, 512:640] = np.where(k > l, -30000.0, 0.0)
    c[:, 640:768] = np.where(k < l, -30000.0, 0.0)
    c[0:32, 768:800] = np.eye(32)
    return c

def fm(v, nchunk):
    return np.ascontiguousarray(v.reshape(nchunk, 128).T)

def prep_l0(inp, b, half):
    f = np.float32
    flip = half == 1
    ctx_ = inp['ctx'][b][::-1] if flip else inp['ctx'][b]
    lat_ = inp['x'][b][::-1] if flip else inp['x'][b]
    m = {}
    m['xs'] = np.ascontiguousarray(np.concatenate([ctx_, lat_], 0), dtype=f)
    cT = np.zeros((128, 8, 2), f)
    cT[:, :, 0] = fm(inp['c'][b], 8); cT[:, :, 1] = fm(inp['c_ctx'], 8)
    m['cT'] = cT
    m['mod_w'] = np.ascontiguousarray(inp['mod_w'][0])
    m['mod_bT'] = fm(inp['mod_b'][0], 48)
    m['lnp'] = np.ascontiguousarray(np.stack([fm(inp['ln1_g'][0], 8), fm(inp['ln1_b'][0], 8), fm(inp['ln2_g'][0], 8), fm(inp['ln2_b'][0], 8)], 1))
    W = inp['ssd_w_in'][0]
    dA = 1 if flip else 0; dB = 1 - dA
    win = np.zeros((8, 1024, 776), f)
    convp = np.zeros((128, 8, 4, 6), f)
    ssdp = np.zeros((1, 160), f)
    cw = inp['ssd_conv_w'][0]; cb = inp['ssd_conv_b'][0]
    if flip: cw = cw[::-1]
    for g in range(8):
        win[g] = np.concatenate([W[:, 256*g:256*g+256], W[:, 2048+256*g:2048+256*g+256], W[:, 4096+128*g:4096+128*g+128],
                                 W[:, 5120+128*g:5120+128*g+128], W[:, 6144+dA*32+4*g:6144+dA*32+4*g+4], W[:, 6144+dB*32+4*g:6144+dB*32+4*g+4]], 1)
        for ch, c0 in enumerate([256*g, 256*g+128, 2048+128*g, 3072+128*g]):
            convp[:, g, ch, 0:5] = cw[:, c0:c0+128].T
            convp[:, g, ch, 5] = cb[c0:c0+128]
        ssdp[0, g*20:g*20+20] = np.concatenate([inp['ssd_dt_bias'][0][dA, 4*g:4*g+4], inp['ssd_dt_bias'][0][dB, 4*g:4*g+4],
                                               inp['ssd_a_log'][0][dA, 4*g:4*g+4], inp['ssd_a_log'][0][dB, 4*g:4*g+4], inp['ssd_d'][0][4*g:4*g+4]])
    m['win'] = win; m['convp'] = convp; m['ssdp'] = ssdp
    m['normw'] = fm(inp['ssd_norm_w'][0], 16)
    m['wout'] = np.ascontiguousarray(inp['ssd_w_out'][0])
    add_moe(m, inp, 0)
    m['cst'] = consts()
    return m

def add_moe(m, inp, i):
    f = np.float32
    m['rw'] = np.ascontiguousarray(inp['router_w'][i].reshape(8, 128, 32).transpose(1, 0, 2))
    m['rb'] = np.ascontiguousarray(inp['router_b'][i].reshape(1, 32))
    wi = inp['moe_w_in'][i]
    m['mw_in'] = np.ascontiguousarray(np.concatenate([wi[:, :, 0:512], wi[:, :, 1024:1536], wi[:, :, 512:1024], wi[:, :, 1536:2048]], 2))
    m['mb_inT'] = np.ascontiguousarray(inp['moe_b_in'][i].reshape(32, 16, 128).transpose(2, 0, 1))
    m['mw_out'] = np.ascontiguousarray(inp['moe_w_out'][i])
    m['mb_out'] = np.ascontiguousarray(inp['moe_b_out'][i])

def gather_l0(results, B=4):
    x1 = np.zeros((B, 4096, 1024), np.float32); ctx1 = np.zeros((B, 256, 1024), np.float32)
    for cid, r in enumerate(results):
        b, half = cid // 2, cid % 2
        o = r['xout']
        if half == 0:
            ctx1[b, 0:128] = o[0:128]; x1[b, 0:2048] = o[128:]
        else:
            ctx1[b, 128:256] = o[0:128][::-1]; x1[b, 2048:4096] = o[128:][::-1]
    return x1, ctx1


def rope_consts():
    f = np.float32
    t = np.arange(4096)
    row = (t // 64).astype(f); col = (t % 64).astype(f)
    inv = (f(10000.0) ** (-np.arange(16, dtype=f) / f(16))).astype(f)
    ang = np.stack([row[:, None] * inv, col[:, None] * inv], axis=1).astype(f)
    cos, sin = np.cos(ang).astype(f), np.sin(ang).astype(f)
    cosT = np.zeros((64, 4096), f); sinT = np.zeros((64, 4096), f)
    for a in range(2):
        for hf in range(2):
            cosT[a * 32 + hf * 16:a * 32 + hf * 16 + 16] = cos[:, a, :].T
            sinT[a * 32 + hf * 16:a * 32 + hf * 16 + 16] = sin[:, a, :].T
    rm = np.zeros((64, 64), f)
    for a in range(2):
        for k in range(16):
            i1 = a * 32 + k; i2 = a * 32 + 16 + k
            rm[i2, i1] = -1.0
            rm[i1, i2] = 1.0
    return cosT, sinT, rm

def prep_l1(inp, x1, ctx1, b, half, rc=None):
    f = np.float32
    cosT, sinT, rm = rc if rc is not None else rope_consts()
    own = slice(half * 2048, (half + 1) * 2048)
    oth = slice((1 - half) * 2048, (2 - half) * 2048)
    m = {}
    m['xs'] = np.ascontiguousarray(np.concatenate([x1[b, own], x1[b, oth], ctx1[b]], 0), dtype=f)
    cT = np.zeros((128, 8, 2), f)
    cT[:, :, 0] = fm(inp['c'][b], 8); cT[:, :, 1] = fm(inp['c_ctx'], 8)
    m['cT'] = cT
    m['mod_w'] = np.ascontiguousarray(inp['mod_w'][1])
    m['mod_bT'] = fm(inp['mod_b'][1], 48)
    m['lnp'] = np.ascontiguousarray(np.stack([fm(inp['ln1_g'][1], 8), fm(inp['ln1_b'][1], 8), fm(inp['ln2_g'][1], 8), fm(inp['ln2_b'][1], 8)], 1))
    m['wa'] = np.ascontiguousarray(inp['mla_w_a'][0])
    m['qnorm'] = fm(inp['mla_q_norm'][0], 3)
    m['kvnorm'] = fm(inp['mla_kv_norm'][0], 2)
    wq = inp['mla_w_qb'][0]
    m['wqb'] = np.ascontiguousarray(np.concatenate([wq[:, h * 192:h * 192 + 128] for h in range(8)] +
                                                  [wq[:, h * 192 + 128:h * 192 + 192] for h in range(8)], 1))
    wk = inp['mla_w_kvb'][0]
    m['wkvb'] = np.ascontiguousarray(np.concatenate([wk[:, h * 256:h * 256 + 128] for h in range(8)] +
                                                   [wk[:, h * 256 + 128:h * 256 + 256] for h in range(8)], 1))
    m['wo'] = np.ascontiguousarray(inp['mla_w_o'][0])
    m['ropeq'] = np.ascontiguousarray(np.stack([cosT[:, own], sinT[:, own]], 1))
    ck = np.concatenate([cosT[:, own], cosT[:, oth], np.ones((64, 256), f)], 1)
    sk = np.concatenate([sinT[:, own], sinT[:, oth], np.zeros((64, 256), f)], 1)
    m['ropek'] = np.ascontiguousarray(np.stack([ck, sk], 1))
    m['rm'] = rm
    add_moe(m, inp, 1)
    m['cst'] = consts()
    return m

def gather_l1(results, B=4):
    out = np.zeros((B, 4096, 1024), np.float32)
    for cid, r in enumerate(results):
        b, half = cid // 2, cid % 2
        out[b, half * 2048:(half + 1) * 2048] = r['xout']
    return out


def prep_fused(inp, b, half, rc):
    f = np.float32
    cosT, sinT, rm = rc
    m0 = prep_l0(inp, b, half)
    m = {("cst" if k == "cst" else "a_" + k): v for k, v in m0.items()}
    idx1 = 4095 - np.arange(2048)
    ck = np.ones((64, 4352), f)
    sk = np.zeros((64, 4352), f)
    for t, (r, mt) in enumerate(xall_tile_map()):
        if mt == 0:
            continue
        base = (mt - 1) * 128 + np.arange(128)
        lat = base if r == 0 else 4095 - base
        ck[:, t * 128:(t + 1) * 128] = cosT[:, lat]
        sk[:, t * 128:(t + 1) * 128] = sinT[:, lat]
    qi = np.arange(2048) if half == 0 else idx1
    m1 = {}
    cT = np.zeros((128, 8, 2), f)
    cT[:, :, 0] = fm(inp['c'][b], 8); cT[:, :, 1] = fm(inp['c_ctx'], 8)
    m1['cT'] = cT
    m1['mod_w'] = np.ascontiguousarray(inp['mod_w'][1])
    m1['mod_bT'] = fm(inp['mod_b'][1], 48)
    m1['lnp'] = np.ascontiguousarray(np.stack([fm(inp['ln1_g'][1], 8), fm(inp['ln1_b'][1], 8), fm(inp['ln2_g'][1], 8), fm(inp['ln2_b'][1], 8)], 1))
    m1['wa'] = np.ascontiguousarray(inp['mla_w_a'][0])
    m1['qnorm'] = fm(inp['mla_q_norm'][0], 3)
    m1['kvnorm'] = fm(inp['mla_kv_norm'][0], 2)
    wq = inp['mla_w_qb'][0]
    m1['wqb'] = np.ascontiguousarray(np.concatenate([wq[:, h * 192:h * 192 + 128] for h in range(8)] +
                                                   [wq[:, h * 192 + 128:h * 192 + 192] for h in range(8)], 1))
    wk = inp['mla_w_kvb'][0]
    m1['wkvb'] = np.ascontiguousarray(np.concatenate([wk[:, h * 256:h * 256 + 128] for h in range(8)] +
                                                    [wk[:, h * 256 + 128:h * 256 + 256] for h in range(8)], 1))
    m1['wo'] = np.ascontiguousarray(inp['mla_w_o'][0])
    m1['ropeq'] = np.ascontiguousarray(np.stack([cosT[:, qi], sinT[:, qi]], 1))
    m1['ropek'] = np.ascontiguousarray(np.stack([ck, sk], 1))
    m1['rm'] = rm
    add_moe(m1, inp, 1)
    for k, v in m1.items():
        m["b_" + k] = v
    return m

def gather_fused(results, B=4):
    out = np.zeros((B, 4096, 1024), np.float32)
    for cid, r in enumerate(results):
        b, half = cid // 2, cid % 2
        o = r['b_xout']
        if half == 0:
            out[b, 0:2048] = o
        else:
            out[b, 2048:4096] = o[::-1]
    return out


def kernel(**inputs):
    inp = {k: np.asarray(v) for k, v in inputs.items()}
    nc = build_fused()
    rc = rope_consts()
    maps = []
    for cid in range(8):
        m = prep_fused(inp, cid // 2, cid % 2, rc)
        maps.append({k: m[k] for k in nc._in_names})
    res = run_bass_kernel_spmd(nc, maps, core_ids=list(range(8)))
    return gather_fused(res.results)
```
